# Optimizing a Trainium2 kernel written in Bass

```python
import math
import jax
import jax.numpy as jnp
from jax import lax
import numpy as np

D_MODEL = 2048
BATCH = 4
SEQ = 2048
DEPTH = 1

GRID_W = 64
CTX_LEN = 256
EXPAND = 2
MIX_WIDTH = EXPAND * D_MODEL
SSD_WIDTH = MIX_WIDTH // 2
CM_WIDTH = MIX_WIDTH - SSD_WIDTH
SSD_HEAD_DIM = 64
SSD_HEADS = SSD_WIDTH // SSD_HEAD_DIM
SSD_GROUPS = 4
SSD_STATE = 128
SSD_CHUNK = 128
CONV_K = 3
BC_WIDTH = SSD_GROUPS * SSD_STATE
CONV_DIM = SSD_WIDTH + 2 * BC_WIDTH
CM_HEADS = 8
CM_HEAD_DIM = CM_WIDTH // CM_HEADS
CM_CHUNK = 128
IN_COLS = SSD_WIDTH + CONV_DIM + 2 * SSD_HEADS + 2 * CM_WIDTH
N_EXPERT_GROUPS = 4
EXPERTS_PER_GROUP = 8
EXPERT_TOP_K = 2
EXPERT_FF = D_MODEL // 4
N_MOD = 6
NORM_EPS = 1e-6
DT_MIN = 1e-3
DT_MAX = 1e-1

kernel_name = 'hybrid_bissd_chunkgmlp_hmoe_block'


def rmsnorm(x, g):
    xf = x.astype(jnp.float32)
    y = xf * lax.rsqrt(jnp.mean(xf * xf, axis=-1, keepdims=True) + NORM_EPS)
    return (y * g).astype(x.dtype)


def layernorm(x, g, b):
    xf = x.astype(jnp.float32)
    mu = jnp.mean(xf, axis=-1, keepdims=True)
    xc = xf - mu
    y = xc * lax.rsqrt(jnp.mean(xc * xc, axis=-1, keepdims=True) + NORM_EPS)
    return (y * g + b).astype(x.dtype)


def modulate(h, shift, scale):
    return h * (1.0 + scale) + shift


def dwconv_centred(x, w, b):
    k = w.shape[0]
    pad = k // 2
    n = x.shape[-2]
    xp = jnp.pad(x, [(0, 0)] * (x.ndim - 2) + [(pad, pad), (0, 0)])
    out = b
    for j in range(k):
        out = out + xp[..., j:j + n, :] * w[j]
    return out


def split_in_proj(p):
    s0 = SSD_WIDTH
    s1 = s0 + CONV_DIM
    s2 = s1 + SSD_HEADS
    s3 = s2 + SSD_HEADS
    s4 = s3 + CM_WIDTH
    return jnp.split(p, [s0, s1, s2, s3, s4], axis=-1)


def ssd_prepare(xbc, dt_f, dt_b, conv_w, conv_b, dt_bias_f, dt_bias_b, rows):
    bsz, n, _ = xbc.shape
    if rows is None:
        xbc = dwconv_centred(xbc, conv_w, conv_b)
    else:
        xbc = dwconv_centred(xbc.reshape(bsz, rows, GRID_W, CONV_DIM), conv_w, conv_b).reshape(bsz, n, CONV_DIM)
    xbc = jax.nn.silu(xbc)
    xs, bm, cm = jnp.split(xbc, [SSD_WIDTH, SSD_WIDTH + BC_WIDTH], axis=-1)
    xs = xs.reshape(bsz, n, SSD_HEADS, SSD_HEAD_DIM)
    bm = bm.reshape(bsz, n, SSD_GROUPS, SSD_STATE)
    cm = cm.reshape(bsz, n, SSD_GROUPS, SSD_STATE)
    dtf = jax.nn.softplus(dt_f.astype(jnp.float32) + dt_bias_f.astype(jnp.float32))
    dtb = jax.nn.softplus(dt_b.astype(jnp.float32) + dt_bias_b.astype(jnp.float32))
    return xs, bm, cm, dtf, dtb


def ssd_scan(xs, dt, a, bm, cm, h0, with_output):
    bsz, n, nh, hp = xs.shape
    g, ns = bm.shape[-2:]
    r = nh // g
    nc = n // SSD_CHUNK
    q = SSD_CHUNK
    dt = dt.reshape(bsz, nc, q, g, r)
    x_dt = xs.reshape(bsz, nc, q, g, r, hp) * dt[..., None]
    bc = bm.reshape(bsz, nc, q, g, ns)
    cc = cm.reshape(bsz, nc, q, g, ns)
    a_cum = jnp.cumsum(dt * a.reshape(g, r), axis=2)
    a_tot = a_cum[:, :, -1]
    decay_end = jnp.exp(a_tot[:, :, None] - a_cum)
    states = jnp.einsum('bcqgn,bcqgrp->bcgrpn', bc, x_dt * decay_end[..., None])

    def step(h, inp):
        s_c, a_c = inp
        return h * jnp.exp(a_c)[..., None, None] + s_c, h

    h_last, h_prev = lax.scan(step, h0.reshape(bsz, g, r, hp, ns).astype(jnp.float32),
                              (jnp.moveaxis(states, 1, 0), jnp.moveaxis(a_tot, 1, 0)))
    h_final = h_last.reshape(bsz, nh, hp, ns)
    if not with_output:
        return None, h_final
    h_prev = jnp.moveaxis(h_prev, 0, 1)
    y_off = jnp.einsum('bcqgn,bcgrpn->bcqgrp', cc, h_prev) * jnp.exp(a_cum)[..., None]
    seg = a_cum[:, :, :, None] - a_cum[:, :, None, :]
    lower = np.tri(q, dtype=bool)[:, :, None, None]
    decay = jnp.exp(jnp.where(lower, seg, -jnp.inf))
    cb = jnp.einsum('bcign,bcjgn->bcijg', cc, bc)
    y_diag = jnp.einsum('bcijgr,bcjgrp->bcigrp', cb[..., None] * decay, x_dt)
    y = (y_diag + y_off).reshape(bsz, n, nh, hp)
    return y, h_final


def bi_ssd(xs, dtf, dtb, a_f, a_b, bm, cm, h0_f, h0_b, with_output):
    y_f, h_f = ssd_scan(xs, dtf, a_f, bm, cm, h0_f, with_output)
    rev = lambda t: jnp.flip(t, axis=1)
    y_b, h_b = ssd_scan(rev(xs), rev(dtb), a_b, rev(bm), rev(cm), h0_b, with_output)
    y = y_f + rev(y_b) if with_output else None
    return y, h_f, h_b


def ssd_finish(y, xs, z, d_skip, norm_g):
    bsz, n = z.shape[:2]
    y = (y + xs * d_skip[:, None]).reshape(bsz, n, SSD_WIDTH).astype(z.dtype)
    return rmsnorm(y * jax.nn.silu(z), norm_g)


def chunk_gmlp(u, v, ln_g, ln_b, w_s, b_s):
    bsz, n, _ = u.shape
    nc = n // CM_CHUNK
    u = jax.nn.gelu(u)
    v = layernorm(jax.nn.gelu(v), ln_g, ln_b).reshape(bsz, nc, CM_CHUNK, CM_HEADS, CM_HEAD_DIM)
    s = jnp.einsum('gij,bcjgd->bcigd', w_s, v) + b_s.T[:, :, None]
    return u * s.reshape(bsz, n, CM_WIDTH)


def hier_moe(t, w_rg, b_rg, w_re, b_re, w_gate, w_up, w_down):
    p_g = jax.nn.softmax((t @ w_rg).astype(jnp.float32) + b_rg, axis=-1)
    top_pg, top_g = lax.top_k(p_g, 1)
    onehot_g = jax.nn.one_hot(top_g[:, 0], N_EXPERT_GROUPS, dtype=jnp.float32)
    logits_e = jnp.einsum('td,gde->tge', t, w_re).astype(jnp.float32) + b_re
    logits_sel = jnp.einsum('tge,tg->te', logits_e, onehot_g)
    top_le, top_e = lax.top_k(logits_sel, EXPERT_TOP_K)
    p_e = jax.nn.softmax(top_le, axis=-1) * top_pg
    w_e = jnp.einsum('tk,tke->te', p_e, jax.nn.one_hot(top_e, EXPERTS_PER_GROUP, dtype=jnp.float32))
    comb = (onehot_g[:, :, None] * w_e[:, None, :]).astype(t.dtype)
    out = jnp.zeros_like(t)
    for g in range(N_EXPERT_GROUPS):
        hid = jax.nn.silu(jnp.einsum('td,edf->tef', t, w_gate[g])) * jnp.einsum('td,edf->tef', t, w_up[g])
        out = out + jnp.einsum('tef,efd->td', hid * comb[:, g, :, None], w_down[g])
    return out


def setup_inputs(seed: int = 0) -> dict:
    key = jax.random.key(seed)
    ks = jax.random.split(key, 32)
    f32 = jnp.float32

    def nrm(k, shape, scale):
        return jax.random.normal(k, shape, f32) * scale

    L = DEPTH
    G, E, F = N_EXPERT_GROUPS, EXPERTS_PER_GROUP, EXPERT_FF
    log_lo, log_hi = math.log(DT_MIN), math.log(DT_MAX)
    dt0 = jnp.exp(jax.random.uniform(ks[8], (L, 2, SSD_HEADS), f32) * (log_hi - log_lo) + log_lo)
    dt_bias = dt0 + jnp.log(-jnp.expm1(-dt0))
    a_log = jnp.log(jax.random.uniform(ks[9], (L, 2, SSD_HEADS), f32, 1.0, 16.0))
    return {
        'x': nrm(ks[0], (BATCH, SEQ, D_MODEL), 1.0),
        'c': nrm(ks[1], (BATCH, D_MODEL), 1.0),
        'ctx': nrm(ks[2], (BATCH, CTX_LEN, D_MODEL), 1.0),
        'c_ctx': nrm(ks[3], (D_MODEL,), 1.0),
        'w_mod': nrm(ks[4], (L, D_MODEL, N_MOD * D_MODEL), 0.5 * D_MODEL ** -0.5),
        'b_mod': nrm(ks[5], (L, N_MOD * D_MODEL), 0.02),
        'norm1_g': 1.0 + nrm(ks[6], (L, D_MODEL), 0.02),
        'w_in': nrm(ks[7], (L, D_MODEL, IN_COLS), D_MODEL ** -0.5),
        'conv_w': nrm(ks[10], (L, CONV_K, CONV_DIM), CONV_K ** -0.5),
        'conv_b': nrm(ks[11], (L, CONV_DIM), 0.02),
        'dt_bias_f': dt_bias[:, 0],
        'dt_bias_b': dt_bias[:, 1],
        'a_log_f': a_log[:, 0],
        'a_log_b': a_log[:, 1],
        'd_skip': 1.0 + nrm(ks[12], (L, SSD_HEADS), 0.1),
        'ssd_norm_g': 1.0 + nrm(ks[13], (L, SSD_WIDTH), 0.02),
        'cm_ln_g': 1.0 + nrm(ks[14], (L, CM_WIDTH), 0.02),
        'cm_ln_b': nrm(ks[15], (L, CM_WIDTH), 0.02),
        'w_spatial': nrm(ks[16], (L, CM_HEADS, CM_CHUNK, CM_CHUNK), CM_CHUNK ** -0.5),
        'b_spatial': 1.0 + nrm(ks[17], (L, CM_HEADS, CM_CHUNK), 0.02),
        'w_out': nrm(ks[18], (L, MIX_WIDTH, D_MODEL), MIX_WIDTH ** -0.5),
        'norm2_g': 1.0 + nrm(ks[19], (L, D_MODEL), 0.02),
        'w_router_group': nrm(ks[20], (L, D_MODEL, G), D_MODEL ** -0.5),
        'b_router_group': nrm(ks[21], (L, G), 0.01),
        'w_router_expert': nrm(ks[22], (L, G, D_MODEL, E), D_MODEL ** -0.5),
        'b_router_expert': nrm(ks[23], (L, G, E), 0.01),
        'w_exp_gate': nrm(ks[24], (L, G, E, D_MODEL, F), D_MODEL ** -0.5),
        'w_exp_up': nrm(ks[25], (L, G, E, D_MODEL, F), D_MODEL ** -0.5),
        'w_exp_down': nrm(ks[26], (L, G, E, F, D_MODEL), F ** -0.5),
        'normf_g': 1.0 + nrm(ks[27], (D_MODEL,), 0.02),
    }


def reference(x, c, ctx, c_ctx, w_mod, b_mod, norm1_g, w_in, conv_w, conv_b, dt_bias_f, dt_bias_b,
              a_log_f, a_log_b, d_skip, ssd_norm_g, cm_ln_g, cm_ln_b, w_spatial, b_spatial, w_out,
              norm2_g, w_router_group, b_router_group, w_router_expert, b_router_expert,
              w_exp_gate, w_exp_up, w_exp_down, normf_g):
    bsz, seq_len, _ = x.shape
    rows = seq_len // GRID_W
    h_zero = jnp.zeros((bsz, SSD_HEADS, SSD_HEAD_DIM, SSD_STATE), jnp.float32)
    for i in range(DEPTH):
        last = i == DEPTH - 1
        mod_x = (jax.nn.silu(c) @ w_mod[i] + b_mod[i])[:, None, :]
        mod_c = jax.nn.silu(c_ctx) @ w_mod[i] + b_mod[i]
        sh1, sc1, g1, sh2, sc2, g2 = jnp.split(mod_x, N_MOD, axis=-1)
        csh1, csc1, cg1, csh2, csc2, cg2 = jnp.split(mod_c, N_MOD, axis=-1)
        a_f = -jnp.exp(a_log_f[i].astype(jnp.float32))
        a_b = -jnp.exp(a_log_b[i].astype(jnp.float32))
        p_x = modulate(rmsnorm(x, norm1_g[i]), sh1, sc1) @ w_in[i]
        p_c = modulate(rmsnorm(ctx, norm1_g[i]), csh1, csc1) @ w_in[i]
        z_x, xbc_x, dtf_x, dtb_x, u_x, v_x = split_in_proj(p_x)
        z_c, xbc_c, dtf_c, dtb_c, u_c, v_c = split_in_proj(p_c)
        xs_c, bm_c, cm_c, dtf_c, dtb_c = ssd_prepare(xbc_c, dtf_c, dtb_c, conv_w[i], conv_b[i],
                                                      dt_bias_f[i], dt_bias_b[i], None)
        y_c, hc_f, hc_b = bi_ssd(xs_c, dtf_c, dtb_c, a_f, a_b, bm_c, cm_c, h_zero, h_zero, not last)
        xs_x, bm_x, cm_x, dtf_x, dtb_x = ssd_prepare(xbc_x, dtf_x, dtb_x, conv_w[i], conv_b[i],
                                                      dt_bias_f[i], dt_bias_b[i], rows)
        y_x, _, _ = bi_ssd(xs_x, dtf_x, dtb_x, a_f, a_b, bm_x, cm_x, hc_f, hc_b, True)
        mix_x = jnp.concatenate([ssd_finish(y_x, xs_x, z_x, d_skip[i], ssd_norm_g[i]),
                                 chunk_gmlp(u_x, v_x, cm_ln_g[i], cm_ln_b[i], w_spatial[i], b_spatial[i])],
                                axis=-1)
        x = x + g1 * (mix_x @ w_out[i])
        moe_w = (w_router_group[i], b_router_group[i], w_router_expert[i], b_router_expert[i],
                 w_exp_gate[i], w_exp_up[i], w_exp_down[i])
        h2 = modulate(rmsnorm(x, norm2_g[i]), sh2, sc2)
        x = x + g2 * hier_moe(h2.reshape(-1, D_MODEL), *moe_w).reshape(x.shape)
        if not last:
            mix_c = jnp.concatenate([ssd_finish(y_c, xs_c, z_c, d_skip[i], ssd_norm_g[i]),
                                     chunk_gmlp(u_c, v_c, cm_ln_g[i], cm_ln_b[i], w_spatial[i], b_spatial[i])],
                                    axis=-1)
            ctx = ctx + cg1 * (mix_c @ w_out[i])
            h2c = modulate(rmsnorm(ctx, norm2_g[i]), csh2, csc2)
            ctx = ctx + cg2 * hier_moe(h2c.reshape(-1, D_MODEL), *moe_w).reshape(ctx.shape)
    return rmsnorm(x, normf_g)
```

```python
from contextlib import ExitStack
import numpy as np
import concourse.bass as bass
import concourse.mybir as mybir
from concourse.bass_utils import run_bass_kernel_spmd

F32 = mybir.dt.float32
BF16 = mybir.dt.bfloat16
AF = mybir.ActivationFunctionType
OP = mybir.AluOpType
AX = mybir.AxisListType

D = 2048
NCORES = 8
EPS = 1e-6
ENGS = ("pe", "act", "dve", "pool", "sp")
SUB = 99
JOBS = None
NOTR = False
NCH = 8
NEXP = 32
NOCONV = False

PV_N1G, PV_N2G, PV_SNG, PV_CW, PV_CB, PV_BMOD = 0, 16, 32, 48, 120, 144
PV_N = 240
RP_DTB, RP_ALOG, RP_DSKIP, RP_BS, RP_BR = 0, 64, 128, 160, 1184
RP_N = 1220
C_ID, C_TU, C_TL, C_MNF, C_MNB, C_ONE = 0, 128, 256, 384, 512, 640
C_N = 768


class Prog:
    def __init__(self, nc, es):
        self.nc = nc
        self.es = es
        self.sems = {}
        self.cnt = {}
        self.ops = {e: [] for e in ENGS}
        self.known = {e: {} for e in ENGS}
        self.last_w = {}
        self.readers = {}
        self.latest = {}
        for e in ENGS:
            self._sem(e)

    def _sem(self, key):
        if key not in self.sems:
            self.sems[key] = self.es.enter_context(self.nc.semaphore("s_" + str(key)))
            self.cnt[key] = 0
        return self.sems[key]

    def op(self, eng, fn, reads=(), writes=(), dma=None):
        waits = {}
        def need(tok):
            sk, v = tok
            if sk == "pe" and eng == "pe":
                return
            if self.known[eng].get(sk, 0) >= v:
                return
            waits[sk] = max(waits.get(sk, 0), v)
        for k in reads:
            if k in self.last_w:
                need(self.last_w[k])
        for k in writes:
            if k in self.last_w:
                need(self.last_w[k])
            for r in self.readers.get(k, ()):
                need(r)
        for sk, v in waits.items():
            self.known[eng][sk] = v
        if dma is not None:
            self._sem(dma)
            self.cnt[dma] += 16
            tok = (dma, self.cnt[dma])
        else:
            self.cnt[eng] += 1
            tok = (eng, self.cnt[eng])
        self.latest[tok[0]] = tok[1]
        for k in writes:
            self.last_w[k] = tok
            self.readers[k] = []
        for k in reads:
            self.readers.setdefault(k, []).append(tok)
        self.ops[eng].append((list(waits.items()), fn, tok, dma is not None))
        return tok

    def barrier(self):
        for e in ENGS:
            waits = []
            for sk, v in self.latest.items():
                if sk == e and e == "pe":
                    continue
                if self.known[e].get(sk, 0) < v:
                    waits.append((sk, v))
                    self.known[e][sk] = v
            if waits:
                self.ops[e].append((waits, None, None, False))
        self.last_w.clear()
        self.readers.clear()

    def flush(self):
        nc = self.nc
        ops = self.ops
        sems = self.sems

        def run(engh, lst):
            for waits, fn, tok, isdma in lst:
                for sk, v in waits:
                    engh.wait_ge(sems[sk], v)
                if fn is None:
                    continue
                ins = fn(engh)
                ins.then_inc(sems[tok[0]], 16 if isdma else 1)

        with nc.Block() as block:
            if ops["sp"]:
                @block.sync
                def _(e):
                    run(e, ops["sp"])
            if ops["pe"]:
                @block.tensor
                def _(e):
                    run(e, ops["pe"])
            if ops["act"]:
                @block.scalar
                def _(e):
                    run(e, ops["act"])
            if ops["dve"]:
                @block.vector
                def _(e):
                    run(e, ops["dve"])
            if ops["pool"]:
                @block.gpsimd
                def _(e):
                    run(e, ops["pool"])
        self.ops = {e: [] for e in ENGS}


def build_nc(stage=99, taps=()):
    nc = bass.Bass("TRN2", target_bir_lowering=False)
    es = ExitStack()
    pg = Prog(nc, es)

    def din(name, shape, dt=F32):
        return nc.dram_tensor(name, list(shape), dt, kind="ExternalInput").ap()

    def dscr(name, shape, dt):
        kind = "ExternalOutput" if name in taps else "Internal"
        return nc.dram_tensor(name, list(shape), dt, kind=kind).ap()

    x_all = din("x_all", [2304, D])
    flags = din("flags", [128, 2])
    cvec = din("cvec", [128, 32])
    pvec = din("pvec", [128, PV_N])
    rowp = din("rowp", [128, RP_N])
    lnrows = din("lnrows", [128, 3 * D])
    consts = din("consts", [128, C_N])
    sel3 = din("sel3", [128, 4096])
    w_mod = din("w_mod", [D, 6 * D])
    w_in = din("w_in", [D, 9280])
    w_out = din("w_out", [2 * D, D])
    wsT = din("wsT", [128, 1024])
    wr = din("wr", [128, 16 * 36])
    w_g = din("w_g", [32, D, 512])
    w_u = din("w_u", [32, D, 512])
    w_d = din("w_d", [32, 512, D])
    out = nc.dram_tensor("out", [1024, D], F32, kind="ExternalOutput").ap()

    def sb(name, shape, dt=F32):
        return es.enter_context(nc.sbuf_tensor(name, list(shape), dt))

    def ps(name, shape, dt=F32):
        return es.enter_context(nc.psum_tensor(name, list(shape), dt))

    cst = sb("cst", [128, C_N])
    idb = sb("idb", [128, 128], BF16)
    sel3b = sb("sel3b", [128, 4096], BF16)
    pv = sb("pv", [128, PV_N])
    rp = sb("rp", [128, RP_N])
    flg = sb("flg", [128, 2])
    cv = sb("cv", [128, 32])
    scT = sb("scT", [128, 32], BF16)
    modT = sb("modT", [128, 192])
    AB = sb("AB", [128, 6, 16])
    G12 = sb("G12", [128, 2, 16])
    dtall = sb("dtall", [128, 18, 64])
    aneg = sb("aneg", [128, 64])
    decall = sb("decall", [128, 18, 64])

    ident = cst[:, C_ID:C_ID + 128]
    ones = cst[:, C_ONE:C_ONE + 128]

    def dma_sp(out_ap, in_ap, sem, reads=(), writes=()):
        pg.op("sp", lambda e: e.dma_start(out=out_ap, in_=in_ap), reads=reads, writes=writes, dma=sem)

    def dma_cast(out_ap, in_ap, sem, reads=(), writes=()):
        pg.op("pool", lambda e: e.dma_start(out=out_ap, in_=in_ap), reads=reads, writes=writes, dma=sem)

    dma_sp(cst[:], consts, "ld_c0", writes=["cst"])
    dma_sp(pv[:], pvec, "ld_c1", writes=["pv"])
    dma_sp(rp[:], rowp, "ld_c3", writes=["rp"])
    dma_sp(flg[:], flags, "ld_c4", writes=["flg"])
    dma_sp(cv[:], cvec, "ld_c5", writes=["cv"])
    dma_cast(sel3b[:], sel3, "ld_c2", writes=["sel3b"])
    pg.op("dve", lambda e: e.tensor_copy(out=idb[:], in_=ident), reads=["cst"], writes=["idb"])
    pg.op("act", lambda e: e.activation(out=scT[:], in_=cv[:], func=AF.Silu), reads=["cv"], writes=["scT"])
    pg.op("act", lambda e: e.activation(out=aneg[:], in_=rp[:, RP_ALOG:RP_ALOG + 64], func=AF.Exp),
          reads=["rp"], writes=["aneg"])
    pg.op("dve", lambda e: e.tensor_scalar_mul(out=aneg[:], in0=aneg[:], scalar1=-1.0),
          reads=["aneg"], writes=["aneg"])

    st = ExitStack()
    wb = [st.enter_context(nc.sbuf_tensor("p0w%d" % i, [128, 16, 512], BF16)) for i in range(2)]
    modps = st.enter_context(nc.psum_tensor("modps", [128, 192], F32))
    w_mod_v = w_mod.rearrange("(kc p) c -> p kc c", p=128)
    mps_cur = [modps]
    def mod_block(blk, wb):
        w = wb[blk % 2]
        key = "p0w%d" % (blk % 2)
        dma_cast(w[:], w_mod_v[:, :, blk * 512:(blk + 1) * 512], "ld_" + key, writes=[key])

        def mm(e, w=w, blk=blk):
            ins = None
            for cc in range(4):
                col = (blk * 4 + cc) * 2
                for kc in range(16):
                    ins = e.matmul(mps_cur[0][:, col:col + 2], lhsT=w[:, kc, cc * 128:(cc + 1) * 128],
                                   rhs=scT[:, kc * 2:kc * 2 + 2], start=(kc == 0), stop=(kc == 15))
            return ins
        pg.op("pe", mm, reads=[key, "scT"], writes=["modps"])

    def mod_finish(c0, c1, modps):
        pg.op("dve", lambda e: e.tensor_tensor(
            out=modT[:, c0 * 2:c1 * 2].rearrange("p (c t) -> p c t", t=2),
            in0=modps[:, c0 * 2:c1 * 2].rearrange("p (c t) -> p c t", t=2),
            in1=pv[:, PV_BMOD + c0:PV_BMOD + c1].unsqueeze(2).to_broadcast([128, c1 - c0, 2]), op=OP.add),
            reads=["modps", "pv"], writes=["modT"])
    for blk in range(8):
        mod_block(blk, wb)
    mod_finish(0, 32, modps)
    mt = modT[:].rearrange("p (m kc t) -> p m kc t", m=6, kc=16, t=2)
    def ab(e_idx, g_off, sc_m, sh_m, which):
        pg.op("dve", lambda e: e.scalar_tensor_tensor(
            out=AB[:, e_idx, :], in0=mt[:, sc_m, :, which], scalar=1.0, in1=pv[:, g_off:g_off + 16],
            op0=OP.add, op1=OP.mult), reads=["modT", "pv"], writes=["AB%d" % e_idx])
        pg.op("dve", lambda e: e.tensor_copy(out=AB[:, e_idx + 1, :], in_=mt[:, sh_m, :, which]),
              reads=["modT"], writes=["AB%d" % (e_idx + 1)])
    ab(0, PV_N1G, 1, 0, 0)
    ab(2, PV_N1G, 1, 0, 1)
    if "t_modT" in taps:
        t_modT = nc.dram_tensor("t_modT", [128, 192], F32, kind="ExternalOutput").ap()
        dma_sp(t_modT, modT[:], "st_tap", reads=["modT"])
    pg.barrier()
    pg.flush()
    st.close()
    if stage <= 0:
        return finish(nc, pg, es, out)


    S_all = dscr("S_all", [18, 2, 128, 2048], F32)
    xs_s = dscr("xs_s", [8, 128, 2048], BF16)
    bt_s = dscr("bt_s", [8, 128, 4, 128], BF16)
    ct_s = dscr("ct_s", [8, 128, 4, 128], BF16)
    z_s = dscr("z_s", [8, 128, 2048], BF16)
    v_s = dscr("v_s", [8, 128, 2048], BF16)
    u_s = dscr("u_s", [8, 128, 16, 128], BF16)
    hp_s = dscr("hp_s", [2, 8, 128, 2048], BF16)
    mix_s = dscr("mix_s", [8, 128, 32, 128], BF16)

    st = ExitStack()
    def sb1(name, shape, dt=F32):
        return st.enter_context(nc.sbuf_tensor(name, list(shape), dt))
    def ps1(name, shape, dt=F32):
        return st.enter_context(nc.psum_tensor(name, list(shape), dt))
    xin = sb1("xin", [128, 4, 2048])
    hT = sb1("hT", [128, 16, 512], BF16)
    wb = [sb1("wb%d" % i, [128, 16, 512], BF16) for i in range(2)]
    t0s = [sb1("t0_%d" % i, [128, 512]) for i in range(2)]
    sbf = [sb1("sbf%d" % i, [128, 512], BF16) for i in range(2)]
    sraws = [sb1("sraw%d" % i, [128, 512]) for i in range(2)]
    xs_tok = sb1("xs_tok", [128, 4, 2048], BF16)
    B_tok = sb1("B_tok", [128, 4, 512], BF16)
    BTs = sb1("BTs", [128, 4, 512], BF16)
    CTs = sb1("CTs", [128, 4, 512], BF16)
    stg = sb1("stg", [128, 4, 2048], BF16)
    lnr = sb1("lnr", [128, 2, 2048])
    xw = [sb1("xw%d" % i, [128, 2048], BF16) for i in range(2)]
    Sst = [sb1("Sst%d" % i, [128, 2048]) for i in range(2)]
    sm = sb1("sm", [128, 16, 64])
    ssq = sb1("ssq", [128, 16])
    wgt_t = sb1("wgt_t", [128, 4, 64])
    pA = [ps1("pA%d" % i, [128, 512]) for i in range(2)]
    pTf = [ps1("pTf%d" % i, [128, 512]) for i in range(2)]
    pS = [ps1("pS%d" % i, [128, 512]) for i in range(2)]
    pm = ps1("pm", [128, 512])
    stg_u = stg[:].rearrange("p t c -> p (t c)").rearrange("p (cc n) -> p cc n", cc=16)

    w_in_v = w_in.rearrange("(kc p) c -> p kc c", p=128)
    dma_sp(lnr[:].rearrange("p a c -> p (a c)"), lnrows[:, 0:2 * D], "ld_lnr", writes=["lnr"])
    wcnt = [0]
    acnt = [0]

    def load_w(c0, ncols):
        i = wcnt[0] % 2
        wcnt[0] += 1
        dma_cast(wb[i][:, :, 0:ncols], w_in_v[:, :, c0:c0 + ncols], "ld_wb%d" % i, writes=["wb%d" % i])
        return wb[i], "wb%d" % i

    def next_pA():
        i = acnt[0] % 2
        acnt[0] += 1
        return pA[i], "pA%d" % i, i

    blocks = [("ctx", 0, 2, 0), ("oth", 256, 4, 2), ("oth", 768, 4, 6), ("own", 1280, 4, 10), ("own", 1792, 4, 14)]
    if stage == 1:
        blocks = blocks[:1] + blocks[3:4]
    if SUB in (2, 3):
        blocks = blocks[:1]
    def do_block(kind, row0, NT, T0):
        N = NT * 128
        own = kind == "own"
        Aidx = 2 if kind == "ctx" else 0
        pg.op("dve", lambda e: e.memset(ssq[:], 0.0), writes=["ssq"])
        for t in range(NT):
            dma_sp(xin[:, t, :], x_all[row0 + t * 128:row0 + (t + 1) * 128, :], "ld_xin%d" % t, writes=[("xin", t)])
            pg.op("act", lambda e, t=t: e.activation(out=xw[0][:], in_=xin[:, t, :], func=AF.Square,
                                                     accum_out=ssq[:, t:t + 1]),
                  reads=[("xin", t), "ssq"], writes=["xw0", ("ssq", t)])
            pg.op("dve", lambda e, t=t: e.tensor_scalar(out=ssq[:, 8 + t:9 + t], in0=ssq[:, t:t + 1], scalar1=1.0 / D,
                                                        scalar2=EPS, op0=OP.mult, op1=OP.add),
                  reads=[("ssq", t)], writes=[("rs", t)])
            pg.op("act", lambda e, t=t: e.sqrt(out=ssq[:, 8 + t:9 + t], in_=ssq[:, 8 + t:9 + t]),
                  reads=[("rs", t)], writes=[("rs", t)])
            pg.op("dve", lambda e, t=t: e.reciprocal(out=ssq[:, 8 + t:9 + t], in_=ssq[:, 8 + t:9 + t]),
                  reads=[("rs", t)], writes=[("rs", t)])
            pg.op("act", lambda e, t=t: e.activation(out=xin[:, t, :], in_=xin[:, t, :], func=AF.Copy,
                                                     scale=ssq[:, 8 + t:9 + t]),
                  reads=[("xin", t), ("rs", t)], writes=[("xin", t)])
        if SUB <= 0:
            return
        for kc in range(16):
            pa, pak, _ = next_pA()
            def tr(e, pa=pa, kc=kc):
                ins = None
                for t in range(NT):
                    ins = e.transpose(out=pa[:, t * 128:(t + 1) * 128], in_=xin[:, t, kc * 128:(kc + 1) * 128],
                                      identity=ident)
                return ins
            pg.op("pe", tr, reads=[("xin", t) for t in range(NT)] + ["cst"], writes=[pak])
            pg.op("act", lambda e, pa=pa, kc=kc: e.activation(
                out=hT[:, kc, 0:N], in_=pa[:, 0:N], func=AF.Identity,
                bias=AB[:, Aidx + 1, kc:kc + 1], scale=AB[:, Aidx, kc:kc + 1]),
                reads=[pak, "AB%d" % Aidx, "AB%d" % (Aidx + 1)], writes=[("hT", kc)])
        hT_keys = [("hT", kc) for kc in range(16)]
        if SUB <= 1:
            return

        pending = []
        def fm_job(c0, jobkind, cb):
            w, wk = load_w(c0, 512)
            for cc in range(4):
                pa, pak, pi = next_pA()
                def mm(e, pa=pa, w=w, cc=cc):
                    ins = None
                    for kc in range(16):
                        ins = e.matmul(pa[:, 0:N], lhsT=w[:, kc, cc * 128:(cc + 1) * 128], rhs=hT[:, kc, 0:N],
                                       start=(kc == 0), stop=(kc == 15))
                    return ins
                pg.op("pe", mm, reads=[wk] + hT_keys, writes=[pak])
                while pending:
                    pending.pop(0)()
                if jobkind == "u":
                    pg.op("act", lambda e, pa=pa, cc=cc: e.activation(out=stg_u[:, cb * 4 + cc, 0:N], in_=pa[:, 0:N],
                                                                      func=AF.Gelu),
                          reads=[pak], writes=["stg"])
                    continue
                chn = (c0 - 2048) // 128 + cc
                L = 256 if kind == "ctx" else 64
                t0 = t0s[pi]
                t0k = "t0_%d" % pi
                sraw = sraws[pi]
                pg.op("act", lambda e, pa=pa, sraw=sraw: e.copy(out=sraw[:, 0:N], in_=pa[:, 0:N]),
                      reads=[pak], writes=["sraw%d" % pi])
                pav = sraw[:, 0:N].rearrange("p (r l) -> p r l", l=L)
                t0v = t0[:, 0:N].rearrange("p (r l) -> p r l", l=L)
                cw = lambda j, chn=chn: pv[:, PV_CW + j * 24 + chn:PV_CW + j * 24 + chn + 1]
                pg.op("act", lambda e, pa=pa, t0=t0, chn=chn, cw=cw: e.activation(
                    out=t0[:, 0:N], in_=pa[:, 0:N], func=AF.Identity,
                    bias=pv[:, PV_CB + chn:PV_CB + chn + 1], scale=cw(1)), reads=[pak, "pv"], writes=[t0k])
                if not NOCONV:
                  pg.op("dve", lambda e, pav=pav, t0v=t0v, cw=cw: e.scalar_tensor_tensor(
                    out=t0v[:, :, 1:L], in0=pav[:, :, 0:L - 1], scalar=cw(0), in1=t0v[:, :, 1:L],
                    op0=OP.mult, op1=OP.add), reads=["sraw%d" % pi, t0k, "pv"], writes=[t0k])
                if not NOCONV:
                  pg.op("dve", lambda e, pav=pav, t0v=t0v, cw=cw: e.scalar_tensor_tensor(
                    out=t0v[:, :, 0:L - 1], in0=pav[:, :, 1:L], scalar=cw(2), in1=t0v[:, :, 0:L - 1],
                    op0=OP.mult, op1=OP.add), reads=["sraw%d" % pi, t0k, "pv"], writes=[t0k])
                if jobkind == "xs":
                    dst, dk = sbf[pi][:, 0:N], "sbf%d" % pi
                elif jobkind == "B":
                    dst, dk = BTs[:, cc, 0:N], ("BTs", cc)
                else:
                    dst, dk = CTs[:, cc, 0:N], ("CTs", cc)
                pg.op("act", lambda e, t0=t0, dst=dst: e.activation(out=dst, in_=t0[:, 0:N], func=AF.Silu),
                      reads=[t0k], writes=[dk])
                if jobkind in ("xs", "B") and not NOTR:
                    def emit_tr(dst=dst, dk=dk, pi=pi, cc=cc, jobkind=jobkind, cb=cb):
                        ptf, ptk = pTf[pi], "pTf%d" % pi
                        def tr2(e):
                            ins = None
                            for t in range(NT):
                                ins = e.matmul(ptf[:, t * 128:(t + 1) * 128], lhsT=dst[:, t * 128:(t + 1) * 128],
                                               rhs=idb[:], start=True, stop=True)
                            return ins
                        pg.op("pe", tr2, reads=[dk, "idb"], writes=[ptk])
                        if jobkind == "xs":
                            o = xs_tok[:, 0:NT, cb * 512 + cc * 128:cb * 512 + (cc + 1) * 128]
                            ok = [("xs_tok", t, cb) for t in range(NT)]
                        else:
                            o = B_tok[:, 0:NT, cc * 128:(cc + 1) * 128]
                            ok = [("B_tok", t) for t in range(NT)]
                        iv = ptf[:, 0:N].rearrange("p (t c) -> p t c", c=128)
                        if cc % 2 == 0:
                            pg.op("dve", lambda e: e.tensor_copy(out=o, in_=iv), reads=[ptk], writes=ok)
                        else:
                            pg.op("act", lambda e: e.copy(out=o, in_=iv), reads=[ptk], writes=ok)
                    pending.append(emit_tr)

        def tm_job(c0, ncols, jobkind, cb):
            if jobkind == "dt":
                w, wk = load_w(4672, 512)
                wofs = 448
            else:
                w, wk = load_w(c0, ncols)
                wofs = 0
            for t in range(NT):
                pa, pak, pi = next_pA()
                def mm(e, pa=pa, w=w, t=t):
                    ins = None
                    for kc in range(16):
                        ins = e.matmul(pa[:, 0:ncols], lhsT=hT[:, kc, t * 128:(t + 1) * 128], rhs=w[:, kc, wofs:wofs + ncols],
                                       start=(kc == 0), stop=(kc == 15))
                    return ins
                pg.op("pe", mm, reads=[wk] + hT_keys, writes=[pak])
                while pending:
                    pending.pop(0)()
                if jobkind == "z":
                    pg.op("act", lambda e, pa=pa, t=t: e.activation(out=stg[:, t, cb * 512:(cb + 1) * 512], in_=pa[:],
                                                                    func=AF.Silu), reads=[pak], writes=["stg"])
                elif jobkind == "v":
                    pg.op("act", lambda e, pa=pa, t=t: e.activation(out=xin[:, t, cb * 512:(cb + 1) * 512], in_=pa[:],
                                                                    func=AF.Gelu), reads=[pak], writes=[("xin", t)])
                else:
                    T = T0 + t
                    a, b_, c_, d_ = sm[:, 0, :], sm[:, 1, :], sm[:, 2, :], sm[:, 3, :]
                    pg.op("dve", lambda e, pa=pa: e.tensor_tensor(out=a, in0=pa[:, 0:64], in1=rp[:, RP_DTB:RP_DTB + 64],
                                                                  op=OP.add), reads=[pak, "rp"], writes=["sm0"])
                    pg.op("dve", lambda e: e.tensor_scalar_mul(out=b_, in0=a, scalar1=-1.0),
                          reads=["sm0"], writes=["sm1"])
                    pg.op("dve", lambda e: e.tensor_tensor(out=b_, in0=b_, in1=a, op=OP.min),
                          reads=["sm0", "sm1"], writes=["sm1"])
                    pg.op("act", lambda e: e.activation(out=c_, in_=b_, func=AF.Exp),
                          reads=["sm1"], writes=["sm2"])
                    pg.op("dve", lambda e: e.tensor_scalar_add(out=c_, in0=c_, scalar1=1.0),
                          reads=["sm2"], writes=["sm2"])
                    pg.op("act", lambda e: e.activation(out=c_, in_=c_, func=AF.Ln),
                          reads=["sm2"], writes=["sm2"])
                    pg.op("dve", lambda e, T=T: e.scalar_tensor_tensor(out=dtall[:, T, :], in0=a, scalar=0.0, in1=c_,
                                                                       op0=OP.max, op1=OP.add),
                          reads=["sm0", "sm2"], writes=[("dtall", T)])
                    if kind == "oth":
                        for dr in range(2):
                            pg.op("dve", lambda e, T=T, dr=dr: e.tensor_scalar_mul(
                                out=dtall[:, T, dr * 32:(dr + 1) * 32], in0=dtall[:, T, dr * 32:(dr + 1) * 32],
                                scalar1=flg[:, dr:dr + 1]), reads=[("dtall", T), "flg"], writes=[("dtall", T)])

        if own:
            for cb in range(4):
                tm_job(cb * 512, 512, "z", cb)
            for t in range(NT):
                dma_sp(z_s[T0 - 10 + t], stg[:, t, :], "st_stg", reads=["stg"])
        for cb in range(4):
            if JOBS is None or "xs" in JOBS:
                fm_job(2048 + cb * 512, "xs", cb)
        if JOBS is None or "B" in JOBS:
            fm_job(4096, "B", 0)
        if own:
            fm_job(4608, "C", 0)
        if JOBS is None or "dt" in JOBS:
            tm_job(5120, 64, "dt", 0)
        while pending:
            pending.pop(0)()
        if own:
            for t in range(NT):
                dma_sp(xs_s[T0 - 10 + t], xs_tok[:, t, :], "st_xs%d" % t, reads=[("xs_tok", t, cb) for cb in range(4)])
                dma_sp(bt_s[T0 - 10 + t], BTs[:, :, t * 128:(t + 1) * 128], "st_bt",
                       reads=[("BTs", cc) for cc in range(4)])
                dma_sp(ct_s[T0 - 10 + t], CTs[:, :, t * 128:(t + 1) * 128], "st_ct",
                       reads=[("CTs", cc) for cc in range(4)])
            for cb in range(4):
                fm_job(5184 + cb * 512, "u", cb)
            for t in range(NT):
                dma_sp(u_s[T0 - 10 + t], stg_u[:, :, t * 128:(t + 1) * 128], "st_stg", reads=["stg"])
            for cb in range(4):
                tm_job(7232 + cb * 512, 512, "v", cb)
            pg.op("dve", lambda e: e.memset(ssq[:], 0.0), writes=["ssq"] + [("ssq", t) for t in range(4)])
            for t in range(NT):
                pg.op("act", lambda e, t=t: e.activation(out=xw[0][:], in_=xin[:, t, :], func=AF.Identity,
                                                         accum_out=ssq[:, t:t + 1]),
                      reads=[("xin", t), "ssq"], writes=["xw0", ("ssq", t)])
                pg.op("act", lambda e, t=t: e.activation(out=xw[0][:], in_=xin[:, t, :], func=AF.Square,
                                                         accum_out=ssq[:, 4 + t:5 + t]),
                      reads=[("xin", t), "ssq"], writes=["xw0", ("ssq", t)])
                mean, var, rs_, nmr = (ssq[:, 8 + t:9 + t], ssq[:, 12 + t:13 + t], ssq[:, 12 + t:13 + t], ssq[:, 8 + t:9 + t])
                k = ("ssq", t)
                pg.op("dve", lambda e, t=t, mean=mean: e.tensor_scalar_mul(out=mean, in0=ssq[:, t:t + 1], scalar1=1.0 / D),
                      reads=[k], writes=[k])
                pg.op("dve", lambda e, t=t, mean=mean, var=var: e.tensor_tensor(out=var, in0=mean, in1=mean, op=OP.mult),
                      reads=[k], writes=[k])
                pg.op("dve", lambda e, t=t, var=var: e.scalar_tensor_tensor(
                    out=var, in0=ssq[:, 4 + t:5 + t], scalar=1.0 / D, in1=var, op0=OP.mult, op1=OP.subtract),
                    reads=[k], writes=[k])
                pg.op("dve", lambda e, var=var: e.tensor_scalar_add(out=var, in0=var, scalar1=EPS), reads=[k], writes=[k])
                pg.op("act", lambda e, var=var: e.sqrt(out=var, in_=var), reads=[k], writes=[k])
                pg.op("dve", lambda e, var=var: e.reciprocal(out=var, in_=var), reads=[k], writes=[k])
                pg.op("dve", lambda e, mean=mean, var=var: e.scalar_tensor_tensor(
                    out=mean, in0=mean, scalar=-1.0, in1=var, op0=OP.mult, op1=OP.mult), reads=[k], writes=[k])
                pg.op("act", lambda e, t=t, mean=mean, var=var: e.activation(
                    out=xin[:, t, :], in_=xin[:, t, :], func=AF.Identity, bias=mean, scale=var),
                    reads=[("xin", t), k], writes=[("xin", t)])
                pg.op("dve", lambda e, t=t: e.tensor_tensor(out=xin[:, t, :], in0=xin[:, t, :], in1=lnr[:, 0, :], op=OP.mult),
                      reads=[("xin", t), "lnr"], writes=[("xin", t)])
                pg.op("dve", lambda e, t=t: e.tensor_tensor(out=stg[:, t, :], in0=xin[:, t, :], in1=lnr[:, 1, :], op=OP.add),
                      reads=[("xin", t), "lnr"], writes=["stg"])
                dma_sp(v_s[T0 - 10 + t], stg[:, t, :], "st_stg", reads=["stg"])

        if SUB <= 2:
            return
        while pending:
            pending.pop(0)()
        W = NT * 64
        dtab, acsb, ddb, wgtb = sm[:, 4:8, :], sm[:, 8:12, :], sm[:, 12:16, :], wgt_t[:]
        f2 = lambda ap: ap[:, 0:NT, :]
        dtk = [("dtall", T0 + t) for t in range(NT)]
        pg.op("dve", lambda e: e.tensor_tensor(out=f2(dtab), in0=dtall[:, T0:T0 + NT, :],
                                               in1=aneg[:].unsqueeze(1).to_broadcast([128, NT, 64]), op=OP.mult),
              reads=dtk + ["aneg"], writes=["sm4"])
        def mm2(e):
            pmv = pm[:, 0:W].rearrange("p (t c) -> p t c", c=64)
            for dr in range(2):
                tri = cst[:, C_TU:C_TU + 128] if dr == 0 else cst[:, C_TL:C_TL + 128]
                e.matmul(pmv[:, :, dr * 32:(dr + 1) * 32], lhsT=tri, rhs=f2(dtab)[:, :, dr * 32:(dr + 1) * 32],
                         start=True, stop=True)
            return e.matmul(pm[:, 256:256 + W].rearrange("p (t c) -> p t c", c=64), lhsT=ones, rhs=f2(dtab),
                            start=True, stop=True)
        pg.op("pe", mm2, reads=["sm4", "cst"], writes=["pm"])
        pg.op("act", lambda e: e.copy(out=f2(acsb), in_=pm[:, 0:W].rearrange("p (t c) -> p t c", c=64)),
              reads=["pm"], writes=["sm5"])
        pg.op("act", lambda e: e.activation(out=decall[:, T0:T0 + NT, :],
                                            in_=pm[:, 256:256 + W].rearrange("p (t c) -> p t c", c=64), func=AF.Exp),
              reads=["pm"], writes=[("decall", T0 + t, d_) for t in range(NT) for d_ in range(2)])
        pg.op("dve", lambda e: e.tensor_tensor(out=f2(ddb), in0=pm[:, 256:256 + W].rearrange("p (t c) -> p t c", c=64),
                                               in1=f2(acsb), op=OP.subtract), reads=["pm", "sm5"], writes=["sm6"])
        pg.op("act", lambda e: e.activation(out=f2(ddb), in_=f2(ddb), func=AF.Exp), reads=["sm6"], writes=["sm6"])
        pg.op("dve", lambda e: e.tensor_tensor(out=f2(wgtb), in0=f2(ddb), in1=dtall[:, T0:T0 + NT, :], op=OP.mult),
              reads=["sm6"] + dtk, writes=["sm7"])
        for t in range(NT):
            T = T0 + t
            for dr in range(2):
                xwt = xw[dr]
                pg.op("dve", lambda e, t=t, dr=dr, xwt=xwt: e.tensor_tensor(
                    out=xwt[:].rearrange("p (h d) -> p h d", d=64),
                    in0=xs_tok[:, t, :].rearrange("p (h d) -> p h d", d=64),
                    in1=wgtb[:, t, dr * 32:(dr + 1) * 32].unsqueeze(2).to_broadcast([128, 32, 64]), op=OP.mult),
                    reads=[("xs_tok", t, cb) for cb in range(4)] + ["sm7"], writes=["xw%d" % dr])
                sst = Sst[dr]
                for g in range(4):
                    psg, psk = pS[g % 2], "pS%d" % (g % 2)
                    pg.op("pe", lambda e, psg=psg, t=t, g=g, xwt=xwt: e.matmul(
                        psg[:], lhsT=B_tok[:, t, g * 128:(g + 1) * 128], rhs=xwt[:, g * 512:(g + 1) * 512],
                        start=True, stop=True), reads=[("B_tok", t), "xw%d" % dr], writes=[psk])
                    if g % 2 == 0:
                        pg.op("act", lambda e, psg=psg, g=g, sst=sst: e.copy(out=sst[:, g * 512:(g + 1) * 512], in_=psg[:]),
                              reads=[psk], writes=[("Sst", dr, g)])
                    else:
                        pg.op("dve", lambda e, psg=psg, g=g, sst=sst: e.tensor_copy(out=sst[:, g * 512:(g + 1) * 512], in_=psg[:]),
                              reads=[psk], writes=[("Sst", dr, g)])
                dma_sp(S_all[T, dr], sst[:], "st_S%d" % dr, reads=[("Sst", dr, g) for g in range(4)])
    for blk_ in blocks:
        do_block(*blk_)
    if "t_dt" in taps:
        t_dt = nc.dram_tensor("t_dt", [128, 18 * 64], F32, kind="ExternalOutput").ap()
        dma_sp(t_dt, dtall[:].rearrange("p a b -> p (a b)"), "st_tap", reads=[("dtall", T) for T in range(18)])
        t_dec = nc.dram_tensor("t_dec", [128, 18 * 64], F32, kind="ExternalOutput").ap()
        dma_sp(t_dec, decall[:].rearrange("p a b -> p (a b)"), "st_tap2", reads=[("decall", T, d_) for T in range(18) for d_ in range(2)])
    pg.barrier()
    pg.flush()
    st.close()
    if stage <= 1:
        return finish(nc, pg, es, out)


    st = ExitStack()
    hst = st.enter_context(nc.sbuf_tensor("hst", [128, 2048], F32))
    Sld = [st.enter_context(nc.sbuf_tensor("Sld%d" % i, [128, 2048], F32)) for i in range(2)]
    hpb = [st.enter_context(nc.sbuf_tensor("hpb%d" % i, [128, 2048], BF16)) for i in range(2)]
    wb3 = [st.enter_context(nc.sbuf_tensor("p3w%d" % i, [128, 16, 512], BF16)) for i in range(2)]
    modps3 = st.enter_context(nc.psum_tensor("modps3", [128, 192], F32))
    mps_cur[0] = modps3
    for blk in range(8, 24):
        mod_block(blk, wb3)
    mod_finish(32, 96, modps3)
    ab(4, PV_N2G, 4, 3, 0)
    pg.op("dve", lambda e: e.tensor_copy(out=G12[:, 0, :], in_=mt[:, 2, :, 0]), reads=["modT"], writes=["G12a"])
    pg.op("dve", lambda e: e.tensor_copy(out=G12[:, 1, :], in_=mt[:, 5, :, 0]), reads=["modT"], writes=["G12b"])
    for dr in range(2):
        pg.op("dve", lambda e: e.memset(hst[:], 0.0), writes=["hst"])
        if dr == 0:
            chain = list(range(0, 18))
        else:
            chain = [1, 0] + list(range(9, 1, -1)) + list(range(17, 9, -1))
        for i, T in enumerate(chain):
            sl, slk = Sld[i % 2], "Sld%d" % (i % 2)
            dma_sp(sl[:], S_all[T, dr], "ld_" + slk, writes=[slk])
            if T >= 10:
                hb, hbk = hpb[i % 2], "hpb%d" % (i % 2)
                pg.op("act", lambda e, hb=hb: e.copy(out=hb[:], in_=hst[:]), reads=["hst"], writes=[hbk])
                dma_sp(hp_s[dr, T - 10], hb[:], "st_" + hbk, reads=[hbk])
            pg.op("dve", lambda e, T=T, dr=dr: e.tensor_tensor(
                out=hst[:].rearrange("p (h d) -> p h d", d=64), in0=hst[:].rearrange("p (h d) -> p h d", d=64),
                in1=decall[:, T, dr * 32:(dr + 1) * 32].unsqueeze(2).to_broadcast([128, 32, 64]), op=OP.mult),
                reads=["hst", ("decall", T, dr)], writes=["hst"])
            pg.op("dve", lambda e, sl=sl: e.tensor_tensor(out=hst[:], in0=hst[:], in1=sl[:], op=OP.add),
                  reads=["hst", slk], writes=["hst"])
    pg.barrier()
    pg.flush()
    st.close()
    if stage <= 3:
        return finish(nc, pg, es, out)


    st = ExitStack()
    def sb4(name, shape, dt=F32):
        return st.enter_context(nc.sbuf_tensor(name, list(shape), dt))
    def ps4(name, shape, dt=F32):
        return st.enter_context(nc.psum_tensor(name, list(shape), dt))
    xs_c = [sb4("xs_c%d" % i, [128, 2048], BF16) for i in range(2)]
    bt_c = [sb4("bt_c%d" % i, [128, 4, 128], BF16) for i in range(2)]
    ct_c = [sb4("ct_c%d" % i, [128, 4, 128], BF16) for i in range(2)]
    hpf_c = [sb4("hpf_c%d" % i, [128, 2048], BF16) for i in range(2)]
    hpb_c = [sb4("hpb_c%d" % i, [128, 2048], BF16) for i in range(2)]
    z_c = [sb4("z_c%d" % i, [128, 2048], BF16) for i in range(2)]
    v_c = [sb4("v_c%d" % i, [128, 2048], BF16) for i in range(2)]
    u_c = [sb4("u_c%d" % i, [128, 16, 128], BF16) for i in range(2)]
    mixb = [sb4("mixb%d" % i, [128, 32, 128], BF16) for i in range(2)]
    wsb = sb4("wsb", [128, 8, 128], BF16)
    dta3 = sb4("dta3", [128, 96])
    acs = sb4("acs", [128, 64])
    nacs = sb4("nacs", [128, 64])
    ecum = sb4("ecum", [128, 64])
    xdt = [sb4("xdt%d" % i, [128, 2048], BF16) for i in range(2)]
    pcs = [sb4("pcs%d" % i, [128, 128], BF16) for i in range(2)]
    tbb = sb4("tbb", [128, 128], BF16)
    Rr = sb4("Rr", [128, 128])
    R2 = sb4("R2", [128, 128])
    cbT = sb4("cbT", [128, 4, 128])
    dwork = sb4("dwork", [128, 8, 128])
    Mm = [sb4("Mm%d" % i, [128, 8, 128], BF16) for i in range(2)]
    t1 = sb4("t1", [128, 512])
    t2 = sb4("t2", [128, 512])
    yb = sb4("yb", [128, 2048])
    yn = sb4("yn", [128, 2048], BF16)
    gt = sb4("gt", [128, 512])
    ss4 = sb4("ss4", [128, 4])
    pm2 = ps4("pm2", [128, 512])
    pcb = ps4("pcb", [128, 512])
    pD = ps4("pD", [128, 1024])
    pY = ps4("pY", [128, 512])
    pOf = ps4("pOf", [128, 512])
    pOb = ps4("pOb", [128, 512])
    pGa = ps4("pGa", [128, 512])
    pG = [pGa, pcb]
    pGk = ["pGa", "pcb"]
    dma_cast(wsb[:].rearrange("p g i -> p (g i)"), wsT, "ld_wsb", writes=["wsb"])
    dsk = rp[:, RP_DSKIP:RP_DSKIP + 32]

    def do_chunk(c):
        T = 10 + c
        i2 = c % 2
        xs, bt, ct, hpf, hpb_, zc, vc, uc, mix = (xs_c[i2], bt_c[i2], ct_c[i2], hpf_c[i2], hpb_c[i2], z_c[i2],
                                                   v_c[i2], u_c[i2], mixb[i2])
        K = lambda n: "%s%d" % (n, i2)
        dma_sp(xs[:], xs_s[c], "ld_" + K("xs"), writes=[K("xs")])
        dma_sp(bt[:], bt_s[c], "ld_" + K("bt"), writes=[K("bt")])
        dma_sp(ct[:], ct_s[c], "ld_" + K("ct"), writes=[K("ct")])
        dma_sp(hpf[:], hp_s[0, c], "ld_" + K("hpf"), writes=[K("hpf")])
        dma_sp(hpb_[:], hp_s[1, c], "ld_" + K("hpb"), writes=[K("hpb")])
        dma_sp(zc[:], z_s[c], "ld_" + K("z"), writes=[K("z")])
        dma_sp(vc[:], v_s[c], "ld_" + K("v"), writes=[K("v")])
        dma_sp(uc[:], u_s[c], "ld_" + K("u"), writes=[K("u")])
        def mmcb(e):
            ins = None
            for g in range(4):
                ins = e.matmul(pcb[:, g * 128:(g + 1) * 128], lhsT=bt[:, g, :], rhs=ct[:, g, :], start=True, stop=True)
            return ins
        pg.op("pe", mmcb, reads=[K("bt"), K("ct")], writes=["pcb"])
        pg.op("act", lambda e: e.copy(out=cbT[:].rearrange("p g i -> p (g i)"), in_=pcb[:]), reads=["pcb"], writes=["cbT"])
        for dr in range(2):
            tri = cst[:, C_TU:C_TU + 128] if dr == 0 else cst[:, C_TL:C_TL + 128]
            pg.op("dve", lambda e, dr=dr: e.tensor_tensor(
                out=dta3[:].rearrange("p (r h) -> p r h", h=32),
                in0=dtall[:, T, dr * 32:(dr + 1) * 32].unsqueeze(1).to_broadcast([128, 3, 32]),
                in1=aneg[:, dr * 32:(dr + 1) * 32].unsqueeze(1).to_broadcast([128, 3, 32]), op=OP.mult),
                reads=["aneg"], writes=["dta3"])
            def mmac(e, dr=dr, tri=tri):
                e.matmul(pm2[:, dr * 32:(dr + 1) * 32], lhsT=tri, rhs=dta3[:, 0:32], start=True, stop=True)
                return e.matmul(pm2[0:96, 64 + dr * 128:64 + (dr + 1) * 128], lhsT=dta3[:, 0:96], rhs=tri,
                                start=True, stop=True)
            pg.op("pe", mmac, reads=["dta3", "cst"], writes=[("pm2", dr)])
            sl = slice(dr * 32, (dr + 1) * 32)
            pg.op("act", lambda e, sl=sl: e.copy(out=acs[:, sl], in_=pm2[:, sl]), reads=[("pm2", dr)], writes=[("acs", dr)])
            pg.op("dve", lambda e, sl=sl: e.tensor_scalar_mul(out=nacs[:, sl], in0=acs[:, sl], scalar1=-1.0),
                  reads=[("acs", dr)], writes=[("nacs", dr)])
            pg.op("act", lambda e, sl=sl: e.activation(out=ecum[:, sl], in_=acs[:, sl], func=AF.Exp),
                  reads=[("acs", dr)], writes=[("ecum", dr)])
            src_ = pm2[:, 64 + dr * 128:64 + (dr + 1) * 128]
            pc = pcs[dr]
            pk = "pcs%d" % dr
            pg.op("act", lambda e, pc=pc, src_=src_: e.copy(out=pc[0:32, :], in_=src_[0:32, :]),
                  reads=[("pm2", dr)], writes=[(pk, 0)])
            for lo in (32, 64):
                pg.op("act", lambda e, src_=src_, lo=lo: e.copy(out=tbb[lo:lo + 32, :], in_=src_[lo:lo + 32, :]),
                      reads=[("pm2", dr)], writes=[("tbb", lo)])
                pg.op("dve", lambda e, src_=src_, lo=lo: e.tensor_tensor(out=Rr[lo:lo + 32, :], in0=src_[lo:lo + 32, :],
                                                                        in1=tbb[lo:lo + 32, :], op=OP.subtract),
                      reads=[("pm2", dr), ("tbb", lo)], writes=[("Rr", lo)])
            pg.op("act", lambda e, pc=pc: e.copy(out=pc[32:64, :], in_=Rr[32:64, :]), reads=[("Rr", 32)], writes=[(pk, 1)])
            pg.op("act", lambda e: e.copy(out=tbb[64:96, :], in_=Rr[64:96, :]), reads=[("Rr", 64)], writes=[("tbb", 64)])
            pg.op("dve", lambda e: e.tensor_tensor(out=R2[64:96, :], in0=Rr[64:96, :], in1=tbb[64:96, :], op=OP.subtract),
                  reads=[("Rr", 64), ("tbb", 64)], writes=["R2"])
            pg.op("act", lambda e, pc=pc: e.copy(out=pc[64:96, :], in_=R2[64:96, :]), reads=["R2"], writes=[(pk, 2)])
            pg.op("pool", lambda e, dr=dr: e.tensor_tensor(
                out=xdt[dr][:].rearrange("p (h d) -> p h d", d=64), in0=xs[:].rearrange("p (h d) -> p h d", d=64),
                in1=dtall[:, T, dr * 32:(dr + 1) * 32].unsqueeze(2).to_broadcast([128, 32, 64]), op=OP.mult),
                reads=[K("xs")], writes=["xdt%d" % dr])
        for g in range(4):
            for dr in range(2):
                mk = cst[:, C_MNF:C_MNF + 128] if dr == 0 else cst[:, C_MNB:C_MNB + 128]
                pc = pcs[dr]
                pk = "pcs%d" % dr
                def mmD(e, g=g, pc=pc):
                    ins = None
                    for hh in range(8):
                        h = g * 8 + hh
                        ins = e.matmul(pD[:, hh * 128:(hh + 1) * 128], lhsT=sel3b[0:96, h * 128:(h + 1) * 128],
                                       rhs=pc[0:96, :], start=True, stop=True)
                    return ins
                pg.op("pe", mmD, reads=[(pk, 0), (pk, 1), (pk, 2), "sel3b"], writes=["pD"])
                for hh in range(8):
                    h = g * 8 + hh
                    pg.op("act", lambda e, hh=hh, h=h, dr=dr: e.activation(
                        out=dwork[:, hh, :], in_=pD[:, hh * 128:(hh + 1) * 128], func=AF.Identity,
                        bias=nacs[:, dr * 32 + h:dr * 32 + h + 1], scale=1.0),
                        reads=["pD", ("nacs", dr)], writes=[("dwork", hh)])
                pg.op("dve", lambda e, mk=mk: e.tensor_tensor(out=dwork[:], in0=dwork[:],
                                                              in1=mk.unsqueeze(1).to_broadcast([128, 8, 128]), op=OP.add),
                      reads=[("dwork", hh) for hh in range(8)] + ["cst"], writes=["dwork"])
                pg.op("act", lambda e: e.activation(out=dwork[:], in_=dwork[:], func=AF.Exp), reads=["dwork"],
                      writes=["dwork"] + [("dwork", hh) for hh in range(8)])
                pg.op("dve", lambda e, g=g, dr=dr: e.tensor_tensor(
                    out=Mm[dr][:], in0=dwork[:], in1=cbT[:, g, :].unsqueeze(1).to_broadcast([128, 8, 128]), op=OP.mult),
                    reads=["dwork", "cbT"], writes=["Mm%d" % dr])
            def mmY(e, g=g):
                ins = None
                for hh in range(8):
                    h = g * 8 + hh
                    e.matmul(pY[:, hh * 64:(hh + 1) * 64], lhsT=Mm[0][:, hh, :], rhs=xdt[0][:, h * 64:(h + 1) * 64],
                             start=True, stop=False)
                    ins = e.matmul(pY[:, hh * 64:(hh + 1) * 64], lhsT=Mm[1][:, hh, :], rhs=xdt[1][:, h * 64:(h + 1) * 64],
                                   start=False, stop=True)
                return ins
            pg.op("pe", mmY, reads=["Mm0", "Mm1", "xdt0", "xdt1"], writes=["pY"])
            pg.op("pe", lambda e, g=g: e.matmul(pOf[:], lhsT=ct[:, g, :], rhs=hpf[:, g * 512:(g + 1) * 512],
                                                start=True, stop=True), reads=[K("ct"), K("hpf")], writes=["pOf"])
            pg.op("pe", lambda e, g=g: e.matmul(pOb[:], lhsT=ct[:, g, :], rhs=hpb_[:, g * 512:(g + 1) * 512],
                                                start=True, stop=True), reads=[K("ct"), K("hpb")], writes=["pOb"])
            v3 = lambda ap: ap.rearrange("p (h d) -> p h d", d=64)
            pg.op("dve", lambda e, g=g: e.tensor_tensor(
                out=v3(t1[:]), in0=v3(pOf[:]), in1=ecum[:, g * 8:(g + 1) * 8].unsqueeze(2).to_broadcast([128, 8, 64]),
                op=OP.mult), reads=["pOf", ("ecum", 0)], writes=["t1"])
            pg.op("dve", lambda e, g=g: e.tensor_tensor(
                out=v3(t2[:]), in0=v3(pOb[:]), in1=ecum[:, 32 + g * 8:32 + (g + 1) * 8].unsqueeze(2).to_broadcast([128, 8, 64]),
                op=OP.mult), reads=["pOb", ("ecum", 1)], writes=["t2"])
            pg.op("pool", lambda e: e.tensor_tensor(out=t1[:], in0=t1[:], in1=t2[:], op=OP.add), reads=["t1", "t2"],
                  writes=["t1"])
            pg.op("dve", lambda e, g=g: e.tensor_tensor(out=yb[:, g * 512:(g + 1) * 512], in0=pY[:], in1=t1[:], op=OP.add),
                  reads=["pY", "t1"], writes=[("yb", g)])
            pg.op("pool", lambda e, g=g: e.tensor_tensor(
                out=v3(t2[:]), in0=v3(xs[:, g * 512:(g + 1) * 512]),
                in1=dsk[:, g * 8:(g + 1) * 8].unsqueeze(2).to_broadcast([128, 8, 64]), op=OP.mult),
                reads=[K("xs"), "rp", "t2"], writes=["t2"])
            pg.op("pool", lambda e, g=g: e.tensor_tensor(out=yb[:, g * 512:(g + 1) * 512], in0=yb[:, g * 512:(g + 1) * 512],
                                                        in1=t2[:], op=OP.add), reads=[("yb", g), "t2"], writes=[("yb", g)])
        ybk = [("yb", g) for g in range(4)]
        pg.op("dve", lambda e: e.tensor_tensor(out=yb[:], in0=yb[:], in1=zc[:], op=OP.mult), reads=ybk + [K("z")], writes=ybk)
        pg.op("dve", lambda e: e.memset(ss4[:], 0.0), writes=["ss4"])
        pg.op("act", lambda e: e.activation(out=yn[:], in_=yb[:], func=AF.Square, accum_out=ss4[:, 0:1]),
              reads=ybk + ["ss4"], writes=["yn", "ss4"])
        pg.op("dve", lambda e: e.tensor_scalar(out=ss4[:, 1:2], in0=ss4[:, 0:1], scalar1=1.0 / D, scalar2=EPS,
                                               op0=OP.mult, op1=OP.add), reads=["ss4"], writes=["ss4"])
        pg.op("act", lambda e: e.sqrt(out=ss4[:, 1:2], in_=ss4[:, 1:2]), reads=["ss4"], writes=["ss4"])
        pg.op("dve", lambda e: e.reciprocal(out=ss4[:, 1:2], in_=ss4[:, 1:2]), reads=["ss4"], writes=["ss4"])
        pg.op("act", lambda e: e.activation(out=yn[:], in_=yb[:], func=AF.Copy, scale=ss4[:, 1:2]),
              reads=ybk + ["ss4"], writes=["yn"])
        for q in range(4):
            pgq, pgk = pG[q % 2], pGk[q % 2]
            def mmT(e, q=q, pgq=pgq):
                ins = None
                for j in range(4):
                    kc = q * 4 + j
                    ins = e.matmul(pgq[:, j * 128:(j + 1) * 128], lhsT=yn[:, kc * 128:(kc + 1) * 128], rhs=idb[:],
                                   start=True, stop=True)
                return ins
            pg.op("pe", mmT, reads=["yn", "idb"], writes=[pgk])
            for j in range(4):
                kc = q * 4 + j
                pg.op("act", lambda e, j=j, kc=kc, pgq=pgq: e.activation(
                    out=mix[:, kc, :], in_=pgq[:, j * 128:(j + 1) * 128], func=AF.Copy,
                    scale=pv[:, PV_SNG + kc:PV_SNG + kc + 1]), reads=[pgk, "pv"], writes=[(K("mix"), kc)])
        for q in range(4):
            pgq, pgk = pG[q % 2], pGk[q % 2]
            def mmG(e, q=q, pgq=pgq):
                ins = None
                for j in range(4):
                    cc = q * 4 + j
                    ins = e.matmul(pgq[:, j * 128:(j + 1) * 128], lhsT=vc[:, cc * 128:(cc + 1) * 128], rhs=wsb[:, cc // 2, :],
                                   start=True, stop=True)
                return ins
            pg.op("pe", mmG, reads=[K("v"), "wsb"], writes=[pgk])
            pg.op("dve", lambda e, q=q, pgq=pgq: e.tensor_tensor(
                out=gt[:].rearrange("p (a b i) -> p a b i", a=2, b=2),
                in0=pgq[:].rearrange("p (a b i) -> p a b i", a=2, b=2),
                in1=rp[:, RP_BS + q * 256:RP_BS + (q + 1) * 256].rearrange("p (a i) -> p a i", a=2).unsqueeze(2)
                .to_broadcast([128, 2, 2, 128]), op=OP.add), reads=[pgk, "rp"], writes=["gt"])
            pg.op("pool", lambda e, q=q: e.tensor_tensor(
                out=mix[:, 16 + q * 4:16 + (q + 1) * 4, :], in0=gt[:].rearrange("p (c i) -> p c i", i=128),
                in1=uc[:, q * 4:(q + 1) * 4, :], op=OP.mult), reads=["gt", K("u")], writes=[(K("mix"), 16 + q)])
        dma_sp(mix_s[c], mix[:], "st_" + K("mix"),
               reads=[(K("mix"), kc) for kc in range(20)])

    nch = NCH
    for c in range(nch):
        do_chunk(c)
    pg.barrier()
    pg.flush()
    st.close()
    if stage <= 4:
        return finish(nc, pg, es, out)


    st5 = ExitStack()
    x1T = st5.enter_context(nc.sbuf_tensor("x1T", [128, 16, 1024], F32))
    banks = [st5.enter_context(nc.psum_tensor("bk%d" % i, [128, 512], F32)) for i in range(8)]
    bkk = ["bk%d" % i for i in range(8)]
    st = ExitStack()
    mixblk = st.enter_context(nc.sbuf_tensor("mixblk", [128, 32, 512], BF16))
    xin5 = st.enter_context(nc.sbuf_tensor("xin5", [128, 4, 2048], F32))
    wo = [st.enter_context(nc.sbuf_tensor("wo%d" % i, [128, 32, 256], BF16)) for i in range(2)]
    tmp5 = [st.enter_context(nc.sbuf_tensor("tmp5_%d" % i, [128, 512], F32)) for i in range(2)]
    w_out_v = w_out.rearrange("(kc p) c -> p kc c", p=128)

    def do_p5(tb):
        for t in range(4):
            dma_sp(mixblk[:, :, t * 128:(t + 1) * 128], mix_s[tb * 4 + t], "ld_mixblk%d" % t, writes=[("mixblk", t)])
            r0 = 1280 + (tb * 4 + t) * 128
            dma_sp(xin5[:, t, :], x_all[r0:r0 + 128, :], "ld_xin5_%d" % t, writes=[("xin5", t)])
        for dcp in range(8):
            w = wo[dcp % 2]
            wk = "wo%d" % (dcp % 2)
            dma_cast(w[:], w_out_v[:, :, dcp * 256:(dcp + 1) * 256], "ld_" + wk, writes=[wk])
            for d2 in range(2):
                dc = dcp * 2 + d2
                i2 = dc % 2
                pa, pak = banks[i2], bkk[i2]
                px, pxk = banks[2 + i2], bkk[2 + i2]
                def mm(e, w=w, d2=d2, pa=pa):
                    ins = None
                    for kc in range(32):
                        ins = e.matmul(pa[:], lhsT=w[:, kc, d2 * 128:(d2 + 1) * 128], rhs=mixblk[:, kc, :],
                                       start=(kc == 0), stop=(kc == 31))
                    return ins
                pg.op("pe", mm, reads=[wk] + [("mixblk", t) for t in range(4)], writes=[pak])
                tm, tmk = tmp5[i2], "tmp5_%d" % i2
                pg.op("act", lambda e, tm=tm, pa=pa, dc=dc: e.activation(out=tm[:], in_=pa[:], func=AF.Copy,
                                                                         scale=G12[:, 0, dc:dc + 1]),
                      reads=[pak, "G12a"], writes=[tmk])
                def trx(e, px=px, dc=dc):
                    ins = None
                    for t in range(4):
                        ins = e.transpose(out=px[:, t * 128:(t + 1) * 128], in_=xin5[:, t, dc * 128:(dc + 1) * 128],
                                          identity=ident)
                    return ins
                pg.op("pe", trx, reads=[("xin5", t) for t in range(4)] + ["cst"], writes=[pxk])
                pg.op("dve", lambda e, px=px, tm=tm, dc=dc: e.tensor_tensor(
                    out=x1T[:, dc, tb * 512:(tb + 1) * 512], in0=px[:], in1=tm[:], op=OP.add),
                    reads=[pxk, tmk], writes=[("x1T", dc, tb)])
    for tb in range(2):
        do_p5(tb)
    if "t_x1T" in taps:
        t_x1T = nc.dram_tensor("t_x1T", [128, 16 * 1024], F32, kind="ExternalOutput").ap()
        dma_sp(t_x1T, x1T[:].rearrange("p a b -> p (a b)"), "st_tap5",
               reads=[("x1T", dc, tb) for dc in range(16) for tb in range(2)])
    pg.barrier()
    pg.flush()
    st.close()
    if stage <= 5:
        st5.close()
        return finish(nc, pg, es, out)


    st6 = ExitStack()
    h2T = st6.enter_context(nc.sbuf_tensor("h2T", [128, 16, 1024], BF16))
    cpc = st6.enter_context(nc.sbuf_tensor("cpc", [128, 1024], BF16))
    st = ExitStack()
    def sb6(name, shape, dt=F32):
        return st.enter_context(nc.sbuf_tensor(name, list(shape), dt))
    sq = [sb6("sq%d" % i, [128, 512]) for i in range(2)]
    rstd = sb6("rstd", [128, 1024])
    tmph = [sb6("tmph%d" % i, [128, 1024]) for i in range(2)]
    wrb = sb6("wrb", [128, 16, 36], BF16)
    lg = sb6("lg", [128, 8, 36])
    mg = sb6("mg", [128, 8])
    eg = sb6("eg", [128, 8, 4])
    sgm = sb6("sgm", [128, 8])
    tpg = sb6("tpg", [128, 8])
    ohg = sb6("ohg", [128, 8, 4])
    selx = sb6("selx", [128, 8, 8])
    tmp8 = sb6("tmp8", [128, 8, 8])
    m1 = sb6("m1", [128, 8])
    m2 = sb6("m2", [128, 8])
    mask1 = sb6("mask1", [128, 8, 8])
    mask2 = sb6("mask2", [128, 8, 8])
    sel2 = sb6("sel2", [128, 8, 8])
    p1 = sb6("p1", [128, 8])
    p2 = sb6("p2", [128, 8])
    wex = sb6("wex", [128, 8, 8])
    comb3 = sb6("comb3", [128, 8, 3, 32])
    ctb = sb6("ctb", [128, 1024], BF16)
    cR = sb6("cR", [128, 1024])
    cR2 = sb6("cR2", [128, 1024])
    dma_cast(wrb[:].rearrange("p a b -> p (a b)"), wr, "ld_wrb", writes=["wrb"])
    x1k = [("x1T", dc, tb) for dc in range(16) for tb in range(2)]
    for tb in range(2):
        for kc in range(16):
            s_, sk = sq[kc % 2], "sq%d" % (kc % 2)
            pg.op("act", lambda e, s_=s_, kc=kc, tb=tb: e.activation(out=s_[:], in_=x1T[:, kc, tb * 512:(tb + 1) * 512],
                                                                     func=AF.Square), reads=[("x1T", kc, tb)], writes=[sk])
            pg.op("pe", lambda e, s_=s_, kc=kc, tb=tb: e.matmul(banks[tb][:], lhsT=ones, rhs=s_[:], start=(kc == 0),
                                                               stop=(kc == 15)), reads=[sk, "cst"], writes=[bkk[tb]])
        sl = slice(tb * 512, (tb + 1) * 512)
        pg.op("dve", lambda e, tb=tb, sl=sl: e.tensor_scalar(out=rstd[:, sl], in0=banks[tb][:], scalar1=1.0 / D, scalar2=EPS,
                                                            op0=OP.mult, op1=OP.add), reads=[bkk[tb]], writes=[("rstd", tb)])
        pg.op("act", lambda e, sl=sl: e.sqrt(out=rstd[:, sl], in_=rstd[:, sl]), reads=[("rstd", tb)], writes=[("rstd", tb)])
        pg.op("dve", lambda e, sl=sl: e.reciprocal(out=rstd[:, sl], in_=rstd[:, sl]), reads=[("rstd", tb)], writes=[("rstd", tb)])
    for kc in range(16):
        th, thk = tmph[kc % 2], "tmph%d" % (kc % 2)
        pg.op("dve", lambda e, th=th, kc=kc: e.tensor_tensor(out=th[:], in0=x1T[:, kc, :], in1=rstd[:], op=OP.mult),
              reads=[("x1T", kc, 0), ("x1T", kc, 1), ("rstd", 0), ("rstd", 1)], writes=[thk])
        pg.op("act", lambda e, th=th, kc=kc: e.activation(out=h2T[:, kc, :], in_=th[:], func=AF.Identity,
                                                          bias=AB[:, 5, kc:kc + 1], scale=AB[:, 4, kc:kc + 1]),
              reads=[thk, "AB4", "AB5"], writes=[("h2T", kc)])
    h2k = [("h2T", kc) for kc in range(16)]
    for t in range(8):
        pr, prk = banks[2 + t % 2], bkk[2 + t % 2]
        def mmr(e, t=t, pr=pr):
            ins = None
            for kc in range(16):
                ins = e.matmul(pr[:, 0:36], lhsT=h2T[:, kc, t * 128:(t + 1) * 128], rhs=wrb[:, kc, :],
                               start=(kc == 0), stop=(kc == 15))
            return ins
        pg.op("pe", mmr, reads=h2k + ["wrb"], writes=[prk])
        pg.op("dve", lambda e, t=t, pr=pr: e.tensor_tensor(out=lg[:, t, :], in0=pr[:, 0:36], in1=rp[:, RP_BR:RP_BR + 36],
                                                          op=OP.add), reads=[prk, "rp"], writes=[("lg", t)])
    lgk = [("lg", t) for t in range(8)]
    lgG = lg[:, :, 0:4]
    bc = lambda ap, n: ap.unsqueeze(2).to_broadcast([128, 8, n])
    R_ = "rt"
    pg.op("dve", lambda e: e.tensor_reduce(out=mg[:], in_=lgG, axis=AX.X, op=OP.max), reads=lgk, writes=[R_])
    pg.op("dve", lambda e: e.tensor_tensor(out=eg[:], in0=lgG, in1=bc(mg[:], 4), op=OP.subtract), reads=lgk + [R_], writes=[R_])
    pg.op("act", lambda e: e.activation(out=eg[:], in_=eg[:], func=AF.Exp), reads=[R_], writes=[R_])
    pg.op("dve", lambda e: e.tensor_reduce(out=sgm[:], in_=eg[:], axis=AX.X, op=OP.add), reads=[R_], writes=[R_])
    pg.op("dve", lambda e: e.reciprocal(out=tpg[:], in_=sgm[:]), reads=[R_], writes=[R_])
    pg.op("dve", lambda e: e.tensor_tensor(out=ohg[:], in0=lgG, in1=bc(mg[:], 4), op=OP.is_equal), reads=lgk + [R_], writes=[R_])
    for g in range(4):
        lgE = lg[:, :, 4 + g * 8:4 + (g + 1) * 8]
        dst = selx if g == 0 else tmp8
        pg.op("dve", lambda e, g=g, lgE=lgE, dst=dst: e.tensor_tensor(
            out=dst[:], in0=lgE, in1=ohg[:, :, g:g + 1].to_broadcast([128, 8, 8]), op=OP.mult), reads=lgk + [R_], writes=[R_])
        if g > 0:
            pg.op("dve", lambda e: e.tensor_tensor(out=selx[:], in0=selx[:], in1=tmp8[:], op=OP.add), reads=[R_], writes=[R_])
    pg.op("dve", lambda e: e.tensor_reduce(out=m1[:], in_=selx[:], axis=AX.X, op=OP.max), reads=[R_], writes=[R_])
    pg.op("dve", lambda e: e.tensor_tensor(out=mask1[:], in0=selx[:], in1=bc(m1[:], 8), op=OP.is_equal), reads=[R_], writes=[R_])
    pg.op("dve", lambda e: e.tensor_scalar_mul(out=sel2[:], in0=mask1[:], scalar1=-1.0e30), reads=[R_], writes=[R_])
    pg.op("dve", lambda e: e.tensor_tensor(out=sel2[:], in0=sel2[:], in1=selx[:], op=OP.add), reads=[R_], writes=[R_])
    pg.op("dve", lambda e: e.tensor_reduce(out=m2[:], in_=sel2[:], axis=AX.X, op=OP.max), reads=[R_], writes=[R_])
    pg.op("dve", lambda e: e.tensor_tensor(out=mask2[:], in0=sel2[:], in1=bc(m2[:], 8), op=OP.is_equal), reads=[R_], writes=[R_])
    pg.op("dve", lambda e: e.tensor_tensor(out=p2[:], in0=m2[:], in1=m1[:], op=OP.subtract), reads=[R_], writes=[R_])
    pg.op("act", lambda e: e.activation(out=p2[:], in_=p2[:], func=AF.Exp), reads=[R_], writes=[R_])
    pg.op("dve", lambda e: e.tensor_scalar_add(out=p1[:], in0=p2[:], scalar1=1.0), reads=[R_], writes=[R_])
    pg.op("dve", lambda e: e.reciprocal(out=p1[:], in_=p1[:]), reads=[R_], writes=[R_])
    pg.op("dve", lambda e: e.tensor_tensor(out=p2[:], in0=p2[:], in1=p1[:], op=OP.mult), reads=[R_], writes=[R_])
    pg.op("dve", lambda e: e.tensor_tensor(out=p1[:], in0=p1[:], in1=tpg[:], op=OP.mult), reads=[R_], writes=[R_])
    pg.op("dve", lambda e: e.tensor_tensor(out=p2[:], in0=p2[:], in1=tpg[:], op=OP.mult), reads=[R_], writes=[R_])
    pg.op("dve", lambda e: e.tensor_tensor(out=wex[:], in0=mask1[:], in1=bc(p1[:], 8), op=OP.mult), reads=[R_], writes=[R_])
    pg.op("dve", lambda e: e.tensor_tensor(out=tmp8[:], in0=mask2[:], in1=bc(p2[:], 8), op=OP.mult), reads=[R_], writes=[R_])
    pg.op("dve", lambda e: e.tensor_tensor(out=wex[:], in0=wex[:], in1=tmp8[:], op=OP.add), reads=[R_], writes=[R_])
    for r in range(3):
        for g in range(4):
            pg.op("dve", lambda e, r=r, g=g: e.tensor_tensor(
                out=comb3[:, :, r, g * 8:(g + 1) * 8], in0=wex[:], in1=ohg[:, :, g:g + 1].to_broadcast([128, 8, 8]),
                op=OP.mult), reads=[R_], writes=[R_, ("comb3", r, g)])
    if "t_comb" in taps:
        t_comb = nc.dram_tensor("t_comb", [128, 8 * 96], F32, kind="ExternalOutput").ap()
        dma_sp(t_comb, comb3[:].rearrange("p a b c -> p (a b c)"), "st_tap6", reads=[R_])
    if "t_h2T" in taps:
        t_h2T = nc.dram_tensor("t_h2T", [128, 16 * 1024], BF16, kind="ExternalOutput").ap()
        dma_sp(t_h2T, h2T[:].rearrange("p a b -> p (a b)"), "st_tap7", reads=h2k)
    for t in range(8):
        pc_, pck = banks[4 + t // 4], bkk[4 + t // 4]
        pg.op("pe", lambda e, t=t, pc_=pc_: e.transpose(out=pc_[0:96, (t % 4) * 128:(t % 4 + 1) * 128],
                                                        in_=comb3[:, t, :, :].rearrange("p r c -> p (r c)"), identity=ident),
              reads=[R_, "cst"], writes=[(pck, t % 4)])
    for hb in range(2):
        src_ = banks[4 + hb]
        sk = [(bkk[4 + hb], j) for j in range(4)]
        sl = slice(hb * 512, (hb + 1) * 512)
        pg.op("act", lambda e, src_=src_, sl=sl: e.copy(out=cpc[0:32, sl], in_=src_[0:32, :]), reads=sk, writes=[("cpc", 0, hb)])
        for lo in (32, 64):
            pg.op("act", lambda e, src_=src_, sl=sl, lo=lo: e.copy(out=ctb[lo:lo + 32, sl], in_=src_[lo:lo + 32, :]),
                  reads=sk, writes=[("ctb", lo, hb)])
            pg.op("dve", lambda e, src_=src_, sl=sl, lo=lo: e.tensor_tensor(
                out=cR[lo:lo + 32, sl], in0=src_[lo:lo + 32, :], in1=ctb[lo:lo + 32, sl], op=OP.subtract),
                reads=sk + [("ctb", lo, hb)], writes=[("cR", lo, hb)])
        pg.op("act", lambda e, sl=sl: e.copy(out=cpc[32:64, sl], in_=cR[32:64, sl]), reads=[("cR", 32, hb)],
              writes=[("cpc", 1, hb)])
        pg.op("act", lambda e, sl=sl: e.copy(out=ctb[64:96, sl], in_=cR[64:96, sl]), reads=[("cR", 64, hb)],
              writes=[("ctb", 64, hb)])
        pg.op("dve", lambda e, sl=sl: e.tensor_tensor(out=cR2[64:96, sl], in0=cR[64:96, sl], in1=ctb[64:96, sl],
                                                      op=OP.subtract), reads=[("cR", 64, hb), ("ctb", 64, hb)],
              writes=[("cR2", hb)])
        pg.op("act", lambda e, sl=sl: e.copy(out=cpc[64:96, sl], in_=cR2[64:96, sl]), reads=[("cR2", hb)],
              writes=[("cpc", 2, hb)])
    pg.barrier()
    pg.flush()
    st.close()
    if stage <= 6:
        st6.close()
        st5.close()
        return finish(nc, pg, es, out)


    st = ExitStack()
    def sb7(name, shape, dt=F32):
        return st.enter_context(nc.sbuf_tensor(name, list(shape), dt))
    wgh = [sb7("wgh%d" % i, [128, 16, 256], BF16) for i in range(2)]
    wuh = [sb7("wuh%d" % i, [128, 16, 256], BF16) for i in range(2)]
    wdn = sb7("wdn", [128, 4, 2048], BF16)
    hid = sb7("hid", [128, 4, 1024], BF16)
    cbc = sb7("cbc", [128, 2, 512])
    sgs = [sb7("sgs%d" % i, [128, 512]) for i in range(2)]
    tus = [sb7("tus%d" % i, [128, 512]) for i in range(2)]
    tmo = [sb7("tmo%d" % i, [128, 512]) for i in range(2)]
    cpk = [("cpc", r, hb) for r in range(3) for hb in range(2)]
    cnt6 = [0]

    def do_expert(ex):
        wg_v = w_g[ex].rearrange("(kc p) f -> p kc f", p=128)
        wu_v = w_u[ex].rearrange("(kc p) f -> p kc f", p=128)
        wd_v = w_d[ex].rearrange("(fc p) d -> p fc d", p=128)
        for tb in range(2):
            pg.op("pe", lambda e, tb=tb: e.matmul(banks[6][:], lhsT=sel3b[0:96, ex * 128:(ex + 1) * 128],
                                                  rhs=cpc[0:96, tb * 512:(tb + 1) * 512], start=True, stop=True),
                  reads=cpk + ["sel3b"], writes=[bkk[6]])
            pg.op("act", lambda e, tb=tb: e.copy(out=cbc[:, tb, :], in_=banks[6][:]), reads=[bkk[6]], writes=[("cbc", tb)])
        for half in range(2):
            wg_, wu_ = wgh[half], wuh[half]
            dma_cast(wg_[:], wg_v[:, :, half * 256:(half + 1) * 256], "ld_wgh%d" % half, writes=["wgh%d" % half])
            dma_cast(wu_[:], wu_v[:, :, half * 256:(half + 1) * 256], "ld_wuh%d" % half, writes=["wuh%d" % half])
            for fcl in range(2):
                fc = half * 2 + fcl
                for tb in range(2):
                    i2 = cnt6[0] % 2
                    cnt6[0] += 1
                    pgt, pgk_ = banks[i2], bkk[i2]
                    pup, puk = banks[2 + i2], bkk[2 + i2]
                    def mmg(e, wg_=wg_, fcl=fcl, tb=tb, pgt=pgt):
                        ins = None
                        for kc in range(16):
                            ins = e.matmul(pgt[:], lhsT=wg_[:, kc, fcl * 128:(fcl + 1) * 128],
                                           rhs=h2T[:, kc, tb * 512:(tb + 1) * 512], start=(kc == 0), stop=(kc == 15))
                        return ins
                    pg.op("pe", mmg, reads=["wgh%d" % half] + h2k, writes=[pgk_])
                    def mmu(e, wu_=wu_, fcl=fcl, tb=tb, pup=pup):
                        ins = None
                        for kc in range(16):
                            ins = e.matmul(pup[:], lhsT=wu_[:, kc, fcl * 128:(fcl + 1) * 128],
                                           rhs=h2T[:, kc, tb * 512:(tb + 1) * 512], start=(kc == 0), stop=(kc == 15))
                        return ins
                    pg.op("pe", mmu, reads=["wuh%d" % half] + h2k, writes=[puk])
                    sg_, sgk = sgs[i2], "sgs%d" % i2
                    tu_, tuk = tus[i2], "tus%d" % i2
                    pg.op("act", lambda e, sg_=sg_, pgt=pgt: e.activation(out=sg_[:], in_=pgt[:], func=AF.Silu),
                          reads=[pgk_], writes=[sgk])
                    pg.op("dve", lambda e, tu_=tu_, pup=pup, sg_=sg_: e.tensor_tensor(out=tu_[:], in0=pup[:], in1=sg_[:],
                                                                                     op=OP.mult),
                          reads=[puk, sgk], writes=[tuk])
                    pg.op("dve", lambda e, tu_=tu_, fc=fc, tb=tb: e.tensor_tensor(
                        out=hid[:, fc, tb * 512:(tb + 1) * 512], in0=tu_[:], in1=cbc[:, tb, :], op=OP.mult),
                        reads=[tuk, ("cbc", tb)], writes=[("hid", fc, tb)])
        dma_cast(wdn[:], wd_v, "ld_wdn", writes=["wdn"])
        for dc in range(16):
            for tb in range(2):
                i2 = cnt6[0] % 2
                cnt6[0] += 1
                po, pok = banks[4 + i2], bkk[4 + i2]
                def mmd(e, dc=dc, tb=tb, po=po):
                    ins = None
                    for fc in range(4):
                        ins = e.matmul(po[:], lhsT=wdn[:, fc, dc * 128:(dc + 1) * 128], rhs=hid[:, fc, tb * 512:(tb + 1) * 512],
                                       start=(fc == 0), stop=(fc == 3))
                    return ins
                pg.op("pe", mmd, reads=["wdn"] + [("hid", fc, tb) for fc in range(4)], writes=[pok])
                tm_, tmk = tmo[i2], "tmo%d" % i2
                pg.op("act", lambda e, tm_=tm_, po=po, dc=dc: e.activation(out=tm_[:], in_=po[:], func=AF.Copy,
                                                                           scale=G12[:, 1, dc:dc + 1]),
                      reads=[pok, "G12b"], writes=[tmk])
                pg.op("dve", lambda e, tm_=tm_, dc=dc, tb=tb: e.tensor_tensor(
                    out=x1T[:, dc, tb * 512:(tb + 1) * 512], in0=x1T[:, dc, tb * 512:(tb + 1) * 512], in1=tm_[:], op=OP.add),
                    reads=[("x1T", dc, tb), tmk], writes=[("x1T", dc, tb)])
    for ex in range(NEXP):
        do_expert(ex)
    pg.barrier()
    pg.flush()
    st.close()
    st6.close()

    st = ExitStack()
    nfr = st.enter_context(nc.sbuf_tensor("nfr", [128, 2048], F32))
    xo = [st.enter_context(nc.sbuf_tensor("xo%d" % i, [128, 2048], F32)) for i in range(2)]
    junk = st.enter_context(nc.sbuf_tensor("junk", [128, 2048], BF16))
    ss7 = st.enter_context(nc.sbuf_tensor("ss7", [128, 16], F32))
    dma_sp(nfr[:], lnrows[:, 2 * D:3 * D], "ld_nfr", writes=["nfr"])
    pg.op("dve", lambda e: e.memset(ss7[:], 0.0), writes=["ss7"])
    for t in range(8):
        xo_, xok = xo[t % 2], "xo%d" % (t % 2)
        for q in range(4):
            pf, pfk = banks[q % 2], bkk[q % 2]
            def trf(e, t=t, q=q, pf=pf):
                ins = None
                for j in range(4):
                    dc = q * 4 + j
                    ins = e.transpose(out=pf[:, j * 128:(j + 1) * 128], in_=x1T[:, dc, t * 128:(t + 1) * 128], identity=ident)
                return ins
            pg.op("pe", trf, reads=[("x1T", q * 4 + j, t // 4) for j in range(4)] + ["cst"], writes=[pfk])
            pg.op("act", lambda e, xo_=xo_, q=q, pf=pf: e.copy(out=xo_[:, q * 512:(q + 1) * 512], in_=pf[:]),
                  reads=[pfk], writes=[(xok, q)])
        xk = [(xok, q) for q in range(4)]
        pg.op("act", lambda e, xo_=xo_, t=t: e.activation(out=junk[:], in_=xo_[:], func=AF.Square, accum_out=ss7[:, t:t + 1]),
              reads=xk + ["ss7"], writes=["junk", ("ss7", t)])
        pg.op("dve", lambda e, t=t: e.tensor_scalar(out=ss7[:, 8 + t:9 + t], in0=ss7[:, t:t + 1], scalar1=1.0 / D, scalar2=EPS,
                                                    op0=OP.mult, op1=OP.add), reads=[("ss7", t)], writes=[("rs7", t)])
        pg.op("act", lambda e, t=t: e.sqrt(out=ss7[:, 8 + t:9 + t], in_=ss7[:, 8 + t:9 + t]), reads=[("rs7", t)], writes=[("rs7", t)])
        pg.op("dve", lambda e, t=t: e.reciprocal(out=ss7[:, 8 + t:9 + t], in_=ss7[:, 8 + t:9 + t]), reads=[("rs7", t)],
              writes=[("rs7", t)])
        pg.op("dve", lambda e, xo_=xo_, t=t: e.scalar_tensor_tensor(out=xo_[:], in0=xo_[:], scalar=ss7[:, 8 + t:9 + t],
                                                                    in1=nfr[:], op0=OP.mult, op1=OP.mult),
              reads=xk + [("rs7", t), "nfr"], writes=xk)
        dma_sp(out[t * 128:(t + 1) * 128, :], xo_[:], "st_out%d" % (t % 2), reads=xk)
    pg.barrier()
    pg.flush()
    st.close()
    st5.close()
    return finish(nc, pg, es, out)


def finish(nc, pg, es, out):
    es.close()
    return nc


def _consts():
    c = np.zeros((128, C_N), np.float32)
    i = np.arange(128)
    c[:, C_ID:C_ID + 128] = np.eye(128, dtype=np.float32)
    c[:, C_TU:C_TU + 128] = (i[:, None] <= i[None, :])
    c[:, C_TL:C_TL + 128] = (i[:, None] >= i[None, :])
    c[:, C_MNF:C_MNF + 128] = np.where(i[:, None] <= i[None, :], 0.0, -30000.0)
    c[:, C_MNB:C_MNB + 128] = np.where(i[:, None] >= i[None, :], 0.0, -30000.0)
    c[:, C_ONE:C_ONE + 128] = 1.0
    s3 = np.zeros((128, 32, 128), np.float32)
    for p in range(96):
        s3[p, p % 32, :] = 1.0
    return c, s3.reshape(128, 4096)


def _pp(v):
    return np.ascontiguousarray(np.asarray(v, np.float32).reshape(-1, 128).T)


def prep_inputs(inp):
    f = lambda a: np.ascontiguousarray(np.asarray(a, np.float32))
    x, c, ctx, c_ctx = f(inp["x"]), f(inp["c"]), f(inp["ctx"]), f(inp["c_ctx"])
    cst, s3 = _consts()
    conv_w = f(inp["conv_w"])[0]
    pvec = np.zeros((128, PV_N), np.float32)
    pvec[:, PV_N1G:PV_N1G + 16] = _pp(inp["norm1_g"][0])
    pvec[:, PV_N2G:PV_N2G + 16] = _pp(inp["norm2_g"][0])
    pvec[:, PV_SNG:PV_SNG + 16] = _pp(inp["ssd_norm_g"][0])
    for j in range(3):
        pvec[:, PV_CW + j * 24:PV_CW + (j + 1) * 24] = _pp(conv_w[j])
    pvec[:, PV_CB:PV_CB + 24] = _pp(inp["conv_b"][0])
    pvec[:, PV_BMOD:PV_BMOD + 96] = _pp(inp["b_mod"][0])
    row = np.zeros((RP_N,), np.float32)
    row[RP_DTB:RP_DTB + 32] = f(inp["dt_bias_f"])[0]
    row[RP_DTB + 32:RP_DTB + 64] = f(inp["dt_bias_b"])[0]
    row[RP_ALOG:RP_ALOG + 32] = f(inp["a_log_f"])[0]
    row[RP_ALOG + 32:RP_ALOG + 64] = f(inp["a_log_b"])[0]
    row[RP_DSKIP:RP_DSKIP + 32] = f(inp["d_skip"])[0]
    row[RP_BS:RP_BS + 1024] = f(inp["b_spatial"])[0].reshape(-1)
    row[RP_BR:RP_BR + 4] = f(inp["b_router_group"])[0]
    row[RP_BR + 4:RP_BR + 36] = f(inp["b_router_expert"])[0].reshape(-1)
    rowp = np.ascontiguousarray(np.broadcast_to(row[None, :], (128, RP_N)))
    lnr = np.concatenate([f(inp["cm_ln_g"])[0], f(inp["cm_ln_b"])[0], f(inp["normf_g"])])
    lnrows = np.ascontiguousarray(np.broadcast_to(lnr[None, :], (128, 3 * D)))
    w_mod = f(inp["w_mod"])[0]
    w_in = f(inp["w_in"])[0]
    w_out = f(inp["w_out"])[0]
    wsT = np.ascontiguousarray(np.transpose(f(inp["w_spatial"])[0], (2, 0, 1)).reshape(128, 1024))
    wrg = f(inp["w_router_group"])[0]
    wre = np.transpose(f(inp["w_router_expert"])[0], (1, 0, 2)).reshape(D, 32)
    wrc = np.concatenate([wrg, wre], axis=1)
    wr = np.ascontiguousarray(wrc.reshape(16, 128, 36).transpose(1, 0, 2).reshape(128, 16 * 36))
    w_g = f(inp["w_exp_gate"])[0].reshape(32, D, 512)
    w_u = f(inp["w_exp_up"])[0].reshape(32, D, 512)
    w_d = f(inp["w_exp_down"])[0].reshape(32, 512, D)
    maps = []
    for k in range(NCORES):
        b, s = k // 2, k % 2
        own = x[b, s * 1024:(s + 1) * 1024]
        oth = x[b, (1 - s) * 1024:(2 - s) * 1024]
        x_all = np.concatenate([ctx[b], oth, own], axis=0)
        fl = np.zeros((128, 2), np.float32)
        fl[:, 0] = 1.0 if s == 1 else 0.0
        fl[:, 1] = 1.0 if s == 0 else 0.0
        cvec = np.stack([_pp(c[b]), _pp(c_ctx)], axis=2).reshape(128, 32)
        maps.append(dict(x_all=x_all, flags=fl, cvec=np.ascontiguousarray(cvec), pvec=pvec, rowp=rowp,
                         lnrows=lnrows, consts=cst, sel3=s3, w_mod=w_mod, w_in=w_in, w_out=w_out,
                         wsT=wsT, wr=wr, w_g=w_g, w_u=w_u, w_d=w_d))
    return maps


def kernel(**inputs):
    maps = prep_inputs(inputs)
    nc = build_nc()
    res = run_bass_kernel_spmd(nc, maps, core_ids=list(range(NCORES)))
    outf = np.zeros((4, 2048, D), np.float32)
    for k in range(NCORES):
        b, s = k // 2, k % 2
        outf[b, s * 1024:(s + 1) * 1024] = res.results[k]["out"]
    return outf
```

```python
from contextlib import ExitStack
import numpy as np
import concourse.bass as bass
import concourse.mybir as mybir
from concourse.bass_utils import run_bass_kernel_spmd

F32 = mybir.dt.float32
BF16 = mybir.dt.bfloat16
AF = mybir.ActivationFunctionType
OP = mybir.AluOpType
AX = mybir.AxisListType

D = 2048
NCORES = 8
EPS = 1e-6
ENGS = ("pe", "act", "dve", "pool", "sp")
SUB = 99
JOBS = None
NOTR = False
NCH = 8
NEXP = 32
NOCONV = False

PV_N1G, PV_N2G, PV_SNG, PV_CW, PV_CB, PV_BMOD = 0, 16, 32, 48, 120, 144
PV_N = 240
RP_DTB, RP_ALOG, RP_DSKIP, RP_BS, RP_BR = 0, 64, 128, 160, 1184
RP_N = 1220
C_ID, C_TU, C_TL, C_MNF, C_MNB, C_ONE = 0, 128, 256, 384, 512, 640
C_N = 768


class Prog:
    def __init__(self, nc, es):
        self.nc = nc
        self.es = es
        self.sems = {}
        self.cnt = {}
        self.ops = {e: [] for e in ENGS}
        self.known = {e: {} for e in ENGS}
        self.last_w = {}
        self.readers = {}
        self.latest = {}
        for e in ENGS:
            self._sem(e)

    def _sem(self, key):
        if key not in self.sems:
            self.sems[key] = self.es.enter_context(self.nc.semaphore("s_" + str(key)))
            self.cnt[key] = 0
        return self.sems[key]

    def op(self, eng, fn, reads=(), writes=(), dma=None):
        waits = {}
        def need(tok):
            sk, v = tok
            if sk == "pe" and eng == "pe":
                return
            if self.known[eng].get(sk, 0) >= v:
                return
            waits[sk] = max(waits.get(sk, 0), v)
        for k in reads:
            if k in self.last_w:
                need(self.last_w[k])
        for k in writes:
            if k in self.last_w:
                need(self.last_w[k])
            for r in self.readers.get(k, ()):
                need(r)
        for sk, v in waits.items():
            self.known[eng][sk] = v
        if dma is not None:
            self._sem(dma)
            self.cnt[dma] += 16
            tok = (dma, self.cnt[dma])
        else:
            self.cnt[eng] += 1
            tok = (eng, self.cnt[eng])
        self.latest[tok[0]] = tok[1]
        for k in writes:
            self.last_w[k] = tok
            self.readers[k] = []
        for k in reads:
            self.readers.setdefault(k, []).append(tok)
        self.ops[eng].append((list(waits.items()), fn, tok, dma is not None))
        return tok

    def barrier(self):
        for e in ENGS:
            waits = []
            for sk, v in self.latest.items():
                if sk == e and e == "pe":
                    continue
                if self.known[e].get(sk, 0) < v:
                    waits.append((sk, v))
                    self.known[e][sk] = v
            if waits:
                self.ops[e].append((waits, None, None, False))
        self.last_w.clear()
        self.readers.clear()

    def flush(self):
        nc = self.nc
        ops = self.ops
        sems = self.sems

        def run(engh, lst):
            for waits, fn, tok, isdma in lst:
                for sk, v in waits:
                    engh.wait_ge(sems[sk], v)
                if fn is None:
                    continue
                ins = fn(engh)
                ins.then_inc(sems[tok[0]], 16 if isdma else 1)

        with nc.Block() as block:
            if ops["sp"]:
                @block.sync
                def _(e):
                    run(e, ops["sp"])
            if ops["pe"]:
                @block.tensor
                def _(e):
                    run(e, ops["pe"])
            if ops["act"]:
                @block.scalar
                def _(e):
                    run(e, ops["act"])
            if ops["dve"]:
                @block.vector
                def _(e):
                    run(e, ops["dve"])
            if ops["pool"]:
                @block.gpsimd
                def _(e):
                    run(e, ops["pool"])
        self.ops = {e: [] for e in ENGS}


def build_nc(stage=99, taps=()):
    nc = bass.Bass("TRN2", target_bir_lowering=False)
    es = ExitStack()
    pg = Prog(nc, es)

    def din(name, shape, dt=F32):
        return nc.dram_tensor(name, list(shape), dt, kind="ExternalInput").ap()

    def dscr(name, shape, dt):
        kind = "ExternalOutput" if name in taps else "Internal"
        return nc.dram_tensor(name, list(shape), dt, kind=kind).ap()

    x_all = din("x_all", [2304, D])
    flags = din("flags", [128, 2])
    cvec = din("cvec", [128, 32])
    pvec = din("pvec", [128, PV_N])
    rowp = din("rowp", [128, RP_N])
    lnrows = din("lnrows", [128, 3 * D])
    consts = din("consts", [128, C_N])
    sel3 = din("sel3", [128, 4096])
    w_mod = din("w_mod", [D, 6 * D])
    w_in = din("w_in", [D, 9280])
    w_out = din("w_out", [2 * D, D])
    wsT = din("wsT", [128, 1024])
    wr = din("wr", [128, 16 * 36])
    w_g = din("w_g", [32, D, 512])
    w_u = din("w_u", [32, D, 512])
    w_d = din("w_d", [32, 512, D])
    out = nc.dram_tensor("out", [1024, D], F32, kind="ExternalOutput").ap()

    def sb(name, shape, dt=F32):
        return es.enter_context(nc.sbuf_tensor(name, list(shape), dt))

    def ps(name, shape, dt=F32):
        return es.enter_context(nc.psum_tensor(name, list(shape), dt))

    cst = sb("cst", [128, C_N])
    idb = sb("idb", [128, 128], BF16)
    pv = sb("pv", [128, PV_N])
    rp = sb("rp", [128, RP_N])
    flg = sb("flg", [128, 2])
    cv = sb("cv", [128, 32])
    scT = sb("scT", [128, 32], BF16)
    modT = sb("modT", [128, 192])
    AB = sb("AB", [128, 6, 16])
    G12 = sb("G12", [128, 2, 16])
    dtall = sb("dtall", [128, 18, 64])
    aneg = sb("aneg", [128, 64])
    decall = sb("decall", [128, 18, 64])

    ident = cst[:, C_ID:C_ID + 128]
    ones = cst[:, C_ONE:C_ONE + 128]

    def dma_sp(out_ap, in_ap, sem, reads=(), writes=()):
        pg.op("sp", lambda e: e.dma_start(out=out_ap, in_=in_ap), reads=reads, writes=writes, dma=sem)

    def dma_cast(out_ap, in_ap, sem, reads=(), writes=()):
        pg.op("pool", lambda e: e.dma_start(out=out_ap, in_=in_ap), reads=reads, writes=writes, dma=sem)

    dma_sp(cst[:], consts, "ld_c0", writes=["cst"])
    dma_sp(pv[:], pvec, "ld_c1", writes=["pv"])
    dma_sp(rp[:], rowp, "ld_c3", writes=["rp"])
    dma_sp(flg[:], flags, "ld_c4", writes=["flg"])
    dma_sp(cv[:], cvec, "ld_c5", writes=["cv"])
    pg.op("dve", lambda e: e.tensor_copy(out=idb[:], in_=ident), reads=["cst"], writes=["idb"])
    pg.op("act", lambda e: e.activation(out=scT[:], in_=cv[:], func=AF.Silu), reads=["cv"], writes=["scT"])
    pg.op("act", lambda e: e.activation(out=aneg[:], in_=rp[:, RP_ALOG:RP_ALOG + 64], func=AF.Exp),
          reads=["rp"], writes=["aneg"])
    pg.op("dve", lambda e: e.tensor_scalar_mul(out=aneg[:], in0=aneg[:], scalar1=-1.0),
          reads=["aneg"], writes=["aneg"])

    st = ExitStack()
    wb = [st.enter_context(nc.sbuf_tensor("p0w%d" % i, [128, 16, 512], BF16)) for i in range(2)]
    modps = st.enter_context(nc.psum_tensor("modps", [128, 192], F32))
    w_mod_v = w_mod.rearrange("(kc p) c -> p kc c", p=128)
    mps_cur = [modps]
    def mod_block(blk, wb):
        w = wb[blk % 2]
        key = "p0w%d" % (blk % 2)
        dma_cast(w[:], w_mod_v[:, :, blk * 512:(blk + 1) * 512], "ld_" + key, writes=[key])

        def mm(e, w=w, blk=blk):
            ins = None
            for cc in range(4):
                col = (blk * 4 + cc) * 2
                for kc in range(16):
                    ins = e.matmul(mps_cur[0][:, col:col + 2], lhsT=w[:, kc, cc * 128:(cc + 1) * 128],
                                   rhs=scT[:, kc * 2:kc * 2 + 2], start=(kc == 0), stop=(kc == 15))
            return ins
        pg.op("pe", mm, reads=[key, "scT"], writes=["modps"])

    def mod_finish(c0, c1, modps):
        pg.op("dve", lambda e: e.tensor_tensor(
            out=modT[:, c0 * 2:c1 * 2].rearrange("p (c t) -> p c t", t=2),
            in0=modps[:, c0 * 2:c1 * 2].rearrange("p (c t) -> p c t", t=2),
            in1=pv[:, PV_BMOD + c0:PV_BMOD + c1].unsqueeze(2).to_broadcast([128, c1 - c0, 2]), op=OP.add),
            reads=["modps", "pv"], writes=["modT"])
    for blk in range(8):
        mod_block(blk, wb)
    mod_finish(0, 32, modps)
    mt = modT[:].rearrange("p (m kc t) -> p m kc t", m=6, kc=16, t=2)
    def ab(e_idx, g_off, sc_m, sh_m, which):
        pg.op("dve", lambda e: e.scalar_tensor_tensor(
            out=AB[:, e_idx, :], in0=mt[:, sc_m, :, which], scalar=1.0, in1=pv[:, g_off:g_off + 16],
            op0=OP.add, op1=OP.mult), reads=["modT", "pv"], writes=["AB%d" % e_idx])
        pg.op("dve", lambda e: e.tensor_copy(out=AB[:, e_idx + 1, :], in_=mt[:, sh_m, :, which]),
              reads=["modT"], writes=["AB%d" % (e_idx + 1)])
    ab(0, PV_N1G, 1, 0, 0)
    ab(2, PV_N1G, 1, 0, 1)
    if "t_modT" in taps:
        t_modT = nc.dram_tensor("t_modT", [128, 192], F32, kind="ExternalOutput").ap()
        dma_sp(t_modT, modT[:], "st_tap", reads=["modT"])
    pg.barrier()
    pg.flush()
    st.close()
    if stage <= 0:
        return finish(nc, pg, es, out)


    S_all = dscr("S_all", [18, 2, 128, 2048], F32)
    xs_s = dscr("xs_s", [8, 128, 2048], BF16)
    bt_s = dscr("bt_s", [8, 128, 4, 128], BF16)
    ct_s = dscr("ct_s", [8, 128, 4, 128], BF16)
    z_s = dscr("z_s", [8, 128, 2048], BF16)
    v_s = dscr("v_s", [8, 128, 2048], BF16)
    u_s = dscr("u_s", [8, 128, 16, 128], BF16)
    hp_s = dscr("hp_s", [2, 8, 128, 2048], BF16)
    mix_s = dscr("mix_s", [8, 128, 32, 128], BF16)

    st = ExitStack()
    def sb1(name, shape, dt=F32):
        return st.enter_context(nc.sbuf_tensor(name, list(shape), dt))
    def ps1(name, shape, dt=F32):
        return st.enter_context(nc.psum_tensor(name, list(shape), dt))
    xin = sb1("xin", [128, 4, 2048])
    hT = sb1("hT", [128, 16, 512], BF16)
    wb = [sb1("wb%d" % i, [128, 16, 512], BF16) for i in range(2)]
    t0s = [sb1("t0_%d" % i, [128, 512]) for i in range(2)]
    sbf = [sb1("sbf%d" % i, [128, 512], BF16) for i in range(2)]
    sraws = [sb1("sraw%d" % i, [128, 512]) for i in range(2)]
    xs_tok = sb1("xs_tok", [128, 4, 2048], BF16)
    B_tok = sb1("B_tok", [128, 4, 512], BF16)
    BTs = sb1("BTs", [128, 4, 512], BF16)
    CTs = sb1("CTs", [128, 4, 512], BF16)
    stg = sb1("stg", [128, 4, 2048], BF16)
    lnr = sb1("lnr", [128, 2, 2048])
    xw = [sb1("xw%d" % i, [128, 2048], BF16) for i in range(2)]
    vout = [sb1("vout%d" % i, [128, 2048], BF16) for i in range(2)]
    Sst = [sb1("Sst%d" % i, [128, 2048]) for i in range(2)]
    sm = sb1("sm", [128, 16, 64])
    ssq = sb1("ssq", [128, 16])
    wgt_t = sb1("wgt_t", [128, 4, 64])
    pA = [ps1("pA%d" % i, [128, 512]) for i in range(2)]
    pTf = [ps1("pTf%d" % i, [128, 512]) for i in range(2)]
    pS = [ps1("pS%d" % i, [128, 512]) for i in range(2)]
    pm = ps1("pm", [128, 512])
    stg_u = stg[:].rearrange("p t c -> p (t c)").rearrange("p (cc n) -> p cc n", cc=16)

    w_in_v = w_in.rearrange("(kc p) c -> p kc c", p=128)
    dma_sp(lnr[:].rearrange("p a c -> p (a c)"), lnrows[:, 0:2 * D], "ld_lnr", writes=["lnr"])
    wcnt = [0]
    acnt = [0]

    def load_w(c0, ncols):
        i = wcnt[0] % 2
        wcnt[0] += 1
        dma_cast(wb[i][:, :, 0:ncols], w_in_v[:, :, c0:c0 + ncols], "ld_wb%d" % i, writes=["wb%d" % i])
        return wb[i], "wb%d" % i

    def next_pA():
        i = acnt[0] % 2
        acnt[0] += 1
        return pA[i], "pA%d" % i, i

    blocks = [("ctx", 0, 2, 0), ("oth", 256, 4, 2), ("oth", 768, 4, 6), ("own", 1280, 4, 10), ("own", 1792, 4, 14)]
    if stage == 1:
        blocks = blocks[:1] + blocks[3:4]
    if SUB in (2, 3):
        blocks = blocks[:1]
    def do_block(kind, row0, NT, T0, prev_units):
        N = NT * 128
        own = kind == "own"
        Aidx = 2 if kind == "ctx" else 0
        pg.op("dve", lambda e: e.memset(ssq[:], 0.0), writes=["ssq"])
        for t in range(NT):
            dma_sp(xin[:, t, :], x_all[row0 + t * 128:row0 + (t + 1) * 128, :], "ld_xin%d" % t, writes=[("xin", t)])
            pg.op("act", lambda e, t=t: e.activation(out=stg[:, t, :], in_=xin[:, t, :], func=AF.Square,
                                                     accum_out=ssq[:, t:t + 1]),
                  reads=[("xin", t), "ssq"], writes=["stg", ("ssq", t)])
            pg.op("dve", lambda e, t=t: e.tensor_scalar(out=ssq[:, 8 + t:9 + t], in0=ssq[:, t:t + 1], scalar1=1.0 / D,
                                                        scalar2=EPS, op0=OP.mult, op1=OP.add),
                  reads=[("ssq", t)], writes=[("rs", t)])
            pg.op("act", lambda e, t=t: e.sqrt(out=ssq[:, 8 + t:9 + t], in_=ssq[:, 8 + t:9 + t]),
                  reads=[("rs", t)], writes=[("rs", t)])
            pg.op("dve", lambda e, t=t: e.reciprocal(out=ssq[:, 8 + t:9 + t], in_=ssq[:, 8 + t:9 + t]),
                  reads=[("rs", t)], writes=[("rs", t)])
            pg.op("act", lambda e, t=t: e.activation(out=xin[:, t, :], in_=xin[:, t, :], func=AF.Copy,
                                                     scale=ssq[:, 8 + t:9 + t]),
                  reads=[("xin", t), ("rs", t)], writes=[("xin", t)])
        if SUB <= 0:
            return
        for kc in range(16):
            pa, pak, _ = next_pA()
            def tr(e, pa=pa, kc=kc):
                ins = None
                for t in range(NT):
                    ins = e.transpose(out=pa[:, t * 128:(t + 1) * 128], in_=xin[:, t, kc * 128:(kc + 1) * 128],
                                      identity=ident)
                return ins
            pg.op("pe", tr, reads=[("xin", t) for t in range(NT)] + ["cst"], writes=[pak])
            pg.op("act", lambda e, pa=pa, kc=kc: e.activation(
                out=hT[:, kc, 0:N], in_=pa[:, 0:N], func=AF.Identity,
                bias=AB[:, Aidx + 1, kc:kc + 1], scale=AB[:, Aidx, kc:kc + 1]),
                reads=[pak, "AB%d" % Aidx, "AB%d" % (Aidx + 1)], writes=[("hT", kc)])
            if prev_units and kc % 2 == 1:
                prev_units.pop(0)()
        while prev_units:
            prev_units.pop(0)()
        hT_keys = [("hT", kc) for kc in range(16)]
        if SUB <= 1:
            return

        pending = []
        def fm_job(c0, jobkind, cb):
            w, wk = load_w(c0, 512)
            for cc in range(4):
                pa, pak, pi = next_pA()
                def mm(e, pa=pa, w=w, cc=cc):
                    ins = None
                    for kc in range(16):
                        ins = e.matmul(pa[:, 0:N], lhsT=w[:, kc, cc * 128:(cc + 1) * 128], rhs=hT[:, kc, 0:N],
                                       start=(kc == 0), stop=(kc == 15))
                    return ins
                pg.op("pe", mm, reads=[wk] + hT_keys, writes=[pak])
                while pending:
                    pending.pop(0)()
                if jobkind == "u":
                    pg.op("act", lambda e, pa=pa, cc=cc: e.activation(out=stg_u[:, cb * 4 + cc, 0:N], in_=pa[:, 0:N],
                                                                      func=AF.Gelu),
                          reads=[pak], writes=["stg"])
                    continue
                chn = (c0 - 2048) // 128 + cc
                L = 256 if kind == "ctx" else 64
                t0 = t0s[pi]
                t0k = "t0_%d" % pi
                sraw = sraws[pi]
                pg.op("act", lambda e, pa=pa, sraw=sraw: e.copy(out=sraw[:, 0:N], in_=pa[:, 0:N]),
                      reads=[pak], writes=["sraw%d" % pi])
                pav = sraw[:, 0:N].rearrange("p (r l) -> p r l", l=L)
                t0v = t0[:, 0:N].rearrange("p (r l) -> p r l", l=L)
                cw = lambda j, chn=chn: pv[:, PV_CW + j * 24 + chn:PV_CW + j * 24 + chn + 1]
                pg.op("act", lambda e, pa=pa, t0=t0, chn=chn, cw=cw: e.activation(
                    out=t0[:, 0:N], in_=pa[:, 0:N], func=AF.Identity,
                    bias=pv[:, PV_CB + chn:PV_CB + chn + 1], scale=cw(1)), reads=[pak, "pv"], writes=[t0k])
                if not NOCONV:
                  pg.op("dve", lambda e, pav=pav, t0v=t0v, cw=cw: e.scalar_tensor_tensor(
                    out=t0v[:, :, 1:L], in0=pav[:, :, 0:L - 1], scalar=cw(0), in1=t0v[:, :, 1:L],
                    op0=OP.mult, op1=OP.add), reads=["sraw%d" % pi, t0k, "pv"], writes=[t0k])
                if not NOCONV:
                  pg.op("dve", lambda e, pav=pav, t0v=t0v, cw=cw: e.scalar_tensor_tensor(
                    out=t0v[:, :, 0:L - 1], in0=pav[:, :, 1:L], scalar=cw(2), in1=t0v[:, :, 0:L - 1],
                    op0=OP.mult, op1=OP.add), reads=["sraw%d" % pi, t0k, "pv"], writes=[t0k])
                if jobkind == "xs":
                    dst, dk = sbf[pi][:, 0:N], "sbf%d" % pi
                elif jobkind == "B":
                    dst, dk = BTs[:, cc, 0:N], ("BTs", cc)
                else:
                    dst, dk = CTs[:, cc, 0:N], ("CTs", cc)
                pg.op("act", lambda e, t0=t0, dst=dst: e.activation(out=dst, in_=t0[:, 0:N], func=AF.Silu),
                      reads=[t0k], writes=[dk])
                if jobkind in ("xs", "B") and not NOTR:
                    def emit_tr(dst=dst, dk=dk, pi=pi, cc=cc, jobkind=jobkind, cb=cb):
                        ptf, ptk = pTf[pi], "pTf%d" % pi
                        def tr2(e):
                            ins = None
                            for t in range(NT):
                                ins = e.matmul(ptf[:, t * 128:(t + 1) * 128], lhsT=dst[:, t * 128:(t + 1) * 128],
                                               rhs=idb[:], start=True, stop=True)
                            return ins
                        pg.op("pe", tr2, reads=[dk, "idb"], writes=[ptk])
                        if jobkind == "xs":
                            o = xs_tok[:, 0:NT, cb * 512 + cc * 128:cb * 512 + (cc + 1) * 128]
                            ok = [("xs_tok", t, cb) for t in range(NT)]
                        else:
                            o = B_tok[:, 0:NT, cc * 128:(cc + 1) * 128]
                            ok = [("B_tok", t) for t in range(NT)]
                        iv = ptf[:, 0:N].rearrange("p (t c) -> p t c", c=128)
                        if cc % 2 == 0:
                            pg.op("dve", lambda e: e.tensor_copy(out=o, in_=iv), reads=[ptk], writes=ok)
                        else:
                            pg.op("act", lambda e: e.copy(out=o, in_=iv), reads=[ptk], writes=ok)
                    pending.append(emit_tr)

        def tm_job(c0, ncols, jobkind, cb):
            if jobkind == "dt":
                w, wk = load_w(4672, 512)
                wofs = 448
            else:
                w, wk = load_w(c0, ncols)
                wofs = 0
            for t in range(NT):
                pa, pak, pi = next_pA()
                def mm(e, pa=pa, w=w, t=t):
                    ins = None
                    for kc in range(16):
                        ins = e.matmul(pa[:, 0:ncols], lhsT=hT[:, kc, t * 128:(t + 1) * 128], rhs=w[:, kc, wofs:wofs + ncols],
                                       start=(kc == 0), stop=(kc == 15))
                    return ins
                pg.op("pe", mm, reads=[wk] + hT_keys, writes=[pak])
                while pending:
                    pending.pop(0)()
                if jobkind == "z":
                    pg.op("act", lambda e, pa=pa, t=t: e.activation(out=stg[:, t, cb * 512:(cb + 1) * 512], in_=pa[:],
                                                                    func=AF.Silu), reads=[pak], writes=["stg"])
                elif jobkind == "v":
                    pg.op("act", lambda e, pa=pa, t=t: e.activation(out=xin[:, t, cb * 512:(cb + 1) * 512], in_=pa[:],
                                                                    func=AF.Gelu), reads=[pak], writes=[("xin", t)])
                else:
                    T = T0 + t
                    a, b_, c_, d_ = sm[:, 0, :], sm[:, 1, :], sm[:, 2, :], sm[:, 3, :]
                    pg.op("dve", lambda e, pa=pa: e.tensor_tensor(out=a, in0=pa[:, 0:64], in1=rp[:, RP_DTB:RP_DTB + 64],
                                                                  op=OP.add), reads=[pak, "rp"], writes=["sm0"])
                    pg.op("dve", lambda e: e.tensor_scalar_mul(out=b_, in0=a, scalar1=-1.0),
                          reads=["sm0"], writes=["sm1"])
                    pg.op("dve", lambda e: e.tensor_tensor(out=b_, in0=b_, in1=a, op=OP.min),
                          reads=["sm0", "sm1"], writes=["sm1"])
                    pg.op("act", lambda e: e.activation(out=c_, in_=b_, func=AF.Exp),
                          reads=["sm1"], writes=["sm2"])
                    pg.op("dve", lambda e: e.tensor_scalar_add(out=c_, in0=c_, scalar1=1.0),
                          reads=["sm2"], writes=["sm2"])
                    pg.op("act", lambda e: e.activation(out=c_, in_=c_, func=AF.Ln),
                          reads=["sm2"], writes=["sm2"])
                    pg.op("dve", lambda e, T=T: e.scalar_tensor_tensor(out=dtall[:, T, :], in0=a, scalar=0.0, in1=c_,
                                                                       op0=OP.max, op1=OP.add),
                          reads=["sm0", "sm2"], writes=[("dtall", T)])
                    if kind == "oth":
                        for dr in range(2):
                            pg.op("dve", lambda e, T=T, dr=dr: e.tensor_scalar_mul(
                                out=dtall[:, T, dr * 32:(dr + 1) * 32], in0=dtall[:, T, dr * 32:(dr + 1) * 32],
                                scalar1=flg[:, dr:dr + 1]), reads=[("dtall", T), "flg"], writes=[("dtall", T)])

        if own:
            for cb in range(4):
                tm_job(cb * 512, 512, "z", cb)
            for t in range(NT):
                dma_sp(z_s[T0 - 10 + t], stg[:, t, :], "st_stg", reads=["stg"])
        for cb in range(4):
            if JOBS is None or "xs" in JOBS:
                fm_job(2048 + cb * 512, "xs", cb)
        if JOBS is None or "B" in JOBS:
            fm_job(4096, "B", 0)
        if own:
            fm_job(4608, "C", 0)
        if JOBS is None or "dt" in JOBS:
            tm_job(5120, 64, "dt", 0)
        while pending:
            pending.pop(0)()
        def build_units():
            units = []
            W = NT * 64
            dtab, acsb, ddb, wgtb = sm[:, 4:8, :], sm[:, 8:12, :], sm[:, 12:16, :], wgt_t[:]
            f2 = lambda ap: ap[:, 0:NT, :]
            dtk = [("dtall", T0 + t) for t in range(NT)]
            def prep_():
                pg.op("dve", lambda e: e.tensor_tensor(out=f2(dtab), in0=dtall[:, T0:T0 + NT, :],
                                                       in1=aneg[:].unsqueeze(1).to_broadcast([128, NT, 64]), op=OP.mult),
                      reads=dtk + ["aneg"], writes=["sm4"])
                def mm2(e):
                    pmv = pm[:, 0:W].rearrange("p (t c) -> p t c", c=64)
                    for dr in range(2):
                        tri = cst[:, C_TU:C_TU + 128] if dr == 0 else cst[:, C_TL:C_TL + 128]
                        e.matmul(pmv[:, :, dr * 32:(dr + 1) * 32], lhsT=tri, rhs=f2(dtab)[:, :, dr * 32:(dr + 1) * 32],
                                 start=True, stop=True)
                    return e.matmul(pm[:, 256:256 + W].rearrange("p (t c) -> p t c", c=64), lhsT=ones, rhs=f2(dtab),
                                    start=True, stop=True)
                pg.op("pe", mm2, reads=["sm4", "cst"], writes=["pm"])
                pg.op("act", lambda e: e.copy(out=f2(acsb), in_=pm[:, 0:W].rearrange("p (t c) -> p t c", c=64)),
                      reads=["pm"], writes=["sm5"])
                pg.op("act", lambda e: e.activation(out=decall[:, T0:T0 + NT, :],
                                                    in_=pm[:, 256:256 + W].rearrange("p (t c) -> p t c", c=64), func=AF.Exp),
                      reads=["pm"], writes=[("decall", T0 + t, d_) for t in range(NT) for d_ in range(2)])
                pg.op("dve", lambda e: e.tensor_tensor(out=f2(ddb), in0=pm[:, 256:256 + W].rearrange("p (t c) -> p t c", c=64),
                                                       in1=f2(acsb), op=OP.subtract), reads=["pm", "sm5"], writes=["sm6"])
                pg.op("act", lambda e: e.activation(out=f2(ddb), in_=f2(ddb), func=AF.Exp), reads=["sm6"], writes=["sm6"])
                pg.op("dve", lambda e: e.tensor_tensor(out=f2(wgtb), in0=f2(ddb), in1=dtall[:, T0:T0 + NT, :], op=OP.mult),
                      reads=["sm6"] + dtk, writes=["sm7"])

            units.append(prep_)
            for t in range(NT):
                for dr in range(2):
                    def unit_(t=t, dr=dr):
                        T = T0 + t
                        xwt = xw[dr]
                        pg.op("dve", lambda e, t=t, dr=dr, xwt=xwt: e.tensor_tensor(
                            out=xwt[:].rearrange("p (h d) -> p h d", d=64),
                            in0=xs_tok[:, t, :].rearrange("p (h d) -> p h d", d=64),
                            in1=wgtb[:, t, dr * 32:(dr + 1) * 32].unsqueeze(2).to_broadcast([128, 32, 64]), op=OP.mult),
                            reads=[("xs_tok", t, cb) for cb in range(4)] + ["sm7"], writes=["xw%d" % dr])
                        sst = Sst[dr]
                        for g in range(4):
                            psg, psk = pS[g % 2], "pS%d" % (g % 2)
                            pg.op("pe", lambda e, psg=psg, t=t, g=g, xwt=xwt: e.matmul(
                                psg[:], lhsT=B_tok[:, t, g * 128:(g + 1) * 128], rhs=xwt[:, g * 512:(g + 1) * 512],
                                start=True, stop=True), reads=[("B_tok", t), "xw%d" % dr], writes=[psk])
                            if g % 2 == 0:
                                pg.op("act", lambda e, psg=psg, g=g, sst=sst: e.copy(out=sst[:, g * 512:(g + 1) * 512], in_=psg[:]),
                                      reads=[psk], writes=[("Sst", dr, g)])
                            else:
                                pg.op("dve", lambda e, psg=psg, g=g, sst=sst: e.tensor_copy(out=sst[:, g * 512:(g + 1) * 512], in_=psg[:]),
                                      reads=[psk], writes=[("Sst", dr, g)])
                        dma_sp(S_all[T, dr], sst[:], "st_S%d" % dr, reads=[("Sst", dr, g) for g in range(4)])

                    units.append(unit_)
            return units
        units = build_units()
        while pending:
            pending.pop(0)()
        if own:
            for t in range(NT):
                dma_sp(xs_s[T0 - 10 + t], xs_tok[:, t, :], "st_xs%d" % t, reads=[("xs_tok", t, cb) for cb in range(4)])
                dma_sp(bt_s[T0 - 10 + t], BTs[:, :, t * 128:(t + 1) * 128], "st_bt",
                       reads=[("BTs", cc) for cc in range(4)])
                dma_sp(ct_s[T0 - 10 + t], CTs[:, :, t * 128:(t + 1) * 128], "st_ct",
                       reads=[("CTs", cc) for cc in range(4)])
            for cb in range(4):
                tm_job(7232 + cb * 512, 512, "v", cb)
                for _ in range(2):
                    if units:
                        units.pop(0)()
            pg.op("dve", lambda e: e.memset(ssq[:], 0.0), writes=["ssq"] + [("ssq", t) for t in range(4)])
            def ln_tile(t):
                pg.op("act", lambda e, t=t: e.activation(out=vout[t % 2][:], in_=xin[:, t, :], func=AF.Identity,
                                                         accum_out=ssq[:, t:t + 1]),
                      reads=[("xin", t), "ssq"], writes=["vout%d" % (t % 2), ("ssq", t)])
                pg.op("act", lambda e, t=t: e.activation(out=vout[t % 2][:], in_=xin[:, t, :], func=AF.Square,
                                                         accum_out=ssq[:, 4 + t:5 + t]),
                      reads=[("xin", t), "ssq"], writes=["vout%d" % (t % 2), ("ssq", t)])
                mean, var, rs_, nmr = (ssq[:, 8 + t:9 + t], ssq[:, 12 + t:13 + t], ssq[:, 12 + t:13 + t], ssq[:, 8 + t:9 + t])
                k = ("ssq", t)
                pg.op("dve", lambda e, t=t, mean=mean: e.tensor_scalar_mul(out=mean, in0=ssq[:, t:t + 1], scalar1=1.0 / D),
                      reads=[k], writes=[k])
                pg.op("dve", lambda e, t=t, mean=mean, var=var: e.tensor_tensor(out=var, in0=mean, in1=mean, op=OP.mult),
                      reads=[k], writes=[k])
                pg.op("dve", lambda e, t=t, var=var: e.scalar_tensor_tensor(
                    out=var, in0=ssq[:, 4 + t:5 + t], scalar=1.0 / D, in1=var, op0=OP.mult, op1=OP.subtract),
                    reads=[k], writes=[k])
                pg.op("dve", lambda e, var=var: e.tensor_scalar_add(out=var, in0=var, scalar1=EPS), reads=[k], writes=[k])
                pg.op("act", lambda e, var=var: e.sqrt(out=var, in_=var), reads=[k], writes=[k])
                pg.op("dve", lambda e, var=var: e.reciprocal(out=var, in_=var), reads=[k], writes=[k])
                pg.op("dve", lambda e, mean=mean, var=var: e.scalar_tensor_tensor(
                    out=mean, in0=mean, scalar=-1.0, in1=var, op0=OP.mult, op1=OP.mult), reads=[k], writes=[k])
                pg.op("act", lambda e, t=t, mean=mean, var=var: e.activation(
                    out=xin[:, t, :], in_=xin[:, t, :], func=AF.Identity, bias=mean, scale=var),
                    reads=[("xin", t), k], writes=[("xin", t)])
                pg.op("dve", lambda e, t=t: e.tensor_tensor(out=xin[:, t, :], in0=xin[:, t, :], in1=lnr[:, 0, :], op=OP.mult),
                      reads=[("xin", t), "lnr"], writes=[("xin", t)])
                pg.op("dve", lambda e, t=t: e.tensor_tensor(out=vout[t % 2][:], in0=xin[:, t, :], in1=lnr[:, 1, :], op=OP.add),
                      reads=[("xin", t), "lnr"], writes=["vout%d" % (t % 2)])
                dma_sp(v_s[T0 - 10 + t], vout[t % 2][:], "st_vout%d" % (t % 2), reads=["vout%d" % (t % 2)])

            for cb in range(4):
                fm_job(5184 + cb * 512, "u", cb)
                if units:
                    units.pop(0)()
                ln_tile(cb)
            for t in range(NT):
                dma_sp(u_s[T0 - 10 + t], stg_u[:, :, t * 128:(t + 1) * 128], "st_stg", reads=["stg"])
        return units

        if SUB <= 2:
            return
    prev_units = []
    for blk_ in blocks:
        prev_units = do_block(*blk_, prev_units)
    while prev_units:
        prev_units.pop(0)()
    if "t_dt" in taps:
        t_dt = nc.dram_tensor("t_dt", [128, 18 * 64], F32, kind="ExternalOutput").ap()
        dma_sp(t_dt, dtall[:].rearrange("p a b -> p (a b)"), "st_tap", reads=[("dtall", T) for T in range(18)])
        t_dec = nc.dram_tensor("t_dec", [128, 18 * 64], F32, kind="ExternalOutput").ap()
        dma_sp(t_dec, decall[:].rearrange("p a b -> p (a b)"), "st_tap2", reads=[("decall", T, d_) for T in range(18) for d_ in range(2)])
    pg.barrier()
    pg.flush()
    st.close()
    if stage <= 1:
        return finish(nc, pg, es, out)


    st = ExitStack()
    hst = st.enter_context(nc.sbuf_tensor("hst", [128, 2048], F32))
    Sld = [st.enter_context(nc.sbuf_tensor("Sld%d" % i, [128, 2048], F32)) for i in range(2)]
    hpb = [st.enter_context(nc.sbuf_tensor("hpb%d" % i, [128, 2048], BF16)) for i in range(2)]
    wb3 = [st.enter_context(nc.sbuf_tensor("p3w%d" % i, [128, 16, 512], BF16)) for i in range(2)]
    modps3 = st.enter_context(nc.psum_tensor("modps3", [128, 192], F32))
    mps_cur[0] = modps3
    for blk in range(8, 24):
        mod_block(blk, wb3)
    mod_finish(32, 96, modps3)
    ab(4, PV_N2G, 4, 3, 0)
    pg.op("dve", lambda e: e.tensor_copy(out=G12[:, 0, :], in_=mt[:, 2, :, 0]), reads=["modT"], writes=["G12a"])
    pg.op("dve", lambda e: e.tensor_copy(out=G12[:, 1, :], in_=mt[:, 5, :, 0]), reads=["modT"], writes=["G12b"])
    for dr in range(2):
        pg.op("dve", lambda e: e.memset(hst[:], 0.0), writes=["hst"])
        if dr == 0:
            chain = list(range(0, 18))
        else:
            chain = [1, 0] + list(range(9, 1, -1)) + list(range(17, 9, -1))
        for i, T in enumerate(chain):
            sl, slk = Sld[i % 2], "Sld%d" % (i % 2)
            dma_sp(sl[:], S_all[T, dr], "ld_" + slk, writes=[slk])
            if T >= 10:
                hb, hbk = hpb[i % 2], "hpb%d" % (i % 2)
                pg.op("act", lambda e, hb=hb: e.copy(out=hb[:], in_=hst[:]), reads=["hst"], writes=[hbk])
                dma_sp(hp_s[dr, T - 10], hb[:], "st_" + hbk, reads=[hbk])
            pg.op("dve", lambda e, T=T, dr=dr: e.tensor_tensor(
                out=hst[:].rearrange("p (h d) -> p h d", d=64), in0=hst[:].rearrange("p (h d) -> p h d", d=64),
                in1=decall[:, T, dr * 32:(dr + 1) * 32].unsqueeze(2).to_broadcast([128, 32, 64]), op=OP.mult),
                reads=["hst", ("decall", T, dr)], writes=["hst"])
            pg.op("dve", lambda e, sl=sl: e.tensor_tensor(out=hst[:], in0=hst[:], in1=sl[:], op=OP.add),
                  reads=["hst", slk], writes=["hst"])
    pg.barrier()
    pg.flush()
    st.close()
    if stage <= 3:
        return finish(nc, pg, es, out)


    sel3b = sb("sel3b", [128, 4096], BF16)
    dma_cast(sel3b[:], sel3, "ld_c2", writes=["sel3b"])
    st = ExitStack()
    def sb4(name, shape, dt=F32):
        return st.enter_context(nc.sbuf_tensor(name, list(shape), dt))
    def ps4(name, shape, dt=F32):
        return st.enter_context(nc.psum_tensor(name, list(shape), dt))
    xs_c = [sb4("xs_c%d" % i, [128, 2048], BF16) for i in range(2)]
    bt_c = [sb4("bt_c%d" % i, [128, 4, 128], BF16) for i in range(2)]
    ct_c = [sb4("ct_c%d" % i, [128, 4, 128], BF16) for i in range(2)]
    hpf_c = [sb4("hpf_c%d" % i, [128, 2048], BF16) for i in range(2)]
    hpb_c = [sb4("hpb_c%d" % i, [128, 2048], BF16) for i in range(2)]
    z_c = [sb4("z_c%d" % i, [128, 2048], BF16) for i in range(2)]
    v_c = [sb4("v_c%d" % i, [128, 2048], BF16) for i in range(2)]
    u_c = [sb4("u_c%d" % i, [128, 16, 128], BF16) for i in range(2)]
    mixb = [sb4("mixb%d" % i, [128, 32, 128], BF16) for i in range(2)]
    wsb = sb4("wsb", [128, 8, 128], BF16)
    dta3 = sb4("dta3", [128, 96])
    acs = sb4("acs", [128, 64])
    nacs = sb4("nacs", [128, 64])
    ecum = sb4("ecum", [128, 64])
    xdt = [sb4("xdt%d" % i, [128, 2048], BF16) for i in range(2)]
    pcs = [sb4("pcs%d" % i, [128, 128], BF16) for i in range(2)]
    tbb = sb4("tbb", [128, 128], BF16)
    Rr = sb4("Rr", [128, 128])
    R2 = sb4("R2", [128, 128])
    cbT = sb4("cbT", [128, 4, 128])
    dws = [sb4("dw%d" % i, [128, 4, 128]) for i in range(2)]
    Mm = [[sb4("Mm%d_%d" % (i, j), [128, 8, 128], BF16) for j in range(2)] for i in range(2)]
    ucnt = [0]
    t1 = sb4("t1", [128, 512])
    t2 = sb4("t2", [128, 512])
    yb = sb4("yb", [128, 2048])
    yn = sb4("yn", [128, 2048], BF16)
    gt = sb4("gt", [128, 512])
    ss4 = sb4("ss4", [128, 4])
    pm2 = ps4("pm2", [128, 512])
    pcb = ps4("pcb", [128, 512])
    pDs = [ps4("pD%d" % i, [128, 512]) for i in range(2)]
    pY = ps4("pY", [128, 512])
    pOf = ps4("pOf", [128, 512])
    pOb = ps4("pOb", [128, 512])
    pGa = ps4("pGa", [128, 512])
    pG = [pGa, pcb]
    pGk = ["pGa", "pcb"]
    dma_cast(wsb[:].rearrange("p g i -> p (g i)"), wsT, "ld_wsb", writes=["wsb"])
    mkb = [sb4("mkb%d" % i, [128, 128], BF16) for i in range(2)]
    pg.op("dve", lambda e: e.tensor_copy(out=mkb[0][:], in_=cst[:, C_MNF:C_MNF + 128]), reads=["cst"], writes=["mkb"])
    pg.op("dve", lambda e: e.tensor_copy(out=mkb[1][:], in_=cst[:, C_MNB:C_MNB + 128]), reads=["cst"], writes=["mkb"])
    dsk = rp[:, RP_DSKIP:RP_DSKIP + 32]

    def do_loads(c):
        i2 = c % 2
        xs, bt, ct, hpf, hpb_, zc, vc, uc, mix = (xs_c[i2], bt_c[i2], ct_c[i2], hpf_c[i2], hpb_c[i2], z_c[i2],
                                                   v_c[i2], u_c[i2], mixb[i2])
        K = lambda n: "%s%d" % (n, i2)
        dma_sp(xs[:], xs_s[c], "ld_" + K("xs"), writes=[K("xs")])
        dma_sp(bt[:], bt_s[c], "ld_" + K("bt"), writes=[K("bt")])
        dma_sp(ct[:], ct_s[c], "ld_" + K("ct"), writes=[K("ct")])
        dma_sp(hpf[:], hp_s[0, c], "ld_" + K("hpf"), writes=[K("hpf")])
        dma_sp(hpb_[:], hp_s[1, c], "ld_" + K("hpb"), writes=[K("hpb")])
        dma_sp(zc[:], z_s[c], "ld_" + K("z"), writes=[K("z")])
        dma_sp(vc[:], v_s[c], "ld_" + K("v"), writes=[K("v")])
        dma_sp(uc[:], u_s[c], "ld_" + K("u"), writes=[K("u")])

    def do_chunk(c):
        T = 10 + c
        i2 = c % 2
        xs, bt, ct, hpf, hpb_, zc, vc, uc, mix = (xs_c[i2], bt_c[i2], ct_c[i2], hpf_c[i2], hpb_c[i2], z_c[i2],
                                                   v_c[i2], u_c[i2], mixb[i2])
        K = lambda n: "%s%d" % (n, i2)
        def mmcb(e):
            ins = None
            for g in range(4):
                ins = e.matmul(pcb[:, g * 128:(g + 1) * 128], lhsT=bt[:, g, :], rhs=ct[:, g, :], start=True, stop=True)
            return ins
        pg.op("pe", mmcb, reads=[K("bt"), K("ct")], writes=["pcb"])
        pg.op("act", lambda e: e.copy(out=cbT[:].rearrange("p g i -> p (g i)"), in_=pcb[:]), reads=["pcb"], writes=["cbT"])
        for dr in range(2):
            tri = cst[:, C_TU:C_TU + 128] if dr == 0 else cst[:, C_TL:C_TL + 128]
            pg.op("dve", lambda e, dr=dr: e.tensor_tensor(
                out=dta3[:].rearrange("p (r h) -> p r h", h=32),
                in0=dtall[:, T, dr * 32:(dr + 1) * 32].unsqueeze(1).to_broadcast([128, 3, 32]),
                in1=aneg[:, dr * 32:(dr + 1) * 32].unsqueeze(1).to_broadcast([128, 3, 32]), op=OP.mult),
                reads=["aneg"], writes=["dta3"])
            def mmac(e, dr=dr, tri=tri):
                e.matmul(pm2[:, dr * 32:(dr + 1) * 32], lhsT=tri, rhs=dta3[:, 0:32], start=True, stop=True)
                return e.matmul(pm2[0:96, 64 + dr * 128:64 + (dr + 1) * 128], lhsT=dta3[:, 0:96], rhs=tri,
                                start=True, stop=True)
            pg.op("pe", mmac, reads=["dta3", "cst"], writes=[("pm2", dr)])
            sl = slice(dr * 32, (dr + 1) * 32)
            pg.op("act", lambda e, sl=sl: e.copy(out=acs[:, sl], in_=pm2[:, sl]), reads=[("pm2", dr)], writes=[("acs", dr)])
            pg.op("dve", lambda e, sl=sl: e.tensor_scalar_mul(out=nacs[:, sl], in0=acs[:, sl], scalar1=-1.0),
                  reads=[("acs", dr)], writes=[("nacs", dr)])
            pg.op("act", lambda e, sl=sl: e.activation(out=ecum[:, sl], in_=acs[:, sl], func=AF.Exp),
                  reads=[("acs", dr)], writes=[("ecum", dr)])
            src_ = pm2[:, 64 + dr * 128:64 + (dr + 1) * 128]
            pc = pcs[dr]
            pk = "pcs%d" % dr
            pg.op("act", lambda e, pc=pc, src_=src_: e.copy(out=pc[0:32, :], in_=src_[0:32, :]),
                  reads=[("pm2", dr)], writes=[(pk, 0)])
            for lo in (32, 64):
                pg.op("act", lambda e, src_=src_, lo=lo: e.copy(out=tbb[lo:lo + 32, :], in_=src_[lo:lo + 32, :]),
                      reads=[("pm2", dr)], writes=[("tbb", lo)])
                pg.op("dve", lambda e, src_=src_, lo=lo: e.tensor_tensor(out=Rr[lo:lo + 32, :], in0=src_[lo:lo + 32, :],
                                                                        in1=tbb[lo:lo + 32, :], op=OP.subtract),
                      reads=[("pm2", dr), ("tbb", lo)], writes=[("Rr", lo)])
            pg.op("act", lambda e, pc=pc: e.copy(out=pc[32:64, :], in_=Rr[32:64, :]), reads=[("Rr", 32)], writes=[(pk, 1)])
            pg.op("act", lambda e: e.copy(out=tbb[64:96, :], in_=Rr[64:96, :]), reads=[("Rr", 64)], writes=[("tbb", 64)])
            pg.op("dve", lambda e: e.tensor_tensor(out=R2[64:96, :], in0=Rr[64:96, :], in1=tbb[64:96, :], op=OP.subtract),
                  reads=[("Rr", 64), ("tbb", 64)], writes=["R2"])
            pg.op("act", lambda e, pc=pc: e.copy(out=pc[64:96, :], in_=R2[64:96, :]), reads=["R2"], writes=[(pk, 2)])
            pg.op("pool", lambda e, dr=dr: e.tensor_tensor(
                out=xdt[dr][:].rearrange("p (h d) -> p h d", d=64), in0=xs[:].rearrange("p (h d) -> p h d", d=64),
                in1=dtall[:, T, dr * 32:(dr + 1) * 32].unsqueeze(2).to_broadcast([128, 32, 64]), op=OP.mult),
                reads=[K("xs")], writes=["xdt%d" % dr])
        def emit_D(g):
            Mg = Mm[g % 2]
            Mk = lambda dr, hf, g=g: ("Mm", g % 2, dr, hf)
            for dr in range(2):
                mk = cst[:, C_MNF:C_MNF + 128] if dr == 0 else cst[:, C_MNB:C_MNB + 128]
                pc = pcs[dr]
                pk = "pcs%d" % dr
                for hf in range(2):
                    bsel = ucnt[0] % 2
                    ucnt[0] += 1
                    pDh, pDk = pDs[bsel], "pD%d" % bsel
                    dw, dwk = dws[bsel], "dw%d" % bsel
                    h0 = g * 8 + hf * 4
                    def mmD(e, h0=h0, pc=pc, pDh=pDh, dr=dr):
                        ins = None
                        for j in range(4):
                            h = h0 + j
                            e.matmul(pDh[:, j * 128:(j + 1) * 128], lhsT=sel3b[0:96, h * 128:(h + 1) * 128],
                                     rhs=pc[0:96, :], start=True, stop=False)
                            ins = e.matmul(pDh[:, j * 128:(j + 1) * 128], lhsT=idb[:], rhs=mkb[dr][:],
                                           start=False, stop=True)
                        return ins
                    pg.op("pe", mmD, reads=[(pk, 0), (pk, 1), (pk, 2), "sel3b", "mkb", "idb"], writes=[pDk])
                    pg.op("dve", lambda e, dw=dw, pDh=pDh, h0=h0, dr=dr: e.tensor_tensor(
                        out=dw[:], in0=pDh[:].rearrange("p (h i) -> p h i", i=128),
                        in1=acs[:, dr * 32 + h0:dr * 32 + h0 + 4].unsqueeze(2).to_broadcast([128, 4, 128]), op=OP.subtract),
                        reads=[pDk, ("acs", dr)], writes=[dwk])
                    pg.op("act", lambda e, dw=dw: e.activation(out=dw[:], in_=dw[:], func=AF.Exp), reads=[dwk], writes=[dwk])
                    pg.op("pool", lambda e, dw=dw, g=g, dr=dr, hf=hf, Mg=Mg: e.tensor_tensor(
                        out=Mg[dr][:, hf * 4:(hf + 1) * 4, :], in0=dw[:],
                        in1=cbT[:, g, :].unsqueeze(1).to_broadcast([128, 4, 128]), op=OP.mult),
                        reads=[dwk, "cbT"], writes=[Mk(dr, hf)])
        def emit_Y(g):
            Mg = Mm[g % 2]
            Mk = lambda dr, hf, g=g: ("Mm", g % 2, dr, hf)
            def mmY(e, g=g, Mg=Mg):
                ins = None
                for hh in range(8):
                    h = g * 8 + hh
                    e.matmul(pY[:, hh * 64:(hh + 1) * 64], lhsT=Mg[0][:, hh, :], rhs=xdt[0][:, h * 64:(h + 1) * 64],
                             start=True, stop=False)
                    ins = e.matmul(pY[:, hh * 64:(hh + 1) * 64], lhsT=Mg[1][:, hh, :], rhs=xdt[1][:, h * 64:(h + 1) * 64],
                                   start=False, stop=True)
                return ins
            pg.op("pe", mmY, reads=[Mk(dr, hf) for dr in range(2) for hf in range(2)] + ["xdt0", "xdt1"], writes=["pY"])
            pg.op("pe", lambda e, g=g: e.matmul(pOf[:], lhsT=ct[:, g, :], rhs=hpf[:, g * 512:(g + 1) * 512],
                                                start=True, stop=True), reads=[K("ct"), K("hpf")], writes=["pOf"])
            pg.op("pe", lambda e, g=g: e.matmul(pOb[:], lhsT=ct[:, g, :], rhs=hpb_[:, g * 512:(g + 1) * 512],
                                                start=True, stop=True), reads=[K("ct"), K("hpb")], writes=["pOb"])
            v3 = lambda ap: ap.rearrange("p (h d) -> p h d", d=64)
            pg.op("dve", lambda e, g=g: e.tensor_tensor(
                out=v3(t1[:]), in0=v3(pOf[:]), in1=ecum[:, g * 8:(g + 1) * 8].unsqueeze(2).to_broadcast([128, 8, 64]),
                op=OP.mult), reads=["pOf", ("ecum", 0)], writes=["t1"])
            pg.op("dve", lambda e, g=g: e.tensor_tensor(
                out=v3(t2[:]), in0=v3(pOb[:]), in1=ecum[:, 32 + g * 8:32 + (g + 1) * 8].unsqueeze(2).to_broadcast([128, 8, 64]),
                op=OP.mult), reads=["pOb", ("ecum", 1)], writes=["t2"])
            pg.op("pool", lambda e: e.tensor_tensor(out=t1[:], in0=t1[:], in1=t2[:], op=OP.add), reads=["t1", "t2"],
                  writes=["t1"])
            pg.op("dve", lambda e, g=g: e.tensor_tensor(out=yb[:, g * 512:(g + 1) * 512], in0=pY[:], in1=t1[:], op=OP.add),
                  reads=["pY", "t1"], writes=[("yb", g)])
            pg.op("pool", lambda e, g=g: e.tensor_tensor(
                out=v3(t2[:]), in0=v3(xs[:, g * 512:(g + 1) * 512]),
                in1=dsk[:, g * 8:(g + 1) * 8].unsqueeze(2).to_broadcast([128, 8, 64]), op=OP.mult),
                reads=[K("xs"), "rp", "t2"], writes=["t2"])
            pg.op("pool", lambda e, g=g: e.tensor_tensor(out=yb[:, g * 512:(g + 1) * 512], in0=yb[:, g * 512:(g + 1) * 512],
                                                        in1=t2[:], op=OP.add), reads=[("yb", g), "t2"], writes=[("yb", g)])

        emit_D(0)
        for g in range(4):
            if g + 1 < 4:
                emit_D(g + 1)
            emit_Y(g)
        ybk = [("yb", g) for g in range(4)]
        pg.op("dve", lambda e: e.tensor_tensor(out=yb[:], in0=yb[:], in1=zc[:], op=OP.mult), reads=ybk + [K("z")], writes=ybk)
        pg.op("dve", lambda e: e.memset(ss4[:], 0.0), writes=["ss4"])
        pg.op("act", lambda e: e.activation(out=yn[:], in_=yb[:], func=AF.Square, accum_out=ss4[:, 0:1]),
              reads=ybk + ["ss4"], writes=["yn", "ss4"])
        pg.op("dve", lambda e: e.tensor_scalar(out=ss4[:, 1:2], in0=ss4[:, 0:1], scalar1=1.0 / D, scalar2=EPS,
                                               op0=OP.mult, op1=OP.add), reads=["ss4"], writes=["ss4"])
        pg.op("act", lambda e: e.sqrt(out=ss4[:, 1:2], in_=ss4[:, 1:2]), reads=["ss4"], writes=["ss4"])
        pg.op("dve", lambda e: e.reciprocal(out=ss4[:, 1:2], in_=ss4[:, 1:2]), reads=["ss4"], writes=["ss4"])
        pg.op("act", lambda e: e.activation(out=yn[:], in_=yb[:], func=AF.Copy, scale=ss4[:, 1:2]),
              reads=ybk + ["ss4"], writes=["yn"])
        for q in range(4):
            pgq, pgk = pG[q % 2], pGk[q % 2]
            def mmT(e, q=q, pgq=pgq):
                ins = None
                for j in range(4):
                    kc = q * 4 + j
                    ins = e.matmul(pgq[:, j * 128:(j + 1) * 128], lhsT=yn[:, kc * 128:(kc + 1) * 128], rhs=idb[:],
                                   start=True, stop=True)
                return ins
            pg.op("pe", mmT, reads=["yn", "idb"], writes=[pgk])
            for j in range(4):
                kc = q * 4 + j
                pg.op("act", lambda e, j=j, kc=kc, pgq=pgq: e.activation(
                    out=mix[:, kc, :], in_=pgq[:, j * 128:(j + 1) * 128], func=AF.Copy,
                    scale=pv[:, PV_SNG + kc:PV_SNG + kc + 1]), reads=[pgk, "pv"], writes=[(K("mix"), kc)])
        for q in range(4):
            pgq, pgk = pG[q % 2], pGk[q % 2]
            def mmG(e, q=q, pgq=pgq):
                ins = None
                for j in range(4):
                    cc = q * 4 + j
                    ins = e.matmul(pgq[:, j * 128:(j + 1) * 128], lhsT=vc[:, cc * 128:(cc + 1) * 128], rhs=wsb[:, cc // 2, :],
                                   start=True, stop=True)
                return ins
            pg.op("pe", mmG, reads=[K("v"), "wsb"], writes=[pgk])
            pg.op("dve", lambda e, q=q, pgq=pgq: e.tensor_tensor(
                out=gt[:].rearrange("p (a b i) -> p a b i", a=2, b=2),
                in0=pgq[:].rearrange("p (a b i) -> p a b i", a=2, b=2),
                in1=rp[:, RP_BS + q * 256:RP_BS + (q + 1) * 256].rearrange("p (a i) -> p a i", a=2).unsqueeze(2)
                .to_broadcast([128, 2, 2, 128]), op=OP.add), reads=[pgk, "rp"], writes=["gt"])
            pg.op("pool", lambda e, q=q: e.tensor_tensor(
                out=mix[:, 16 + q * 4:16 + (q + 1) * 4, :], in0=gt[:].rearrange("p (c i) -> p c i", i=128),
                in1=uc[:, q * 4:(q + 1) * 4, :], op=OP.mult), reads=["gt", K("u")], writes=[(K("mix"), 16 + q)])
        dma_sp(mix_s[c], mix[:], "st_" + K("mix"),
               reads=[(K("mix"), kc) for kc in range(20)])

    nch = NCH
    do_loads(0)
    for c in range(nch):
        if c + 1 < nch:
            do_loads(c + 1)
        do_chunk(c)
    pg.barrier()
    pg.flush()
    st.close()
    if stage <= 4:
        return finish(nc, pg, es, out)


    st5 = ExitStack()
    x1T = st5.enter_context(nc.sbuf_tensor("x1T", [128, 16, 1024], F32))
    banks = [st5.enter_context(nc.psum_tensor("bk%d" % i, [128, 512], F32)) for i in range(8)]
    bkk = ["bk%d" % i for i in range(8)]
    st = ExitStack()
    mixblk = st.enter_context(nc.sbuf_tensor("mixblk", [128, 32, 512], BF16))
    xin5 = st.enter_context(nc.sbuf_tensor("xin5", [128, 4, 2048], F32))
    wo = [st.enter_context(nc.sbuf_tensor("wo%d" % i, [128, 32, 256], BF16)) for i in range(2)]
    tmp5 = [st.enter_context(nc.sbuf_tensor("tmp5_%d" % i, [128, 512], F32)) for i in range(2)]
    w_out_v = w_out.rearrange("(kc p) c -> p kc c", p=128)

    def do_p5(tb):
        for t in range(4):
            dma_sp(mixblk[:, :, t * 128:(t + 1) * 128], mix_s[tb * 4 + t], "ld_mixblk%d" % t, writes=[("mixblk", t)])
            r0 = 1280 + (tb * 4 + t) * 128
            dma_sp(xin5[:, t, :], x_all[r0:r0 + 128, :], "ld_xin5_%d" % t, writes=[("xin5", t)])
        for dcp in range(8):
            w = wo[dcp % 2]
            wk = "wo%d" % (dcp % 2)
            dma_cast(w[:], w_out_v[:, :, dcp * 256:(dcp + 1) * 256], "ld_" + wk, writes=[wk])
            for d2 in range(2):
                dc = dcp * 2 + d2
                i2 = dc % 2
                pa, pak = banks[i2], bkk[i2]
                px, pxk = banks[2 + i2], bkk[2 + i2]
                def mm(e, w=w, d2=d2, pa=pa):
                    ins = None
                    for kc in range(32):
                        ins = e.matmul(pa[:], lhsT=w[:, kc, d2 * 128:(d2 + 1) * 128], rhs=mixblk[:, kc, :],
                                       start=(kc == 0), stop=(kc == 31))
                    return ins
                pg.op("pe", mm, reads=[wk] + [("mixblk", t) for t in range(4)], writes=[pak])
                tm, tmk = tmp5[i2], "tmp5_%d" % i2
                pg.op("act", lambda e, tm=tm, pa=pa, dc=dc: e.activation(out=tm[:], in_=pa[:], func=AF.Copy,
                                                                         scale=G12[:, 0, dc:dc + 1]),
                      reads=[pak, "G12a"], writes=[tmk])
                def trx(e, px=px, dc=dc):
                    ins = None
                    for t in range(4):
                        ins = e.transpose(out=px[:, t * 128:(t + 1) * 128], in_=xin5[:, t, dc * 128:(dc + 1) * 128],
                                          identity=ident)
                    return ins
                pg.op("pe", trx, reads=[("xin5", t) for t in range(4)] + ["cst"], writes=[pxk])
                pg.op("dve", lambda e, px=px, tm=tm, dc=dc: e.tensor_tensor(
                    out=x1T[:, dc, tb * 512:(tb + 1) * 512], in0=px[:], in1=tm[:], op=OP.add),
                    reads=[pxk, tmk], writes=[("x1T", dc, tb)])
    for tb in range(2):
        do_p5(tb)
    if "t_x1T" in taps:
        t_x1T = nc.dram_tensor("t_x1T", [128, 16 * 1024], F32, kind="ExternalOutput").ap()
        dma_sp(t_x1T, x1T[:].rearrange("p a b -> p (a b)"), "st_tap5",
               reads=[("x1T", dc, tb) for dc in range(16) for tb in range(2)])
    pg.barrier()
    pg.flush()
    st.close()
    if stage <= 5:
        st5.close()
        return finish(nc, pg, es, out)


    st6 = ExitStack()
    h2T = st6.enter_context(nc.sbuf_tensor("h2T", [128, 16, 1024], BF16))
    cpc = st6.enter_context(nc.sbuf_tensor("cpc", [128, 1024], BF16))
    st = ExitStack()
    def sb6(name, shape, dt=F32):
        return st.enter_context(nc.sbuf_tensor(name, list(shape), dt))
    sq = [sb6("sq%d" % i, [128, 512]) for i in range(2)]
    rstd = sb6("rstd", [128, 1024])
    tmph = [sb6("tmph%d" % i, [128, 1024]) for i in range(2)]
    wrb = sb6("wrb", [128, 16, 36], BF16)
    lg = sb6("lg", [128, 8, 36])
    mg = sb6("mg", [128, 8])
    eg = sb6("eg", [128, 8, 4])
    sgm = sb6("sgm", [128, 8])
    tpg = sb6("tpg", [128, 8])
    ohg = sb6("ohg", [128, 8, 4])
    selx = sb6("selx", [128, 8, 8])
    tmp8 = sb6("tmp8", [128, 8, 8])
    m1 = sb6("m1", [128, 8])
    m2 = sb6("m2", [128, 8])
    mask1 = sb6("mask1", [128, 8, 8])
    mask2 = sb6("mask2", [128, 8, 8])
    sel2 = sb6("sel2", [128, 8, 8])
    p1 = sb6("p1", [128, 8])
    p2 = sb6("p2", [128, 8])
    wex = sb6("wex", [128, 8, 8])
    comb3 = sb6("comb3", [128, 8, 3, 32])
    ctb = sb6("ctb", [128, 1024], BF16)
    cR = sb6("cR", [128, 1024])
    cR2 = sb6("cR2", [128, 1024])
    dma_cast(wrb[:].rearrange("p a b -> p (a b)"), wr, "ld_wrb", writes=["wrb"])
    x1k = [("x1T", dc, tb) for dc in range(16) for tb in range(2)]
    for tb in range(2):
        for kc in range(16):
            s_, sk = sq[kc % 2], "sq%d" % (kc % 2)
            pg.op("act", lambda e, s_=s_, kc=kc, tb=tb: e.activation(out=s_[:], in_=x1T[:, kc, tb * 512:(tb + 1) * 512],
                                                                     func=AF.Square), reads=[("x1T", kc, tb)], writes=[sk])
            pg.op("pe", lambda e, s_=s_, kc=kc, tb=tb: e.matmul(banks[tb][:], lhsT=ones, rhs=s_[:], start=(kc == 0),
                                                               stop=(kc == 15)), reads=[sk, "cst"], writes=[bkk[tb]])
        sl = slice(tb * 512, (tb + 1) * 512)
        pg.op("dve", lambda e, tb=tb, sl=sl: e.tensor_scalar(out=rstd[:, sl], in0=banks[tb][:], scalar1=1.0 / D, scalar2=EPS,
                                                            op0=OP.mult, op1=OP.add), reads=[bkk[tb]], writes=[("rstd", tb)])
        pg.op("act", lambda e, sl=sl: e.sqrt(out=rstd[:, sl], in_=rstd[:, sl]), reads=[("rstd", tb)], writes=[("rstd", tb)])
        pg.op("dve", lambda e, sl=sl: e.reciprocal(out=rstd[:, sl], in_=rstd[:, sl]), reads=[("rstd", tb)], writes=[("rstd", tb)])
    for kc in range(16):
        th, thk = tmph[kc % 2], "tmph%d" % (kc % 2)
        pg.op("dve", lambda e, th=th, kc=kc: e.tensor_tensor(out=th[:], in0=x1T[:, kc, :], in1=rstd[:], op=OP.mult),
              reads=[("x1T", kc, 0), ("x1T", kc, 1), ("rstd", 0), ("rstd", 1)], writes=[thk])
        pg.op("act", lambda e, th=th, kc=kc: e.activation(out=h2T[:, kc, :], in_=th[:], func=AF.Identity,
                                                          bias=AB[:, 5, kc:kc + 1], scale=AB[:, 4, kc:kc + 1]),
              reads=[thk, "AB4", "AB5"], writes=[("h2T", kc)])
    h2k = [("h2T", kc) for kc in range(16)]
    for t in range(8):
        pr, prk = banks[2 + t % 2], bkk[2 + t % 2]
        def mmr(e, t=t, pr=pr):
            ins = None
            for kc in range(16):
                ins = e.matmul(pr[:, 0:36], lhsT=h2T[:, kc, t * 128:(t + 1) * 128], rhs=wrb[:, kc, :],
                               start=(kc == 0), stop=(kc == 15))
            return ins
        pg.op("pe", mmr, reads=h2k + ["wrb"], writes=[prk])
        pg.op("dve", lambda e, t=t, pr=pr: e.tensor_tensor(out=lg[:, t, :], in0=pr[:, 0:36], in1=rp[:, RP_BR:RP_BR + 36],
                                                          op=OP.add), reads=[prk, "rp"], writes=[("lg", t)])
    lgk = [("lg", t) for t in range(8)]
    lgG = lg[:, :, 0:4]
    bc = lambda ap, n: ap.unsqueeze(2).to_broadcast([128, 8, n])
    R_ = "rt"
    pg.op("dve", lambda e: e.tensor_reduce(out=mg[:], in_=lgG, axis=AX.X, op=OP.max), reads=lgk, writes=[R_])
    pg.op("dve", lambda e: e.tensor_tensor(out=eg[:], in0=lgG, in1=bc(mg[:], 4), op=OP.subtract), reads=lgk + [R_], writes=[R_])
    pg.op("act", lambda e: e.activation(out=eg[:], in_=eg[:], func=AF.Exp), reads=[R_], writes=[R_])
    pg.op("dve", lambda e: e.tensor_reduce(out=sgm[:], in_=eg[:], axis=AX.X, op=OP.add), reads=[R_], writes=[R_])
    pg.op("dve", lambda e: e.reciprocal(out=tpg[:], in_=sgm[:]), reads=[R_], writes=[R_])
    pg.op("dve", lambda e: e.tensor_tensor(out=ohg[:], in0=lgG, in1=bc(mg[:], 4), op=OP.is_equal), reads=lgk + [R_], writes=[R_])
    for g in range(4):
        lgE = lg[:, :, 4 + g * 8:4 + (g + 1) * 8]
        dst = selx if g == 0 else tmp8
        pg.op("dve", lambda e, g=g, lgE=lgE, dst=dst: e.tensor_tensor(
            out=dst[:], in0=lgE, in1=ohg[:, :, g:g + 1].to_broadcast([128, 8, 8]), op=OP.mult), reads=lgk + [R_], writes=[R_])
        if g > 0:
            pg.op("dve", lambda e: e.tensor_tensor(out=selx[:], in0=selx[:], in1=tmp8[:], op=OP.add), reads=[R_], writes=[R_])
    pg.op("dve", lambda e: e.tensor_reduce(out=m1[:], in_=selx[:], axis=AX.X, op=OP.max), reads=[R_], writes=[R_])
    pg.op("dve", lambda e: e.tensor_tensor(out=mask1[:], in0=selx[:], in1=bc(m1[:], 8), op=OP.is_equal), reads=[R_], writes=[R_])
    pg.op("dve", lambda e: e.tensor_scalar_mul(out=sel2[:], in0=mask1[:], scalar1=-1.0e30), reads=[R_], writes=[R_])
    pg.op("dve", lambda e: e.tensor_tensor(out=sel2[:], in0=sel2[:], in1=selx[:], op=OP.add), reads=[R_], writes=[R_])
    pg.op("dve", lambda e: e.tensor_reduce(out=m2[:], in_=sel2[:], axis=AX.X, op=OP.max), reads=[R_], writes=[R_])
    pg.op("dve", lambda e: e.tensor_tensor(out=mask2[:], in0=sel2[:], in1=bc(m2[:], 8), op=OP.is_equal), reads=[R_], writes=[R_])
    pg.op("dve", lambda e: e.tensor_tensor(out=p2[:], in0=m2[:], in1=m1[:], op=OP.subtract), reads=[R_], writes=[R_])
    pg.op("act", lambda e: e.activation(out=p2[:], in_=p2[:], func=AF.Exp), reads=[R_], writes=[R_])
    pg.op("dve", lambda e: e.tensor_scalar_add(out=p1[:], in0=p2[:], scalar1=1.0), reads=[R_], writes=[R_])
    pg.op("dve", lambda e: e.reciprocal(out=p1[:], in_=p1[:]), reads=[R_], writes=[R_])
    pg.op("dve", lambda e: e.tensor_tensor(out=p2[:], in0=p2[:], in1=p1[:], op=OP.mult), reads=[R_], writes=[R_])
    pg.op("dve", lambda e: e.tensor_tensor(out=p1[:], in0=p1[:], in1=tpg[:], op=OP.mult), reads=[R_], writes=[R_])
    pg.op("dve", lambda e: e.tensor_tensor(out=p2[:], in0=p2[:], in1=tpg[:], op=OP.mult), reads=[R_], writes=[R_])
    pg.op("dve", lambda e: e.tensor_tensor(out=wex[:], in0=mask1[:], in1=bc(p1[:], 8), op=OP.mult), reads=[R_], writes=[R_])
    pg.op("dve", lambda e: e.tensor_tensor(out=tmp8[:], in0=mask2[:], in1=bc(p2[:], 8), op=OP.mult), reads=[R_], writes=[R_])
    pg.op("dve", lambda e: e.tensor_tensor(out=wex[:], in0=wex[:], in1=tmp8[:], op=OP.add), reads=[R_], writes=[R_])
    for r in range(3):
        for g in range(4):
            pg.op("dve", lambda e, r=r, g=g: e.tensor_tensor(
                out=comb3[:, :, r, g * 8:(g + 1) * 8], in0=wex[:], in1=ohg[:, :, g:g + 1].to_broadcast([128, 8, 8]),
                op=OP.mult), reads=[R_], writes=[R_, ("comb3", r, g)])
    if "t_comb" in taps:
        t_comb = nc.dram_tensor("t_comb", [128, 8 * 96], F32, kind="ExternalOutput").ap()
        dma_sp(t_comb, comb3[:].rearrange("p a b c -> p (a b c)"), "st_tap6", reads=[R_])
    if "t_h2T" in taps:
        t_h2T = nc.dram_tensor("t_h2T", [128, 16 * 1024], BF16, kind="ExternalOutput").ap()
        dma_sp(t_h2T, h2T[:].rearrange("p a b -> p (a b)"), "st_tap7", reads=h2k)
    for t in range(8):
        pc_, pck = banks[4 + t // 4], bkk[4 + t // 4]
        pg.op("pe", lambda e, t=t, pc_=pc_: e.transpose(out=pc_[0:96, (t % 4) * 128:(t % 4 + 1) * 128],
                                                        in_=comb3[:, t, :, :].rearrange("p r c -> p (r c)"), identity=ident),
              reads=[R_, "cst"], writes=[(pck, t % 4)])
    for hb in range(2):
        src_ = banks[4 + hb]
        sk = [(bkk[4 + hb], j) for j in range(4)]
        sl = slice(hb * 512, (hb + 1) * 512)
        pg.op("act", lambda e, src_=src_, sl=sl: e.copy(out=cpc[0:32, sl], in_=src_[0:32, :]), reads=sk, writes=[("cpc", 0, hb)])
        for lo in (32, 64):
            pg.op("act", lambda e, src_=src_, sl=sl, lo=lo: e.copy(out=ctb[lo:lo + 32, sl], in_=src_[lo:lo + 32, :]),
                  reads=sk, writes=[("ctb", lo, hb)])
            pg.op("dve", lambda e, src_=src_, sl=sl, lo=lo: e.tensor_tensor(
                out=cR[lo:lo + 32, sl], in0=src_[lo:lo + 32, :], in1=ctb[lo:lo + 32, sl], op=OP.subtract),
                reads=sk + [("ctb", lo, hb)], writes=[("cR", lo, hb)])
        pg.op("act", lambda e, sl=sl: e.copy(out=cpc[32:64, sl], in_=cR[32:64, sl]), reads=[("cR", 32, hb)],
              writes=[("cpc", 1, hb)])
        pg.op("act", lambda e, sl=sl: e.copy(out=ctb[64:96, sl], in_=cR[64:96, sl]), reads=[("cR", 64, hb)],
              writes=[("ctb", 64, hb)])
        pg.op("dve", lambda e, sl=sl: e.tensor_tensor(out=cR2[64:96, sl], in0=cR[64:96, sl], in1=ctb[64:96, sl],
                                                      op=OP.subtract), reads=[("cR", 64, hb), ("ctb", 64, hb)],
              writes=[("cR2", hb)])
        pg.op("act", lambda e, sl=sl: e.copy(out=cpc[64:96, sl], in_=cR2[64:96, sl]), reads=[("cR2", hb)],
              writes=[("cpc", 2, hb)])
    pg.barrier()
    pg.flush()
    st.close()
    if stage <= 6:
        st6.close()
        st5.close()
        return finish(nc, pg, es, out)


    st = ExitStack()
    def sb7(name, shape, dt=F32):
        return st.enter_context(nc.sbuf_tensor(name, list(shape), dt))
    wgh = [sb7("wgh%d" % i, [128, 16, 256], BF16) for i in range(2)]
    wuh = [sb7("wuh%d" % i, [128, 16, 256], BF16) for i in range(2)]
    wdn = sb7("wdn", [128, 4, 2048], BF16)
    hid = sb7("hid", [128, 4, 1024], BF16)
    cbc = sb7("cbc", [128, 2, 512])
    sgs = [sb7("sgs%d" % i, [128, 512]) for i in range(2)]
    tus = [sb7("tus%d" % i, [128, 512]) for i in range(2)]
    tmo = [sb7("tmo%d" % i, [128, 512]) for i in range(2)]
    cpk = [("cpc", r, hb) for r in range(3) for hb in range(2)]
    cnt6 = [0]

    def do_expert(ex):
        wg_v = w_g[ex].rearrange("(kc p) f -> p kc f", p=128)
        wu_v = w_u[ex].rearrange("(kc p) f -> p kc f", p=128)
        wd_v = w_d[ex].rearrange("(fc p) d -> p fc d", p=128)
        for tb in range(2):
            pg.op("pe", lambda e, tb=tb: e.matmul(banks[6][:], lhsT=sel3b[0:96, ex * 128:(ex + 1) * 128],
                                                  rhs=cpc[0:96, tb * 512:(tb + 1) * 512], start=True, stop=True),
                  reads=cpk + ["sel3b"], writes=[bkk[6]])
            pg.op("act", lambda e, tb=tb: e.copy(out=cbc[:, tb, :], in_=banks[6][:]), reads=[bkk[6]], writes=[("cbc", tb)])
        for half in range(2):
            wg_, wu_ = wgh[half], wuh[half]
            dma_cast(wg_[:], wg_v[:, :, half * 256:(half + 1) * 256], "ld_wgh%d" % half, writes=["wgh%d" % half])
            dma_cast(wu_[:], wu_v[:, :, half * 256:(half + 1) * 256], "ld_wuh%d" % half, writes=["wuh%d" % half])
            for fcl in range(2):
                fc = half * 2 + fcl
                for tb in range(2):
                    i2 = cnt6[0] % 2
                    cnt6[0] += 1
                    pgt, pgk_ = banks[i2], bkk[i2]
                    pup, puk = banks[2 + i2], bkk[2 + i2]
                    def mmg(e, wg_=wg_, fcl=fcl, tb=tb, pgt=pgt):
                        ins = None
                        for kc in range(16):
                            ins = e.matmul(pgt[:], lhsT=wg_[:, kc, fcl * 128:(fcl + 1) * 128],
                                           rhs=h2T[:, kc, tb * 512:(tb + 1) * 512], start=(kc == 0), stop=(kc == 15))
                        return ins
                    pg.op("pe", mmg, reads=["wgh%d" % half] + h2k, writes=[pgk_])
                    def mmu(e, wu_=wu_, fcl=fcl, tb=tb, pup=pup):
                        ins = None
                        for kc in range(16):
                            ins = e.matmul(pup[:], lhsT=wu_[:, kc, fcl * 128:(fcl + 1) * 128],
                                           rhs=h2T[:, kc, tb * 512:(tb + 1) * 512], start=(kc == 0), stop=(kc == 15))
                        return ins
                    pg.op("pe", mmu, reads=["wuh%d" % half] + h2k, writes=[puk])
                    sg_, sgk = sgs[i2], "sgs%d" % i2
                    tu_, tuk = tus[i2], "tus%d" % i2
                    pg.op("act", lambda e, sg_=sg_, pgt=pgt: e.activation(out=sg_[:], in_=pgt[:], func=AF.Silu),
                          reads=[pgk_], writes=[sgk])
                    pg.op("dve", lambda e, tu_=tu_, pup=pup, sg_=sg_: e.tensor_tensor(out=tu_[:], in0=pup[:], in1=sg_[:],
                                                                                     op=OP.mult),
                          reads=[puk, sgk], writes=[tuk])
                    pg.op("dve", lambda e, tu_=tu_, fc=fc, tb=tb: e.tensor_tensor(
                        out=hid[:, fc, tb * 512:(tb + 1) * 512], in0=tu_[:], in1=cbc[:, tb, :], op=OP.mult),
                        reads=[tuk, ("cbc", tb)], writes=[("hid", fc, tb)])
        dma_cast(wdn[:], wd_v, "ld_wdn", writes=["wdn"])
        for dc in range(16):
            for tb in range(2):
                i2 = cnt6[0] % 2
                cnt6[0] += 1
                po, pok = banks[4 + i2], bkk[4 + i2]
                def mmd(e, dc=dc, tb=tb, po=po):
                    ins = None
                    for fc in range(4):
                        ins = e.matmul(po[:], lhsT=wdn[:, fc, dc * 128:(dc + 1) * 128], rhs=hid[:, fc, tb * 512:(tb + 1) * 512],
                                       start=(fc == 0), stop=(fc == 3))
                    return ins
                pg.op("pe", mmd, reads=["wdn"] + [("hid", fc, tb) for fc in range(4)], writes=[pok])
                tm_, tmk = tmo[i2], "tmo%d" % i2
                pg.op("act", lambda e, tm_=tm_, po=po, dc=dc: e.activation(out=tm_[:], in_=po[:], func=AF.Copy,
                                                                           scale=G12[:, 1, dc:dc + 1]),
                      reads=[pok, "G12b"], writes=[tmk])
                pg.op("dve", lambda e, tm_=tm_, dc=dc, tb=tb: e.tensor_tensor(
                    out=x1T[:, dc, tb * 512:(tb + 1) * 512], in0=x1T[:, dc, tb * 512:(tb + 1) * 512], in1=tm_[:], op=OP.add),
                    reads=[("x1T", dc, tb), tmk], writes=[("x1T", dc, tb)])
    for ex in range(NEXP):
        do_expert(ex)
    pg.barrier()
    pg.flush()
    st.close()
    st6.close()

    st = ExitStack()
    nfr = st.enter_context(nc.sbuf_tensor("nfr", [128, 2048], F32))
    xo = [st.enter_context(nc.sbuf_tensor("xo%d" % i, [128, 2048], F32)) for i in range(2)]
    junk = st.enter_context(nc.sbuf_tensor("junk", [128, 2048], BF16))
    ss7 = st.enter_context(nc.sbuf_tensor("ss7", [128, 16], F32))
    dma_sp(nfr[:], lnrows[:, 2 * D:3 * D], "ld_nfr", writes=["nfr"])
    pg.op("dve", lambda e: e.memset(ss7[:], 0.0), writes=["ss7"])
    for t in range(8):
        xo_, xok = xo[t % 2], "xo%d" % (t % 2)
        for q in range(4):
            pf, pfk = banks[q % 2], bkk[q % 2]
            def trf(e, t=t, q=q, pf=pf):
                ins = None
                for j in range(4):
                    dc = q * 4 + j
                    ins = e.transpose(out=pf[:, j * 128:(j + 1) * 128], in_=x1T[:, dc, t * 128:(t + 1) * 128], identity=ident)
                return ins
            pg.op("pe", trf, reads=[("x1T", q * 4 + j, t // 4) for j in range(4)] + ["cst"], writes=[pfk])
            pg.op("act", lambda e, xo_=xo_, q=q, pf=pf: e.copy(out=xo_[:, q * 512:(q + 1) * 512], in_=pf[:]),
                  reads=[pfk], writes=[(xok, q)])
        xk = [(xok, q) for q in range(4)]
        pg.op("act", lambda e, xo_=xo_, t=t: e.activation(out=junk[:], in_=xo_[:], func=AF.Square, accum_out=ss7[:, t:t + 1]),
              reads=xk + ["ss7"], writes=["junk", ("ss7", t)])
        pg.op("dve", lambda e, t=t: e.tensor_scalar(out=ss7[:, 8 + t:9 + t], in0=ss7[:, t:t + 1], scalar1=1.0 / D, scalar2=EPS,
                                                    op0=OP.mult, op1=OP.add), reads=[("ss7", t)], writes=[("rs7", t)])
        pg.op("act", lambda e, t=t: e.sqrt(out=ss7[:, 8 + t:9 + t], in_=ss7[:, 8 + t:9 + t]), reads=[("rs7", t)], writes=[("rs7", t)])
        pg.op("dve", lambda e, t=t: e.reciprocal(out=ss7[:, 8 + t:9 + t], in_=ss7[:, 8 + t:9 + t]), reads=[("rs7", t)],
              writes=[("rs7", t)])
        pg.op("dve", lambda e, xo_=xo_, t=t: e.scalar_tensor_tensor(out=xo_[:], in0=xo_[:], scalar=ss7[:, 8 + t:9 + t],
                                                                    in1=nfr[:], op0=OP.mult, op1=OP.mult),
              reads=xk + [("rs7", t), "nfr"], writes=xk)
        dma_sp(out[t * 128:(t + 1) * 128, :], xo_[:], "st_out%d" % (t % 2), reads=xk)
    pg.barrier()
    pg.flush()
    st.close()
    st5.close()
    return finish(nc, pg, es, out)


def finish(nc, pg, es, out):
    es.close()
    return nc


def _consts():
    c = np.zeros((128, C_N), np.float32)
    i = np.arange(128)
    c[:, C_ID:C_ID + 128] = np.eye(128, dtype=np.float32)
    c[:, C_TU:C_TU + 128] = (i[:, None] <= i[None, :])
    c[:, C_TL:C_TL + 128] = (i[:, None] >= i[None, :])
    c[:, C_MNF:C_MNF + 128] = np.where(i[:, None] <= i[None, :], 0.0, -30000.0)
    c[:, C_MNB:C_MNB + 128] = np.where(i[:, None] >= i[None, :], 0.0, -30000.0)
    c[:, C_ONE:C_ONE + 128] = 1.0
    s3 = np.zeros((128, 32, 128), np.float32)
    for p in range(96):
        s3[p, p % 32, :] = 1.0
    return c, s3.reshape(128, 4096)


def _pp(v):
    return np.ascontiguousarray(np.asarray(v, np.float32).reshape(-1, 128).T)


def prep_inputs(inp):
    f = lambda a: np.ascontiguousarray(np.asarray(a, np.float32))
    x, c, ctx, c_ctx = f(inp["x"]), f(inp["c"]), f(inp["ctx"]), f(inp["c_ctx"])
    cst, s3 = _consts()
    conv_w = f(inp["conv_w"])[0]
    pvec = np.zeros((128, PV_N), np.float32)
    pvec[:, PV_N1G:PV_N1G + 16] = _pp(inp["norm1_g"][0])
    pvec[:, PV_N2G:PV_N2G + 16] = _pp(inp["norm2_g"][0])
    pvec[:, PV_SNG:PV_SNG + 16] = _pp(inp["ssd_norm_g"][0])
    for j in range(3):
        pvec[:, PV_CW + j * 24:PV_CW + (j + 1) * 24] = _pp(conv_w[j])
    pvec[:, PV_CB:PV_CB + 24] = _pp(inp["conv_b"][0])
    pvec[:, PV_BMOD:PV_BMOD + 96] = _pp(inp["b_mod"][0])
    row = np.zeros((RP_N,), np.float32)
    row[RP_DTB:RP_DTB + 32] = f(inp["dt_bias_f"])[0]
    row[RP_DTB + 32:RP_DTB + 64] = f(inp["dt_bias_b"])[0]
    row[RP_ALOG:RP_ALOG + 32] = f(inp["a_log_f"])[0]
    row[RP_ALOG + 32:RP_ALOG + 64] = f(inp["a_log_b"])[0]
    row[RP_DSKIP:RP_DSKIP + 32] = f(inp["d_skip"])[0]
    row[RP_BS:RP_BS + 1024] = f(inp["b_spatial"])[0].reshape(-1)
    row[RP_BR:RP_BR + 4] = f(inp["b_router_group"])[0]
    row[RP_BR + 4:RP_BR + 36] = f(inp["b_router_expert"])[0].reshape(-1)
    rowp = np.ascontiguousarray(np.broadcast_to(row[None, :], (128, RP_N)))
    lnr = np.concatenate([f(inp["cm_ln_g"])[0], f(inp["cm_ln_b"])[0], f(inp["normf_g"])])
    lnrows = np.ascontiguousarray(np.broadcast_to(lnr[None, :], (128, 3 * D)))
    w_mod = f(inp["w_mod"])[0]
    w_in = f(inp["w_in"])[0]
    w_out = f(inp["w_out"])[0]
    wsT = np.ascontiguousarray(np.transpose(f(inp["w_spatial"])[0], (2, 0, 1)).reshape(128, 1024))
    wrg = f(inp["w_router_group"])[0]
    wre = np.transpose(f(inp["w_router_expert"])[0], (1, 0, 2)).reshape(D, 32)
    wrc = np.concatenate([wrg, wre], axis=1)
    wr = np.ascontiguousarray(wrc.reshape(16, 128, 36).transpose(1, 0, 2).reshape(128, 16 * 36))
    w_g = f(inp["w_exp_gate"])[0].reshape(32, D, 512)
    w_u = f(inp["w_exp_up"])[0].reshape(32, D, 512)
    w_d = f(inp["w_exp_down"])[0].reshape(32, 512, D)
    maps = []
    for k in range(NCORES):
        b, s = k // 2, k % 2
        own = x[b, s * 1024:(s + 1) * 1024]
        oth = x[b, (1 - s) * 1024:(2 - s) * 1024]
        x_all = np.concatenate([ctx[b], oth, own], axis=0)
        fl = np.zeros((128, 2), np.float32)
        fl[:, 0] = 1.0 if s == 1 else 0.0
        fl[:, 1] = 1.0 if s == 0 else 0.0
        cvec = np.stack([_pp(c[b]), _pp(c_ctx)], axis=2).reshape(128, 32)
        maps.append(dict(x_all=x_all, flags=fl, cvec=np.ascontiguousarray(cvec), pvec=pvec, rowp=rowp,
                         lnrows=lnrows, consts=cst, sel3=s3, w_mod=w_mod, w_in=w_in, w_out=w_out,
                         wsT=wsT, wr=wr, w_g=w_g, w_u=w_u, w_d=w_d))
    return maps


def kernel(**inputs):
    maps = prep_inputs(inputs)
    nc = build_nc()
    res = run_bass_kernel_spmd(nc, maps, core_ids=list(range(NCORES)))
    outf = np.zeros((4, 2048, D), np.float32)
    for k in range(NCORES):
        b, s = k // 2, k % 2
        outf[b, s * 1024:(s + 1) * 1024] = res.results[k]["out"]
    return outf
```

```python
from contextlib import ExitStack
import numpy as np
import concourse.bass as bass
import concourse.mybir as mybir
from concourse.bass_utils import run_bass_kernel_spmd

F32 = mybir.dt.float32
BF16 = mybir.dt.bfloat16
AF = mybir.ActivationFunctionType
OP = mybir.AluOpType
AX = mybir.AxisListType

D = 2048
NCORES = 8
EPS = 1e-6
ENGS = ("pe", "act", "dve", "pool", "sp")
SUB = 99
JOBS = None
NOTR = False
NCH = 8
NEXP = 32
NOCONV = False

PV_N1G, PV_N2G, PV_SNG, PV_CW, PV_CB, PV_BMOD = 0, 16, 32, 48, 120, 144
PV_N = 240
RP_DTB, RP_ALOG, RP_DSKIP, RP_BS, RP_BR = 0, 64, 128, 160, 1184
RP_N = 1220
C_ID, C_TU, C_TL, C_MNF, C_MNB, C_ONE = 0, 128, 256, 384, 512, 640
C_N = 768


class Prog:
    def __init__(self, nc, es):
        self.nc = nc
        self.es = es
        self.sems = {}
        self.cnt = {}
        self.ops = {e: [] for e in ENGS}
        self.known = {e: {} for e in ENGS}
        self.last_w = {}
        self.readers = {}
        self.latest = {}
        for e in ENGS:
            self._sem(e)

    def _sem(self, key):
        if key not in self.sems:
            self.sems[key] = self.es.enter_context(self.nc.semaphore("s_" + str(key)))
            self.cnt[key] = 0
        return self.sems[key]

    def op(self, eng, fn, reads=(), writes=(), dma=None):
        waits = {}
        def need(tok):
            sk, v = tok
            if sk == "pe" and eng == "pe":
                return
            if self.known[eng].get(sk, 0) >= v:
                return
            waits[sk] = max(waits.get(sk, 0), v)
        for k in reads:
            if k in self.last_w:
                need(self.last_w[k])
        for k in writes:
            if k in self.last_w:
                need(self.last_w[k])
            for r in self.readers.get(k, ()):
                need(r)
        for sk, v in waits.items():
            self.known[eng][sk] = v
        if dma is not None:
            self._sem(dma)
            self.cnt[dma] += 16
            tok = (dma, self.cnt[dma])
        else:
            self.cnt[eng] += 1
            tok = (eng, self.cnt[eng])
        self.latest[tok[0]] = tok[1]
        for k in writes:
            self.last_w[k] = tok
            self.readers[k] = []
        for k in reads:
            self.readers.setdefault(k, []).append(tok)
        self.ops[eng].append((list(waits.items()), fn, tok, dma is not None))
        return tok

    def barrier(self):
        for e in ENGS:
            waits = []
            for sk, v in self.latest.items():
                if sk == e and e == "pe":
                    continue
                if self.known[e].get(sk, 0) < v:
                    waits.append((sk, v))
                    self.known[e][sk] = v
            if waits:
                self.ops[e].append((waits, None, None, False))
        self.last_w.clear()
        self.readers.clear()

    def flush(self):
        nc = self.nc
        ops = self.ops
        sems = self.sems

        def run(engh, lst):
            for waits, fn, tok, isdma in lst:
                for sk, v in waits:
                    engh.wait_ge(sems[sk], v)
                if fn is None:
                    continue
                ins = fn(engh)
                ins.then_inc(sems[tok[0]], 16 if isdma else 1)

        with nc.Block() as block:
            if ops["sp"]:
                @block.sync
                def _(e):
                    run(e, ops["sp"])
            if ops["pe"]:
                @block.tensor
                def _(e):
                    run(e, ops["pe"])
            if ops["act"]:
                @block.scalar
                def _(e):
                    run(e, ops["act"])
            if ops["dve"]:
                @block.vector
                def _(e):
                    run(e, ops["dve"])
            if ops["pool"]:
                @block.gpsimd
                def _(e):
                    run(e, ops["pool"])
        self.ops = {e: [] for e in ENGS}


def build_nc(stage=99, taps=()):
    nc = bass.Bass("TRN2", target_bir_lowering=False)
    es = ExitStack()
    pg = Prog(nc, es)

    def din(name, shape, dt=F32):
        return nc.dram_tensor(name, list(shape), dt, kind="ExternalInput").ap()

    def dscr(name, shape, dt):
        kind = "ExternalOutput" if name in taps else "Internal"
        return nc.dram_tensor(name, list(shape), dt, kind=kind).ap()

    x_all = din("x_all", [2304, D])
    flags = din("flags", [128, 2])
    cvec = din("cvec", [128, 32])
    pvec = din("pvec", [128, PV_N])
    rowp = din("rowp", [128, RP_N])
    lnrows = din("lnrows", [128, 3 * D])
    consts = din("consts", [128, C_N])
    sel3 = din("sel3", [128, 4096])
    w_mod = din("w_mod", [D, 6 * D])
    w_in = din("w_in", [D, 9280])
    w_out = din("w_out", [2 * D, D])
    wsT = din("wsT", [128, 1024])
    wr = din("wr", [128, 16 * 36])
    w_g = din("w_g", [32, D, 512])
    w_u = din("w_u", [32, D, 512])
    w_d = din("w_d", [32, 512, D])
    out = nc.dram_tensor("out", [1024, D], F32, kind="ExternalOutput").ap()

    def sb(name, shape, dt=F32):
        return es.enter_context(nc.sbuf_tensor(name, list(shape), dt))

    def ps(name, shape, dt=F32):
        return es.enter_context(nc.psum_tensor(name, list(shape), dt))

    cst = sb("cst", [128, C_N])
    idb = sb("idb", [128, 128], BF16)
    pv = sb("pv", [128, PV_N])
    rp = sb("rp", [128, RP_N])
    flg = sb("flg", [128, 2])
    cv = sb("cv", [128, 32])
    scT = sb("scT", [128, 32], BF16)
    modT = sb("modT", [128, 192])
    AB = sb("AB", [128, 6, 16])
    G12 = sb("G12", [128, 2, 16])
    dtall = sb("dtall", [128, 18, 64])
    aneg = sb("aneg", [128, 64])
    decall = sb("decall", [128, 18, 64])

    ident = cst[:, C_ID:C_ID + 128]
    ones = cst[:, C_ONE:C_ONE + 128]

    def dma_sp(out_ap, in_ap, sem, reads=(), writes=()):
        pg.op("sp", lambda e: e.dma_start(out=out_ap, in_=in_ap), reads=reads, writes=writes, dma=sem)

    def dma_cast(out_ap, in_ap, sem, reads=(), writes=()):
        pg.op("pool", lambda e: e.dma_start(out=out_ap, in_=in_ap), reads=reads, writes=writes, dma=sem)

    dma_sp(cst[:], consts, "ld_c0", writes=["cst"])
    dma_sp(pv[:], pvec, "ld_c1", writes=["pv"])
    dma_sp(rp[:], rowp, "ld_c3", writes=["rp"])
    dma_sp(flg[:], flags, "ld_c4", writes=["flg"])
    dma_sp(cv[:], cvec, "ld_c5", writes=["cv"])
    pg.op("dve", lambda e: e.tensor_copy(out=idb[:], in_=ident), reads=["cst"], writes=["idb"])
    pg.op("act", lambda e: e.activation(out=scT[:], in_=cv[:], func=AF.Silu), reads=["cv"], writes=["scT"])
    pg.op("act", lambda e: e.activation(out=aneg[:], in_=rp[:, RP_ALOG:RP_ALOG + 64], func=AF.Exp),
          reads=["rp"], writes=["aneg"])
    pg.op("dve", lambda e: e.tensor_scalar_mul(out=aneg[:], in0=aneg[:], scalar1=-1.0),
          reads=["aneg"], writes=["aneg"])

    st = ExitStack()
    wb = [st.enter_context(nc.sbuf_tensor("p0w%d" % i, [128, 16, 512], BF16)) for i in range(2)]
    modps = st.enter_context(nc.psum_tensor("modps", [128, 192], F32))
    w_mod_v = w_mod.rearrange("(kc p) c -> p kc c", p=128)
    mps_cur = [modps]
    mps_off = [0]
    def mod_block(blk, wb, part=3):
        w = wb[blk % 2]
        key = "p0w%d" % (blk % 2)
        if part & 1:
            dma_cast(w[:], w_mod_v[:, :, blk * 512:(blk + 1) * 512], "ld_" + key, writes=[key])
        if not (part & 2):
            return

        def mm(e, w=w, blk=blk):
            ins = None
            for cc in range(4):
                col = (blk * 4 + cc) * 2 - mps_off[0]
                for kc in range(16):
                    ins = e.matmul(mps_cur[0][:, col:col + 2], lhsT=w[:, kc, cc * 128:(cc + 1) * 128],
                                   rhs=scT[:, kc * 2:kc * 2 + 2], start=(kc == 0), stop=(kc == 15))
            return ins
        pg.op("pe", mm, reads=[key, "scT"], writes=["modps"])

    def mod_finish(c0, c1, modps, base=0):
        pg.op("dve", lambda e: e.tensor_tensor(
            out=modT[:, c0 * 2:c1 * 2].rearrange("p (c t) -> p c t", t=2),
            in0=modps[:, (c0 - base) * 2:(c1 - base) * 2].rearrange("p (c t) -> p c t", t=2),
            in1=pv[:, PV_BMOD + c0:PV_BMOD + c1].unsqueeze(2).to_broadcast([128, c1 - c0, 2]), op=OP.add),
            reads=["modps", "pv"], writes=["modT"])
    for blk in range(8):
        mod_block(blk, wb)
    mod_finish(0, 32, modps)
    mt = modT[:].rearrange("p (m kc t) -> p m kc t", m=6, kc=16, t=2)
    def ab(e_idx, g_off, sc_m, sh_m, which):
        pg.op("dve", lambda e: e.scalar_tensor_tensor(
            out=AB[:, e_idx, :], in0=mt[:, sc_m, :, which], scalar=1.0, in1=pv[:, g_off:g_off + 16],
            op0=OP.add, op1=OP.mult), reads=["modT", "pv"], writes=["AB%d" % e_idx])
        pg.op("dve", lambda e: e.tensor_copy(out=AB[:, e_idx + 1, :], in_=mt[:, sh_m, :, which]),
              reads=["modT"], writes=["AB%d" % (e_idx + 1)])
    ab(0, PV_N1G, 1, 0, 0)
    ab(2, PV_N1G, 1, 0, 1)
    if "t_modT" in taps:
        t_modT = nc.dram_tensor("t_modT", [128, 192], F32, kind="ExternalOutput").ap()
        dma_sp(t_modT, modT[:], "st_tap", reads=["modT"])
    pg.barrier()
    pg.flush()
    st.close()
    if stage <= 0:
        return finish(nc, pg, es, out)


    S_all = dscr("S_all", [18, 2, 128, 2048], F32)
    xs_s = dscr("xs_s", [8, 128, 2048], BF16)
    bt_s = dscr("bt_s", [8, 128, 4, 128], BF16)
    ct_s = dscr("ct_s", [8, 128, 4, 128], BF16)
    z_s = dscr("z_s", [8, 128, 2048], BF16)
    v_s = dscr("v_s", [8, 128, 2048], BF16)
    u_s = dscr("u_s", [8, 128, 16, 128], BF16)
    hp_s = dscr("hp_s", [2, 8, 128, 2048], BF16)
    mix_s = dscr("mix_s", [8, 128, 32, 128], BF16)

    st = ExitStack()
    def sb1(name, shape, dt=F32):
        return st.enter_context(nc.sbuf_tensor(name, list(shape), dt))
    def ps1(name, shape, dt=F32):
        return st.enter_context(nc.psum_tensor(name, list(shape), dt))
    xin = sb1("xin", [128, 4, 2048])
    hT = sb1("hT", [128, 16, 512], BF16)
    wb = [sb1("wb%d" % i, [128, 16, 512], BF16) for i in range(2)]
    t0s = [sb1("t0_%d" % i, [128, 512]) for i in range(2)]
    sbf = [sb1("sbf%d" % i, [128, 512], BF16) for i in range(2)]
    sraws = [sb1("sraw%d" % i, [128, 512]) for i in range(2)]
    xs_tok = sb1("xs_tok", [128, 4, 2048], BF16)
    B_tok = sb1("B_tok", [128, 4, 512], BF16)
    BTs = sb1("BTs", [128, 4, 512], BF16)
    CTs = sb1("CTs", [128, 4, 512], BF16)
    stg = sb1("stg", [128, 4, 2048], BF16)
    lnr = sb1("lnr", [128, 2, 2048])
    xw = [sb1("xw%d" % i, [128, 2048], BF16) for i in range(2)]
    vout = [sb1("vout%d" % i, [128, 2048], BF16) for i in range(2)]
    Sst = [sb1("Sst%d" % i, [128, 2048]) for i in range(2)]
    sm = sb1("sm", [128, 16, 64])
    ssq = sb1("ssq", [128, 16])
    wgt_t = sb1("wgt_t", [128, 4, 64])
    pA = [ps1("pA%d" % i, [128, 512]) for i in range(2)]
    pTf = [ps1("pTf%d" % i, [128, 512]) for i in range(2)]
    pS = [ps1("pS%d" % i, [128, 512]) for i in range(2)]
    pm = ps1("pm", [128, 512])
    stg_u = stg[:].rearrange("p t c -> p (t c)").rearrange("p (cc n) -> p cc n", cc=16)

    w_in_v = w_in.rearrange("(kc p) c -> p kc c", p=128)
    dma_sp(lnr[:].rearrange("p a c -> p (a c)"), lnrows[:, 0:2 * D], "ld_lnr", writes=["lnr"])
    wcnt = [0]
    acnt = [0]

    def load_w(c0, ncols):
        i = wcnt[0] % 2
        wcnt[0] += 1
        dma_cast(wb[i][:, :, 0:ncols], w_in_v[:, :, c0:c0 + ncols], "ld_wb%d" % i, writes=["wb%d" % i])
        return wb[i], "wb%d" % i

    def next_pA():
        i = acnt[0] % 2
        acnt[0] += 1
        return pA[i], "pA%d" % i, i

    blocks = [("ctx", 0, 2, 0), ("oth", 256, 4, 2), ("oth", 768, 4, 6), ("own", 1280, 4, 10), ("own", 1792, 4, 14)]
    if stage == 1:
        blocks = blocks[:1] + blocks[3:4]
    if SUB in (2, 3):
        blocks = blocks[:1]
    def do_block(kind, row0, NT, T0, prev_units):
        N = NT * 128
        own = kind == "own"
        Aidx = 2 if kind == "ctx" else 0
        pg.op("dve", lambda e: e.memset(ssq[:], 0.0), writes=["ssq"])
        for t in range(NT):
            dma_sp(xin[:, t, :], x_all[row0 + t * 128:row0 + (t + 1) * 128, :], "ld_xin%d" % t, writes=[("xin", t)])
            pg.op("act", lambda e, t=t: e.activation(out=stg[:, t, :], in_=xin[:, t, :], func=AF.Square,
                                                     accum_out=ssq[:, t:t + 1]),
                  reads=[("xin", t), "ssq"], writes=["stg", ("ssq", t)])
            pg.op("dve", lambda e, t=t: e.tensor_scalar(out=ssq[:, 8 + t:9 + t], in0=ssq[:, t:t + 1], scalar1=1.0 / D,
                                                        scalar2=EPS, op0=OP.mult, op1=OP.add),
                  reads=[("ssq", t)], writes=[("rs", t)])
            pg.op("act", lambda e, t=t: e.sqrt(out=ssq[:, 8 + t:9 + t], in_=ssq[:, 8 + t:9 + t]),
                  reads=[("rs", t)], writes=[("rs", t)])
            pg.op("dve", lambda e, t=t: e.reciprocal(out=ssq[:, 8 + t:9 + t], in_=ssq[:, 8 + t:9 + t]),
                  reads=[("rs", t)], writes=[("rs", t)])
            pg.op("act", lambda e, t=t: e.activation(out=xin[:, t, :], in_=xin[:, t, :], func=AF.Copy,
                                                     scale=ssq[:, 8 + t:9 + t]),
                  reads=[("xin", t), ("rs", t)], writes=[("xin", t)])
        if SUB <= 0:
            return
        for kc in range(16):
            pa, pak, _ = next_pA()
            def tr(e, pa=pa, kc=kc):
                ins = None
                for t in range(NT):
                    ins = e.transpose(out=pa[:, t * 128:(t + 1) * 128], in_=xin[:, t, kc * 128:(kc + 1) * 128],
                                      identity=ident)
                return ins
            pg.op("pe", tr, reads=[("xin", t) for t in range(NT)] + ["cst"], writes=[pak])
            pg.op("act", lambda e, pa=pa, kc=kc: e.activation(
                out=hT[:, kc, 0:N], in_=pa[:, 0:N], func=AF.Identity,
                bias=AB[:, Aidx + 1, kc:kc + 1], scale=AB[:, Aidx, kc:kc + 1]),
                reads=[pak, "AB%d" % Aidx, "AB%d" % (Aidx + 1)], writes=[("hT", kc)])
            if prev_units and kc % 2 == 1:
                prev_units.pop(0)()
        while prev_units:
            prev_units.pop(0)()
        hT_keys = [("hT", kc) for kc in range(16)]
        if SUB <= 1:
            return

        pending = []
        def fm_job(c0, jobkind, cb):
            w, wk = load_w(c0, 512)
            for cc in range(4):
                pa, pak, pi = next_pA()
                def mm(e, pa=pa, w=w, cc=cc):
                    ins = None
                    for kc in range(16):
                        ins = e.matmul(pa[:, 0:N], lhsT=w[:, kc, cc * 128:(cc + 1) * 128], rhs=hT[:, kc, 0:N],
                                       start=(kc == 0), stop=(kc == 15))
                    return ins
                pg.op("pe", mm, reads=[wk] + hT_keys, writes=[pak])
                while pending:
                    pending.pop(0)()
                if jobkind == "u":
                    pg.op("act", lambda e, pa=pa, cc=cc: e.activation(out=stg_u[:, cb * 4 + cc, 0:N], in_=pa[:, 0:N],
                                                                      func=AF.Gelu),
                          reads=[pak], writes=["stg"])
                    continue
                chn = (c0 - 2048) // 128 + cc
                L = 256 if kind == "ctx" else 64
                t0 = t0s[pi]
                t0k = "t0_%d" % pi
                sraw = sraws[pi]
                pg.op("act", lambda e, pa=pa, sraw=sraw: e.copy(out=sraw[:, 0:N], in_=pa[:, 0:N]),
                      reads=[pak], writes=["sraw%d" % pi])
                pav = sraw[:, 0:N].rearrange("p (r l) -> p r l", l=L)
                t0v = t0[:, 0:N].rearrange("p (r l) -> p r l", l=L)
                cw = lambda j, chn=chn: pv[:, PV_CW + j * 24 + chn:PV_CW + j * 24 + chn + 1]
                pg.op("act", lambda e, pa=pa, t0=t0, chn=chn, cw=cw: e.activation(
                    out=t0[:, 0:N], in_=pa[:, 0:N], func=AF.Identity,
                    bias=pv[:, PV_CB + chn:PV_CB + chn + 1], scale=cw(1)), reads=[pak, "pv"], writes=[t0k])
                if not NOCONV:
                  pg.op("dve", lambda e, pav=pav, t0v=t0v, cw=cw: e.scalar_tensor_tensor(
                    out=t0v[:, :, 1:L], in0=pav[:, :, 0:L - 1], scalar=cw(0), in1=t0v[:, :, 1:L],
                    op0=OP.mult, op1=OP.add), reads=["sraw%d" % pi, t0k, "pv"], writes=[t0k])
                if not NOCONV:
                  pg.op("dve", lambda e, pav=pav, t0v=t0v, cw=cw: e.scalar_tensor_tensor(
                    out=t0v[:, :, 0:L - 1], in0=pav[:, :, 1:L], scalar=cw(2), in1=t0v[:, :, 0:L - 1],
                    op0=OP.mult, op1=OP.add), reads=["sraw%d" % pi, t0k, "pv"], writes=[t0k])
                if jobkind == "xs":
                    dst, dk = sbf[pi][:, 0:N], "sbf%d" % pi
                elif jobkind == "B":
                    dst, dk = BTs[:, cc, 0:N], ("BTs", cc)
                else:
                    dst, dk = CTs[:, cc, 0:N], ("CTs", cc)
                pg.op("act", lambda e, t0=t0, dst=dst: e.activation(out=dst, in_=t0[:, 0:N], func=AF.Silu),
                      reads=[t0k], writes=[dk])
                if jobkind in ("xs", "B") and not NOTR:
                    def emit_tr(dst=dst, dk=dk, pi=pi, cc=cc, jobkind=jobkind, cb=cb):
                        ptf, ptk = pTf[pi], "pTf%d" % pi
                        def tr2(e):
                            ins = None
                            for t in range(NT):
                                ins = e.matmul(ptf[:, t * 128:(t + 1) * 128], lhsT=dst[:, t * 128:(t + 1) * 128],
                                               rhs=idb[:], start=True, stop=True)
                            return ins
                        pg.op("pe", tr2, reads=[dk, "idb"], writes=[ptk])
                        if jobkind == "xs":
                            o = xs_tok[:, 0:NT, cb * 512 + cc * 128:cb * 512 + (cc + 1) * 128]
                            ok = [("xs_tok", t, cb) for t in range(NT)]
                        else:
                            o = B_tok[:, 0:NT, cc * 128:(cc + 1) * 128]
                            ok = [("B_tok", t) for t in range(NT)]
                        iv = ptf[:, 0:N].rearrange("p (t c) -> p t c", c=128)
                        if cc % 2 == 0:
                            pg.op("dve", lambda e: e.tensor_copy(out=o, in_=iv), reads=[ptk], writes=ok)
                        else:
                            pg.op("act", lambda e: e.copy(out=o, in_=iv), reads=[ptk], writes=ok)
                    pending.append(emit_tr)

        def tm_job(c0, ncols, jobkind, cb):
            if jobkind == "dt":
                w, wk = load_w(4672, 512)
                wofs = 448
            else:
                w, wk = load_w(c0, ncols)
                wofs = 0
            for t in range(NT):
                pa, pak, pi = next_pA()
                def mm(e, pa=pa, w=w, t=t):
                    ins = None
                    for kc in range(16):
                        ins = e.matmul(pa[:, 0:ncols], lhsT=hT[:, kc, t * 128:(t + 1) * 128], rhs=w[:, kc, wofs:wofs + ncols],
                                       start=(kc == 0), stop=(kc == 15))
                    return ins
                pg.op("pe", mm, reads=[wk] + hT_keys, writes=[pak])
                while pending:
                    pending.pop(0)()
                if jobkind == "z":
                    pg.op("act", lambda e, pa=pa, t=t: e.activation(out=stg[:, t, cb * 512:(cb + 1) * 512], in_=pa[:],
                                                                    func=AF.Silu), reads=[pak], writes=["stg"])
                elif jobkind == "v":
                    pg.op("act", lambda e, pa=pa, t=t: e.activation(out=xin[:, t, cb * 512:(cb + 1) * 512], in_=pa[:],
                                                                    func=AF.Gelu), reads=[pak], writes=[("xin", t)])
                else:
                    T = T0 + t
                    a, b_, c_, d_ = sm[:, 0, :], sm[:, 1, :], sm[:, 2, :], sm[:, 3, :]
                    pg.op("dve", lambda e, pa=pa: e.tensor_tensor(out=a, in0=pa[:, 0:64], in1=rp[:, RP_DTB:RP_DTB + 64],
                                                                  op=OP.add), reads=[pak, "rp"], writes=["sm0"])
                    pg.op("dve", lambda e: e.tensor_scalar_mul(out=b_, in0=a, scalar1=-1.0),
                          reads=["sm0"], writes=["sm1"])
                    pg.op("dve", lambda e: e.tensor_tensor(out=b_, in0=b_, in1=a, op=OP.min),
                          reads=["sm0", "sm1"], writes=["sm1"])
                    pg.op("act", lambda e: e.activation(out=c_, in_=b_, func=AF.Exp),
                          reads=["sm1"], writes=["sm2"])
                    pg.op("dve", lambda e: e.tensor_scalar_add(out=c_, in0=c_, scalar1=1.0),
                          reads=["sm2"], writes=["sm2"])
                    pg.op("act", lambda e: e.activation(out=c_, in_=c_, func=AF.Ln),
                          reads=["sm2"], writes=["sm2"])
                    pg.op("dve", lambda e, T=T: e.scalar_tensor_tensor(out=dtall[:, T, :], in0=a, scalar=0.0, in1=c_,
                                                                       op0=OP.max, op1=OP.add),
                          reads=["sm0", "sm2"], writes=[("dtall", T)])
                    if kind == "oth":
                        for dr in range(2):
                            pg.op("dve", lambda e, T=T, dr=dr: e.tensor_scalar_mul(
                                out=dtall[:, T, dr * 32:(dr + 1) * 32], in0=dtall[:, T, dr * 32:(dr + 1) * 32],
                                scalar1=flg[:, dr:dr + 1]), reads=[("dtall", T), "flg"], writes=[("dtall", T)])

        if own:
            for cb in range(4):
                tm_job(cb * 512, 512, "z", cb)
            for t in range(NT):
                dma_sp(z_s[T0 - 10 + t], stg[:, t, :], "st_stg", reads=["stg"])
        for cb in range(4):
            if JOBS is None or "xs" in JOBS:
                fm_job(2048 + cb * 512, "xs", cb)
        if JOBS is None or "B" in JOBS:
            fm_job(4096, "B", 0)
        if own:
            fm_job(4608, "C", 0)
        if JOBS is None or "dt" in JOBS:
            tm_job(5120, 64, "dt", 0)
        while pending:
            pending.pop(0)()
        def build_units():
            units = []
            W = NT * 64
            dtab, acsb, ddb, wgtb = sm[:, 4:8, :], sm[:, 8:12, :], sm[:, 12:16, :], wgt_t[:]
            f2 = lambda ap: ap[:, 0:NT, :]
            dtk = [("dtall", T0 + t) for t in range(NT)]
            def prep_():
                pg.op("dve", lambda e: e.tensor_tensor(out=f2(dtab), in0=dtall[:, T0:T0 + NT, :],
                                                       in1=aneg[:].unsqueeze(1).to_broadcast([128, NT, 64]), op=OP.mult),
                      reads=dtk + ["aneg"], writes=["sm4"])
                def mm2(e):
                    pmv = pm[:, 0:W].rearrange("p (t c) -> p t c", c=64)
                    for dr in range(2):
                        tri = cst[:, C_TU:C_TU + 128] if dr == 0 else cst[:, C_TL:C_TL + 128]
                        e.matmul(pmv[:, :, dr * 32:(dr + 1) * 32], lhsT=tri, rhs=f2(dtab)[:, :, dr * 32:(dr + 1) * 32],
                                 start=True, stop=True)
                    return e.matmul(pm[:, 256:256 + W].rearrange("p (t c) -> p t c", c=64), lhsT=ones, rhs=f2(dtab),
                                    start=True, stop=True)
                pg.op("pe", mm2, reads=["sm4", "cst"], writes=["pm"])
                pg.op("act", lambda e: e.copy(out=f2(acsb), in_=pm[:, 0:W].rearrange("p (t c) -> p t c", c=64)),
                      reads=["pm"], writes=["sm5"])
                pg.op("act", lambda e: e.activation(out=decall[:, T0:T0 + NT, :],
                                                    in_=pm[:, 256:256 + W].rearrange("p (t c) -> p t c", c=64), func=AF.Exp),
                      reads=["pm"], writes=[("decall", T0 + t, d_) for t in range(NT) for d_ in range(2)])
                pg.op("dve", lambda e: e.tensor_tensor(out=f2(ddb), in0=pm[:, 256:256 + W].rearrange("p (t c) -> p t c", c=64),
                                                       in1=f2(acsb), op=OP.subtract), reads=["pm", "sm5"], writes=["sm6"])
                pg.op("act", lambda e: e.activation(out=f2(ddb), in_=f2(ddb), func=AF.Exp), reads=["sm6"], writes=["sm6"])
                pg.op("dve", lambda e: e.tensor_tensor(out=f2(wgtb), in0=f2(ddb), in1=dtall[:, T0:T0 + NT, :], op=OP.mult),
                      reads=["sm6"] + dtk, writes=["sm7"])

            units.append(prep_)
            for t in range(NT):
                for dr in range(2):
                    def unit_(t=t, dr=dr):
                        T = T0 + t
                        xwt = xw[dr]
                        pg.op("dve", lambda e, t=t, dr=dr, xwt=xwt: e.tensor_tensor(
                            out=xwt[:].rearrange("p (h d) -> p h d", d=64),
                            in0=xs_tok[:, t, :].rearrange("p (h d) -> p h d", d=64),
                            in1=wgtb[:, t, dr * 32:(dr + 1) * 32].unsqueeze(2).to_broadcast([128, 32, 64]), op=OP.mult),
                            reads=[("xs_tok", t, cb) for cb in range(4)] + ["sm7"], writes=["xw%d" % dr])
                        sst = Sst[dr]
                        for g in range(4):
                            psg, psk = pS[g % 2], "pS%d" % (g % 2)
                            pg.op("pe", lambda e, psg=psg, t=t, g=g, xwt=xwt: e.matmul(
                                psg[:], lhsT=B_tok[:, t, g * 128:(g + 1) * 128], rhs=xwt[:, g * 512:(g + 1) * 512],
                                start=True, stop=True), reads=[("B_tok", t), "xw%d" % dr], writes=[psk])
                            if g % 2 == 0:
                                pg.op("act", lambda e, psg=psg, g=g, sst=sst: e.copy(out=sst[:, g * 512:(g + 1) * 512], in_=psg[:]),
                                      reads=[psk], writes=[("Sst", dr, g)])
                            else:
                                pg.op("dve", lambda e, psg=psg, g=g, sst=sst: e.tensor_copy(out=sst[:, g * 512:(g + 1) * 512], in_=psg[:]),
                                      reads=[psk], writes=[("Sst", dr, g)])
                        dma_sp(S_all[T, dr], sst[:], "st_S%d" % dr, reads=[("Sst", dr, g) for g in range(4)])

                    units.append(unit_)
            return units
        units = build_units()
        while pending:
            pending.pop(0)()
        if own:
            for t in range(NT):
                dma_sp(xs_s[T0 - 10 + t], xs_tok[:, t, :], "st_xs%d" % t, reads=[("xs_tok", t, cb) for cb in range(4)])
                dma_sp(bt_s[T0 - 10 + t], BTs[:, :, t * 128:(t + 1) * 128], "st_bt",
                       reads=[("BTs", cc) for cc in range(4)])
                dma_sp(ct_s[T0 - 10 + t], CTs[:, :, t * 128:(t + 1) * 128], "st_ct",
                       reads=[("CTs", cc) for cc in range(4)])
            for cb in range(4):
                tm_job(7232 + cb * 512, 512, "v", cb)
                for _ in range(2):
                    if units:
                        units.pop(0)()
            pg.op("dve", lambda e: e.memset(ssq[:], 0.0), writes=["ssq"] + [("ssq", t) for t in range(4)])
            def ln_tile(t):
                pg.op("act", lambda e, t=t: e.activation(out=vout[t % 2][:], in_=xin[:, t, :], func=AF.Identity,
                                                         accum_out=ssq[:, t:t + 1]),
                      reads=[("xin", t), "ssq"], writes=["vout%d" % (t % 2), ("ssq", t)])
                pg.op("act", lambda e, t=t: e.activation(out=vout[t % 2][:], in_=xin[:, t, :], func=AF.Square,
                                                         accum_out=ssq[:, 4 + t:5 + t]),
                      reads=[("xin", t), "ssq"], writes=["vout%d" % (t % 2), ("ssq", t)])
                mean, var, rs_, nmr = (ssq[:, 8 + t:9 + t], ssq[:, 12 + t:13 + t], ssq[:, 12 + t:13 + t], ssq[:, 8 + t:9 + t])
                k = ("ssq", t)
                pg.op("dve", lambda e, t=t, mean=mean: e.tensor_scalar_mul(out=mean, in0=ssq[:, t:t + 1], scalar1=1.0 / D),
                      reads=[k], writes=[k])
                pg.op("dve", lambda e, t=t, mean=mean, var=var: e.tensor_tensor(out=var, in0=mean, in1=mean, op=OP.mult),
                      reads=[k], writes=[k])
                pg.op("dve", lambda e, t=t, var=var: e.scalar_tensor_tensor(
                    out=var, in0=ssq[:, 4 + t:5 + t], scalar=1.0 / D, in1=var, op0=OP.mult, op1=OP.subtract),
                    reads=[k], writes=[k])
                pg.op("dve", lambda e, var=var: e.tensor_scalar_add(out=var, in0=var, scalar1=EPS), reads=[k], writes=[k])
                pg.op("act", lambda e, var=var: e.sqrt(out=var, in_=var), reads=[k], writes=[k])
                pg.op("dve", lambda e, var=var: e.reciprocal(out=var, in_=var), reads=[k], writes=[k])
                pg.op("dve", lambda e, mean=mean, var=var: e.scalar_tensor_tensor(
                    out=mean, in0=mean, scalar=-1.0, in1=var, op0=OP.mult, op1=OP.mult), reads=[k], writes=[k])
                pg.op("act", lambda e, t=t, mean=mean, var=var: e.activation(
                    out=xin[:, t, :], in_=xin[:, t, :], func=AF.Identity, bias=mean, scale=var),
                    reads=[("xin", t), k], writes=[("xin", t)])
                pg.op("dve", lambda e, t=t: e.tensor_tensor(out=xin[:, t, :], in0=xin[:, t, :], in1=lnr[:, 0, :], op=OP.mult),
                      reads=[("xin", t), "lnr"], writes=[("xin", t)])
                pg.op("dve", lambda e, t=t: e.tensor_tensor(out=vout[t % 2][:], in0=xin[:, t, :], in1=lnr[:, 1, :], op=OP.add),
                      reads=[("xin", t), "lnr"], writes=["vout%d" % (t % 2)])
                dma_sp(v_s[T0 - 10 + t], vout[t % 2][:], "st_vout%d" % (t % 2), reads=["vout%d" % (t % 2)])

            for cb in range(4):
                fm_job(5184 + cb * 512, "u", cb)
                if units:
                    units.pop(0)()
                ln_tile(cb)
            for t in range(NT):
                dma_sp(u_s[T0 - 10 + t], stg_u[:, :, t * 128:(t + 1) * 128], "st_stg", reads=["stg"])
        return units

        if SUB <= 2:
            return
    prev_units = []
    for blk_ in blocks:
        prev_units = do_block(*blk_, prev_units)
    while prev_units:
        prev_units.pop(0)()
    if "t_dt" in taps:
        t_dt = nc.dram_tensor("t_dt", [128, 18 * 64], F32, kind="ExternalOutput").ap()
        dma_sp(t_dt, dtall[:].rearrange("p a b -> p (a b)"), "st_tap", reads=[("dtall", T) for T in range(18)])
        t_dec = nc.dram_tensor("t_dec", [128, 18 * 64], F32, kind="ExternalOutput").ap()
        dma_sp(t_dec, decall[:].rearrange("p a b -> p (a b)"), "st_tap2", reads=[("decall", T, d_) for T in range(18) for d_ in range(2)])
    pg.barrier()
    pg.flush()
    st.close()
    if stage <= 1:
        return finish(nc, pg, es, out)


    st = ExitStack()
    hsts = [st.enter_context(nc.sbuf_tensor("hst%d" % i, [128, 2048], F32)) for i in range(2)]
    Sld = [[st.enter_context(nc.sbuf_tensor("Sld%d_%d" % (d_, i), [128, 2048], F32)) for i in range(2)] for d_ in range(2)]
    hpb = [[st.enter_context(nc.sbuf_tensor("hpb%d_%d" % (d_, i), [128, 2048], BF16)) for i in range(2)] for d_ in range(2)]
    chains = [list(range(0, 18)), [1, 0] + list(range(9, 1, -1)) + list(range(17, 9, -1))]
    for dr in range(2):
        pg.op("dve" if dr == 0 else "pool", lambda e, dr=dr: e.memset(hsts[dr][:], 0.0), writes=["hst%d" % dr])
    for i in range(18):
        for dr in range(2):
            T = chains[dr][i]
            hst = hsts[dr]
            hk = "hst%d" % dr
            sl, slk = Sld[dr][i % 2], "Sld%d_%d" % (dr, i % 2)
            dma_sp(sl[:], S_all[T, dr], "ld_" + slk, writes=[slk])
            if T >= 10:
                hb, hbk = hpb[dr][i % 2], "hpb%d_%d" % (dr, i % 2)
                pg.op("act", lambda e, hb=hb, hst=hst: e.copy(out=hb[:], in_=hst[:]), reads=[hk], writes=[hbk])
                dma_sp(hp_s[dr, T - 10], hb[:], "st_" + hbk, reads=[hbk])
            pg.op("dve", lambda e, T=T, dr=dr, hst=hst: e.tensor_tensor(
                out=hst[:].rearrange("p (h d) -> p h d", d=64), in0=hst[:].rearrange("p (h d) -> p h d", d=64),
                in1=decall[:, T, dr * 32:(dr + 1) * 32].unsqueeze(2).to_broadcast([128, 32, 64]), op=OP.mult),
                reads=[hk, ("decall", T, dr)], writes=[hk])
            pg.op("dve", lambda e, sl=sl, hst=hst: e.tensor_tensor(out=hst[:], in0=hst[:], in1=sl[:], op=OP.add),
                  reads=[hk, slk], writes=[hk])
    pg.barrier()
    pg.flush()
    st.close()
    if stage <= 3:
        return finish(nc, pg, es, out)


    sel3b = sb("sel3b", [128, 4096], BF16)
    dma_cast(sel3b[:], sel3, "ld_c2", writes=["sel3b"])
    st = ExitStack()
    def sb4(name, shape, dt=F32):
        return st.enter_context(nc.sbuf_tensor(name, list(shape), dt))
    def ps4(name, shape, dt=F32):
        return st.enter_context(nc.psum_tensor(name, list(shape), dt))
    xs_c = [sb4("xs_c%d" % i, [128, 2048], BF16) for i in range(2)]
    bt_c = [sb4("bt_c%d" % i, [128, 4, 128], BF16) for i in range(2)]
    ct_c = [sb4("ct_c%d" % i, [128, 4, 128], BF16) for i in range(2)]
    hpf_c = [sb4("hpf_c%d" % i, [128, 2048], BF16) for i in range(2)]
    hpb_c = [sb4("hpb_c%d" % i, [128, 2048], BF16) for i in range(2)]
    z_c = [sb4("z_c%d" % i, [128, 2048], BF16) for i in range(2)]
    v_c = [sb4("v_c%d" % i, [128, 2048], BF16) for i in range(2)]
    u_c = [sb4("u_c%d" % i, [128, 16, 128], BF16) for i in range(2)]
    mixb = [sb4("mixb%d" % i, [128, 32, 128], BF16) for i in range(2)]
    wsb = sb4("wsb", [128, 8, 128], BF16)
    dta3 = sb4("dta3", [128, 96])
    acs = sb4("acs", [128, 64])
    nacs = sb4("nacs", [128, 64])
    ecum = sb4("ecum", [128, 64])
    xdt = [sb4("xdt%d" % i, [128, 2048], BF16) for i in range(2)]
    pcs = [sb4("pcs%d" % i, [128, 128], BF16) for i in range(2)]
    tbb = sb4("tbb", [128, 128], BF16)
    Rr = sb4("Rr", [128, 128])
    R2 = sb4("R2", [128, 128])
    cbT = sb4("cbT", [128, 4, 128])
    dws = [sb4("dw%d" % i, [128, 4, 128]) for i in range(2)]
    Mm = [[sb4("Mm%d_%d" % (i, j), [128, 8, 128], BF16) for j in range(2)] for i in range(2)]
    ucnt = [0]
    t1 = sb4("t1", [128, 512])
    t2 = sb4("t2", [128, 512])
    yb = sb4("yb", [128, 2048])
    yn = sb4("yn", [128, 2048], BF16)
    gt = sb4("gt", [128, 512])
    ss4 = sb4("ss4", [128, 4])
    pm2 = ps4("pm2", [128, 512])
    pcb = ps4("pcb", [128, 512])
    pDs = [ps4("pD%d" % i, [128, 512]) for i in range(2)]
    pY = ps4("pY", [128, 512])
    pOf = ps4("pOf", [128, 512])
    pOb = ps4("pOb", [128, 512])
    pGa = ps4("pGa", [128, 512])
    pG = [pGa, pcb]
    pGk = ["pGa", "pcb"]
    dma_cast(wsb[:].rearrange("p g i -> p (g i)"), wsT, "ld_wsb", writes=["wsb"])
    mkb = [sb4("mkb%d" % i, [128, 128], BF16) for i in range(2)]
    pg.op("dve", lambda e: e.tensor_copy(out=mkb[0][:], in_=cst[:, C_MNF:C_MNF + 128]), reads=["cst"], writes=["mkb"])
    pg.op("dve", lambda e: e.tensor_copy(out=mkb[1][:], in_=cst[:, C_MNB:C_MNB + 128]), reads=["cst"], writes=["mkb"])
    dsk = rp[:, RP_DSKIP:RP_DSKIP + 32]

    def do_loads(c):
        i2 = c % 2
        xs, bt, ct, hpf, hpb_, zc, vc, uc, mix = (xs_c[i2], bt_c[i2], ct_c[i2], hpf_c[i2], hpb_c[i2], z_c[i2],
                                                   v_c[i2], u_c[i2], mixb[i2])
        K = lambda n: "%s%d" % (n, i2)
        dma_sp(xs[:], xs_s[c], "ld_" + K("xs"), writes=[K("xs")])
        dma_sp(bt[:], bt_s[c], "ld_" + K("bt"), writes=[K("bt")])
        dma_sp(ct[:], ct_s[c], "ld_" + K("ct"), writes=[K("ct")])
        dma_sp(hpf[:], hp_s[0, c], "ld_" + K("hpf"), writes=[K("hpf")])
        dma_sp(hpb_[:], hp_s[1, c], "ld_" + K("hpb"), writes=[K("hpb")])
        dma_sp(zc[:], z_s[c], "ld_" + K("z"), writes=[K("z")])
        dma_sp(vc[:], v_s[c], "ld_" + K("v"), writes=[K("v")])
        dma_sp(uc[:], u_s[c], "ld_" + K("u"), writes=[K("u")])

    def do_chunk(c):
        T = 10 + c
        i2 = c % 2
        xs, bt, ct, hpf, hpb_, zc, vc, uc, mix = (xs_c[i2], bt_c[i2], ct_c[i2], hpf_c[i2], hpb_c[i2], z_c[i2],
                                                   v_c[i2], u_c[i2], mixb[i2])
        K = lambda n: "%s%d" % (n, i2)
        def mmcb(e):
            ins = None
            for g in range(4):
                ins = e.matmul(pcb[:, g * 128:(g + 1) * 128], lhsT=bt[:, g, :], rhs=ct[:, g, :], start=True, stop=True)
            return ins
        pg.op("pe", mmcb, reads=[K("bt"), K("ct")], writes=["pcb"])
        pg.op("act", lambda e: e.copy(out=cbT[:].rearrange("p g i -> p (g i)"), in_=pcb[:]), reads=["pcb"], writes=["cbT"])
        for dr in range(2):
            tri = cst[:, C_TU:C_TU + 128] if dr == 0 else cst[:, C_TL:C_TL + 128]
            pg.op("dve", lambda e, dr=dr: e.tensor_tensor(
                out=dta3[:].rearrange("p (r h) -> p r h", h=32),
                in0=dtall[:, T, dr * 32:(dr + 1) * 32].unsqueeze(1).to_broadcast([128, 3, 32]),
                in1=aneg[:, dr * 32:(dr + 1) * 32].unsqueeze(1).to_broadcast([128, 3, 32]), op=OP.mult),
                reads=["aneg"], writes=["dta3"])
            def mmac(e, dr=dr, tri=tri):
                e.matmul(pm2[:, dr * 32:(dr + 1) * 32], lhsT=tri, rhs=dta3[:, 0:32], start=True, stop=True)
                return e.matmul(pm2[0:96, 64 + dr * 128:64 + (dr + 1) * 128], lhsT=dta3[:, 0:96], rhs=tri,
                                start=True, stop=True)
            pg.op("pe", mmac, reads=["dta3", "cst"], writes=[("pm2", dr)])
            sl = slice(dr * 32, (dr + 1) * 32)
            pg.op("act", lambda e, sl=sl: e.copy(out=acs[:, sl], in_=pm2[:, sl]), reads=[("pm2", dr)], writes=[("acs", dr)])
            pg.op("dve", lambda e, sl=sl: e.tensor_scalar_mul(out=nacs[:, sl], in0=acs[:, sl], scalar1=-1.0),
                  reads=[("acs", dr)], writes=[("nacs", dr)])
            pg.op("act", lambda e, sl=sl: e.activation(out=ecum[:, sl], in_=acs[:, sl], func=AF.Exp),
                  reads=[("acs", dr)], writes=[("ecum", dr)])
            src_ = pm2[:, 64 + dr * 128:64 + (dr + 1) * 128]
            pc = pcs[dr]
            pk = "pcs%d" % dr
            pg.op("act", lambda e, pc=pc, src_=src_: e.copy(out=pc[0:32, :], in_=src_[0:32, :]),
                  reads=[("pm2", dr)], writes=[(pk, 0)])
            for lo in (32, 64):
                pg.op("act", lambda e, src_=src_, lo=lo: e.copy(out=tbb[lo:lo + 32, :], in_=src_[lo:lo + 32, :]),
                      reads=[("pm2", dr)], writes=[("tbb", lo)])
                pg.op("dve", lambda e, src_=src_, lo=lo: e.tensor_tensor(out=Rr[lo:lo + 32, :], in0=src_[lo:lo + 32, :],
                                                                        in1=tbb[lo:lo + 32, :], op=OP.subtract),
                      reads=[("pm2", dr), ("tbb", lo)], writes=[("Rr", lo)])
            pg.op("act", lambda e, pc=pc: e.copy(out=pc[32:64, :], in_=Rr[32:64, :]), reads=[("Rr", 32)], writes=[(pk, 1)])
            pg.op("act", lambda e: e.copy(out=tbb[64:96, :], in_=Rr[64:96, :]), reads=[("Rr", 64)], writes=[("tbb", 64)])
            pg.op("dve", lambda e: e.tensor_tensor(out=R2[64:96, :], in0=Rr[64:96, :], in1=tbb[64:96, :], op=OP.subtract),
                  reads=[("Rr", 64), ("tbb", 64)], writes=["R2"])
            pg.op("act", lambda e, pc=pc: e.copy(out=pc[64:96, :], in_=R2[64:96, :]), reads=["R2"], writes=[(pk, 2)])
            pg.op("pool", lambda e, dr=dr: e.tensor_tensor(
                out=xdt[dr][:].rearrange("p (h d) -> p h d", d=64), in0=xs[:].rearrange("p (h d) -> p h d", d=64),
                in1=dtall[:, T, dr * 32:(dr + 1) * 32].unsqueeze(2).to_broadcast([128, 32, 64]), op=OP.mult),
                reads=[K("xs")], writes=["xdt%d" % dr])
        def emit_D(g):
            Mg = Mm[g % 2]
            Mk = lambda dr, hf, g=g: ("Mm", g % 2, dr, hf)
            for dr in range(2):
                mk = cst[:, C_MNF:C_MNF + 128] if dr == 0 else cst[:, C_MNB:C_MNB + 128]
                pc = pcs[dr]
                pk = "pcs%d" % dr
                for hf in range(2):
                    bsel = ucnt[0] % 2
                    ucnt[0] += 1
                    pDh, pDk = pDs[bsel], "pD%d" % bsel
                    dw, dwk = dws[bsel], "dw%d" % bsel
                    h0 = g * 8 + hf * 4
                    def mmD(e, h0=h0, pc=pc, pDh=pDh, dr=dr):
                        ins = None
                        for j in range(4):
                            h = h0 + j
                            e.matmul(pDh[:, j * 128:(j + 1) * 128], lhsT=sel3b[0:96, h * 128:(h + 1) * 128],
                                     rhs=pc[0:96, :], start=True, stop=False)
                            ins = e.matmul(pDh[:, j * 128:(j + 1) * 128], lhsT=idb[:], rhs=mkb[dr][:],
                                           start=False, stop=True)
                        return ins
                    pg.op("pe", mmD, reads=[(pk, 0), (pk, 1), (pk, 2), "sel3b", "mkb", "idb"], writes=[pDk])
                    pg.op("dve", lambda e, dw=dw, pDh=pDh, h0=h0, dr=dr: e.tensor_tensor(
                        out=dw[:], in0=pDh[:].rearrange("p (h i) -> p h i", i=128),
                        in1=acs[:, dr * 32 + h0:dr * 32 + h0 + 4].unsqueeze(2).to_broadcast([128, 4, 128]), op=OP.subtract),
                        reads=[pDk, ("acs", dr)], writes=[dwk])
                    pg.op("act", lambda e, dw=dw: e.activation(out=dw[:], in_=dw[:], func=AF.Exp), reads=[dwk], writes=[dwk])
                    pg.op("pool", lambda e, dw=dw, g=g, dr=dr, hf=hf, Mg=Mg: e.tensor_tensor(
                        out=Mg[dr][:, hf * 4:(hf + 1) * 4, :], in0=dw[:],
                        in1=cbT[:, g, :].unsqueeze(1).to_broadcast([128, 4, 128]), op=OP.mult),
                        reads=[dwk, "cbT"], writes=[Mk(dr, hf)])
        def emit_Y(g):
            Mg = Mm[g % 2]
            Mk = lambda dr, hf, g=g: ("Mm", g % 2, dr, hf)
            def mmY(e, g=g, Mg=Mg):
                ins = None
                for hh in range(8):
                    h = g * 8 + hh
                    e.matmul(pY[:, hh * 64:(hh + 1) * 64], lhsT=Mg[0][:, hh, :], rhs=xdt[0][:, h * 64:(h + 1) * 64],
                             start=True, stop=False)
                    ins = e.matmul(pY[:, hh * 64:(hh + 1) * 64], lhsT=Mg[1][:, hh, :], rhs=xdt[1][:, h * 64:(h + 1) * 64],
                                   start=False, stop=True)
                return ins
            pg.op("pe", mmY, reads=[Mk(dr, hf) for dr in range(2) for hf in range(2)] + ["xdt0", "xdt1"], writes=["pY"])
            pg.op("pe", lambda e, g=g: e.matmul(pOf[:], lhsT=ct[:, g, :], rhs=hpf[:, g * 512:(g + 1) * 512],
                                                start=True, stop=True), reads=[K("ct"), K("hpf")], writes=["pOf"])
            pg.op("pe", lambda e, g=g: e.matmul(pOb[:], lhsT=ct[:, g, :], rhs=hpb_[:, g * 512:(g + 1) * 512],
                                                start=True, stop=True), reads=[K("ct"), K("hpb")], writes=["pOb"])
            v3 = lambda ap: ap.rearrange("p (h d) -> p h d", d=64)
            pg.op("dve", lambda e, g=g: e.tensor_tensor(
                out=v3(t1[:]), in0=v3(pOf[:]), in1=ecum[:, g * 8:(g + 1) * 8].unsqueeze(2).to_broadcast([128, 8, 64]),
                op=OP.mult), reads=["pOf", ("ecum", 0)], writes=["t1"])
            pg.op("dve", lambda e, g=g: e.tensor_tensor(
                out=v3(t2[:]), in0=v3(pOb[:]), in1=ecum[:, 32 + g * 8:32 + (g + 1) * 8].unsqueeze(2).to_broadcast([128, 8, 64]),
                op=OP.mult), reads=["pOb", ("ecum", 1)], writes=["t2"])
            pg.op("pool", lambda e: e.tensor_tensor(out=t1[:], in0=t1[:], in1=t2[:], op=OP.add), reads=["t1", "t2"],
                  writes=["t1"])
            pg.op("dve", lambda e, g=g: e.tensor_tensor(out=yb[:, g * 512:(g + 1) * 512], in0=pY[:], in1=t1[:], op=OP.add),
                  reads=["pY", "t1"], writes=[("yb", g)])
            pg.op("pool", lambda e, g=g: e.tensor_tensor(
                out=v3(t2[:]), in0=v3(xs[:, g * 512:(g + 1) * 512]),
                in1=dsk[:, g * 8:(g + 1) * 8].unsqueeze(2).to_broadcast([128, 8, 64]), op=OP.mult),
                reads=[K("xs"), "rp", "t2"], writes=["t2"])
            pg.op("pool", lambda e, g=g: e.tensor_tensor(out=yb[:, g * 512:(g + 1) * 512], in0=yb[:, g * 512:(g + 1) * 512],
                                                        in1=t2[:], op=OP.add), reads=[("yb", g), "t2"], writes=[("yb", g)])

        emit_D(0)
        for g in range(4):
            if g + 1 < 4:
                emit_D(g + 1)
            emit_Y(g)
        ybk = [("yb", g) for g in range(4)]
        pg.op("dve", lambda e: e.tensor_tensor(out=yb[:], in0=yb[:], in1=zc[:], op=OP.mult), reads=ybk + [K("z")], writes=ybk)
        pg.op("dve", lambda e: e.memset(ss4[:], 0.0), writes=["ss4"])
        pg.op("act", lambda e: e.activation(out=yn[:], in_=yb[:], func=AF.Square, accum_out=ss4[:, 0:1]),
              reads=ybk + ["ss4"], writes=["yn", "ss4"])
        pg.op("dve", lambda e: e.tensor_scalar(out=ss4[:, 1:2], in0=ss4[:, 0:1], scalar1=1.0 / D, scalar2=EPS,
                                               op0=OP.mult, op1=OP.add), reads=["ss4"], writes=["ss4"])
        pg.op("act", lambda e: e.sqrt(out=ss4[:, 1:2], in_=ss4[:, 1:2]), reads=["ss4"], writes=["ss4"])
        pg.op("dve", lambda e: e.reciprocal(out=ss4[:, 1:2], in_=ss4[:, 1:2]), reads=["ss4"], writes=["ss4"])
        pg.op("act", lambda e: e.activation(out=yn[:], in_=yb[:], func=AF.Copy, scale=ss4[:, 1:2]),
              reads=ybk + ["ss4"], writes=["yn"])
        for q in range(4):
            pgq, pgk = pG[q % 2], pGk[q % 2]
            def mmT(e, q=q, pgq=pgq):
                ins = None
                for j in range(4):
                    kc = q * 4 + j
                    ins = e.matmul(pgq[:, j * 128:(j + 1) * 128], lhsT=yn[:, kc * 128:(kc + 1) * 128], rhs=idb[:],
                                   start=True, stop=True)
                return ins
            pg.op("pe", mmT, reads=["yn", "idb"], writes=[pgk])
            for j in range(4):
                kc = q * 4 + j
                pg.op("act", lambda e, j=j, kc=kc, pgq=pgq: e.activation(
                    out=mix[:, kc, :], in_=pgq[:, j * 128:(j + 1) * 128], func=AF.Copy,
                    scale=pv[:, PV_SNG + kc:PV_SNG + kc + 1]), reads=[pgk, "pv"], writes=[(K("mix"), kc)])
        for q in range(4):
            pgq, pgk = pG[q % 2], pGk[q % 2]
            def mmG(e, q=q, pgq=pgq):
                ins = None
                for j in range(4):
                    cc = q * 4 + j
                    ins = e.matmul(pgq[:, j * 128:(j + 1) * 128], lhsT=vc[:, cc * 128:(cc + 1) * 128], rhs=wsb[:, cc // 2, :],
                                   start=True, stop=True)
                return ins
            pg.op("pe", mmG, reads=[K("v"), "wsb"], writes=[pgk])
            pg.op("dve", lambda e, q=q, pgq=pgq: e.tensor_tensor(
                out=gt[:].rearrange("p (a b i) -> p a b i", a=2, b=2),
                in0=pgq[:].rearrange("p (a b i) -> p a b i", a=2, b=2),
                in1=rp[:, RP_BS + q * 256:RP_BS + (q + 1) * 256].rearrange("p (a i) -> p a i", a=2).unsqueeze(2)
                .to_broadcast([128, 2, 2, 128]), op=OP.add), reads=[pgk, "rp"], writes=["gt"])
            pg.op("pool", lambda e, q=q: e.tensor_tensor(
                out=mix[:, 16 + q * 4:16 + (q + 1) * 4, :], in0=gt[:].rearrange("p (c i) -> p c i", i=128),
                in1=uc[:, q * 4:(q + 1) * 4, :], op=OP.mult), reads=["gt", K("u")], writes=[(K("mix"), 16 + q)])
        dma_sp(mix_s[c], mix[:], "st_" + K("mix"),
               reads=[(K("mix"), kc) for kc in range(20)])

    nch = NCH
    wb3 = [sb4("p3w%d" % i, [128, 16, 512], BF16) for i in range(2)]

    class _MP:
        def __getitem__(self, key):
            p_, c_ = key
            return pm2[p_, 320 + c_.start:320 + c_.stop]
    mps_cur[0] = _MP()
    mps_off[0] = 64
    do_loads(0)
    for c in range(nch):
        if c + 1 < nch:
            do_loads(c + 1)
        mod_block(8 + 2 * c, wb3, part=1)
        mod_block(9 + 2 * c, wb3, part=1)
        do_chunk(c)
        mod_block(8 + 2 * c, wb3, part=2)
        mod_block(9 + 2 * c, wb3, part=2)
    mod_finish(32, 96, pm2[:, 320:512], base=32)
    ab(4, PV_N2G, 4, 3, 0)
    pg.op("dve", lambda e: e.tensor_copy(out=G12[:, 0, :], in_=mt[:, 2, :, 0]), reads=["modT"], writes=["G12a"])
    pg.op("dve", lambda e: e.tensor_copy(out=G12[:, 1, :], in_=mt[:, 5, :, 0]), reads=["modT"], writes=["G12b"])
    pg.barrier()
    pg.flush()
    st.close()
    if stage <= 4:
        return finish(nc, pg, es, out)


    st5 = ExitStack()
    x1T = st5.enter_context(nc.sbuf_tensor("x1T", [128, 16, 1024], F32))
    banks = [st5.enter_context(nc.psum_tensor("bk%d" % i, [128, 512], F32)) for i in range(8)]
    bkk = ["bk%d" % i for i in range(8)]
    st = ExitStack()
    mixblk = st.enter_context(nc.sbuf_tensor("mixblk", [128, 32, 512], BF16))
    xin5 = st.enter_context(nc.sbuf_tensor("xin5", [128, 4, 2048], F32))
    wo = [st.enter_context(nc.sbuf_tensor("wo%d" % i, [128, 32, 256], BF16)) for i in range(2)]
    tmp5 = [st.enter_context(nc.sbuf_tensor("tmp5_%d" % i, [128, 512], F32)) for i in range(2)]
    w_out_v = w_out.rearrange("(kc p) c -> p kc c", p=128)

    def do_p5(tb):
        for t in range(4):
            dma_sp(mixblk[:, :, t * 128:(t + 1) * 128], mix_s[tb * 4 + t], "ld_mixblk%d" % t, writes=[("mixblk", t)])
            r0 = 1280 + (tb * 4 + t) * 128
            dma_sp(xin5[:, t, :], x_all[r0:r0 + 128, :], "ld_xin5_%d" % t, writes=[("xin5", t)])
        for dcp in range(8):
            w = wo[dcp % 2]
            wk = "wo%d" % (dcp % 2)
            dma_cast(w[:], w_out_v[:, :, dcp * 256:(dcp + 1) * 256], "ld_" + wk, writes=[wk])
            for d2 in range(2):
                dc = dcp * 2 + d2
                i2 = dc % 2
                pa, pak = banks[i2], bkk[i2]
                px, pxk = banks[2 + i2], bkk[2 + i2]
                def mm(e, w=w, d2=d2, pa=pa):
                    ins = None
                    for kc in range(32):
                        ins = e.matmul(pa[:], lhsT=w[:, kc, d2 * 128:(d2 + 1) * 128], rhs=mixblk[:, kc, :],
                                       start=(kc == 0), stop=(kc == 31))
                    return ins
                pg.op("pe", mm, reads=[wk] + [("mixblk", t) for t in range(4)], writes=[pak])
                tm, tmk = tmp5[i2], "tmp5_%d" % i2
                pg.op("act", lambda e, tm=tm, pa=pa, dc=dc: e.activation(out=tm[:], in_=pa[:], func=AF.Copy,
                                                                         scale=G12[:, 0, dc:dc + 1]),
                      reads=[pak, "G12a"], writes=[tmk])
                def trx(e, px=px, dc=dc):
                    ins = None
                    for t in range(4):
                        ins = e.transpose(out=px[:, t * 128:(t + 1) * 128], in_=xin5[:, t, dc * 128:(dc + 1) * 128],
                                          identity=ident)
                    return ins
                pg.op("pe", trx, reads=[("xin5", t) for t in range(4)] + ["cst"], writes=[pxk])
                pg.op("dve", lambda e, px=px, tm=tm, dc=dc: e.tensor_tensor(
                    out=x1T[:, dc, tb * 512:(tb + 1) * 512], in0=px[:], in1=tm[:], op=OP.add),
                    reads=[pxk, tmk], writes=[("x1T", dc, tb)])
    for tb in range(2):
        do_p5(tb)
    if "t_x1T" in taps:
        t_x1T = nc.dram_tensor("t_x1T", [128, 16 * 1024], F32, kind="ExternalOutput").ap()
        dma_sp(t_x1T, x1T[:].rearrange("p a b -> p (a b)"), "st_tap5",
               reads=[("x1T", dc, tb) for dc in range(16) for tb in range(2)])
    pg.barrier()
    pg.flush()
    st.close()
    if stage <= 5:
        st5.close()
        return finish(nc, pg, es, out)


    st6 = ExitStack()
    h2T = st6.enter_context(nc.sbuf_tensor("h2T", [128, 16, 1024], BF16))
    cpc = st6.enter_context(nc.sbuf_tensor("cpc", [128, 1024], BF16))
    st = ExitStack()
    def sb6(name, shape, dt=F32):
        return st.enter_context(nc.sbuf_tensor(name, list(shape), dt))
    sq = [sb6("sq%d" % i, [128, 512]) for i in range(2)]
    rstd = sb6("rstd", [128, 1024])
    tmph = [sb6("tmph%d" % i, [128, 1024]) for i in range(2)]
    wrb = sb6("wrb", [128, 16, 36], BF16)
    lg = sb6("lg", [128, 8, 36])
    mg = sb6("mg", [128, 8])
    eg = sb6("eg", [128, 8, 4])
    sgm = sb6("sgm", [128, 8])
    tpg = sb6("tpg", [128, 8])
    ohg = sb6("ohg", [128, 8, 4])
    selx = sb6("selx", [128, 8, 8])
    tmp8 = sb6("tmp8", [128, 8, 8])
    m1 = sb6("m1", [128, 8])
    m2 = sb6("m2", [128, 8])
    mask1 = sb6("mask1", [128, 8, 8])
    mask2 = sb6("mask2", [128, 8, 8])
    sel2 = sb6("sel2", [128, 8, 8])
    p1 = sb6("p1", [128, 8])
    p2 = sb6("p2", [128, 8])
    wex = sb6("wex", [128, 8, 8])
    comb3 = sb6("comb3", [128, 8, 3, 32])
    ctb = sb6("ctb", [128, 1024], BF16)
    cR = sb6("cR", [128, 1024])
    cR2 = sb6("cR2", [128, 1024])
    dma_cast(wrb[:].rearrange("p a b -> p (a b)"), wr, "ld_wrb", writes=["wrb"])
    x1k = [("x1T", dc, tb) for dc in range(16) for tb in range(2)]
    for tb in range(2):
        for kc in range(16):
            s_, sk = sq[kc % 2], "sq%d" % (kc % 2)
            pg.op("act", lambda e, s_=s_, kc=kc, tb=tb: e.activation(out=s_[:], in_=x1T[:, kc, tb * 512:(tb + 1) * 512],
                                                                     func=AF.Square), reads=[("x1T", kc, tb)], writes=[sk])
            pg.op("pe", lambda e, s_=s_, kc=kc, tb=tb: e.matmul(banks[tb][:], lhsT=ones, rhs=s_[:], start=(kc == 0),
                                                               stop=(kc == 15)), reads=[sk, "cst"], writes=[bkk[tb]])
        sl = slice(tb * 512, (tb + 1) * 512)
        pg.op("dve", lambda e, tb=tb, sl=sl: e.tensor_scalar(out=rstd[:, sl], in0=banks[tb][:], scalar1=1.0 / D, scalar2=EPS,
                                                            op0=OP.mult, op1=OP.add), reads=[bkk[tb]], writes=[("rstd", tb)])
        pg.op("act", lambda e, sl=sl: e.sqrt(out=rstd[:, sl], in_=rstd[:, sl]), reads=[("rstd", tb)], writes=[("rstd", tb)])
        pg.op("dve", lambda e, sl=sl: e.reciprocal(out=rstd[:, sl], in_=rstd[:, sl]), reads=[("rstd", tb)], writes=[("rstd", tb)])
    for kc in range(16):
        th, thk = tmph[kc % 2], "tmph%d" % (kc % 2)
        pg.op("dve", lambda e, th=th, kc=kc: e.tensor_tensor(out=th[:], in0=x1T[:, kc, :], in1=rstd[:], op=OP.mult),
              reads=[("x1T", kc, 0), ("x1T", kc, 1), ("rstd", 0), ("rstd", 1)], writes=[thk])
        pg.op("act", lambda e, th=th, kc=kc: e.activation(out=h2T[:, kc, :], in_=th[:], func=AF.Identity,
                                                          bias=AB[:, 5, kc:kc + 1], scale=AB[:, 4, kc:kc + 1]),
              reads=[thk, "AB4", "AB5"], writes=[("h2T", kc)])
    h2k = [("h2T", kc) for kc in range(16)]
    for t in range(8):
        pr, prk = banks[2 + t % 2], bkk[2 + t % 2]
        def mmr(e, t=t, pr=pr):
            ins = None
            for kc in range(16):
                ins = e.matmul(pr[:, 0:36], lhsT=h2T[:, kc, t * 128:(t + 1) * 128], rhs=wrb[:, kc, :],
                               start=(kc == 0), stop=(kc == 15))
            return ins
        pg.op("pe", mmr, reads=h2k + ["wrb"], writes=[prk])
        pg.op("dve", lambda e, t=t, pr=pr: e.tensor_tensor(out=lg[:, t, :], in0=pr[:, 0:36], in1=rp[:, RP_BR:RP_BR + 36],
                                                          op=OP.add), reads=[prk, "rp"], writes=[("lg", t)])
    lgk = [("lg", t) for t in range(8)]
    lgG = lg[:, :, 0:4]
    bc = lambda ap, n: ap.unsqueeze(2).to_broadcast([128, 8, n])
    R_ = "rt"
    pg.op("dve", lambda e: e.tensor_reduce(out=mg[:], in_=lgG, axis=AX.X, op=OP.max), reads=lgk, writes=[R_])
    pg.op("dve", lambda e: e.tensor_tensor(out=eg[:], in0=lgG, in1=bc(mg[:], 4), op=OP.subtract), reads=lgk + [R_], writes=[R_])
    pg.op("act", lambda e: e.activation(out=eg[:], in_=eg[:], func=AF.Exp), reads=[R_], writes=[R_])
    pg.op("dve", lambda e: e.tensor_reduce(out=sgm[:], in_=eg[:], axis=AX.X, op=OP.add), reads=[R_], writes=[R_])
    pg.op("dve", lambda e: e.reciprocal(out=tpg[:], in_=sgm[:]), reads=[R_], writes=[R_])
    pg.op("dve", lambda e: e.tensor_tensor(out=ohg[:], in0=lgG, in1=bc(mg[:], 4), op=OP.is_equal), reads=lgk + [R_], writes=[R_])
    for g in range(4):
        lgE = lg[:, :, 4 + g * 8:4 + (g + 1) * 8]
        dst = selx if g == 0 else tmp8
        pg.op("dve", lambda e, g=g, lgE=lgE, dst=dst: e.tensor_tensor(
            out=dst[:], in0=lgE, in1=ohg[:, :, g:g + 1].to_broadcast([128, 8, 8]), op=OP.mult), reads=lgk + [R_], writes=[R_])
        if g > 0:
            pg.op("dve", lambda e: e.tensor_tensor(out=selx[:], in0=selx[:], in1=tmp8[:], op=OP.add), reads=[R_], writes=[R_])
    pg.op("dve", lambda e: e.tensor_reduce(out=m1[:], in_=selx[:], axis=AX.X, op=OP.max), reads=[R_], writes=[R_])
    pg.op("dve", lambda e: e.tensor_tensor(out=mask1[:], in0=selx[:], in1=bc(m1[:], 8), op=OP.is_equal), reads=[R_], writes=[R_])
    pg.op("dve", lambda e: e.tensor_scalar_mul(out=sel2[:], in0=mask1[:], scalar1=-1.0e30), reads=[R_], writes=[R_])
    pg.op("dve", lambda e: e.tensor_tensor(out=sel2[:], in0=sel2[:], in1=selx[:], op=OP.add), reads=[R_], writes=[R_])
    pg.op("dve", lambda e: e.tensor_reduce(out=m2[:], in_=sel2[:], axis=AX.X, op=OP.max), reads=[R_], writes=[R_])
    pg.op("dve", lambda e: e.tensor_tensor(out=mask2[:], in0=sel2[:], in1=bc(m2[:], 8), op=OP.is_equal), reads=[R_], writes=[R_])
    pg.op("dve", lambda e: e.tensor_tensor(out=p2[:], in0=m2[:], in1=m1[:], op=OP.subtract), reads=[R_], writes=[R_])
    pg.op("act", lambda e: e.activation(out=p2[:], in_=p2[:], func=AF.Exp), reads=[R_], writes=[R_])
    pg.op("dve", lambda e: e.tensor_scalar_add(out=p1[:], in0=p2[:], scalar1=1.0), reads=[R_], writes=[R_])
    pg.op("dve", lambda e: e.reciprocal(out=p1[:], in_=p1[:]), reads=[R_], writes=[R_])
    pg.op("dve", lambda e: e.tensor_tensor(out=p2[:], in0=p2[:], in1=p1[:], op=OP.mult), reads=[R_], writes=[R_])
    pg.op("dve", lambda e: e.tensor_tensor(out=p1[:], in0=p1[:], in1=tpg[:], op=OP.mult), reads=[R_], writes=[R_])
    pg.op("dve", lambda e: e.tensor_tensor(out=p2[:], in0=p2[:], in1=tpg[:], op=OP.mult), reads=[R_], writes=[R_])
    pg.op("dve", lambda e: e.tensor_tensor(out=wex[:], in0=mask1[:], in1=bc(p1[:], 8), op=OP.mult), reads=[R_], writes=[R_])
    pg.op("dve", lambda e: e.tensor_tensor(out=tmp8[:], in0=mask2[:], in1=bc(p2[:], 8), op=OP.mult), reads=[R_], writes=[R_])
    pg.op("dve", lambda e: e.tensor_tensor(out=wex[:], in0=wex[:], in1=tmp8[:], op=OP.add), reads=[R_], writes=[R_])
    for r in range(3):
        for g in range(4):
            pg.op("dve", lambda e, r=r, g=g: e.tensor_tensor(
                out=comb3[:, :, r, g * 8:(g + 1) * 8], in0=wex[:], in1=ohg[:, :, g:g + 1].to_broadcast([128, 8, 8]),
                op=OP.mult), reads=[R_], writes=[R_, ("comb3", r, g)])
    if "t_comb" in taps:
        t_comb = nc.dram_tensor("t_comb", [128, 8 * 96], F32, kind="ExternalOutput").ap()
        dma_sp(t_comb, comb3[:].rearrange("p a b c -> p (a b c)"), "st_tap6", reads=[R_])
    if "t_h2T" in taps:
        t_h2T = nc.dram_tensor("t_h2T", [128, 16 * 1024], BF16, kind="ExternalOutput").ap()
        dma_sp(t_h2T, h2T[:].rearrange("p a b -> p (a b)"), "st_tap7", reads=h2k)
    for t in range(8):
        pc_, pck = banks[4 + t // 4], bkk[4 + t // 4]
        pg.op("pe", lambda e, t=t, pc_=pc_: e.transpose(out=pc_[0:96, (t % 4) * 128:(t % 4 + 1) * 128],
                                                        in_=comb3[:, t, :, :].rearrange("p r c -> p (r c)"), identity=ident),
              reads=[R_, "cst"], writes=[(pck, t % 4)])
    for hb in range(2):
        src_ = banks[4 + hb]
        sk = [(bkk[4 + hb], j) for j in range(4)]
        sl = slice(hb * 512, (hb + 1) * 512)
        pg.op("act", lambda e, src_=src_, sl=sl: e.copy(out=cpc[0:32, sl], in_=src_[0:32, :]), reads=sk, writes=[("cpc", 0, hb)])
        for lo in (32, 64):
            pg.op("act", lambda e, src_=src_, sl=sl, lo=lo: e.copy(out=ctb[lo:lo + 32, sl], in_=src_[lo:lo + 32, :]),
                  reads=sk, writes=[("ctb", lo, hb)])
            pg.op("dve", lambda e, src_=src_, sl=sl, lo=lo: e.tensor_tensor(
                out=cR[lo:lo + 32, sl], in0=src_[lo:lo + 32, :], in1=ctb[lo:lo + 32, sl], op=OP.subtract),
                reads=sk + [("ctb", lo, hb)], writes=[("cR", lo, hb)])
        pg.op("act", lambda e, sl=sl: e.copy(out=cpc[32:64, sl], in_=cR[32:64, sl]), reads=[("cR", 32, hb)],
              writes=[("cpc", 1, hb)])
        pg.op("act", lambda e, sl=sl: e.copy(out=ctb[64:96, sl], in_=cR[64:96, sl]), reads=[("cR", 64, hb)],
              writes=[("ctb", 64, hb)])
        pg.op("dve", lambda e, sl=sl: e.tensor_tensor(out=cR2[64:96, sl], in0=cR[64:96, sl], in1=ctb[64:96, sl],
                                                      op=OP.subtract), reads=[("cR", 64, hb), ("ctb", 64, hb)],
              writes=[("cR2", hb)])
        pg.op("act", lambda e, sl=sl: e.copy(out=cpc[64:96, sl], in_=cR2[64:96, sl]), reads=[("cR2", hb)],
              writes=[("cpc", 2, hb)])
    pg.barrier()
    pg.flush()
    st.close()
    if stage <= 6:
        st6.close()
        st5.close()
        return finish(nc, pg, es, out)


    st = ExitStack()
    def sb7(name, shape, dt=F32):
        return st.enter_context(nc.sbuf_tensor(name, list(shape), dt))
    wgh = [sb7("wgh%d" % i, [128, 16, 256], BF16) for i in range(2)]
    wuh = [sb7("wuh%d" % i, [128, 16, 256], BF16) for i in range(2)]
    wdn = sb7("wdn", [128, 4, 2048], BF16)
    hid = sb7("hid", [128, 4, 1024], BF16)
    cbc = sb7("cbc", [128, 2, 512])
    sgs = [sb7("sgs%d" % i, [128, 512]) for i in range(2)]
    tus = [sb7("tus%d" % i, [128, 512]) for i in range(2)]
    tmo = [sb7("tmo%d" % i, [128, 512]) for i in range(2)]
    cpk = [("cpc", r, hb) for r in range(3) for hb in range(2)]
    cnt6 = [0]

    def do_expert(ex):
        wg_v = w_g[ex].rearrange("(kc p) f -> p kc f", p=128)
        wu_v = w_u[ex].rearrange("(kc p) f -> p kc f", p=128)
        wd_v = w_d[ex].rearrange("(fc p) d -> p fc d", p=128)
        for tb in range(2):
            pg.op("pe", lambda e, tb=tb: e.matmul(banks[6][:], lhsT=sel3b[0:96, ex * 128:(ex + 1) * 128],
                                                  rhs=cpc[0:96, tb * 512:(tb + 1) * 512], start=True, stop=True),
                  reads=cpk + ["sel3b"], writes=[bkk[6]])
            pg.op("act", lambda e, tb=tb: e.copy(out=cbc[:, tb, :], in_=banks[6][:]), reads=[bkk[6]], writes=[("cbc", tb)])
        for half in range(2):
            wg_, wu_ = wgh[half], wuh[half]
            dma_cast(wg_[:], wg_v[:, :, half * 256:(half + 1) * 256], "ld_wgh%d" % half, writes=["wgh%d" % half])
            dma_cast(wu_[:], wu_v[:, :, half * 256:(half + 1) * 256], "ld_wuh%d" % half, writes=["wuh%d" % half])
            for fcl in range(2):
                fc = half * 2 + fcl
                for tb in range(2):
                    i2 = cnt6[0] % 2
                    cnt6[0] += 1
                    pgt, pgk_ = banks[i2], bkk[i2]
                    pup, puk = banks[2 + i2], bkk[2 + i2]
                    def mmg(e, wg_=wg_, fcl=fcl, tb=tb, pgt=pgt):
                        ins = None
                        for kc in range(16):
                            ins = e.matmul(pgt[:], lhsT=wg_[:, kc, fcl * 128:(fcl + 1) * 128],
                                           rhs=h2T[:, kc, tb * 512:(tb + 1) * 512], start=(kc == 0), stop=(kc == 15))
                        return ins
                    pg.op("pe", mmg, reads=["wgh%d" % half] + h2k, writes=[pgk_])
                    def mmu(e, wu_=wu_, fcl=fcl, tb=tb, pup=pup):
                        ins = None
                        for kc in range(16):
                            ins = e.matmul(pup[:], lhsT=wu_[:, kc, fcl * 128:(fcl + 1) * 128],
                                           rhs=h2T[:, kc, tb * 512:(tb + 1) * 512], start=(kc == 0), stop=(kc == 15))
                        return ins
                    pg.op("pe", mmu, reads=["wuh%d" % half] + h2k, writes=[puk])
                    sg_, sgk = sgs[i2], "sgs%d" % i2
                    tu_, tuk = tus[i2], "tus%d" % i2
                    pg.op("act", lambda e, sg_=sg_, pgt=pgt: e.activation(out=sg_[:], in_=pgt[:], func=AF.Silu),
                          reads=[pgk_], writes=[sgk])
                    pg.op("dve", lambda e, tu_=tu_, pup=pup, sg_=sg_: e.tensor_tensor(out=tu_[:], in0=pup[:], in1=sg_[:],
                                                                                     op=OP.mult),
                          reads=[puk, sgk], writes=[tuk])
                    pg.op("dve", lambda e, tu_=tu_, fc=fc, tb=tb: e.tensor_tensor(
                        out=hid[:, fc, tb * 512:(tb + 1) * 512], in0=tu_[:], in1=cbc[:, tb, :], op=OP.mult),
                        reads=[tuk, ("cbc", tb)], writes=[("hid", fc, tb)])
        dma_cast(wdn[:], wd_v, "ld_wdn", writes=["wdn"])
        for dc in range(16):
            for tb in range(2):
                i2 = cnt6[0] % 2
                cnt6[0] += 1
                po, pok = banks[4 + i2], bkk[4 + i2]
                def mmd(e, dc=dc, tb=tb, po=po):
                    ins = None
                    for fc in range(4):
                        ins = e.matmul(po[:], lhsT=wdn[:, fc, dc * 128:(dc + 1) * 128], rhs=hid[:, fc, tb * 512:(tb + 1) * 512],
                                       start=(fc == 0), stop=(fc == 3))
                    return ins
                pg.op("pe", mmd, reads=["wdn"] + [("hid", fc, tb) for fc in range(4)], writes=[pok])
                tm_, tmk = tmo[i2], "tmo%d" % i2
                pg.op("act", lambda e, tm_=tm_, po=po, dc=dc: e.activation(out=tm_[:], in_=po[:], func=AF.Copy,
                                                                           scale=G12[:, 1, dc:dc + 1]),
                      reads=[pok, "G12b"], writes=[tmk])
                pg.op("dve", lambda e, tm_=tm_, dc=dc, tb=tb: e.tensor_tensor(
                    out=x1T[:, dc, tb * 512:(tb + 1) * 512], in0=x1T[:, dc, tb * 512:(tb + 1) * 512], in1=tm_[:], op=OP.add),
                    reads=[("x1T", dc, tb), tmk], writes=[("x1T", dc, tb)])
    for ex in range(NEXP):
        do_expert(ex)
    pg.barrier()
    pg.flush()
    st.close()
    st6.close()

    st = ExitStack()
    nfr = st.enter_context(nc.sbuf_tensor("nfr", [128, 2048], F32))
    xo = [st.enter_context(nc.sbuf_tensor("xo%d" % i, [128, 2048], F32)) for i in range(2)]
    junk = st.enter_context(nc.sbuf_tensor("junk", [128, 2048], BF16))
    ss7 = st.enter_context(nc.sbuf_tensor("ss7", [128, 16], F32))
    dma_sp(nfr[:], lnrows[:, 2 * D:3 * D], "ld_nfr", writes=["nfr"])
    pg.op("dve", lambda e: e.memset(ss7[:], 0.0), writes=["ss7"])
    for t in range(8):
        xo_, xok = xo[t % 2], "xo%d" % (t % 2)
        for q in range(4):
            pf, pfk = banks[q % 2], bkk[q % 2]
            def trf(e, t=t, q=q, pf=pf):
                ins = None
                for j in range(4):
                    dc = q * 4 + j
                    ins = e.transpose(out=pf[:, j * 128:(j + 1) * 128], in_=x1T[:, dc, t * 128:(t + 1) * 128], identity=ident)
                return ins
            pg.op("pe", trf, reads=[("x1T", q * 4 + j, t // 4) for j in range(4)] + ["cst"], writes=[pfk])
            pg.op("act", lambda e, xo_=xo_, q=q, pf=pf: e.copy(out=xo_[:, q * 512:(q + 1) * 512], in_=pf[:]),
                  reads=[pfk], writes=[(xok, q)])
        xk = [(xok, q) for q in range(4)]
        pg.op("act", lambda e, xo_=xo_, t=t: e.activation(out=junk[:], in_=xo_[:], func=AF.Square, accum_out=ss7[:, t:t + 1]),
              reads=xk + ["ss7"], writes=["junk", ("ss7", t)])
        pg.op("dve", lambda e, t=t: e.tensor_scalar(out=ss7[:, 8 + t:9 + t], in0=ss7[:, t:t + 1], scalar1=1.0 / D, scalar2=EPS,
                                                    op0=OP.mult, op1=OP.add), reads=[("ss7", t)], writes=[("rs7", t)])
        pg.op("act", lambda e, t=t: e.sqrt(out=ss7[:, 8 + t:9 + t], in_=ss7[:, 8 + t:9 + t]), reads=[("rs7", t)], writes=[("rs7", t)])
        pg.op("dve", lambda e, t=t: e.reciprocal(out=ss7[:, 8 + t:9 + t], in_=ss7[:, 8 + t:9 + t]), reads=[("rs7", t)],
              writes=[("rs7", t)])
        pg.op("dve", lambda e, xo_=xo_, t=t: e.scalar_tensor_tensor(out=xo_[:], in0=xo_[:], scalar=ss7[:, 8 + t:9 + t],
                                                                    in1=nfr[:], op0=OP.mult, op1=OP.mult),
              reads=xk + [("rs7", t), "nfr"], writes=xk)
        dma_sp(out[t * 128:(t + 1) * 128, :], xo_[:], "st_out%d" % (t % 2), reads=xk)
    pg.barrier()
    pg.flush()
    st.close()
    st5.close()
    return finish(nc, pg, es, out)


def finish(nc, pg, es, out):
    es.close()
    return nc


def _consts():
    c = np.zeros((128, C_N), np.float32)
    i = np.arange(128)
    c[:, C_ID:C_ID + 128] = np.eye(128, dtype=np.float32)
    c[:, C_TU:C_TU + 128] = (i[:, None] <= i[None, :])
    c[:, C_TL:C_TL + 128] = (i[:, None] >= i[None, :])
    c[:, C_MNF:C_MNF + 128] = np.where(i[:, None] <= i[None, :], 0.0, -30000.0)
    c[:, C_MNB:C_MNB + 128] = np.where(i[:, None] >= i[None, :], 0.0, -30000.0)
    c[:, C_ONE:C_ONE + 128] = 1.0
    s3 = np.zeros((128, 32, 128), np.float32)
    for p in range(96):
        s3[p, p % 32, :] = 1.0
    return c, s3.reshape(128, 4096)


def _pp(v):
    return np.ascontiguousarray(np.asarray(v, np.float32).reshape(-1, 128).T)


def prep_inputs(inp):
    f = lambda a: np.ascontiguousarray(np.asarray(a, np.float32))
    x, c, ctx, c_ctx = f(inp["x"]), f(inp["c"]), f(inp["ctx"]), f(inp["c_ctx"])
    cst, s3 = _consts()
    conv_w = f(inp["conv_w"])[0]
    pvec = np.zeros((128, PV_N), np.float32)
    pvec[:, PV_N1G:PV_N1G + 16] = _pp(inp["norm1_g"][0])
    pvec[:, PV_N2G:PV_N2G + 16] = _pp(inp["norm2_g"][0])
    pvec[:, PV_SNG:PV_SNG + 16] = _pp(inp["ssd_norm_g"][0])
    for j in range(3):
        pvec[:, PV_CW + j * 24:PV_CW + (j + 1) * 24] = _pp(conv_w[j])
    pvec[:, PV_CB:PV_CB + 24] = _pp(inp["conv_b"][0])
    pvec[:, PV_BMOD:PV_BMOD + 96] = _pp(inp["b_mod"][0])
    row = np.zeros((RP_N,), np.float32)
    row[RP_DTB:RP_DTB + 32] = f(inp["dt_bias_f"])[0]
    row[RP_DTB + 32:RP_DTB + 64] = f(inp["dt_bias_b"])[0]
    row[RP_ALOG:RP_ALOG + 32] = f(inp["a_log_f"])[0]
    row[RP_ALOG + 32:RP_ALOG + 64] = f(inp["a_log_b"])[0]
    row[RP_DSKIP:RP_DSKIP + 32] = f(inp["d_skip"])[0]
    row[RP_BS:RP_BS + 1024] = f(inp["b_spatial"])[0].reshape(-1)
    row[RP_BR:RP_BR + 4] = f(inp["b_router_group"])[0]
    row[RP_BR + 4:RP_BR + 36] = f(inp["b_router_expert"])[0].reshape(-1)
    rowp = np.ascontiguousarray(np.broadcast_to(row[None, :], (128, RP_N)))
    lnr = np.concatenate([f(inp["cm_ln_g"])[0], f(inp["cm_ln_b"])[0], f(inp["normf_g"])])
    lnrows = np.ascontiguousarray(np.broadcast_to(lnr[None, :], (128, 3 * D)))
    w_mod = f(inp["w_mod"])[0]
    w_in = f(inp["w_in"])[0]
    w_out = f(inp["w_out"])[0]
    wsT = np.ascontiguousarray(np.transpose(f(inp["w_spatial"])[0], (2, 0, 1)).reshape(128, 1024))
    wrg = f(inp["w_router_group"])[0]
    wre = np.transpose(f(inp["w_router_expert"])[0], (1, 0, 2)).reshape(D, 32)
    wrc = np.concatenate([wrg, wre], axis=1)
    wr = np.ascontiguousarray(wrc.reshape(16, 128, 36).transpose(1, 0, 2).reshape(128, 16 * 36))
    w_g = f(inp["w_exp_gate"])[0].reshape(32, D, 512)
    w_u = f(inp["w_exp_up"])[0].reshape(32, D, 512)
    w_d = f(inp["w_exp_down"])[0].reshape(32, 512, D)
    maps = []
    for k in range(NCORES):
        b, s = k // 2, k % 2
        own = x[b, s * 1024:(s + 1) * 1024]
        oth = x[b, (1 - s) * 1024:(2 - s) * 1024]
        x_all = np.concatenate([ctx[b], oth, own], axis=0)
        fl = np.zeros((128, 2), np.float32)
        fl[:, 0] = 1.0 if s == 1 else 0.0
        fl[:, 1] = 1.0 if s == 0 else 0.0
        cvec = np.stack([_pp(c[b]), _pp(c_ctx)], axis=2).reshape(128, 32)
        maps.append(dict(x_all=x_all, flags=fl, cvec=np.ascontiguousarray(cvec), pvec=pvec, rowp=rowp,
                         lnrows=lnrows, consts=cst, sel3=s3, w_mod=w_mod, w_in=w_in, w_out=w_out,
                         wsT=wsT, wr=wr, w_g=w_g, w_u=w_u, w_d=w_d))
    return maps


def kernel(**inputs):
    maps = prep_inputs(inputs)
    nc = build_nc()
    res = run_bass_kernel_spmd(nc, maps, core_ids=list(range(NCORES)))
    outf = np.zeros((4, 2048, D), np.float32)
    for k in range(NCORES):
        b, s = k // 2, k % 2
        outf[b, s * 1024:(s + 1) * 1024] = res.results[k]["out"]
    return outf
```

```python
from contextlib import ExitStack
import numpy as np
import concourse.bass as bass
import concourse.mybir as mybir
from concourse.bass_utils import run_bass_kernel_spmd

F32 = mybir.dt.float32
BF16 = mybir.dt.bfloat16
AF = mybir.ActivationFunctionType
OP = mybir.AluOpType
AX = mybir.AxisListType

D = 2048
NCORES = 8
EPS = 1e-6
ENGS = ("pe", "act", "dve", "pool", "sp")
SUB = 99
JOBS = None
NOTR = False
NCH = 8
NEXP = 32
NOCONV = False

PV_N1G, PV_N2G, PV_SNG, PV_CW, PV_CB, PV_BMOD = 0, 16, 32, 48, 120, 144
PV_N = 240
RP_DTB, RP_ALOG, RP_DSKIP, RP_BS, RP_BR = 0, 64, 128, 160, 1184
RP_N = 1220
C_ID, C_TU, C_TL, C_MNF, C_MNB, C_ONE = 0, 128, 256, 384, 512, 640
C_N = 768


class Prog:
    def __init__(self, nc, es):
        self.nc = nc
        self.es = es
        self.sems = {}
        self.cnt = {}
        self.ops = {e: [] for e in ENGS}
        self.known = {e: {} for e in ENGS}
        self.last_w = {}
        self.readers = {}
        self.latest = {}
        for e in ENGS:
            self._sem(e)

    def _sem(self, key):
        if key not in self.sems:
            self.sems[key] = self.es.enter_context(self.nc.semaphore("s_" + str(key)))
            self.cnt[key] = 0
        return self.sems[key]

    def op(self, eng, fn, reads=(), writes=(), dma=None):
        waits = {}
        def need(tok):
            sk, v = tok
            if sk == "pe" and eng == "pe":
                return
            if self.known[eng].get(sk, 0) >= v:
                return
            waits[sk] = max(waits.get(sk, 0), v)
        for k in reads:
            if k in self.last_w:
                need(self.last_w[k])
        for k in writes:
            if k in self.last_w:
                need(self.last_w[k])
            for r in self.readers.get(k, ()):
                need(r)
        for sk, v in waits.items():
            self.known[eng][sk] = v
        if dma is not None:
            self._sem(dma)
            self.cnt[dma] += 16
            tok = (dma, self.cnt[dma])
        else:
            self.cnt[eng] += 1
            tok = (eng, self.cnt[eng])
        self.latest[tok[0]] = tok[1]
        for k in writes:
            self.last_w[k] = tok
            self.readers[k] = []
        for k in reads:
            self.readers.setdefault(k, []).append(tok)
        self.ops[eng].append((list(waits.items()), fn, tok, dma is not None))
        return tok

    def barrier(self):
        for e in ENGS:
            waits = []
            for sk, v in self.latest.items():
                if sk == e and e == "pe":
                    continue
                if self.known[e].get(sk, 0) < v:
                    waits.append((sk, v))
                    self.known[e][sk] = v
            if waits:
                self.ops[e].append((waits, None, None, False))
        self.last_w.clear()
        self.readers.clear()

    def flush(self):
        nc = self.nc
        ops = self.ops
        sems = self.sems

        def run(engh, lst):
            for waits, fn, tok, isdma in lst:
                for sk, v in waits:
                    engh.wait_ge(sems[sk], v)
                if fn is None:
                    continue
                ins = fn(engh)
                ins.then_inc(sems[tok[0]], 16 if isdma else 1)

        with nc.Block() as block:
            if ops["sp"]:
                @block.sync
                def _(e):
                    run(e, ops["sp"])
            if ops["pe"]:
                @block.tensor
                def _(e):
                    run(e, ops["pe"])
            if ops["act"]:
                @block.scalar
                def _(e):
                    run(e, ops["act"])
            if ops["dve"]:
                @block.vector
                def _(e):
                    run(e, ops["dve"])
            if ops["pool"]:
                @block.gpsimd
                def _(e):
                    run(e, ops["pool"])
        self.ops = {e: [] for e in ENGS}


def build_nc(stage=99, taps=()):
    nc = bass.Bass("TRN2", target_bir_lowering=False)
    es = ExitStack()
    pg = Prog(nc, es)

    def din(name, shape, dt=F32):
        return nc.dram_tensor(name, list(shape), dt, kind="ExternalInput").ap()

    def dscr(name, shape, dt):
        kind = "ExternalOutput" if name in taps else "Internal"
        return nc.dram_tensor(name, list(shape), dt, kind=kind).ap()

    x_all = din("x_all", [2304, D])
    flags = din("flags", [128, 2])
    cvec = din("cvec", [128, 32])
    pvec = din("pvec", [128, PV_N])
    rowp = din("rowp", [128, RP_N])
    lnrows = din("lnrows", [128, 3 * D])
    consts = din("consts", [128, C_N])
    sel3 = din("sel3", [128, 4096])
    w_mod = din("w_mod", [D, 6 * D])
    w_in = din("w_in", [D, 9280])
    w_out = din("w_out", [2 * D, D])
    wsT = din("wsT", [128, 1024])
    wr = din("wr", [128, 16 * 36])
    w_g = din("w_g", [32, D, 512])
    w_u = din("w_u", [32, D, 512])
    w_d = din("w_d", [32, 512, D])
    out = nc.dram_tensor("out", [1024, D], F32, kind="ExternalOutput").ap()

    def sb(name, shape, dt=F32):
        return es.enter_context(nc.sbuf_tensor(name, list(shape), dt))

    def ps(name, shape, dt=F32):
        return es.enter_context(nc.psum_tensor(name, list(shape), dt))

    cst = sb("cst", [128, C_N])
    idb = sb("idb", [128, 128], BF16)
    pv = sb("pv", [128, PV_N])
    rp = sb("rp", [128, RP_N])
    flg = sb("flg", [128, 2])
    cv = sb("cv", [128, 32])
    scT = sb("scT", [128, 32], BF16)
    modT = sb("modT", [128, 192])
    AB = sb("AB", [128, 6, 16])
    G12 = sb("G12", [128, 2, 16])
    dtall = sb("dtall", [128, 18, 64])
    aneg = sb("aneg", [128, 64])
    decall = sb("decall", [128, 18, 64])

    ident = cst[:, C_ID:C_ID + 128]
    ones = cst[:, C_ONE:C_ONE + 128]

    def dma_sp(out_ap, in_ap, sem, reads=(), writes=()):
        pg.op("sp", lambda e: e.dma_start(out=out_ap, in_=in_ap), reads=reads, writes=writes, dma=sem)

    def dma_cast(out_ap, in_ap, sem, reads=(), writes=()):
        pg.op("pool", lambda e: e.dma_start(out=out_ap, in_=in_ap), reads=reads, writes=writes, dma=sem)

    dma_sp(cst[:], consts, "ld_c0", writes=["cst"])
    dma_sp(pv[:], pvec, "ld_c1", writes=["pv"])
    dma_sp(rp[:], rowp, "ld_c3", writes=["rp"])
    dma_sp(flg[:], flags, "ld_c4", writes=["flg"])
    dma_sp(cv[:], cvec, "ld_c5", writes=["cv"])
    pg.op("dve", lambda e: e.tensor_copy(out=idb[:], in_=ident), reads=["cst"], writes=["idb"])
    pg.op("act", lambda e: e.activation(out=scT[:], in_=cv[:], func=AF.Silu), reads=["cv"], writes=["scT"])
    pg.op("act", lambda e: e.activation(out=aneg[:], in_=rp[:, RP_ALOG:RP_ALOG + 64], func=AF.Exp),
          reads=["rp"], writes=["aneg"])
    pg.op("dve", lambda e: e.tensor_scalar_mul(out=aneg[:], in0=aneg[:], scalar1=-1.0),
          reads=["aneg"], writes=["aneg"])

    st = ExitStack()
    wb = [st.enter_context(nc.sbuf_tensor("p0w%d" % i, [128, 16, 512], BF16)) for i in range(2)]
    modps = st.enter_context(nc.psum_tensor("modps", [128, 192], F32))
    w_mod_v = w_mod.rearrange("(kc p) c -> p kc c", p=128)
    mps_cur = [modps]
    mps_off = [0]
    def mod_block(blk, wb, part=3):
        w = wb[blk % 2]
        key = "p0w%d" % (blk % 2)
        if part & 1:
            dma_cast(w[:], w_mod_v[:, :, blk * 512:(blk + 1) * 512], "ld_" + key, writes=[key])
        if not (part & 2):
            return

        def mm(e, w=w, blk=blk):
            ins = None
            for cc in range(4):
                col = (blk * 4 + cc) * 2 - mps_off[0]
                for kc in range(16):
                    ins = e.matmul(mps_cur[0][:, col:col + 2], lhsT=w[:, kc, cc * 128:(cc + 1) * 128],
                                   rhs=scT[:, kc * 2:kc * 2 + 2], start=(kc == 0), stop=(kc == 15))
            return ins
        pg.op("pe", mm, reads=[key, "scT"], writes=["modps"])

    def mod_finish(c0, c1, modps, base=0):
        pg.op("dve", lambda e: e.tensor_tensor(
            out=modT[:, c0 * 2:c1 * 2].rearrange("p (c t) -> p c t", t=2),
            in0=modps[:, (c0 - base) * 2:(c1 - base) * 2].rearrange("p (c t) -> p c t", t=2),
            in1=pv[:, PV_BMOD + c0:PV_BMOD + c1].unsqueeze(2).to_broadcast([128, c1 - c0, 2]), op=OP.add),
            reads=["modps", "pv"], writes=["modT"])
    for blk in range(8):
        mod_block(blk, wb)
    mod_finish(0, 32, modps)
    mt = modT[:].rearrange("p (m kc t) -> p m kc t", m=6, kc=16, t=2)
    def ab(e_idx, g_off, sc_m, sh_m, which):
        pg.op("dve", lambda e: e.scalar_tensor_tensor(
            out=AB[:, e_idx, :], in0=mt[:, sc_m, :, which], scalar=1.0, in1=pv[:, g_off:g_off + 16],
            op0=OP.add, op1=OP.mult), reads=["modT", "pv"], writes=["AB%d" % e_idx])
        pg.op("dve", lambda e: e.tensor_copy(out=AB[:, e_idx + 1, :], in_=mt[:, sh_m, :, which]),
              reads=["modT"], writes=["AB%d" % (e_idx + 1)])
    ab(0, PV_N1G, 1, 0, 0)
    ab(2, PV_N1G, 1, 0, 1)
    if "t_modT" in taps:
        t_modT = nc.dram_tensor("t_modT", [128, 192], F32, kind="ExternalOutput").ap()
        dma_sp(t_modT, modT[:], "st_tap", reads=["modT"])
    pg.barrier()
    pg.flush()
    st.close()
    if stage <= 0:
        return finish(nc, pg, es, out)


    S_all = dscr("S_all", [18, 2, 128, 2048], F32)
    xs_s = dscr("xs_s", [8, 128, 2048], BF16)
    bt_s = dscr("bt_s", [8, 128, 4, 128], BF16)
    ct_s = dscr("ct_s", [8, 128, 4, 128], BF16)
    z_s = dscr("z_s", [8, 128, 2048], BF16)
    v_s = dscr("v_s", [8, 128, 2048], BF16)
    u_s = dscr("u_s", [8, 128, 16, 128], BF16)
    hp_s = dscr("hp_s", [2, 8, 128, 2048], BF16)
    mix_s = dscr("mix_s", [8, 128, 32, 128], BF16)

    st = ExitStack()
    def sb1(name, shape, dt=F32):
        return st.enter_context(nc.sbuf_tensor(name, list(shape), dt))
    def ps1(name, shape, dt=F32):
        return st.enter_context(nc.psum_tensor(name, list(shape), dt))
    xin = sb1("xin", [128, 4, 2048])
    hT = sb1("hT", [128, 16, 512], BF16)
    wb = [sb1("wb%d" % i, [128, 16, 512], BF16) for i in range(2)]
    t0s = [sb1("t0_%d" % i, [128, 512]) for i in range(2)]
    sbf = [sb1("sbf%d" % i, [128, 512], BF16) for i in range(2)]
    sraws = [sb1("sraw%d" % i, [128, 512]) for i in range(2)]
    xs_tok = sb1("xs_tok", [128, 4, 2048], BF16)
    B_tok = sb1("B_tok", [128, 4, 512], BF16)
    BTs = sb1("BTs", [128, 4, 512], BF16)
    CTs = sb1("CTs", [128, 4, 512], BF16)
    stg = sb1("stg", [128, 4, 2048], BF16)
    lnr = sb1("lnr", [128, 2, 2048])
    xw = [sb1("xw%d" % i, [128, 2048], BF16) for i in range(2)]
    vout = [sb1("vout%d" % i, [128, 2048], BF16) for i in range(2)]
    Sst = [sb1("Sst%d" % i, [128, 2048]) for i in range(2)]
    sm = sb1("sm", [128, 16, 64])
    ssq = sb1("ssq", [128, 16])
    wgt_t = sb1("wgt_t", [128, 4, 64])
    pA = [ps1("pA%d" % i, [128, 512]) for i in range(2)]
    pTf = [ps1("pTf%d" % i, [128, 512]) for i in range(2)]
    pS = [ps1("pS%d" % i, [128, 512]) for i in range(2)]
    pm = ps1("pm", [128, 512])
    stg_u = stg[:].rearrange("p t c -> p (t c)").rearrange("p (cc n) -> p cc n", cc=16)

    w_in_v = w_in.rearrange("(kc p) c -> p kc c", p=128)
    dma_sp(lnr[:].rearrange("p a c -> p (a c)"), lnrows[:, 0:2 * D], "ld_lnr", writes=["lnr"])
    wcnt = [0]
    acnt = [0]

    def load_w(c0, ncols):
        i = wcnt[0] % 2
        wcnt[0] += 1
        dma_cast(wb[i][:, :, 0:ncols], w_in_v[:, :, c0:c0 + ncols], "ld_wb%d" % i, writes=["wb%d" % i])
        return wb[i], "wb%d" % i

    def next_pA():
        i = acnt[0] % 2
        acnt[0] += 1
        return pA[i], "pA%d" % i, i

    blocks = [("ctx", 0, 2, 0), ("oth", 256, 4, 2), ("oth", 768, 4, 6), ("own", 1280, 4, 10), ("own", 1792, 4, 14)]
    if stage == 1:
        blocks = blocks[:1] + blocks[3:4]
    if SUB in (2, 3):
        blocks = blocks[:1]
    def do_block(kind, row0, NT, T0, prev_units):
        N = NT * 128
        own = kind == "own"
        Aidx = 2 if kind == "ctx" else 0
        pg.op("dve", lambda e: e.memset(ssq[:], 0.0), writes=["ssq"])
        for t in range(NT):
            dma_sp(xin[:, t, :], x_all[row0 + t * 128:row0 + (t + 1) * 128, :], "ld_xin%d" % t, writes=[("xin", t)])
            pg.op("act", lambda e, t=t: e.activation(out=stg[:, t, :], in_=xin[:, t, :], func=AF.Square,
                                                     accum_out=ssq[:, t:t + 1]),
                  reads=[("xin", t), "ssq"], writes=["stg", ("ssq", t)])
            pg.op("dve", lambda e, t=t: e.tensor_scalar(out=ssq[:, 8 + t:9 + t], in0=ssq[:, t:t + 1], scalar1=1.0 / D,
                                                        scalar2=EPS, op0=OP.mult, op1=OP.add),
                  reads=[("ssq", t)], writes=[("rs", t)])
            pg.op("act", lambda e, t=t: e.sqrt(out=ssq[:, 8 + t:9 + t], in_=ssq[:, 8 + t:9 + t]),
                  reads=[("rs", t)], writes=[("rs", t)])
            pg.op("dve", lambda e, t=t: e.reciprocal(out=ssq[:, 8 + t:9 + t], in_=ssq[:, 8 + t:9 + t]),
                  reads=[("rs", t)], writes=[("rs", t)])
            pg.op("act", lambda e, t=t: e.activation(out=xin[:, t, :], in_=xin[:, t, :], func=AF.Copy,
                                                     scale=ssq[:, 8 + t:9 + t]),
                  reads=[("xin", t), ("rs", t)], writes=[("xin", t)])
        if SUB <= 0:
            return
        for kc in range(16):
            pa, pak, _ = next_pA()
            def tr(e, pa=pa, kc=kc):
                ins = None
                for t in range(NT):
                    ins = e.transpose(out=pa[:, t * 128:(t + 1) * 128], in_=xin[:, t, kc * 128:(kc + 1) * 128],
                                      identity=ident)
                return ins
            pg.op("pe", tr, reads=[("xin", t) for t in range(NT)] + ["cst"], writes=[pak])
            pg.op("act", lambda e, pa=pa, kc=kc: e.activation(
                out=hT[:, kc, 0:N], in_=pa[:, 0:N], func=AF.Identity,
                bias=AB[:, Aidx + 1, kc:kc + 1], scale=AB[:, Aidx, kc:kc + 1]),
                reads=[pak, "AB%d" % Aidx, "AB%d" % (Aidx + 1)], writes=[("hT", kc)])
            if prev_units and kc % 2 == 1:
                prev_units.pop(0)()
        while prev_units:
            prev_units.pop(0)()
        hT_keys = [("hT", kc) for kc in range(16)]
        if SUB <= 1:
            return

        pending = []
        def fm_job(c0, jobkind, cb):
            w, wk = load_w(c0, 512)
            for cc in range(4):
                pa, pak, pi = next_pA()
                def mm(e, pa=pa, w=w, cc=cc):
                    ins = None
                    for kc in range(16):
                        ins = e.matmul(pa[:, 0:N], lhsT=w[:, kc, cc * 128:(cc + 1) * 128], rhs=hT[:, kc, 0:N],
                                       start=(kc == 0), stop=(kc == 15))
                    return ins
                pg.op("pe", mm, reads=[wk] + hT_keys, writes=[pak])
                while pending:
                    pending.pop(0)()
                if jobkind == "u":
                    pg.op("act", lambda e, pa=pa, cc=cc: e.activation(out=stg_u[:, cb * 4 + cc, 0:N], in_=pa[:, 0:N],
                                                                      func=AF.Gelu),
                          reads=[pak], writes=["stg"])
                    continue
                chn = (c0 - 2048) // 128 + cc
                L = 256 if kind == "ctx" else 64
                t0 = t0s[pi]
                t0k = "t0_%d" % pi
                sraw = sraws[pi]
                pg.op("act", lambda e, pa=pa, sraw=sraw: e.copy(out=sraw[:, 0:N], in_=pa[:, 0:N]),
                      reads=[pak], writes=["sraw%d" % pi])
                pav = sraw[:, 0:N].rearrange("p (r l) -> p r l", l=L)
                t0v = t0[:, 0:N].rearrange("p (r l) -> p r l", l=L)
                cw = lambda j, chn=chn: pv[:, PV_CW + j * 24 + chn:PV_CW + j * 24 + chn + 1]
                pg.op("act", lambda e, pa=pa, t0=t0, chn=chn, cw=cw: e.activation(
                    out=t0[:, 0:N], in_=pa[:, 0:N], func=AF.Identity,
                    bias=pv[:, PV_CB + chn:PV_CB + chn + 1], scale=cw(1)), reads=[pak, "pv"], writes=[t0k])
                if not NOCONV:
                  pg.op("dve", lambda e, pav=pav, t0v=t0v, cw=cw: e.scalar_tensor_tensor(
                    out=t0v[:, :, 1:L], in0=pav[:, :, 0:L - 1], scalar=cw(0), in1=t0v[:, :, 1:L],
                    op0=OP.mult, op1=OP.add), reads=["sraw%d" % pi, t0k, "pv"], writes=[t0k])
                if not NOCONV:
                  pg.op("dve", lambda e, pav=pav, t0v=t0v, cw=cw: e.scalar_tensor_tensor(
                    out=t0v[:, :, 0:L - 1], in0=pav[:, :, 1:L], scalar=cw(2), in1=t0v[:, :, 0:L - 1],
                    op0=OP.mult, op1=OP.add), reads=["sraw%d" % pi, t0k, "pv"], writes=[t0k])
                if jobkind == "xs":
                    dst, dk = sbf[pi][:, 0:N], "sbf%d" % pi
                elif jobkind == "B":
                    dst, dk = BTs[:, cc, 0:N], ("BTs", cc)
                else:
                    dst, dk = CTs[:, cc, 0:N], ("CTs", cc)
                pg.op("act", lambda e, t0=t0, dst=dst: e.activation(out=dst, in_=t0[:, 0:N], func=AF.Silu),
                      reads=[t0k], writes=[dk])
                if jobkind in ("xs", "B") and not NOTR:
                    def emit_tr(dst=dst, dk=dk, pi=pi, cc=cc, jobkind=jobkind, cb=cb):
                        ptf, ptk = pTf[pi], "pTf%d" % pi
                        def tr2(e):
                            ins = None
                            for t in range(NT):
                                ins = e.matmul(ptf[:, t * 128:(t + 1) * 128], lhsT=dst[:, t * 128:(t + 1) * 128],
                                               rhs=idb[:], start=True, stop=True)
                            return ins
                        pg.op("pe", tr2, reads=[dk, "idb"], writes=[ptk])
                        if jobkind == "xs":
                            o = xs_tok[:, 0:NT, cb * 512 + cc * 128:cb * 512 + (cc + 1) * 128]
                            ok = [("xs_tok", t, cb) for t in range(NT)]
                        else:
                            o = B_tok[:, 0:NT, cc * 128:(cc + 1) * 128]
                            ok = [("B_tok", t) for t in range(NT)]
                        iv = ptf[:, 0:N].rearrange("p (t c) -> p t c", c=128)
                        if cc % 2 == 0:
                            pg.op("dve", lambda e: e.tensor_copy(out=o, in_=iv), reads=[ptk], writes=ok)
                        else:
                            pg.op("act", lambda e: e.copy(out=o, in_=iv), reads=[ptk], writes=ok)
                    pending.append(emit_tr)

        def tm_job(c0, ncols, jobkind, cb):
            if jobkind == "dt":
                w, wk = load_w(4672, 512)
                wofs = 448
            else:
                w, wk = load_w(c0, ncols)
                wofs = 0
            for t in range(NT):
                pa, pak, pi = next_pA()
                def mm(e, pa=pa, w=w, t=t):
                    ins = None
                    for kc in range(16):
                        ins = e.matmul(pa[:, 0:ncols], lhsT=hT[:, kc, t * 128:(t + 1) * 128], rhs=w[:, kc, wofs:wofs + ncols],
                                       start=(kc == 0), stop=(kc == 15))
                    return ins
                pg.op("pe", mm, reads=[wk] + hT_keys, writes=[pak])
                while pending:
                    pending.pop(0)()
                if jobkind == "z":
                    pg.op("act", lambda e, pa=pa, t=t: e.activation(out=stg[:, t, cb * 512:(cb + 1) * 512], in_=pa[:],
                                                                    func=AF.Silu), reads=[pak], writes=["stg"])
                elif jobkind == "v":
                    pg.op("act", lambda e, pa=pa, t=t: e.activation(out=xin[:, t, cb * 512:(cb + 1) * 512], in_=pa[:],
                                                                    func=AF.Gelu), reads=[pak], writes=[("xin", t)])
                else:
                    T = T0 + t
                    a, b_, c_, d_ = sm[:, 0, :], sm[:, 1, :], sm[:, 2, :], sm[:, 3, :]
                    pg.op("dve", lambda e, pa=pa: e.tensor_tensor(out=a, in0=pa[:, 0:64], in1=rp[:, RP_DTB:RP_DTB + 64],
                                                                  op=OP.add), reads=[pak, "rp"], writes=["sm0"])
                    pg.op("dve", lambda e: e.tensor_scalar_mul(out=b_, in0=a, scalar1=-1.0),
                          reads=["sm0"], writes=["sm1"])
                    pg.op("dve", lambda e: e.tensor_tensor(out=b_, in0=b_, in1=a, op=OP.min),
                          reads=["sm0", "sm1"], writes=["sm1"])
                    pg.op("act", lambda e: e.activation(out=c_, in_=b_, func=AF.Exp),
                          reads=["sm1"], writes=["sm2"])
                    pg.op("dve", lambda e: e.tensor_scalar_add(out=c_, in0=c_, scalar1=1.0),
                          reads=["sm2"], writes=["sm2"])
                    pg.op("act", lambda e: e.activation(out=c_, in_=c_, func=AF.Ln),
                          reads=["sm2"], writes=["sm2"])
                    pg.op("dve", lambda e, T=T: e.scalar_tensor_tensor(out=dtall[:, T, :], in0=a, scalar=0.0, in1=c_,
                                                                       op0=OP.max, op1=OP.add),
                          reads=["sm0", "sm2"], writes=[("dtall", T)])
                    if kind == "oth":
                        for dr in range(2):
                            pg.op("dve", lambda e, T=T, dr=dr: e.tensor_scalar_mul(
                                out=dtall[:, T, dr * 32:(dr + 1) * 32], in0=dtall[:, T, dr * 32:(dr + 1) * 32],
                                scalar1=flg[:, dr:dr + 1]), reads=[("dtall", T), "flg"], writes=[("dtall", T)])

        if own:
            for cb in range(4):
                tm_job(cb * 512, 512, "z", cb)
            for t in range(NT):
                dma_sp(z_s[T0 - 10 + t], stg[:, t, :], "st_stg", reads=["stg"])
        for cb in range(4):
            if JOBS is None or "xs" in JOBS:
                fm_job(2048 + cb * 512, "xs", cb)
        if JOBS is None or "B" in JOBS:
            fm_job(4096, "B", 0)
        if own:
            fm_job(4608, "C", 0)
        if JOBS is None or "dt" in JOBS:
            tm_job(5120, 64, "dt", 0)
        while pending:
            pending.pop(0)()
        def build_units():
            units = []
            W = NT * 64
            dtab, acsb, ddb, wgtb = sm[:, 4:8, :], sm[:, 8:12, :], sm[:, 12:16, :], wgt_t[:]
            f2 = lambda ap: ap[:, 0:NT, :]
            dtk = [("dtall", T0 + t) for t in range(NT)]
            def prep_():
                pg.op("dve", lambda e: e.tensor_tensor(out=f2(dtab), in0=dtall[:, T0:T0 + NT, :],
                                                       in1=aneg[:].unsqueeze(1).to_broadcast([128, NT, 64]), op=OP.mult),
                      reads=dtk + ["aneg"], writes=["sm4"])
                def mm2(e):
                    pmv = pm[:, 0:W].rearrange("p (t c) -> p t c", c=64)
                    for dr in range(2):
                        tri = cst[:, C_TU:C_TU + 128] if dr == 0 else cst[:, C_TL:C_TL + 128]
                        e.matmul(pmv[:, :, dr * 32:(dr + 1) * 32], lhsT=tri, rhs=f2(dtab)[:, :, dr * 32:(dr + 1) * 32],
                                 start=True, stop=True)
                    return e.matmul(pm[:, 256:256 + W].rearrange("p (t c) -> p t c", c=64), lhsT=ones, rhs=f2(dtab),
                                    start=True, stop=True)
                pg.op("pe", mm2, reads=["sm4", "cst"], writes=["pm"])
                pg.op("act", lambda e: e.copy(out=f2(acsb), in_=pm[:, 0:W].rearrange("p (t c) -> p t c", c=64)),
                      reads=["pm"], writes=["sm5"])
                pg.op("act", lambda e: e.activation(out=decall[:, T0:T0 + NT, :],
                                                    in_=pm[:, 256:256 + W].rearrange("p (t c) -> p t c", c=64), func=AF.Exp),
                      reads=["pm"], writes=[("decall", T0 + t, d_) for t in range(NT) for d_ in range(2)])
                pg.op("dve", lambda e: e.tensor_tensor(out=f2(ddb), in0=pm[:, 256:256 + W].rearrange("p (t c) -> p t c", c=64),
                                                       in1=f2(acsb), op=OP.subtract), reads=["pm", "sm5"], writes=["sm6"])
                pg.op("act", lambda e: e.activation(out=f2(ddb), in_=f2(ddb), func=AF.Exp), reads=["sm6"], writes=["sm6"])
                pg.op("dve", lambda e: e.tensor_tensor(out=f2(wgtb), in0=f2(ddb), in1=dtall[:, T0:T0 + NT, :], op=OP.mult),
                      reads=["sm6"] + dtk, writes=["sm7"])

            units.append(prep_)
            for t in range(NT):
                for dr in range(2):
                    def unit_(t=t, dr=dr):
                        T = T0 + t
                        xwt = xw[dr]
                        pg.op("dve", lambda e, t=t, dr=dr, xwt=xwt: e.tensor_tensor(
                            out=xwt[:].rearrange("p (h d) -> p h d", d=64),
                            in0=xs_tok[:, t, :].rearrange("p (h d) -> p h d", d=64),
                            in1=wgtb[:, t, dr * 32:(dr + 1) * 32].unsqueeze(2).to_broadcast([128, 32, 64]), op=OP.mult),
                            reads=[("xs_tok", t, cb) for cb in range(4)] + ["sm7"], writes=["xw%d" % dr])
                        sst = Sst[dr]
                        for g in range(4):
                            psg, psk = pS[g % 2], "pS%d" % (g % 2)
                            pg.op("pe", lambda e, psg=psg, t=t, g=g, xwt=xwt: e.matmul(
                                psg[:], lhsT=B_tok[:, t, g * 128:(g + 1) * 128], rhs=xwt[:, g * 512:(g + 1) * 512],
                                start=True, stop=True), reads=[("B_tok", t), "xw%d" % dr], writes=[psk])
                            if g % 2 == 0:
                                pg.op("act", lambda e, psg=psg, g=g, sst=sst: e.copy(out=sst[:, g * 512:(g + 1) * 512], in_=psg[:]),
                                      reads=[psk], writes=[("Sst", dr, g)])
                            else:
                                pg.op("dve", lambda e, psg=psg, g=g, sst=sst: e.tensor_copy(out=sst[:, g * 512:(g + 1) * 512], in_=psg[:]),
                                      reads=[psk], writes=[("Sst", dr, g)])
                        dma_sp(S_all[T, dr], sst[:], "st_S%d" % dr, reads=[("Sst", dr, g) for g in range(4)])

                    units.append(unit_)
            return units
        units = build_units()
        while pending:
            pending.pop(0)()
        if own:
            for t in range(NT):
                dma_sp(xs_s[T0 - 10 + t], xs_tok[:, t, :], "st_xs%d" % t, reads=[("xs_tok", t, cb) for cb in range(4)])
                dma_sp(bt_s[T0 - 10 + t], BTs[:, :, t * 128:(t + 1) * 128], "st_bt",
                       reads=[("BTs", cc) for cc in range(4)])
                dma_sp(ct_s[T0 - 10 + t], CTs[:, :, t * 128:(t + 1) * 128], "st_ct",
                       reads=[("CTs", cc) for cc in range(4)])
            for cb in range(4):
                tm_job(7232 + cb * 512, 512, "v", cb)
                for _ in range(2):
                    if units:
                        units.pop(0)()
            pg.op("dve", lambda e: e.memset(ssq[:], 0.0), writes=["ssq"] + [("ssq", t) for t in range(4)])
            def ln_tile(t):
                pg.op("act", lambda e, t=t: e.activation(out=vout[t % 2][:], in_=xin[:, t, :], func=AF.Identity,
                                                         accum_out=ssq[:, t:t + 1]),
                      reads=[("xin", t), "ssq"], writes=["vout%d" % (t % 2), ("ssq", t)])
                pg.op("act", lambda e, t=t: e.activation(out=vout[t % 2][:], in_=xin[:, t, :], func=AF.Square,
                                                         accum_out=ssq[:, 4 + t:5 + t]),
                      reads=[("xin", t), "ssq"], writes=["vout%d" % (t % 2), ("ssq", t)])
                mean, var, rs_, nmr = (ssq[:, 8 + t:9 + t], ssq[:, 12 + t:13 + t], ssq[:, 12 + t:13 + t], ssq[:, 8 + t:9 + t])
                k = ("ssq", t)
                pg.op("dve", lambda e, t=t, mean=mean: e.tensor_scalar_mul(out=mean, in0=ssq[:, t:t + 1], scalar1=1.0 / D),
                      reads=[k], writes=[k])
                pg.op("dve", lambda e, t=t, mean=mean, var=var: e.tensor_tensor(out=var, in0=mean, in1=mean, op=OP.mult),
                      reads=[k], writes=[k])
                pg.op("dve", lambda e, t=t, var=var: e.scalar_tensor_tensor(
                    out=var, in0=ssq[:, 4 + t:5 + t], scalar=1.0 / D, in1=var, op0=OP.mult, op1=OP.subtract),
                    reads=[k], writes=[k])
                pg.op("dve", lambda e, var=var: e.tensor_scalar_add(out=var, in0=var, scalar1=EPS), reads=[k], writes=[k])
                pg.op("act", lambda e, var=var: e.sqrt(out=var, in_=var), reads=[k], writes=[k])
                pg.op("dve", lambda e, var=var: e.reciprocal(out=var, in_=var), reads=[k], writes=[k])
                pg.op("dve", lambda e, mean=mean, var=var: e.scalar_tensor_tensor(
                    out=mean, in0=mean, scalar=-1.0, in1=var, op0=OP.mult, op1=OP.mult), reads=[k], writes=[k])
                pg.op("act", lambda e, t=t, mean=mean, var=var: e.activation(
                    out=xin[:, t, :], in_=xin[:, t, :], func=AF.Identity, bias=mean, scale=var),
                    reads=[("xin", t), k], writes=[("xin", t)])
                pg.op("dve", lambda e, t=t: e.tensor_tensor(out=xin[:, t, :], in0=xin[:, t, :], in1=lnr[:, 0, :], op=OP.mult),
                      reads=[("xin", t), "lnr"], writes=[("xin", t)])
                pg.op("dve", lambda e, t=t: e.tensor_tensor(out=vout[t % 2][:], in0=xin[:, t, :], in1=lnr[:, 1, :], op=OP.add),
                      reads=[("xin", t), "lnr"], writes=["vout%d" % (t % 2)])
                dma_sp(v_s[T0 - 10 + t], vout[t % 2][:], "st_vout%d" % (t % 2), reads=["vout%d" % (t % 2)])

            for cb in range(4):
                fm_job(5184 + cb * 512, "u", cb)
                if units:
                    units.pop(0)()
                ln_tile(cb)
            for t in range(NT):
                dma_sp(u_s[T0 - 10 + t], stg_u[:, :, t * 128:(t + 1) * 128], "st_stg", reads=["stg"])
        return units

        if SUB <= 2:
            return
    prev_units = []
    for blk_ in blocks:
        prev_units = do_block(*blk_, prev_units)
    while prev_units:
        prev_units.pop(0)()
    if "t_dt" in taps:
        t_dt = nc.dram_tensor("t_dt", [128, 18 * 64], F32, kind="ExternalOutput").ap()
        dma_sp(t_dt, dtall[:].rearrange("p a b -> p (a b)"), "st_tap", reads=[("dtall", T) for T in range(18)])
        t_dec = nc.dram_tensor("t_dec", [128, 18 * 64], F32, kind="ExternalOutput").ap()
        dma_sp(t_dec, decall[:].rearrange("p a b -> p (a b)"), "st_tap2", reads=[("decall", T, d_) for T in range(18) for d_ in range(2)])
    pg.barrier()
    pg.flush()
    st.close()
    if stage <= 1:
        return finish(nc, pg, es, out)


    st = ExitStack()
    hsts = [st.enter_context(nc.sbuf_tensor("hst%d" % i, [128, 2048], F32)) for i in range(2)]
    Sld = [[st.enter_context(nc.sbuf_tensor("Sld%d_%d" % (d_, i), [128, 2048], F32)) for i in range(2)] for d_ in range(2)]
    hpb = [[st.enter_context(nc.sbuf_tensor("hpb%d_%d" % (d_, i), [128, 2048], BF16)) for i in range(2)] for d_ in range(2)]
    chains = [list(range(0, 18)), [1, 0] + list(range(9, 1, -1)) + list(range(17, 9, -1))]
    for dr in range(2):
        pg.op("dve" if dr == 0 else "pool", lambda e, dr=dr: e.memset(hsts[dr][:], 0.0), writes=["hst%d" % dr])
    for i in range(18):
        for dr in range(2):
            T = chains[dr][i]
            hst = hsts[dr]
            hk = "hst%d" % dr
            sl, slk = Sld[dr][i % 2], "Sld%d_%d" % (dr, i % 2)
            dma_sp(sl[:], S_all[T, dr], "ld_" + slk, writes=[slk])
            if T >= 10:
                hb, hbk = hpb[dr][i % 2], "hpb%d_%d" % (dr, i % 2)
                pg.op("act", lambda e, hb=hb, hst=hst: e.copy(out=hb[:], in_=hst[:]), reads=[hk], writes=[hbk])
                dma_sp(hp_s[dr, T - 10], hb[:], "st_" + hbk, reads=[hbk])
            pg.op("dve", lambda e, T=T, dr=dr, hst=hst: e.tensor_tensor(
                out=hst[:].rearrange("p (h d) -> p h d", d=64), in0=hst[:].rearrange("p (h d) -> p h d", d=64),
                in1=decall[:, T, dr * 32:(dr + 1) * 32].unsqueeze(2).to_broadcast([128, 32, 64]), op=OP.mult),
                reads=[hk, ("decall", T, dr)], writes=[hk])
            pg.op("dve", lambda e, sl=sl, hst=hst: e.tensor_tensor(out=hst[:], in0=hst[:], in1=sl[:], op=OP.add),
                  reads=[hk, slk], writes=[hk])
    pg.barrier()
    pg.flush()
    st.close()
    if stage <= 3:
        return finish(nc, pg, es, out)


    sel3b = sb("sel3b", [128, 4096], BF16)
    dma_cast(sel3b[:], sel3, "ld_c2", writes=["sel3b"])
    st = ExitStack()
    def sb4(name, shape, dt=F32):
        return st.enter_context(nc.sbuf_tensor(name, list(shape), dt))
    def ps4(name, shape, dt=F32):
        return st.enter_context(nc.psum_tensor(name, list(shape), dt))
    xs_c = [sb4("xs_c%d" % i, [128, 2048], BF16) for i in range(2)]
    bt_c = [sb4("bt_c%d" % i, [128, 4, 128], BF16) for i in range(2)]
    ct_c = [sb4("ct_c%d" % i, [128, 4, 128], BF16) for i in range(2)]
    hpf_c = [sb4("hpf_c%d" % i, [128, 2048], BF16) for i in range(2)]
    hpb_c = [sb4("hpb_c%d" % i, [128, 2048], BF16) for i in range(2)]
    z_c = [sb4("z_c%d" % i, [128, 2048], BF16) for i in range(2)]
    v_c = [sb4("v_c%d" % i, [128, 2048], BF16) for i in range(2)]
    u_c = [sb4("u_c%d" % i, [128, 16, 128], BF16) for i in range(2)]
    mixb = [sb4("mixb%d" % i, [128, 32, 128], BF16) for i in range(2)]
    wsb = sb4("wsb", [128, 8, 128], BF16)
    dta3 = sb4("dta3", [128, 96])
    acs = sb4("acs", [128, 64])
    nacs = sb4("nacs", [128, 64])
    ecum = sb4("ecum", [128, 64])
    xdt = [sb4("xdt%d" % i, [128, 2048], BF16) for i in range(2)]
    pcs = [sb4("pcs%d" % i, [128, 128], BF16) for i in range(2)]
    tbb = sb4("tbb", [128, 128], BF16)
    Rr = sb4("Rr", [128, 128])
    R2 = sb4("R2", [128, 128])
    cbT = sb4("cbT", [128, 4, 128])
    dws = [sb4("dw%d" % i, [128, 4, 128]) for i in range(2)]
    Mm = [[sb4("Mm%d_%d" % (i, j), [128, 8, 128], BF16) for j in range(2)] for i in range(2)]
    ucnt = [0]
    t1 = sb4("t1", [128, 512])
    t2 = sb4("t2", [128, 512])
    yb = sb4("yb", [128, 2048])
    yn = sb4("yn", [128, 2048], BF16)
    gt = sb4("gt", [128, 512])
    ss4 = sb4("ss4", [128, 4])
    pm2 = ps4("pm2", [128, 512])
    pcb = ps4("pcb", [128, 512])
    pDs = [ps4("pD%d" % i, [128, 512]) for i in range(2)]
    pY = ps4("pY", [128, 512])
    pOf = ps4("pOf", [128, 512])
    pOb = ps4("pOb", [128, 512])
    pGa = ps4("pGa", [128, 512])
    pG = [pGa, pcb]
    pGk = ["pGa", "pcb"]
    dma_cast(wsb[:].rearrange("p g i -> p (g i)"), wsT, "ld_wsb", writes=["wsb"])
    mkb = [sb4("mkb%d" % i, [128, 128], BF16) for i in range(2)]
    pg.op("dve", lambda e: e.tensor_copy(out=mkb[0][:], in_=cst[:, C_MNF:C_MNF + 128]), reads=["cst"], writes=["mkb"])
    pg.op("dve", lambda e: e.tensor_copy(out=mkb[1][:], in_=cst[:, C_MNB:C_MNB + 128]), reads=["cst"], writes=["mkb"])
    dsk = rp[:, RP_DSKIP:RP_DSKIP + 32]

    def do_loads(c):
        i2 = c % 2
        xs, bt, ct, hpf, hpb_, zc, vc, uc, mix = (xs_c[i2], bt_c[i2], ct_c[i2], hpf_c[i2], hpb_c[i2], z_c[i2],
                                                   v_c[i2], u_c[i2], mixb[i2])
        K = lambda n: "%s%d" % (n, i2)
        dma_sp(xs[:], xs_s[c], "ld_" + K("xs"), writes=[K("xs")])
        dma_sp(bt[:], bt_s[c], "ld_" + K("bt"), writes=[K("bt")])
        dma_sp(ct[:], ct_s[c], "ld_" + K("ct"), writes=[K("ct")])
        dma_sp(hpf[:], hp_s[0, c], "ld_" + K("hpf"), writes=[K("hpf")])
        dma_sp(hpb_[:], hp_s[1, c], "ld_" + K("hpb"), writes=[K("hpb")])
        dma_sp(zc[:], z_s[c], "ld_" + K("z"), writes=[K("z")])
        dma_sp(vc[:], v_s[c], "ld_" + K("v"), writes=[K("v")])
        dma_sp(uc[:], u_s[c], "ld_" + K("u"), writes=[K("u")])

    def do_chunk(c):
        T = 10 + c
        i2 = c % 2
        xs, bt, ct, hpf, hpb_, zc, vc, uc, mix = (xs_c[i2], bt_c[i2], ct_c[i2], hpf_c[i2], hpb_c[i2], z_c[i2],
                                                   v_c[i2], u_c[i2], mixb[i2])
        K = lambda n: "%s%d" % (n, i2)
        def mmcb(e):
            ins = None
            for g in range(4):
                ins = e.matmul(pcb[:, g * 128:(g + 1) * 128], lhsT=bt[:, g, :], rhs=ct[:, g, :], start=True, stop=True)
            return ins
        pg.op("pe", mmcb, reads=[K("bt"), K("ct")], writes=["pcb"])
        pg.op("act", lambda e: e.copy(out=cbT[:].rearrange("p g i -> p (g i)"), in_=pcb[:]), reads=["pcb"], writes=["cbT"])
        for dr in range(2):
            tri = cst[:, C_TU:C_TU + 128] if dr == 0 else cst[:, C_TL:C_TL + 128]
            pg.op("dve", lambda e, dr=dr: e.tensor_tensor(
                out=dta3[:].rearrange("p (r h) -> p r h", h=32),
                in0=dtall[:, T, dr * 32:(dr + 1) * 32].unsqueeze(1).to_broadcast([128, 3, 32]),
                in1=aneg[:, dr * 32:(dr + 1) * 32].unsqueeze(1).to_broadcast([128, 3, 32]), op=OP.mult),
                reads=["aneg"], writes=["dta3"])
            def mmac(e, dr=dr, tri=tri):
                e.matmul(pm2[:, dr * 32:(dr + 1) * 32], lhsT=tri, rhs=dta3[:, 0:32], start=True, stop=True)
                return e.matmul(pm2[0:96, 64 + dr * 128:64 + (dr + 1) * 128], lhsT=dta3[:, 0:96], rhs=tri,
                                start=True, stop=True)
            pg.op("pe", mmac, reads=["dta3", "cst"], writes=[("pm2", dr)])
            sl = slice(dr * 32, (dr + 1) * 32)
            pg.op("act", lambda e, sl=sl: e.copy(out=acs[:, sl], in_=pm2[:, sl]), reads=[("pm2", dr)], writes=[("acs", dr)])
            pg.op("dve", lambda e, sl=sl: e.tensor_scalar_mul(out=nacs[:, sl], in0=acs[:, sl], scalar1=-1.0),
                  reads=[("acs", dr)], writes=[("nacs", dr)])
            pg.op("act", lambda e, sl=sl: e.activation(out=ecum[:, sl], in_=acs[:, sl], func=AF.Exp),
                  reads=[("acs", dr)], writes=[("ecum", dr)])
            src_ = pm2[:, 64 + dr * 128:64 + (dr + 1) * 128]
            pc = pcs[dr]
            pk = "pcs%d" % dr
            pg.op("act", lambda e, pc=pc, src_=src_: e.copy(out=pc[0:32, :], in_=src_[0:32, :]),
                  reads=[("pm2", dr)], writes=[(pk, 0)])
            for lo in (32, 64):
                pg.op("act", lambda e, src_=src_, lo=lo: e.copy(out=tbb[lo:lo + 32, :], in_=src_[lo:lo + 32, :]),
                      reads=[("pm2", dr)], writes=[("tbb", lo)])
                pg.op("dve", lambda e, src_=src_, lo=lo: e.tensor_tensor(out=Rr[lo:lo + 32, :], in0=src_[lo:lo + 32, :],
                                                                        in1=tbb[lo:lo + 32, :], op=OP.subtract),
                      reads=[("pm2", dr), ("tbb", lo)], writes=[("Rr", lo)])
            pg.op("act", lambda e, pc=pc: e.copy(out=pc[32:64, :], in_=Rr[32:64, :]), reads=[("Rr", 32)], writes=[(pk, 1)])
            pg.op("act", lambda e: e.copy(out=tbb[64:96, :], in_=Rr[64:96, :]), reads=[("Rr", 64)], writes=[("tbb", 64)])
            pg.op("dve", lambda e: e.tensor_tensor(out=R2[64:96, :], in0=Rr[64:96, :], in1=tbb[64:96, :], op=OP.subtract),
                  reads=[("Rr", 64), ("tbb", 64)], writes=["R2"])
            pg.op("act", lambda e, pc=pc: e.copy(out=pc[64:96, :], in_=R2[64:96, :]), reads=["R2"], writes=[(pk, 2)])
            pg.op("pool", lambda e, dr=dr: e.tensor_tensor(
                out=xdt[dr][:].rearrange("p (h d) -> p h d", d=64), in0=xs[:].rearrange("p (h d) -> p h d", d=64),
                in1=dtall[:, T, dr * 32:(dr + 1) * 32].unsqueeze(2).to_broadcast([128, 32, 64]), op=OP.mult),
                reads=[K("xs")], writes=["xdt%d" % dr])
        def emit_D(g):
            Mg = Mm[g % 2]
            Mk = lambda dr, hf, g=g: ("Mm", g % 2, dr, hf)
            for dr in range(2):
                mk = cst[:, C_MNF:C_MNF + 128] if dr == 0 else cst[:, C_MNB:C_MNB + 128]
                pc = pcs[dr]
                pk = "pcs%d" % dr
                for hf in range(2):
                    bsel = ucnt[0] % 2
                    ucnt[0] += 1
                    pDh, pDk = pDs[bsel], "pD%d" % bsel
                    dw, dwk = dws[bsel], "dw%d" % bsel
                    h0 = g * 8 + hf * 4
                    def mmD(e, h0=h0, pc=pc, pDh=pDh, dr=dr):
                        ins = None
                        for j in range(4):
                            h = h0 + j
                            e.matmul(pDh[:, j * 128:(j + 1) * 128], lhsT=sel3b[0:96, h * 128:(h + 1) * 128],
                                     rhs=pc[0:96, :], start=True, stop=False)
                            ins = e.matmul(pDh[:, j * 128:(j + 1) * 128], lhsT=idb[:], rhs=mkb[dr][:],
                                           start=False, stop=True)
                        return ins
                    pg.op("pe", mmD, reads=[(pk, 0), (pk, 1), (pk, 2), "sel3b", "mkb", "idb"], writes=[pDk])
                    pg.op("dve", lambda e, dw=dw, pDh=pDh, h0=h0, dr=dr: e.tensor_tensor(
                        out=dw[:], in0=pDh[:].rearrange("p (h i) -> p h i", i=128),
                        in1=acs[:, dr * 32 + h0:dr * 32 + h0 + 4].unsqueeze(2).to_broadcast([128, 4, 128]), op=OP.subtract),
                        reads=[pDk, ("acs", dr)], writes=[dwk])
                    pg.op("act", lambda e, dw=dw: e.activation(out=dw[:], in_=dw[:], func=AF.Exp), reads=[dwk], writes=[dwk])
                    pg.op("pool", lambda e, dw=dw, g=g, dr=dr, hf=hf, Mg=Mg: e.tensor_tensor(
                        out=Mg[dr][:, hf * 4:(hf + 1) * 4, :], in0=dw[:],
                        in1=cbT[:, g, :].unsqueeze(1).to_broadcast([128, 4, 128]), op=OP.mult),
                        reads=[dwk, "cbT"], writes=[Mk(dr, hf)])
        def emit_Y(g):
            Mg = Mm[g % 2]
            Mk = lambda dr, hf, g=g: ("Mm", g % 2, dr, hf)
            def mmY(e, g=g, Mg=Mg):
                ins = None
                for hh in range(8):
                    h = g * 8 + hh
                    e.matmul(pY[:, hh * 64:(hh + 1) * 64], lhsT=Mg[0][:, hh, :], rhs=xdt[0][:, h * 64:(h + 1) * 64],
                             start=True, stop=False)
                    ins = e.matmul(pY[:, hh * 64:(hh + 1) * 64], lhsT=Mg[1][:, hh, :], rhs=xdt[1][:, h * 64:(h + 1) * 64],
                                   start=False, stop=True)
                return ins
            pg.op("pe", mmY, reads=[Mk(dr, hf) for dr in range(2) for hf in range(2)] + ["xdt0", "xdt1"], writes=["pY"])
            pg.op("pe", lambda e, g=g: e.matmul(pOf[:], lhsT=ct[:, g, :], rhs=hpf[:, g * 512:(g + 1) * 512],
                                                start=True, stop=True), reads=[K("ct"), K("hpf")], writes=["pOf"])
            pg.op("pe", lambda e, g=g: e.matmul(pOb[:], lhsT=ct[:, g, :], rhs=hpb_[:, g * 512:(g + 1) * 512],
                                                start=True, stop=True), reads=[K("ct"), K("hpb")], writes=["pOb"])
            v3 = lambda ap: ap.rearrange("p (h d) -> p h d", d=64)
            pg.op("dve", lambda e, g=g: e.tensor_tensor(
                out=v3(t1[:]), in0=v3(pOf[:]), in1=ecum[:, g * 8:(g + 1) * 8].unsqueeze(2).to_broadcast([128, 8, 64]),
                op=OP.mult), reads=["pOf", ("ecum", 0)], writes=["t1"])
            pg.op("dve", lambda e, g=g: e.tensor_tensor(
                out=v3(t2[:]), in0=v3(pOb[:]), in1=ecum[:, 32 + g * 8:32 + (g + 1) * 8].unsqueeze(2).to_broadcast([128, 8, 64]),
                op=OP.mult), reads=["pOb", ("ecum", 1)], writes=["t2"])
            pg.op("pool", lambda e: e.tensor_tensor(out=t1[:], in0=t1[:], in1=t2[:], op=OP.add), reads=["t1", "t2"],
                  writes=["t1"])
            pg.op("dve", lambda e, g=g: e.tensor_tensor(out=yb[:, g * 512:(g + 1) * 512], in0=pY[:], in1=t1[:], op=OP.add),
                  reads=["pY", "t1"], writes=[("yb", g)])
            pg.op("pool", lambda e, g=g: e.tensor_tensor(
                out=v3(t2[:]), in0=v3(xs[:, g * 512:(g + 1) * 512]),
                in1=dsk[:, g * 8:(g + 1) * 8].unsqueeze(2).to_broadcast([128, 8, 64]), op=OP.mult),
                reads=[K("xs"), "rp", "t2"], writes=["t2"])
            pg.op("pool", lambda e, g=g: e.tensor_tensor(out=yb[:, g * 512:(g + 1) * 512], in0=yb[:, g * 512:(g + 1) * 512],
                                                        in1=t2[:], op=OP.add), reads=[("yb", g), "t2"], writes=[("yb", g)])

        emit_D(0)
        for g in range(4):
            if g + 1 < 4:
                emit_D(g + 1)
            emit_Y(g)
        ybk = [("yb", g) for g in range(4)]
        pg.op("dve", lambda e: e.tensor_tensor(out=yb[:], in0=yb[:], in1=zc[:], op=OP.mult), reads=ybk + [K("z")], writes=ybk)
        pg.op("dve", lambda e: e.memset(ss4[:], 0.0), writes=["ss4"])
        pg.op("act", lambda e: e.activation(out=yn[:], in_=yb[:], func=AF.Square, accum_out=ss4[:, 0:1]),
              reads=ybk + ["ss4"], writes=["yn", "ss4"])
        pg.op("dve", lambda e: e.tensor_scalar(out=ss4[:, 1:2], in0=ss4[:, 0:1], scalar1=1.0 / D, scalar2=EPS,
                                               op0=OP.mult, op1=OP.add), reads=["ss4"], writes=["ss4"])
        pg.op("act", lambda e: e.sqrt(out=ss4[:, 1:2], in_=ss4[:, 1:2]), reads=["ss4"], writes=["ss4"])
        pg.op("dve", lambda e: e.reciprocal(out=ss4[:, 1:2], in_=ss4[:, 1:2]), reads=["ss4"], writes=["ss4"])
        pg.op("act", lambda e: e.activation(out=yn[:], in_=yb[:], func=AF.Copy, scale=ss4[:, 1:2]),
              reads=ybk + ["ss4"], writes=["yn"])
        for q in range(4):
            pgq, pgk = pG[q % 2], pGk[q % 2]
            def mmT(e, q=q, pgq=pgq):
                ins = None
                for j in range(4):
                    kc = q * 4 + j
                    ins = e.matmul(pgq[:, j * 128:(j + 1) * 128], lhsT=yn[:, kc * 128:(kc + 1) * 128], rhs=idb[:],
                                   start=True, stop=True)
                return ins
            pg.op("pe", mmT, reads=["yn", "idb"], writes=[pgk])
            for j in range(4):
                kc = q * 4 + j
                pg.op("act", lambda e, j=j, kc=kc, pgq=pgq: e.activation(
                    out=mix[:, kc, :], in_=pgq[:, j * 128:(j + 1) * 128], func=AF.Copy,
                    scale=pv[:, PV_SNG + kc:PV_SNG + kc + 1]), reads=[pgk, "pv"], writes=[(K("mix"), kc)])
        for q in range(4):
            pgq, pgk = pG[q % 2], pGk[q % 2]
            def mmG(e, q=q, pgq=pgq):
                ins = None
                for j in range(4):
                    cc = q * 4 + j
                    ins = e.matmul(pgq[:, j * 128:(j + 1) * 128], lhsT=vc[:, cc * 128:(cc + 1) * 128], rhs=wsb[:, cc // 2, :],
                                   start=True, stop=True)
                return ins
            pg.op("pe", mmG, reads=[K("v"), "wsb"], writes=[pgk])
            pg.op("dve", lambda e, q=q, pgq=pgq: e.tensor_tensor(
                out=gt[:].rearrange("p (a b i) -> p a b i", a=2, b=2),
                in0=pgq[:].rearrange("p (a b i) -> p a b i", a=2, b=2),
                in1=rp[:, RP_BS + q * 256:RP_BS + (q + 1) * 256].rearrange("p (a i) -> p a i", a=2).unsqueeze(2)
                .to_broadcast([128, 2, 2, 128]), op=OP.add), reads=[pgk, "rp"], writes=["gt"])
            pg.op("pool", lambda e, q=q: e.tensor_tensor(
                out=mix[:, 16 + q * 4:16 + (q + 1) * 4, :], in0=gt[:].rearrange("p (c i) -> p c i", i=128),
                in1=uc[:, q * 4:(q + 1) * 4, :], op=OP.mult), reads=["gt", K("u")], writes=[(K("mix"), 16 + q)])
        dma_sp(mix_s[c], mix[:], "st_" + K("mix"),
               reads=[(K("mix"), kc) for kc in range(20)])

    nch = NCH
    wb3 = [sb4("p3w%d" % i, [128, 16, 512], BF16) for i in range(2)]

    class _MP:
        def __getitem__(self, key):
            p_, c_ = key
            return pm2[p_, 320 + c_.start:320 + c_.stop]
    mps_cur[0] = _MP()
    mps_off[0] = 64
    do_loads(0)
    for c in range(nch):
        if c + 1 < nch:
            do_loads(c + 1)
        mod_block(8 + 2 * c, wb3, part=1)
        mod_block(9 + 2 * c, wb3, part=1)
        do_chunk(c)
        mod_block(8 + 2 * c, wb3, part=2)
        mod_block(9 + 2 * c, wb3, part=2)
    mod_finish(32, 96, pm2[:, 320:512], base=32)
    ab(4, PV_N2G, 4, 3, 0)
    pg.op("dve", lambda e: e.tensor_copy(out=G12[:, 0, :], in_=mt[:, 2, :, 0]), reads=["modT"], writes=["G12a"])
    pg.op("dve", lambda e: e.tensor_copy(out=G12[:, 1, :], in_=mt[:, 5, :, 0]), reads=["modT"], writes=["G12b"])
    pg.barrier()
    pg.flush()
    st.close()
    if stage <= 4:
        return finish(nc, pg, es, out)


    st5 = ExitStack()
    x1T = st5.enter_context(nc.sbuf_tensor("x1T", [128, 16, 1024], F32))
    banks = [st5.enter_context(nc.psum_tensor("bk%d" % i, [128, 512], F32)) for i in range(8)]
    bkk = ["bk%d" % i for i in range(8)]
    st = ExitStack()
    mixblk = st.enter_context(nc.sbuf_tensor("mixblk", [128, 32, 512], BF16))
    xin5 = st.enter_context(nc.sbuf_tensor("xin5", [128, 4, 2048], F32))
    wo = [st.enter_context(nc.sbuf_tensor("wo%d" % i, [128, 32, 256], BF16)) for i in range(2)]
    tmp5 = [st.enter_context(nc.sbuf_tensor("tmp5_%d" % i, [128, 512], F32)) for i in range(2)]
    w_out_v = w_out.rearrange("(kc p) c -> p kc c", p=128)

    def do_p5(tb):
        for t in range(4):
            dma_sp(mixblk[:, :, t * 128:(t + 1) * 128], mix_s[tb * 4 + t], "ld_mixblk%d" % t, writes=[("mixblk", t)])
            r0 = 1280 + (tb * 4 + t) * 128
            dma_sp(xin5[:, t, :], x_all[r0:r0 + 128, :], "ld_xin5_%d" % t, writes=[("xin5", t)])
        for dcp in range(8):
            w = wo[dcp % 2]
            wk = "wo%d" % (dcp % 2)
            dma_cast(w[:], w_out_v[:, :, dcp * 256:(dcp + 1) * 256], "ld_" + wk, writes=[wk])
            for d2 in range(2):
                dc = dcp * 2 + d2
                i2 = dc % 2
                pa, pak = banks[i2], bkk[i2]
                px, pxk = banks[2 + i2], bkk[2 + i2]
                def mm(e, w=w, d2=d2, pa=pa):
                    ins = None
                    for kc in range(32):
                        ins = e.matmul(pa[:], lhsT=w[:, kc, d2 * 128:(d2 + 1) * 128], rhs=mixblk[:, kc, :],
                                       start=(kc == 0), stop=(kc == 31))
                    return ins
                pg.op("pe", mm, reads=[wk] + [("mixblk", t) for t in range(4)], writes=[pak])
                tm, tmk = tmp5[i2], "tmp5_%d" % i2
                pg.op("act", lambda e, tm=tm, pa=pa, dc=dc: e.activation(out=tm[:], in_=pa[:], func=AF.Copy,
                                                                         scale=G12[:, 0, dc:dc + 1]),
                      reads=[pak, "G12a"], writes=[tmk])
                def trx(e, px=px, dc=dc):
                    ins = None
                    for t in range(4):
                        ins = e.transpose(out=px[:, t * 128:(t + 1) * 128], in_=xin5[:, t, dc * 128:(dc + 1) * 128],
                                          identity=ident)
                    return ins
                pg.op("pe", trx, reads=[("xin5", t) for t in range(4)] + ["cst"], writes=[pxk])
                pg.op("dve", lambda e, px=px, tm=tm, dc=dc: e.tensor_tensor(
                    out=x1T[:, dc, tb * 512:(tb + 1) * 512], in0=px[:], in1=tm[:], op=OP.add),
                    reads=[pxk, tmk], writes=[("x1T", dc, tb)])
    for tb in range(2):
        do_p5(tb)
    if "t_x1T" in taps:
        t_x1T = nc.dram_tensor("t_x1T", [128, 16 * 1024], F32, kind="ExternalOutput").ap()
        dma_sp(t_x1T, x1T[:].rearrange("p a b -> p (a b)"), "st_tap5",
               reads=[("x1T", dc, tb) for dc in range(16) for tb in range(2)])
    pg.barrier()
    pg.flush()
    st.close()
    if stage <= 5:
        st5.close()
        return finish(nc, pg, es, out)


    st6 = ExitStack()
    h2T = st6.enter_context(nc.sbuf_tensor("h2T", [128, 16, 1024], BF16))
    cpc = st6.enter_context(nc.sbuf_tensor("cpc", [128, 1024], BF16))
    st = ExitStack()
    def sb6(name, shape, dt=F32):
        return st.enter_context(nc.sbuf_tensor(name, list(shape), dt))
    sq = [sb6("sq%d" % i, [128, 512]) for i in range(2)]
    rstd = sb6("rstd", [128, 1024])
    tmph = [sb6("tmph%d" % i, [128, 1024]) for i in range(2)]
    wrb = sb6("wrb", [128, 16, 36], BF16)
    lg = sb6("lg", [128, 8, 36])
    mg = sb6("mg", [128, 8])
    eg = sb6("eg", [128, 8, 4])
    sgm = sb6("sgm", [128, 8])
    tpg = sb6("tpg", [128, 8])
    ohg = sb6("ohg", [128, 8, 4])
    selx = sb6("selx", [128, 8, 8])
    tmp8 = sb6("tmp8", [128, 8, 8])
    m1 = sb6("m1", [128, 8])
    m2 = sb6("m2", [128, 8])
    mask1 = sb6("mask1", [128, 8, 8])
    mask2 = sb6("mask2", [128, 8, 8])
    sel2 = sb6("sel2", [128, 8, 8])
    p1 = sb6("p1", [128, 8])
    p2 = sb6("p2", [128, 8])
    wex = sb6("wex", [128, 8, 8])
    comb3 = sb6("comb3", [128, 8, 3, 32])
    ctb = sb6("ctb", [128, 1024], BF16)
    cR = sb6("cR", [128, 1024])
    cR2 = sb6("cR2", [128, 1024])
    dma_cast(wrb[:].rearrange("p a b -> p (a b)"), wr, "ld_wrb", writes=["wrb"])
    x1k = [("x1T", dc, tb) for dc in range(16) for tb in range(2)]
    for tb in range(2):
        for kc in range(16):
            s_, sk = sq[kc % 2], "sq%d" % (kc % 2)
            pg.op("act", lambda e, s_=s_, kc=kc, tb=tb: e.activation(out=s_[:], in_=x1T[:, kc, tb * 512:(tb + 1) * 512],
                                                                     func=AF.Square), reads=[("x1T", kc, tb)], writes=[sk])
            pg.op("pe", lambda e, s_=s_, kc=kc, tb=tb: e.matmul(banks[tb][:], lhsT=ones, rhs=s_[:], start=(kc == 0),
                                                               stop=(kc == 15)), reads=[sk, "cst"], writes=[bkk[tb]])
        sl = slice(tb * 512, (tb + 1) * 512)
        pg.op("dve", lambda e, tb=tb, sl=sl: e.tensor_scalar(out=rstd[:, sl], in0=banks[tb][:], scalar1=1.0 / D, scalar2=EPS,
                                                            op0=OP.mult, op1=OP.add), reads=[bkk[tb]], writes=[("rstd", tb)])
        pg.op("act", lambda e, sl=sl: e.sqrt(out=rstd[:, sl], in_=rstd[:, sl]), reads=[("rstd", tb)], writes=[("rstd", tb)])
        pg.op("dve", lambda e, sl=sl: e.reciprocal(out=rstd[:, sl], in_=rstd[:, sl]), reads=[("rstd", tb)], writes=[("rstd", tb)])
    for kc in range(16):
        th, thk = tmph[kc % 2], "tmph%d" % (kc % 2)
        pg.op("dve", lambda e, th=th, kc=kc: e.tensor_tensor(out=th[:], in0=x1T[:, kc, :], in1=rstd[:], op=OP.mult),
              reads=[("x1T", kc, 0), ("x1T", kc, 1), ("rstd", 0), ("rstd", 1)], writes=[thk])
        pg.op("act", lambda e, th=th, kc=kc: e.activation(out=h2T[:, kc, :], in_=th[:], func=AF.Identity,
                                                          bias=AB[:, 5, kc:kc + 1], scale=AB[:, 4, kc:kc + 1]),
              reads=[thk, "AB4", "AB5"], writes=[("h2T", kc)])
    h2k = [("h2T", kc) for kc in range(16)]
    for t in range(8):
        pr, prk = banks[2 + t % 2], bkk[2 + t % 2]
        def mmr(e, t=t, pr=pr):
            ins = None
            for kc in range(16):
                ins = e.matmul(pr[:, 0:36], lhsT=h2T[:, kc, t * 128:(t + 1) * 128], rhs=wrb[:, kc, :],
                               start=(kc == 0), stop=(kc == 15))
            return ins
        pg.op("pe", mmr, reads=h2k + ["wrb"], writes=[prk])
        pg.op("dve", lambda e, t=t, pr=pr: e.tensor_tensor(out=lg[:, t, :], in0=pr[:, 0:36], in1=rp[:, RP_BR:RP_BR + 36],
                                                          op=OP.add), reads=[prk, "rp"], writes=[("lg", t)])
    lgk = [("lg", t) for t in range(8)]
    lgG = lg[:, :, 0:4]
    bc = lambda ap, n: ap.unsqueeze(2).to_broadcast([128, 8, n])
    R_ = "rt"
    pg.op("dve", lambda e: e.tensor_reduce(out=mg[:], in_=lgG, axis=AX.X, op=OP.max), reads=lgk, writes=[R_])
    pg.op("dve", lambda e: e.tensor_tensor(out=eg[:], in0=lgG, in1=bc(mg[:], 4), op=OP.subtract), reads=lgk + [R_], writes=[R_])
    pg.op("act", lambda e: e.activation(out=eg[:], in_=eg[:], func=AF.Exp), reads=[R_], writes=[R_])
    pg.op("dve", lambda e: e.tensor_reduce(out=sgm[:], in_=eg[:], axis=AX.X, op=OP.add), reads=[R_], writes=[R_])
    pg.op("dve", lambda e: e.reciprocal(out=tpg[:], in_=sgm[:]), reads=[R_], writes=[R_])
    pg.op("dve", lambda e: e.tensor_tensor(out=ohg[:], in0=lgG, in1=bc(mg[:], 4), op=OP.is_equal), reads=lgk + [R_], writes=[R_])
    for g in range(4):
        lgE = lg[:, :, 4 + g * 8:4 + (g + 1) * 8]
        dst = selx if g == 0 else tmp8
        pg.op("dve", lambda e, g=g, lgE=lgE, dst=dst: e.tensor_tensor(
            out=dst[:], in0=lgE, in1=ohg[:, :, g:g + 1].to_broadcast([128, 8, 8]), op=OP.mult), reads=lgk + [R_], writes=[R_])
        if g > 0:
            pg.op("dve", lambda e: e.tensor_tensor(out=selx[:], in0=selx[:], in1=tmp8[:], op=OP.add), reads=[R_], writes=[R_])
    pg.op("dve", lambda e: e.tensor_reduce(out=m1[:], in_=selx[:], axis=AX.X, op=OP.max), reads=[R_], writes=[R_])
    pg.op("dve", lambda e: e.tensor_tensor(out=mask1[:], in0=selx[:], in1=bc(m1[:], 8), op=OP.is_equal), reads=[R_], writes=[R_])
    pg.op("dve", lambda e: e.tensor_scalar_mul(out=sel2[:], in0=mask1[:], scalar1=-1.0e30), reads=[R_], writes=[R_])
    pg.op("dve", lambda e: e.tensor_tensor(out=sel2[:], in0=sel2[:], in1=selx[:], op=OP.add), reads=[R_], writes=[R_])
    pg.op("dve", lambda e: e.tensor_reduce(out=m2[:], in_=sel2[:], axis=AX.X, op=OP.max), reads=[R_], writes=[R_])
    pg.op("dve", lambda e: e.tensor_tensor(out=mask2[:], in0=sel2[:], in1=bc(m2[:], 8), op=OP.is_equal), reads=[R_], writes=[R_])
    pg.op("dve", lambda e: e.tensor_tensor(out=p2[:], in0=m2[:], in1=m1[:], op=OP.subtract), reads=[R_], writes=[R_])
    pg.op("act", lambda e: e.activation(out=p2[:], in_=p2[:], func=AF.Exp), reads=[R_], writes=[R_])
    pg.op("dve", lambda e: e.tensor_scalar_add(out=p1[:], in0=p2[:], scalar1=1.0), reads=[R_], writes=[R_])
    pg.op("dve", lambda e: e.reciprocal(out=p1[:], in_=p1[:]), reads=[R_], writes=[R_])
    pg.op("dve", lambda e: e.tensor_tensor(out=p2[:], in0=p2[:], in1=p1[:], op=OP.mult), reads=[R_], writes=[R_])
    pg.op("dve", lambda e: e.tensor_tensor(out=p1[:], in0=p1[:], in1=tpg[:], op=OP.mult), reads=[R_], writes=[R_])
    pg.op("dve", lambda e: e.tensor_tensor(out=p2[:], in0=p2[:], in1=tpg[:], op=OP.mult), reads=[R_], writes=[R_])
    pg.op("dve", lambda e: e.tensor_tensor(out=wex[:], in0=mask1[:], in1=bc(p1[:], 8), op=OP.mult), reads=[R_], writes=[R_])
    pg.op("dve", lambda e: e.tensor_tensor(out=tmp8[:], in0=mask2[:], in1=bc(p2[:], 8), op=OP.mult), reads=[R_], writes=[R_])
    pg.op("dve", lambda e: e.tensor_tensor(out=wex[:], in0=wex[:], in1=tmp8[:], op=OP.add), reads=[R_], writes=[R_])
    for r in range(3):
        for g in range(4):
            pg.op("dve", lambda e, r=r, g=g: e.tensor_tensor(
                out=comb3[:, :, r, g * 8:(g + 1) * 8], in0=wex[:], in1=ohg[:, :, g:g + 1].to_broadcast([128, 8, 8]),
                op=OP.mult), reads=[R_], writes=[R_, ("comb3", r, g)])
    if "t_comb" in taps:
        t_comb = nc.dram_tensor("t_comb", [128, 8 * 96], F32, kind="ExternalOutput").ap()
        dma_sp(t_comb, comb3[:].rearrange("p a b c -> p (a b c)"), "st_tap6", reads=[R_])
    if "t_h2T" in taps:
        t_h2T = nc.dram_tensor("t_h2T", [128, 16 * 1024], BF16, kind="ExternalOutput").ap()
        dma_sp(t_h2T, h2T[:].rearrange("p a b -> p (a b)"), "st_tap7", reads=h2k)
    for t in range(8):
        pc_, pck = banks[4 + t // 4], bkk[4 + t // 4]
        pg.op("pe", lambda e, t=t, pc_=pc_: e.transpose(out=pc_[0:96, (t % 4) * 128:(t % 4 + 1) * 128],
                                                        in_=comb3[:, t, :, :].rearrange("p r c -> p (r c)"), identity=ident),
              reads=[R_, "cst"], writes=[(pck, t % 4)])
    for hb in range(2):
        src_ = banks[4 + hb]
        sk = [(bkk[4 + hb], j) for j in range(4)]
        sl = slice(hb * 512, (hb + 1) * 512)
        pg.op("act", lambda e, src_=src_, sl=sl: e.copy(out=cpc[0:32, sl], in_=src_[0:32, :]), reads=sk, writes=[("cpc", 0, hb)])
        for lo in (32, 64):
            pg.op("act", lambda e, src_=src_, sl=sl, lo=lo: e.copy(out=ctb[lo:lo + 32, sl], in_=src_[lo:lo + 32, :]),
                  reads=sk, writes=[("ctb", lo, hb)])
            pg.op("dve", lambda e, src_=src_, sl=sl, lo=lo: e.tensor_tensor(
                out=cR[lo:lo + 32, sl], in0=src_[lo:lo + 32, :], in1=ctb[lo:lo + 32, sl], op=OP.subtract),
                reads=sk + [("ctb", lo, hb)], writes=[("cR", lo, hb)])
        pg.op("act", lambda e, sl=sl: e.copy(out=cpc[32:64, sl], in_=cR[32:64, sl]), reads=[("cR", 32, hb)],
              writes=[("cpc", 1, hb)])
        pg.op("act", lambda e, sl=sl: e.copy(out=ctb[64:96, sl], in_=cR[64:96, sl]), reads=[("cR", 64, hb)],
              writes=[("ctb", 64, hb)])
        pg.op("dve", lambda e, sl=sl: e.tensor_tensor(out=cR2[64:96, sl], in0=cR[64:96, sl], in1=ctb[64:96, sl],
                                                      op=OP.subtract), reads=[("cR", 64, hb), ("ctb", 64, hb)],
              writes=[("cR2", hb)])
        pg.op("act", lambda e, sl=sl: e.copy(out=cpc[64:96, sl], in_=cR2[64:96, sl]), reads=[("cR2", hb)],
              writes=[("cpc", 2, hb)])
    pg.barrier()
    pg.flush()
    st.close()
    if stage <= 6:
        st6.close()
        st5.close()
        return finish(nc, pg, es, out)


    st = ExitStack()
    def sb7(name, shape, dt=F32):
        return st.enter_context(nc.sbuf_tensor(name, list(shape), dt))
    wgh = [sb7("wgh%d" % i, [128, 16, 256], BF16) for i in range(2)]
    wuh = [sb7("wuh%d" % i, [128, 16, 256], BF16) for i in range(2)]
    wdn = sb7("wdn", [128, 4, 2048], BF16)
    hid = sb7("hid", [128, 4, 1024], BF16)
    cbc = sb7("cbc", [128, 2, 512])
    sgs = [sb7("sgs%d" % i, [128, 512]) for i in range(2)]
    tus = [sb7("tus%d" % i, [128, 512]) for i in range(2)]
    tmo = [sb7("tmo%d" % i, [128, 512]) for i in range(3)]
    cpk = [("cpc", r, hb) for r in range(3) for hb in range(2)]
    cnt6 = [0]

    def do_expert(ex):
        wg_v = w_g[ex].rearrange("(kc p) f -> p kc f", p=128)
        wu_v = w_u[ex].rearrange("(kc p) f -> p kc f", p=128)
        wd_v = w_d[ex].rearrange("(fc p) d -> p fc d", p=128)
        for tb in range(2):
            pg.op("pe", lambda e, tb=tb: e.matmul(banks[6][:], lhsT=sel3b[0:96, ex * 128:(ex + 1) * 128],
                                                  rhs=cpc[0:96, tb * 512:(tb + 1) * 512], start=True, stop=True),
                  reads=cpk + ["sel3b"], writes=[bkk[6]])
            pg.op("act", lambda e, tb=tb: e.copy(out=cbc[:, tb, :], in_=banks[6][:]), reads=[bkk[6]], writes=[("cbc", tb)])
        for half in range(2):
            wg_, wu_ = wgh[half], wuh[half]
            dma_cast(wg_[:], wg_v[:, :, half * 256:(half + 1) * 256], "ld_wgh%d" % half, writes=["wgh%d" % half])
            dma_cast(wu_[:], wu_v[:, :, half * 256:(half + 1) * 256], "ld_wuh%d" % half, writes=["wuh%d" % half])
            for fcl in range(2):
                fc = half * 2 + fcl
                for tb in range(2):
                    i2 = cnt6[0] % 2
                    cnt6[0] += 1
                    pgt, pgk_ = banks[i2], bkk[i2]
                    pup, puk = banks[2 + i2], bkk[2 + i2]
                    def mmg(e, wg_=wg_, fcl=fcl, tb=tb, pgt=pgt):
                        ins = None
                        for kc in range(16):
                            ins = e.matmul(pgt[:], lhsT=wg_[:, kc, fcl * 128:(fcl + 1) * 128],
                                           rhs=h2T[:, kc, tb * 512:(tb + 1) * 512], start=(kc == 0), stop=(kc == 15))
                        return ins
                    pg.op("pe", mmg, reads=["wgh%d" % half] + h2k, writes=[pgk_])
                    def mmu(e, wu_=wu_, fcl=fcl, tb=tb, pup=pup):
                        ins = None
                        for kc in range(16):
                            ins = e.matmul(pup[:], lhsT=wu_[:, kc, fcl * 128:(fcl + 1) * 128],
                                           rhs=h2T[:, kc, tb * 512:(tb + 1) * 512], start=(kc == 0), stop=(kc == 15))
                        return ins
                    pg.op("pe", mmu, reads=["wuh%d" % half] + h2k, writes=[puk])
                    sg_, sgk = sgs[i2], "sgs%d" % i2
                    tu_, tuk = tus[i2], "tus%d" % i2
                    pg.op("act", lambda e, sg_=sg_, pgt=pgt: e.activation(out=sg_[:], in_=pgt[:], func=AF.Silu),
                          reads=[pgk_], writes=[sgk])
                    pg.op("dve", lambda e, tu_=tu_, pup=pup, sg_=sg_: e.tensor_tensor(out=tu_[:], in0=pup[:], in1=sg_[:],
                                                                                     op=OP.mult),
                          reads=[puk, sgk], writes=[tuk])
                    pg.op("dve", lambda e, tu_=tu_, fc=fc, tb=tb: e.tensor_tensor(
                        out=hid[:, fc, tb * 512:(tb + 1) * 512], in0=tu_[:], in1=cbc[:, tb, :], op=OP.mult),
                        reads=[tuk, ("cbc", tb)], writes=[("hid", fc, tb)])
        dma_cast(wdn[:], wd_v, "ld_wdn", writes=["wdn"])
        for dc in range(16):
            for tb in range(2):
                i2 = cnt6[0] % 3
                cnt6[0] += 1
                pbi = (4, 5, 7)[i2]
                po, pok = banks[pbi], bkk[pbi]
                def mmd(e, dc=dc, tb=tb, po=po):
                    ins = None
                    for fc in range(4):
                        ins = e.matmul(po[:], lhsT=wdn[:, fc, dc * 128:(dc + 1) * 128], rhs=hid[:, fc, tb * 512:(tb + 1) * 512],
                                       start=(fc == 0), stop=(fc == 3))
                    return ins
                pg.op("pe", mmd, reads=["wdn"] + [("hid", fc, tb) for fc in range(4)], writes=[pok])
                tm_, tmk = tmo[i2], "tmo%d" % i2
                pg.op("act", lambda e, tm_=tm_, po=po, dc=dc: e.activation(out=tm_[:], in_=po[:], func=AF.Copy,
                                                                           scale=G12[:, 1, dc:dc + 1]),
                      reads=[pok, "G12b"], writes=[tmk])
                pg.op("dve", lambda e, tm_=tm_, dc=dc, tb=tb: e.tensor_tensor(
                    out=x1T[:, dc, tb * 512:(tb + 1) * 512], in0=x1T[:, dc, tb * 512:(tb + 1) * 512], in1=tm_[:], op=OP.add),
                    reads=[("x1T", dc, tb), tmk], writes=[("x1T", dc, tb)])
    for ex in range(NEXP):
        do_expert(ex)
    pg.barrier()
    pg.flush()
    st.close()
    st6.close()

    st = ExitStack()
    nfr = st.enter_context(nc.sbuf_tensor("nfr", [128, 2048], F32))
    xo = [st.enter_context(nc.sbuf_tensor("xo%d" % i, [128, 2048], F32)) for i in range(2)]
    junk = st.enter_context(nc.sbuf_tensor("junk", [128, 2048], BF16))
    ss7 = st.enter_context(nc.sbuf_tensor("ss7", [128, 16], F32))
    dma_sp(nfr[:], lnrows[:, 2 * D:3 * D], "ld_nfr", writes=["nfr"])
    pg.op("dve", lambda e: e.memset(ss7[:], 0.0), writes=["ss7"])
    for t in range(8):
        xo_, xok = xo[t % 2], "xo%d" % (t % 2)
        for q in range(4):
            pf, pfk = banks[q % 2], bkk[q % 2]
            def trf(e, t=t, q=q, pf=pf):
                ins = None
                for j in range(4):
                    dc = q * 4 + j
                    ins = e.transpose(out=pf[:, j * 128:(j + 1) * 128], in_=x1T[:, dc, t * 128:(t + 1) * 128], identity=ident)
                return ins
            pg.op("pe", trf, reads=[("x1T", q * 4 + j, t // 4) for j in range(4)] + ["cst"], writes=[pfk])
            pg.op("act", lambda e, xo_=xo_, q=q, pf=pf: e.copy(out=xo_[:, q * 512:(q + 1) * 512], in_=pf[:]),
                  reads=[pfk], writes=[(xok, q)])
        xk = [(xok, q) for q in range(4)]
        pg.op("act", lambda e, xo_=xo_, t=t: e.activation(out=junk[:], in_=xo_[:], func=AF.Square, accum_out=ss7[:, t:t + 1]),
              reads=xk + ["ss7"], writes=["junk", ("ss7", t)])
        pg.op("dve", lambda e, t=t: e.tensor_scalar(out=ss7[:, 8 + t:9 + t], in0=ss7[:, t:t + 1], scalar1=1.0 / D, scalar2=EPS,
                                                    op0=OP.mult, op1=OP.add), reads=[("ss7", t)], writes=[("rs7", t)])
        pg.op("act", lambda e, t=t: e.sqrt(out=ss7[:, 8 + t:9 + t], in_=ss7[:, 8 + t:9 + t]), reads=[("rs7", t)], writes=[("rs7", t)])
        pg.op("dve", lambda e, t=t: e.reciprocal(out=ss7[:, 8 + t:9 + t], in_=ss7[:, 8 + t:9 + t]), reads=[("rs7", t)],
              writes=[("rs7", t)])
        pg.op("dve", lambda e, xo_=xo_, t=t: e.scalar_tensor_tensor(out=xo_[:], in0=xo_[:], scalar=ss7[:, 8 + t:9 + t],
                                                                    in1=nfr[:], op0=OP.mult, op1=OP.mult),
              reads=xk + [("rs7", t), "nfr"], writes=xk)
        dma_sp(out[t * 128:(t + 1) * 128, :], xo_[:], "st_out%d" % (t % 2), reads=xk)
    pg.barrier()
    pg.flush()
    st.close()
    st5.close()
    return finish(nc, pg, es, out)


def finish(nc, pg, es, out):
    es.close()
    return nc


def _consts():
    c = np.zeros((128, C_N), np.float32)
    i = np.arange(128)
    c[:, C_ID:C_ID + 128] = np.eye(128, dtype=np.float32)
    c[:, C_TU:C_TU + 128] = (i[:, None] <= i[None, :])
    c[:, C_TL:C_TL + 128] = (i[:, None] >= i[None, :])
    c[:, C_MNF:C_MNF + 128] = np.where(i[:, None] <= i[None, :], 0.0, -30000.0)
    c[:, C_MNB:C_MNB + 128] = np.where(i[:, None] >= i[None, :], 0.0, -30000.0)
    c[:, C_ONE:C_ONE + 128] = 1.0
    s3 = np.zeros((128, 32, 128), np.float32)
    for p in range(96):
        s3[p, p % 32, :] = 1.0
    return c, s3.reshape(128, 4096)


def _pp(v):
    return np.ascontiguousarray(np.asarray(v, np.float32).reshape(-1, 128).T)


def prep_inputs(inp):
    f = lambda a: np.ascontiguousarray(np.asarray(a, np.float32))
    x, c, ctx, c_ctx = f(inp["x"]), f(inp["c"]), f(inp["ctx"]), f(inp["c_ctx"])
    cst, s3 = _consts()
    conv_w = f(inp["conv_w"])[0]
    pvec = np.zeros((128, PV_N), np.float32)
    pvec[:, PV_N1G:PV_N1G + 16] = _pp(inp["norm1_g"][0])
    pvec[:, PV_N2G:PV_N2G + 16] = _pp(inp["norm2_g"][0])
    pvec[:, PV_SNG:PV_SNG + 16] = _pp(inp["ssd_norm_g"][0])
    for j in range(3):
        pvec[:, PV_CW + j * 24:PV_CW + (j + 1) * 24] = _pp(conv_w[j])
    pvec[:, PV_CB:PV_CB + 24] = _pp(inp["conv_b"][0])
    pvec[:, PV_BMOD:PV_BMOD + 96] = _pp(inp["b_mod"][0])
    row = np.zeros((RP_N,), np.float32)
    row[RP_DTB:RP_DTB + 32] = f(inp["dt_bias_f"])[0]
    row[RP_DTB + 32:RP_DTB + 64] = f(inp["dt_bias_b"])[0]
    row[RP_ALOG:RP_ALOG + 32] = f(inp["a_log_f"])[0]
    row[RP_ALOG + 32:RP_ALOG + 64] = f(inp["a_log_b"])[0]
    row[RP_DSKIP:RP_DSKIP + 32] = f(inp["d_skip"])[0]
    row[RP_BS:RP_BS + 1024] = f(inp["b_spatial"])[0].reshape(-1)
    row[RP_BR:RP_BR + 4] = f(inp["b_router_group"])[0]
    row[RP_BR + 4:RP_BR + 36] = f(inp["b_router_expert"])[0].reshape(-1)
    rowp = np.ascontiguousarray(np.broadcast_to(row[None, :], (128, RP_N)))
    lnr = np.concatenate([f(inp["cm_ln_g"])[0], f(inp["cm_ln_b"])[0], f(inp["normf_g"])])
    lnrows = np.ascontiguousarray(np.broadcast_to(lnr[None, :], (128, 3 * D)))
    w_mod = f(inp["w_mod"])[0]
    w_in = f(inp["w_in"])[0]
    w_out = f(inp["w_out"])[0]
    wsT = np.ascontiguousarray(np.transpose(f(inp["w_spatial"])[0], (2, 0, 1)).reshape(128, 1024))
    wrg = f(inp["w_router_group"])[0]
    wre = np.transpose(f(inp["w_router_expert"])[0], (1, 0, 2)).reshape(D, 32)
    wrc = np.concatenate([wrg, wre], axis=1)
    wr = np.ascontiguousarray(wrc.reshape(16, 128, 36).transpose(1, 0, 2).reshape(128, 16 * 36))
    w_g = f(inp["w_exp_gate"])[0].reshape(32, D, 512)
    w_u = f(inp["w_exp_up"])[0].reshape(32, D, 512)
    w_d = f(inp["w_exp_down"])[0].reshape(32, 512, D)
    maps = []
    for k in range(NCORES):
        b, s = k // 2, k % 2
        own = x[b, s * 1024:(s + 1) * 1024]
        oth = x[b, (1 - s) * 1024:(2 - s) * 1024]
        x_all = np.concatenate([ctx[b], oth, own], axis=0)
        fl = np.zeros((128, 2), np.float32)
        fl[:, 0] = 1.0 if s == 1 else 0.0
        fl[:, 1] = 1.0 if s == 0 else 0.0
        cvec = np.stack([_pp(c[b]), _pp(c_ctx)], axis=2).reshape(128, 32)
        maps.append(dict(x_all=x_all, flags=fl, cvec=np.ascontiguousarray(cvec), pvec=pvec, rowp=rowp,
                         lnrows=lnrows, consts=cst, sel3=s3, w_mod=w_mod, w_in=w_in, w_out=w_out,
                         wsT=wsT, wr=wr, w_g=w_g, w_u=w_u, w_d=w_d))
    return maps


def kernel(**inputs):
    maps = prep_inputs(inputs)
    nc = build_nc()
    res = run_bass_kernel_spmd(nc, maps, core_ids=list(range(NCORES)))
    outf = np.zeros((4, 2048, D), np.float32)
    for k in range(NCORES):
        b, s = k // 2, k % 2
        outf[b, s * 1024:(s + 1) * 1024] = res.results[k]["out"]
    return outf
```

```python
from contextlib import ExitStack
import numpy as np
import concourse.bass as bass
import concourse.mybir as mybir
from concourse.bass_utils import run_bass_kernel_spmd

F32 = mybir.dt.float32
BF16 = mybir.dt.bfloat16
AF = mybir.ActivationFunctionType
OP = mybir.AluOpType
AX = mybir.AxisListType

D = 2048
NCORES = 8
EPS = 1e-6
ENGS = ("pe", "act", "dve", "pool", "sp")
SUB = 99
JOBS = None
NOTR = False
NCH = 8
NEXP = 32
NOCONV = False

PV_N1G, PV_N2G, PV_SNG, PV_CW, PV_CB, PV_BMOD = 0, 16, 32, 48, 120, 144
PV_N = 240
RP_DTB, RP_ALOG, RP_DSKIP, RP_BS, RP_BR = 0, 64, 128, 160, 1184
RP_N = 1220
C_ID, C_TU, C_TL, C_MNF, C_MNB, C_ONE = 0, 128, 256, 384, 512, 640
C_N = 768


class Prog:
    def __init__(self, nc, es):
        self.nc = nc
        self.es = es
        self.sems = {}
        self.cnt = {}
        self.ops = {e: [] for e in ENGS}
        self.known = {e: {} for e in ENGS}
        self.last_w = {}
        self.readers = {}
        self.latest = {}
        for e in ENGS:
            self._sem(e)

    def _sem(self, key):
        if key not in self.sems:
            self.sems[key] = self.es.enter_context(self.nc.semaphore("s_" + str(key)))
            self.cnt[key] = 0
        return self.sems[key]

    def op(self, eng, fn, reads=(), writes=(), dma=None):
        waits = {}
        def need(tok):
            sk, v = tok
            if sk == "pe" and eng == "pe":
                return
            if self.known[eng].get(sk, 0) >= v:
                return
            waits[sk] = max(waits.get(sk, 0), v)
        for k in reads:
            if k in self.last_w:
                need(self.last_w[k])
        for k in writes:
            if k in self.last_w:
                need(self.last_w[k])
            for r in self.readers.get(k, ()):
                need(r)
        for sk, v in waits.items():
            self.known[eng][sk] = v
        if dma is not None:
            self._sem(dma)
            self.cnt[dma] += 16
            tok = (dma, self.cnt[dma])
        else:
            self.cnt[eng] += 1
            tok = (eng, self.cnt[eng])
        self.latest[tok[0]] = tok[1]
        for k in writes:
            self.last_w[k] = tok
            self.readers[k] = []
        for k in reads:
            self.readers.setdefault(k, []).append(tok)
        self.ops[eng].append((list(waits.items()), fn, tok, dma is not None))
        return tok

    def barrier(self):
        for e in ENGS:
            waits = []
            for sk, v in self.latest.items():
                if sk == e and e == "pe":
                    continue
                if self.known[e].get(sk, 0) < v:
                    waits.append((sk, v))
                    self.known[e][sk] = v
            if waits:
                self.ops[e].append((waits, None, None, False))
        self.last_w.clear()
        self.readers.clear()

    def flush(self):
        nc = self.nc
        ops = self.ops
        sems = self.sems

        def run(engh, lst):
            for waits, fn, tok, isdma in lst:
                for sk, v in waits:
                    engh.wait_ge(sems[sk], v)
                if fn is None:
                    continue
                ins = fn(engh)
                ins.then_inc(sems[tok[0]], 16 if isdma else 1)

        with nc.Block() as block:
            if ops["sp"]:
                @block.sync
                def _(e):
                    run(e, ops["sp"])
            if ops["pe"]:
                @block.tensor
                def _(e):
                    run(e, ops["pe"])
            if ops["act"]:
                @block.scalar
                def _(e):
                    run(e, ops["act"])
            if ops["dve"]:
                @block.vector
                def _(e):
                    run(e, ops["dve"])
            if ops["pool"]:
                @block.gpsimd
                def _(e):
                    run(e, ops["pool"])
        self.ops = {e: [] for e in ENGS}


def build_nc(stage=99, taps=()):
    nc = bass.Bass("TRN2", target_bir_lowering=False)
    es = ExitStack()
    pg = Prog(nc, es)

    def din(name, shape, dt=F32):
        return nc.dram_tensor(name, list(shape), dt, kind="ExternalInput").ap()

    def dscr(name, shape, dt):
        kind = "ExternalOutput" if name in taps else "Internal"
        return nc.dram_tensor(name, list(shape), dt, kind=kind).ap()

    x_all = din("x_all", [2304, D])
    flags = din("flags", [128, 2])
    cvec = din("cvec", [128, 32])
    pvec = din("pvec", [128, PV_N])
    rowp = din("rowp", [128, RP_N])
    lnrows = din("lnrows", [128, 3 * D])
    consts = din("consts", [128, C_N])
    sel3 = din("sel3", [128, 4096])
    w_mod = din("w_mod", [D, 6 * D])
    w_in = din("w_in", [D, 9280])
    w_out = din("w_out", [2 * D, D])
    wsT = din("wsT", [128, 1024])
    wr = din("wr", [128, 16 * 36])
    w_g = din("w_g", [32, D, 512])
    w_u = din("w_u", [32, D, 512])
    w_d = din("w_d", [32, 512, D])
    out = nc.dram_tensor("out", [1024, D], F32, kind="ExternalOutput").ap()

    def sb(name, shape, dt=F32):
        return es.enter_context(nc.sbuf_tensor(name, list(shape), dt))

    def ps(name, shape, dt=F32):
        return es.enter_context(nc.psum_tensor(name, list(shape), dt))

    cst = sb("cst", [128, C_N])
    idb = sb("idb", [128, 128], BF16)
    pv = sb("pv", [128, PV_N])
    rp = sb("rp", [128, RP_N])
    flg = sb("flg", [128, 2])
    cv = sb("cv", [128, 32])
    scT = sb("scT", [128, 32], BF16)
    modT = sb("modT", [128, 192])
    AB = sb("AB", [128, 6, 16])
    G12 = sb("G12", [128, 2, 16])
    dtall = sb("dtall", [128, 18, 64])
    aneg = sb("aneg", [128, 64])
    decall = sb("decall", [128, 18, 64])

    ident = cst[:, C_ID:C_ID + 128]
    ones = cst[:, C_ONE:C_ONE + 128]

    def dma_sp(out_ap, in_ap, sem, reads=(), writes=()):
        pg.op("sp", lambda e: e.dma_start(out=out_ap, in_=in_ap), reads=reads, writes=writes, dma=sem)

    def dma_cast(out_ap, in_ap, sem, reads=(), writes=()):
        pg.op("pool", lambda e: e.dma_start(out=out_ap, in_=in_ap), reads=reads, writes=writes, dma=sem)

    dma_sp(cst[:], consts, "ld_c0", writes=["cst"])
    dma_sp(pv[:], pvec, "ld_c1", writes=["pv"])
    dma_sp(rp[:], rowp, "ld_c3", writes=["rp"])
    dma_sp(flg[:], flags, "ld_c4", writes=["flg"])
    dma_sp(cv[:], cvec, "ld_c5", writes=["cv"])
    pg.op("dve", lambda e: e.tensor_copy(out=idb[:], in_=ident), reads=["cst"], writes=["idb"])
    pg.op("act", lambda e: e.activation(out=scT[:], in_=cv[:], func=AF.Silu), reads=["cv"], writes=["scT"])
    pg.op("act", lambda e: e.activation(out=aneg[:], in_=rp[:, RP_ALOG:RP_ALOG + 64], func=AF.Exp),
          reads=["rp"], writes=["aneg"])
    pg.op("dve", lambda e: e.tensor_scalar_mul(out=aneg[:], in0=aneg[:], scalar1=-1.0),
          reads=["aneg"], writes=["aneg"])

    st = ExitStack()
    wb = [st.enter_context(nc.sbuf_tensor("p0w%d" % i, [128, 16, 512], BF16)) for i in range(2)]
    modps = st.enter_context(nc.psum_tensor("modps", [128, 192], F32))
    w_mod_v = w_mod.rearrange("(kc p) c -> p kc c", p=128)
    mps_cur = [modps]
    mps_off = [0]
    def mod_block(blk, wb, part=3):
        w = wb[blk % 2]
        key = "p0w%d" % (blk % 2)
        if part & 1:
            dma_cast(w[:], w_mod_v[:, :, blk * 512:(blk + 1) * 512], "ld_" + key, writes=[key])
        if not (part & 2):
            return

        def mm(e, w=w, blk=blk):
            ins = None
            for cc in range(4):
                col = (blk * 4 + cc) * 2 - mps_off[0]
                for kc in range(16):
                    ins = e.matmul(mps_cur[0][:, col:col + 2], lhsT=w[:, kc, cc * 128:(cc + 1) * 128],
                                   rhs=scT[:, kc * 2:kc * 2 + 2], start=(kc == 0), stop=(kc == 15))
            return ins
        pg.op("pe", mm, reads=[key, "scT"], writes=["modps"])

    def mod_finish(c0, c1, modps, base=0):
        pg.op("dve", lambda e: e.tensor_tensor(
            out=modT[:, c0 * 2:c1 * 2].rearrange("p (c t) -> p c t", t=2),
            in0=modps[:, (c0 - base) * 2:(c1 - base) * 2].rearrange("p (c t) -> p c t", t=2),
            in1=pv[:, PV_BMOD + c0:PV_BMOD + c1].unsqueeze(2).to_broadcast([128, c1 - c0, 2]), op=OP.add),
            reads=["modps", "pv"], writes=["modT"])
    for blk in range(8):
        mod_block(blk, wb)
    mod_finish(0, 32, modps)
    mt = modT[:].rearrange("p (m kc t) -> p m kc t", m=6, kc=16, t=2)
    def ab(e_idx, g_off, sc_m, sh_m, which):
        pg.op("dve", lambda e: e.scalar_tensor_tensor(
            out=AB[:, e_idx, :], in0=mt[:, sc_m, :, which], scalar=1.0, in1=pv[:, g_off:g_off + 16],
            op0=OP.add, op1=OP.mult), reads=["modT", "pv"], writes=["AB%d" % e_idx])
        pg.op("dve", lambda e: e.tensor_copy(out=AB[:, e_idx + 1, :], in_=mt[:, sh_m, :, which]),
              reads=["modT"], writes=["AB%d" % (e_idx + 1)])
    ab(0, PV_N1G, 1, 0, 0)
    ab(2, PV_N1G, 1, 0, 1)
    if "t_modT" in taps:
        t_modT = nc.dram_tensor("t_modT", [128, 192], F32, kind="ExternalOutput").ap()
        dma_sp(t_modT, modT[:], "st_tap", reads=["modT"])
    pg.barrier()
    pg.flush()
    st.close()
    if stage <= 0:
        return finish(nc, pg, es, out)


    S_all = dscr("S_all", [18, 2, 128, 2048], F32)
    xs_s = dscr("xs_s", [8, 128, 2048], BF16)
    bt_s = dscr("bt_s", [8, 128, 4, 128], BF16)
    ct_s = dscr("ct_s", [8, 128, 4, 128], BF16)
    z_s = dscr("z_s", [8, 128, 2048], BF16)
    v_s = dscr("v_s", [8, 128, 2048], BF16)
    u_s = dscr("u_s", [8, 128, 16, 128], BF16)
    hp_s = dscr("hp_s", [2, 8, 128, 2048], BF16)
    mix_s = dscr("mix_s", [8, 128, 32, 128], BF16)

    st = ExitStack()
    def sb1(name, shape, dt=F32):
        return st.enter_context(nc.sbuf_tensor(name, list(shape), dt))
    def ps1(name, shape, dt=F32):
        return st.enter_context(nc.psum_tensor(name, list(shape), dt))
    xin = sb1("xin", [128, 4, 2048])
    hT = sb1("hT", [128, 16, 512], BF16)
    wb = [sb1("wb%d" % i, [128, 16, 512], BF16) for i in range(2)]
    t0s = [sb1("t0_%d" % i, [128, 512]) for i in range(2)]
    sbf = [sb1("sbf%d" % i, [128, 512], BF16) for i in range(2)]
    sraws = [sb1("sraw%d" % i, [128, 512]) for i in range(2)]
    xs_tok = sb1("xs_tok", [128, 4, 2048], BF16)
    B_tok = sb1("B_tok", [128, 4, 512], BF16)
    BTs = sb1("BTs", [128, 4, 512], BF16)
    CTs = sb1("CTs", [128, 4, 512], BF16)
    stg = sb1("stg", [128, 4, 2048], BF16)
    lnr = sb1("lnr", [128, 2, 2048])
    xw = [sb1("xw%d" % i, [128, 2048], BF16) for i in range(2)]
    vout = [sb1("vout%d" % i, [128, 2048], BF16) for i in range(2)]
    Sst = [sb1("Sst%d" % i, [128, 2048]) for i in range(2)]
    sm = sb1("sm", [128, 16, 64])
    ssq = sb1("ssq", [128, 16])
    wgt_t = sb1("wgt_t", [128, 4, 64])
    pA = [ps1("pA%d" % i, [128, 512]) for i in range(2)]
    pTf = [ps1("pTf%d" % i, [128, 512]) for i in range(2)]
    pS = [ps1("pS%d" % i, [128, 512]) for i in range(3)]
    scnt = [0]
    pm = ps1("pm", [128, 512])
    stg_u = stg[:].rearrange("p t c -> p (t c)").rearrange("p (cc n) -> p cc n", cc=16)

    w_in_v = w_in.rearrange("(kc p) c -> p kc c", p=128)
    dma_sp(lnr[:].rearrange("p a c -> p (a c)"), lnrows[:, 0:2 * D], "ld_lnr", writes=["lnr"])
    wcnt = [0]
    acnt = [0]

    def load_w(c0, ncols):
        i = wcnt[0] % 2
        wcnt[0] += 1
        dma_cast(wb[i][:, :, 0:ncols], w_in_v[:, :, c0:c0 + ncols], "ld_wb%d" % i, writes=["wb%d" % i])
        return wb[i], "wb%d" % i

    def next_pA():
        i = acnt[0] % 2
        acnt[0] += 1
        return pA[i], "pA%d" % i, i

    blocks = [("ctx", 0, 2, 0), ("oth", 256, 4, 2), ("oth", 768, 4, 6), ("own", 1280, 4, 10), ("own", 1792, 4, 14)]
    if stage == 1:
        blocks = blocks[:1] + blocks[3:4]
    if SUB in (2, 3):
        blocks = blocks[:1]
    def do_block(kind, row0, NT, T0, prev_units):
        N = NT * 128
        own = kind == "own"
        Aidx = 2 if kind == "ctx" else 0
        pg.op("dve", lambda e: e.memset(ssq[:], 0.0), writes=["ssq"])
        for t in range(NT):
            dma_sp(xin[:, t, :], x_all[row0 + t * 128:row0 + (t + 1) * 128, :], "ld_xin%d" % t, writes=[("xin", t)])
            pg.op("act", lambda e, t=t: e.activation(out=stg[:, t, :], in_=xin[:, t, :], func=AF.Square,
                                                     accum_out=ssq[:, t:t + 1]),
                  reads=[("xin", t), "ssq"], writes=["stg", ("ssq", t)])
            pg.op("dve", lambda e, t=t: e.tensor_scalar(out=ssq[:, 8 + t:9 + t], in0=ssq[:, t:t + 1], scalar1=1.0 / D,
                                                        scalar2=EPS, op0=OP.mult, op1=OP.add),
                  reads=[("ssq", t)], writes=[("rs", t)])
            pg.op("act", lambda e, t=t: e.sqrt(out=ssq[:, 8 + t:9 + t], in_=ssq[:, 8 + t:9 + t]),
                  reads=[("rs", t)], writes=[("rs", t)])
            pg.op("dve", lambda e, t=t: e.reciprocal(out=ssq[:, 8 + t:9 + t], in_=ssq[:, 8 + t:9 + t]),
                  reads=[("rs", t)], writes=[("rs", t)])
            pg.op("act", lambda e, t=t: e.activation(out=xin[:, t, :], in_=xin[:, t, :], func=AF.Copy,
                                                     scale=ssq[:, 8 + t:9 + t]),
                  reads=[("xin", t), ("rs", t)], writes=[("xin", t)])
        if SUB <= 0:
            return
        for kc in range(16):
            pa, pak, _ = next_pA()
            def tr(e, pa=pa, kc=kc):
                ins = None
                for t in range(NT):
                    ins = e.transpose(out=pa[:, t * 128:(t + 1) * 128], in_=xin[:, t, kc * 128:(kc + 1) * 128],
                                      identity=ident)
                return ins
            pg.op("pe", tr, reads=[("xin", t) for t in range(NT)] + ["cst"], writes=[pak])
            pg.op("act", lambda e, pa=pa, kc=kc: e.activation(
                out=hT[:, kc, 0:N], in_=pa[:, 0:N], func=AF.Identity,
                bias=AB[:, Aidx + 1, kc:kc + 1], scale=AB[:, Aidx, kc:kc + 1]),
                reads=[pak, "AB%d" % Aidx, "AB%d" % (Aidx + 1)], writes=[("hT", kc)])
            if prev_units and kc % 2 == 1:
                prev_units.pop(0)()
        while prev_units:
            prev_units.pop(0)()
        hT_keys = [("hT", kc) for kc in range(16)]
        if SUB <= 1:
            return

        pending = []
        def fm_job(c0, jobkind, cb):
            w, wk = load_w(c0, 512)
            for cc in range(4):
                pa, pak, pi = next_pA()
                def mm(e, pa=pa, w=w, cc=cc):
                    ins = None
                    for kc in range(16):
                        ins = e.matmul(pa[:, 0:N], lhsT=w[:, kc, cc * 128:(cc + 1) * 128], rhs=hT[:, kc, 0:N],
                                       start=(kc == 0), stop=(kc == 15))
                    return ins
                pg.op("pe", mm, reads=[wk] + hT_keys, writes=[pak])
                while pending:
                    pending.pop(0)()
                if jobkind == "u":
                    pg.op("act", lambda e, pa=pa, cc=cc: e.activation(out=stg_u[:, cb * 4 + cc, 0:N], in_=pa[:, 0:N],
                                                                      func=AF.Gelu),
                          reads=[pak], writes=["stg"])
                    continue
                chn = (c0 - 2048) // 128 + cc
                L = 256 if kind == "ctx" else 64
                t0 = t0s[pi]
                t0k = "t0_%d" % pi
                sraw = sraws[pi]
                pg.op("act", lambda e, pa=pa, sraw=sraw: e.copy(out=sraw[:, 0:N], in_=pa[:, 0:N]),
                      reads=[pak], writes=["sraw%d" % pi])
                pav = sraw[:, 0:N].rearrange("p (r l) -> p r l", l=L)
                t0v = t0[:, 0:N].rearrange("p (r l) -> p r l", l=L)
                cw = lambda j, chn=chn: pv[:, PV_CW + j * 24 + chn:PV_CW + j * 24 + chn + 1]
                pg.op("act", lambda e, pa=pa, t0=t0, chn=chn, cw=cw: e.activation(
                    out=t0[:, 0:N], in_=pa[:, 0:N], func=AF.Identity,
                    bias=pv[:, PV_CB + chn:PV_CB + chn + 1], scale=cw(1)), reads=[pak, "pv"], writes=[t0k])
                if not NOCONV:
                  pg.op("dve", lambda e, pav=pav, t0v=t0v, cw=cw: e.scalar_tensor_tensor(
                    out=t0v[:, :, 1:L], in0=pav[:, :, 0:L - 1], scalar=cw(0), in1=t0v[:, :, 1:L],
                    op0=OP.mult, op1=OP.add), reads=["sraw%d" % pi, t0k, "pv"], writes=[t0k])
                if not NOCONV:
                  pg.op("dve", lambda e, pav=pav, t0v=t0v, cw=cw: e.scalar_tensor_tensor(
                    out=t0v[:, :, 0:L - 1], in0=pav[:, :, 1:L], scalar=cw(2), in1=t0v[:, :, 0:L - 1],
                    op0=OP.mult, op1=OP.add), reads=["sraw%d" % pi, t0k, "pv"], writes=[t0k])
                if jobkind == "xs":
                    dst, dk = sbf[pi][:, 0:N], "sbf%d" % pi
                elif jobkind == "B":
                    dst, dk = BTs[:, cc, 0:N], ("BTs", cc)
                else:
                    dst, dk = CTs[:, cc, 0:N], ("CTs", cc)
                pg.op("act", lambda e, t0=t0, dst=dst: e.activation(out=dst, in_=t0[:, 0:N], func=AF.Silu),
                      reads=[t0k], writes=[dk])
                if jobkind in ("xs", "B") and not NOTR:
                    def emit_tr(dst=dst, dk=dk, pi=pi, cc=cc, jobkind=jobkind, cb=cb):
                        ptf, ptk = pTf[pi], "pTf%d" % pi
                        def tr2(e):
                            ins = None
                            for t in range(NT):
                                ins = e.matmul(ptf[:, t * 128:(t + 1) * 128], lhsT=dst[:, t * 128:(t + 1) * 128],
                                               rhs=idb[:], start=True, stop=True)
                            return ins
                        pg.op("pe", tr2, reads=[dk, "idb"], writes=[ptk])
                        if jobkind == "xs":
                            o = xs_tok[:, 0:NT, cb * 512 + cc * 128:cb * 512 + (cc + 1) * 128]
                            ok = [("xs_tok", t, cb) for t in range(NT)]
                        else:
                            o = B_tok[:, 0:NT, cc * 128:(cc + 1) * 128]
                            ok = [("B_tok", t) for t in range(NT)]
                        iv = ptf[:, 0:N].rearrange("p (t c) -> p t c", c=128)
                        if cc % 2 == 0:
                            pg.op("dve", lambda e: e.tensor_copy(out=o, in_=iv), reads=[ptk], writes=ok)
                        else:
                            pg.op("act", lambda e: e.copy(out=o, in_=iv), reads=[ptk], writes=ok)
                    pending.append(emit_tr)

        def tm_job(c0, ncols, jobkind, cb):
            if jobkind == "dt":
                w, wk = load_w(4672, 512)
                wofs = 448
            else:
                w, wk = load_w(c0, ncols)
                wofs = 0
            for t in range(NT):
                pa, pak, pi = next_pA()
                def mm(e, pa=pa, w=w, t=t):
                    ins = None
                    for kc in range(16):
                        ins = e.matmul(pa[:, 0:ncols], lhsT=hT[:, kc, t * 128:(t + 1) * 128], rhs=w[:, kc, wofs:wofs + ncols],
                                       start=(kc == 0), stop=(kc == 15))
                    return ins
                pg.op("pe", mm, reads=[wk] + hT_keys, writes=[pak])
                while pending:
                    pending.pop(0)()
                if jobkind == "z":
                    pg.op("act", lambda e, pa=pa, t=t: e.activation(out=stg[:, t, cb * 512:(cb + 1) * 512], in_=pa[:],
                                                                    func=AF.Silu), reads=[pak], writes=["stg"])
                elif jobkind == "v":
                    pg.op("act", lambda e, pa=pa, t=t: e.activation(out=xin[:, t, cb * 512:(cb + 1) * 512], in_=pa[:],
                                                                    func=AF.Gelu), reads=[pak], writes=[("xin", t)])
                else:
                    T = T0 + t
                    a, b_, c_, d_ = sm[:, 0, :], sm[:, 1, :], sm[:, 2, :], sm[:, 3, :]
                    pg.op("dve", lambda e, pa=pa: e.tensor_tensor(out=a, in0=pa[:, 0:64], in1=rp[:, RP_DTB:RP_DTB + 64],
                                                                  op=OP.add), reads=[pak, "rp"], writes=["sm0"])
                    pg.op("dve", lambda e: e.tensor_scalar_mul(out=b_, in0=a, scalar1=-1.0),
                          reads=["sm0"], writes=["sm1"])
                    pg.op("dve", lambda e: e.tensor_tensor(out=b_, in0=b_, in1=a, op=OP.min),
                          reads=["sm0", "sm1"], writes=["sm1"])
                    pg.op("act", lambda e: e.activation(out=c_, in_=b_, func=AF.Exp),
                          reads=["sm1"], writes=["sm2"])
                    pg.op("dve", lambda e: e.tensor_scalar_add(out=c_, in0=c_, scalar1=1.0),
                          reads=["sm2"], writes=["sm2"])
                    pg.op("act", lambda e: e.activation(out=c_, in_=c_, func=AF.Ln),
                          reads=["sm2"], writes=["sm2"])
                    pg.op("dve", lambda e, T=T: e.scalar_tensor_tensor(out=dtall[:, T, :], in0=a, scalar=0.0, in1=c_,
                                                                       op0=OP.max, op1=OP.add),
                          reads=["sm0", "sm2"], writes=[("dtall", T)])
                    if kind == "oth":
                        for dr in range(2):
                            pg.op("dve", lambda e, T=T, dr=dr: e.tensor_scalar_mul(
                                out=dtall[:, T, dr * 32:(dr + 1) * 32], in0=dtall[:, T, dr * 32:(dr + 1) * 32],
                                scalar1=flg[:, dr:dr + 1]), reads=[("dtall", T), "flg"], writes=[("dtall", T)])

        if own:
            for cb in range(4):
                tm_job(cb * 512, 512, "z", cb)
            for t in range(NT):
                dma_sp(z_s[T0 - 10 + t], stg[:, t, :], "st_stg", reads=["stg"])
        for cb in range(4):
            if JOBS is None or "xs" in JOBS:
                fm_job(2048 + cb * 512, "xs", cb)
        if JOBS is None or "B" in JOBS:
            fm_job(4096, "B", 0)
        if own:
            fm_job(4608, "C", 0)
        if JOBS is None or "dt" in JOBS:
            tm_job(5120, 64, "dt", 0)
        while pending:
            pending.pop(0)()
        def build_units():
            units = []
            W = NT * 64
            dtab, acsb, ddb, wgtb = sm[:, 4:8, :], sm[:, 8:12, :], sm[:, 12:16, :], wgt_t[:]
            f2 = lambda ap: ap[:, 0:NT, :]
            dtk = [("dtall", T0 + t) for t in range(NT)]
            def prep_():
                pg.op("dve", lambda e: e.tensor_tensor(out=f2(dtab), in0=dtall[:, T0:T0 + NT, :],
                                                       in1=aneg[:].unsqueeze(1).to_broadcast([128, NT, 64]), op=OP.mult),
                      reads=dtk + ["aneg"], writes=["sm4"])
                def mm2(e):
                    pmv = pm[:, 0:W].rearrange("p (t c) -> p t c", c=64)
                    for dr in range(2):
                        tri = cst[:, C_TU:C_TU + 128] if dr == 0 else cst[:, C_TL:C_TL + 128]
                        e.matmul(pmv[:, :, dr * 32:(dr + 1) * 32], lhsT=tri, rhs=f2(dtab)[:, :, dr * 32:(dr + 1) * 32],
                                 start=True, stop=True)
                    return e.matmul(pm[:, 256:256 + W].rearrange("p (t c) -> p t c", c=64), lhsT=ones, rhs=f2(dtab),
                                    start=True, stop=True)
                pg.op("pe", mm2, reads=["sm4", "cst"], writes=["pm"])
                pg.op("act", lambda e: e.copy(out=f2(acsb), in_=pm[:, 0:W].rearrange("p (t c) -> p t c", c=64)),
                      reads=["pm"], writes=["sm5"])
                pg.op("act", lambda e: e.activation(out=decall[:, T0:T0 + NT, :],
                                                    in_=pm[:, 256:256 + W].rearrange("p (t c) -> p t c", c=64), func=AF.Exp),
                      reads=["pm"], writes=[("decall", T0 + t, d_) for t in range(NT) for d_ in range(2)])
                pg.op("dve", lambda e: e.tensor_tensor(out=f2(ddb), in0=pm[:, 256:256 + W].rearrange("p (t c) -> p t c", c=64),
                                                       in1=f2(acsb), op=OP.subtract), reads=["pm", "sm5"], writes=["sm6"])
                pg.op("act", lambda e: e.activation(out=f2(ddb), in_=f2(ddb), func=AF.Exp), reads=["sm6"], writes=["sm6"])
                pg.op("dve", lambda e: e.tensor_tensor(out=f2(wgtb), in0=f2(ddb), in1=dtall[:, T0:T0 + NT, :], op=OP.mult),
                      reads=["sm6"] + dtk, writes=["sm7"])

            units.append(prep_)
            for t in range(NT):
                for dr in range(2):
                    def unit_(t=t, dr=dr):
                        T = T0 + t
                        xwt = xw[dr]
                        pg.op("dve", lambda e, t=t, dr=dr, xwt=xwt: e.tensor_tensor(
                            out=xwt[:].rearrange("p (h d) -> p h d", d=64),
                            in0=xs_tok[:, t, :].rearrange("p (h d) -> p h d", d=64),
                            in1=wgtb[:, t, dr * 32:(dr + 1) * 32].unsqueeze(2).to_broadcast([128, 32, 64]), op=OP.mult),
                            reads=[("xs_tok", t, cb) for cb in range(4)] + ["sm7"], writes=["xw%d" % dr])
                        sst = Sst[dr]
                        for g in range(4):
                            si = scnt[0] % 3
                            scnt[0] += 1
                            psg, psk = pS[si], "pS%d" % si
                            pg.op("pe", lambda e, psg=psg, t=t, g=g, xwt=xwt: e.matmul(
                                psg[:], lhsT=B_tok[:, t, g * 128:(g + 1) * 128], rhs=xwt[:, g * 512:(g + 1) * 512],
                                start=True, stop=True), reads=[("B_tok", t), "xw%d" % dr], writes=[psk])
                            if g % 2 == 0:
                                pg.op("act", lambda e, psg=psg, g=g, sst=sst: e.copy(out=sst[:, g * 512:(g + 1) * 512], in_=psg[:]),
                                      reads=[psk], writes=[("Sst", dr, g)])
                            else:
                                pg.op("dve", lambda e, psg=psg, g=g, sst=sst: e.tensor_copy(out=sst[:, g * 512:(g + 1) * 512], in_=psg[:]),
                                      reads=[psk], writes=[("Sst", dr, g)])
                        dma_sp(S_all[T, dr], sst[:], "st_S%d" % dr, reads=[("Sst", dr, g) for g in range(4)])

                    units.append(unit_)
            return units
        units = build_units()
        while pending:
            pending.pop(0)()
        if own:
            for t in range(NT):
                dma_sp(xs_s[T0 - 10 + t], xs_tok[:, t, :], "st_xs%d" % t, reads=[("xs_tok", t, cb) for cb in range(4)])
                dma_sp(bt_s[T0 - 10 + t], BTs[:, :, t * 128:(t + 1) * 128], "st_bt",
                       reads=[("BTs", cc) for cc in range(4)])
                dma_sp(ct_s[T0 - 10 + t], CTs[:, :, t * 128:(t + 1) * 128], "st_ct",
                       reads=[("CTs", cc) for cc in range(4)])
            for cb in range(4):
                tm_job(7232 + cb * 512, 512, "v", cb)
                for _ in range(2):
                    if units:
                        units.pop(0)()
            pg.op("dve", lambda e: e.memset(ssq[:], 0.0), writes=["ssq"] + [("ssq", t) for t in range(4)])
            def ln_tile(t):
                pg.op("act", lambda e, t=t: e.activation(out=vout[t % 2][:], in_=xin[:, t, :], func=AF.Identity,
                                                         accum_out=ssq[:, t:t + 1]),
                      reads=[("xin", t), "ssq"], writes=["vout%d" % (t % 2), ("ssq", t)])
                pg.op("act", lambda e, t=t: e.activation(out=vout[t % 2][:], in_=xin[:, t, :], func=AF.Square,
                                                         accum_out=ssq[:, 4 + t:5 + t]),
                      reads=[("xin", t), "ssq"], writes=["vout%d" % (t % 2), ("ssq", t)])
                mean, var, rs_, nmr = (ssq[:, 8 + t:9 + t], ssq[:, 12 + t:13 + t], ssq[:, 12 + t:13 + t], ssq[:, 8 + t:9 + t])
                k = ("ssq", t)
                pg.op("dve", lambda e, t=t, mean=mean: e.tensor_scalar_mul(out=mean, in0=ssq[:, t:t + 1], scalar1=1.0 / D),
                      reads=[k], writes=[k])
                pg.op("dve", lambda e, t=t, mean=mean, var=var: e.tensor_tensor(out=var, in0=mean, in1=mean, op=OP.mult),
                      reads=[k], writes=[k])
                pg.op("dve", lambda e, t=t, var=var: e.scalar_tensor_tensor(
                    out=var, in0=ssq[:, 4 + t:5 + t], scalar=1.0 / D, in1=var, op0=OP.mult, op1=OP.subtract),
                    reads=[k], writes=[k])
                pg.op("dve", lambda e, var=var: e.tensor_scalar_add(out=var, in0=var, scalar1=EPS), reads=[k], writes=[k])
                pg.op("act", lambda e, var=var: e.sqrt(out=var, in_=var), reads=[k], writes=[k])
                pg.op("dve", lambda e, var=var: e.reciprocal(out=var, in_=var), reads=[k], writes=[k])
                pg.op("dve", lambda e, mean=mean, var=var: e.scalar_tensor_tensor(
                    out=mean, in0=mean, scalar=-1.0, in1=var, op0=OP.mult, op1=OP.mult), reads=[k], writes=[k])
                pg.op("act", lambda e, t=t, mean=mean, var=var: e.activation(
                    out=xin[:, t, :], in_=xin[:, t, :], func=AF.Identity, bias=mean, scale=var),
                    reads=[("xin", t), k], writes=[("xin", t)])
                pg.op("dve", lambda e, t=t: e.tensor_tensor(out=xin[:, t, :], in0=xin[:, t, :], in1=lnr[:, 0, :], op=OP.mult),
                      reads=[("xin", t), "lnr"], writes=[("xin", t)])
                pg.op("dve", lambda e, t=t: e.tensor_tensor(out=vout[t % 2][:], in0=xin[:, t, :], in1=lnr[:, 1, :], op=OP.add),
                      reads=[("xin", t), "lnr"], writes=["vout%d" % (t % 2)])
                dma_sp(v_s[T0 - 10 + t], vout[t % 2][:], "st_vout%d" % (t % 2), reads=["vout%d" % (t % 2)])

            for cb in range(4):
                fm_job(5184 + cb * 512, "u", cb)
                if units:
                    units.pop(0)()
                ln_tile(cb)
            for t in range(NT):
                dma_sp(u_s[T0 - 10 + t], stg_u[:, :, t * 128:(t + 1) * 128], "st_stg", reads=["stg"])
        return units

        if SUB <= 2:
            return
    prev_units = []
    for blk_ in blocks:
        prev_units = do_block(*blk_, prev_units)
    while prev_units:
        prev_units.pop(0)()
    if "t_dt" in taps:
        t_dt = nc.dram_tensor("t_dt", [128, 18 * 64], F32, kind="ExternalOutput").ap()
        dma_sp(t_dt, dtall[:].rearrange("p a b -> p (a b)"), "st_tap", reads=[("dtall", T) for T in range(18)])
        t_dec = nc.dram_tensor("t_dec", [128, 18 * 64], F32, kind="ExternalOutput").ap()
        dma_sp(t_dec, decall[:].rearrange("p a b -> p (a b)"), "st_tap2", reads=[("decall", T, d_) for T in range(18) for d_ in range(2)])
    pg.barrier()
    pg.flush()
    st.close()
    if stage <= 1:
        return finish(nc, pg, es, out)


    st = ExitStack()
    hsts = [st.enter_context(nc.sbuf_tensor("hst%d" % i, [128, 2048], F32)) for i in range(2)]
    Sld = [[st.enter_context(nc.sbuf_tensor("Sld%d_%d" % (d_, i), [128, 2048], F32)) for i in range(2)] for d_ in range(2)]
    hpb = [[st.enter_context(nc.sbuf_tensor("hpb%d_%d" % (d_, i), [128, 2048], BF16)) for i in range(2)] for d_ in range(2)]
    chains = [list(range(0, 18)), [1, 0] + list(range(9, 1, -1)) + list(range(17, 9, -1))]
    for dr in range(2):
        pg.op("dve" if dr == 0 else "pool", lambda e, dr=dr: e.memset(hsts[dr][:], 0.0), writes=["hst%d" % dr])
    for i in range(18):
        for dr in range(2):
            T = chains[dr][i]
            hst = hsts[dr]
            hk = "hst%d" % dr
            sl, slk = Sld[dr][i % 2], "Sld%d_%d" % (dr, i % 2)
            dma_sp(sl[:], S_all[T, dr], "ld_" + slk, writes=[slk])
            if T >= 10:
                hb, hbk = hpb[dr][i % 2], "hpb%d_%d" % (dr, i % 2)
                pg.op("act", lambda e, hb=hb, hst=hst: e.copy(out=hb[:], in_=hst[:]), reads=[hk], writes=[hbk])
                dma_sp(hp_s[dr, T - 10], hb[:], "st_" + hbk, reads=[hbk])
            pg.op("dve", lambda e, T=T, dr=dr, hst=hst: e.tensor_tensor(
                out=hst[:].rearrange("p (h d) -> p h d", d=64), in0=hst[:].rearrange("p (h d) -> p h d", d=64),
                in1=decall[:, T, dr * 32:(dr + 1) * 32].unsqueeze(2).to_broadcast([128, 32, 64]), op=OP.mult),
                reads=[hk, ("decall", T, dr)], writes=[hk])
            pg.op("dve", lambda e, sl=sl, hst=hst: e.tensor_tensor(out=hst[:], in0=hst[:], in1=sl[:], op=OP.add),
                  reads=[hk, slk], writes=[hk])
    pg.barrier()
    pg.flush()
    st.close()
    if stage <= 3:
        return finish(nc, pg, es, out)


    sel3b = sb("sel3b", [128, 4096], BF16)
    dma_cast(sel3b[:], sel3, "ld_c2", writes=["sel3b"])
    st = ExitStack()
    def sb4(name, shape, dt=F32):
        return st.enter_context(nc.sbuf_tensor(name, list(shape), dt))
    def ps4(name, shape, dt=F32):
        return st.enter_context(nc.psum_tensor(name, list(shape), dt))
    xs_c = [sb4("xs_c%d" % i, [128, 2048], BF16) for i in range(2)]
    bt_c = [sb4("bt_c%d" % i, [128, 4, 128], BF16) for i in range(2)]
    ct_c = [sb4("ct_c%d" % i, [128, 4, 128], BF16) for i in range(2)]
    hpf_c = [sb4("hpf_c%d" % i, [128, 2048], BF16) for i in range(2)]
    hpb_c = [sb4("hpb_c%d" % i, [128, 2048], BF16) for i in range(2)]
    z_c = [sb4("z_c%d" % i, [128, 2048], BF16) for i in range(2)]
    v_c = [sb4("v_c%d" % i, [128, 2048], BF16) for i in range(2)]
    u_c = [sb4("u_c%d" % i, [128, 16, 128], BF16) for i in range(2)]
    mixb = [sb4("mixb%d" % i, [128, 32, 128], BF16) for i in range(2)]
    wsb = sb4("wsb", [128, 8, 128], BF16)
    dta3 = sb4("dta3", [128, 96])
    acs = sb4("acs", [128, 64])
    nacs = sb4("nacs", [128, 64])
    ecum = sb4("ecum", [128, 64])
    xdt = [sb4("xdt%d" % i, [128, 2048], BF16) for i in range(2)]
    pcs = [sb4("pcs%d" % i, [128, 128], BF16) for i in range(2)]
    tbb = sb4("tbb", [128, 128], BF16)
    Rr = sb4("Rr", [128, 128])
    R2 = sb4("R2", [128, 128])
    cbT = sb4("cbT", [128, 4, 128])
    dws = [sb4("dw%d" % i, [128, 4, 128]) for i in range(2)]
    Mm = [[sb4("Mm%d_%d" % (i, j), [128, 8, 128], BF16) for j in range(2)] for i in range(2)]
    ucnt = [0]
    t1 = sb4("t1", [128, 512])
    t2 = sb4("t2", [128, 512])
    yb = sb4("yb", [128, 2048])
    yn = sb4("yn", [128, 2048], BF16)
    gt = sb4("gt", [128, 512])
    ss4 = sb4("ss4", [128, 4])
    pm2 = ps4("pm2", [128, 512])
    pcb = ps4("pcb", [128, 512])
    pDs = [ps4("pD%d" % i, [128, 512]) for i in range(2)]
    pY = ps4("pY", [128, 512])
    pOf = ps4("pOf", [128, 512])
    pOb = ps4("pOb", [128, 512])
    pGa = ps4("pGa", [128, 512])
    pG = [pGa, pcb]
    pGk = ["pGa", "pcb"]
    dma_cast(wsb[:].rearrange("p g i -> p (g i)"), wsT, "ld_wsb", writes=["wsb"])
    mkb = [sb4("mkb%d" % i, [128, 128], BF16) for i in range(2)]
    pg.op("dve", lambda e: e.tensor_copy(out=mkb[0][:], in_=cst[:, C_MNF:C_MNF + 128]), reads=["cst"], writes=["mkb"])
    pg.op("dve", lambda e: e.tensor_copy(out=mkb[1][:], in_=cst[:, C_MNB:C_MNB + 128]), reads=["cst"], writes=["mkb"])
    dsk = rp[:, RP_DSKIP:RP_DSKIP + 32]

    def do_loads(c):
        i2 = c % 2
        xs, bt, ct, hpf, hpb_, zc, vc, uc, mix = (xs_c[i2], bt_c[i2], ct_c[i2], hpf_c[i2], hpb_c[i2], z_c[i2],
                                                   v_c[i2], u_c[i2], mixb[i2])
        K = lambda n: "%s%d" % (n, i2)
        dma_sp(xs[:], xs_s[c], "ld_" + K("xs"), writes=[K("xs")])
        dma_sp(bt[:], bt_s[c], "ld_" + K("bt"), writes=[K("bt")])
        dma_sp(ct[:], ct_s[c], "ld_" + K("ct"), writes=[K("ct")])
        dma_sp(hpf[:], hp_s[0, c], "ld_" + K("hpf"), writes=[K("hpf")])
        dma_sp(hpb_[:], hp_s[1, c], "ld_" + K("hpb"), writes=[K("hpb")])
        dma_sp(zc[:], z_s[c], "ld_" + K("z"), writes=[K("z")])
        dma_sp(vc[:], v_s[c], "ld_" + K("v"), writes=[K("v")])
        dma_sp(uc[:], u_s[c], "ld_" + K("u"), writes=[K("u")])

    def do_chunk(c):
        T = 10 + c
        i2 = c % 2
        xs, bt, ct, hpf, hpb_, zc, vc, uc, mix = (xs_c[i2], bt_c[i2], ct_c[i2], hpf_c[i2], hpb_c[i2], z_c[i2],
                                                   v_c[i2], u_c[i2], mixb[i2])
        K = lambda n: "%s%d" % (n, i2)
        def mmcb(e):
            ins = None
            for g in range(4):
                ins = e.matmul(pcb[:, g * 128:(g + 1) * 128], lhsT=bt[:, g, :], rhs=ct[:, g, :], start=True, stop=True)
            return ins
        pg.op("pe", mmcb, reads=[K("bt"), K("ct")], writes=["pcb"])
        pg.op("act", lambda e: e.copy(out=cbT[:].rearrange("p g i -> p (g i)"), in_=pcb[:]), reads=["pcb"], writes=["cbT"])
        for dr in range(2):
            tri = cst[:, C_TU:C_TU + 128] if dr == 0 else cst[:, C_TL:C_TL + 128]
            pg.op("dve", lambda e, dr=dr: e.tensor_tensor(
                out=dta3[:].rearrange("p (r h) -> p r h", h=32),
                in0=dtall[:, T, dr * 32:(dr + 1) * 32].unsqueeze(1).to_broadcast([128, 3, 32]),
                in1=aneg[:, dr * 32:(dr + 1) * 32].unsqueeze(1).to_broadcast([128, 3, 32]), op=OP.mult),
                reads=["aneg"], writes=["dta3"])
            def mmac(e, dr=dr, tri=tri):
                e.matmul(pm2[:, dr * 32:(dr + 1) * 32], lhsT=tri, rhs=dta3[:, 0:32], start=True, stop=True)
                return e.matmul(pm2[0:96, 64 + dr * 128:64 + (dr + 1) * 128], lhsT=dta3[:, 0:96], rhs=tri,
                                start=True, stop=True)
            pg.op("pe", mmac, reads=["dta3", "cst"], writes=[("pm2", dr)])
            sl = slice(dr * 32, (dr + 1) * 32)
            pg.op("act", lambda e, sl=sl: e.copy(out=acs[:, sl], in_=pm2[:, sl]), reads=[("pm2", dr)], writes=[("acs", dr)])
            pg.op("dve", lambda e, sl=sl: e.tensor_scalar_mul(out=nacs[:, sl], in0=acs[:, sl], scalar1=-1.0),
                  reads=[("acs", dr)], writes=[("nacs", dr)])
            pg.op("act", lambda e, sl=sl: e.activation(out=ecum[:, sl], in_=acs[:, sl], func=AF.Exp),
                  reads=[("acs", dr)], writes=[("ecum", dr)])
            src_ = pm2[:, 64 + dr * 128:64 + (dr + 1) * 128]
            pc = pcs[dr]
            pk = "pcs%d" % dr
            pg.op("act", lambda e, pc=pc, src_=src_: e.copy(out=pc[0:32, :], in_=src_[0:32, :]),
                  reads=[("pm2", dr)], writes=[(pk, 0)])
            for lo in (32, 64):
                pg.op("act", lambda e, src_=src_, lo=lo: e.copy(out=tbb[lo:lo + 32, :], in_=src_[lo:lo + 32, :]),
                      reads=[("pm2", dr)], writes=[("tbb", lo)])
                pg.op("dve", lambda e, src_=src_, lo=lo: e.tensor_tensor(out=Rr[lo:lo + 32, :], in0=src_[lo:lo + 32, :],
                                                                        in1=tbb[lo:lo + 32, :], op=OP.subtract),
                      reads=[("pm2", dr), ("tbb", lo)], writes=[("Rr", lo)])
            pg.op("act", lambda e, pc=pc: e.copy(out=pc[32:64, :], in_=Rr[32:64, :]), reads=[("Rr", 32)], writes=[(pk, 1)])
            pg.op("act", lambda e: e.copy(out=tbb[64:96, :], in_=Rr[64:96, :]), reads=[("Rr", 64)], writes=[("tbb", 64)])
            pg.op("dve", lambda e: e.tensor_tensor(out=R2[64:96, :], in0=Rr[64:96, :], in1=tbb[64:96, :], op=OP.subtract),
                  reads=[("Rr", 64), ("tbb", 64)], writes=["R2"])
            pg.op("act", lambda e, pc=pc: e.copy(out=pc[64:96, :], in_=R2[64:96, :]), reads=["R2"], writes=[(pk, 2)])
            pg.op("pool", lambda e, dr=dr: e.tensor_tensor(
                out=xdt[dr][:].rearrange("p (h d) -> p h d", d=64), in0=xs[:].rearrange("p (h d) -> p h d", d=64),
                in1=dtall[:, T, dr * 32:(dr + 1) * 32].unsqueeze(2).to_broadcast([128, 32, 64]), op=OP.mult),
                reads=[K("xs")], writes=["xdt%d" % dr])
        def emit_D(g):
            Mg = Mm[g % 2]
            Mk = lambda dr, hf, g=g: ("Mm", g % 2, dr, hf)
            for dr in range(2):
                mk = cst[:, C_MNF:C_MNF + 128] if dr == 0 else cst[:, C_MNB:C_MNB + 128]
                pc = pcs[dr]
                pk = "pcs%d" % dr
                for hf in range(2):
                    bsel = ucnt[0] % 2
                    ucnt[0] += 1
                    pDh, pDk = pDs[bsel], "pD%d" % bsel
                    dw, dwk = dws[bsel], "dw%d" % bsel
                    h0 = g * 8 + hf * 4
                    def mmD(e, h0=h0, pc=pc, pDh=pDh, dr=dr):
                        ins = None
                        for j in range(4):
                            h = h0 + j
                            e.matmul(pDh[:, j * 128:(j + 1) * 128], lhsT=sel3b[0:96, h * 128:(h + 1) * 128],
                                     rhs=pc[0:96, :], start=True, stop=False)
                            ins = e.matmul(pDh[:, j * 128:(j + 1) * 128], lhsT=idb[:], rhs=mkb[dr][:],
                                           start=False, stop=True)
                        return ins
                    pg.op("pe", mmD, reads=[(pk, 0), (pk, 1), (pk, 2), "sel3b", "mkb", "idb"], writes=[pDk])
                    pg.op("dve", lambda e, dw=dw, pDh=pDh, h0=h0, dr=dr: e.tensor_tensor(
                        out=dw[:], in0=pDh[:].rearrange("p (h i) -> p h i", i=128),
                        in1=acs[:, dr * 32 + h0:dr * 32 + h0 + 4].unsqueeze(2).to_broadcast([128, 4, 128]), op=OP.subtract),
                        reads=[pDk, ("acs", dr)], writes=[dwk])
                    pg.op("act", lambda e, dw=dw: e.activation(out=dw[:], in_=dw[:], func=AF.Exp), reads=[dwk], writes=[dwk])
                    pg.op("pool", lambda e, dw=dw, g=g, dr=dr, hf=hf, Mg=Mg: e.tensor_tensor(
                        out=Mg[dr][:, hf * 4:(hf + 1) * 4, :], in0=dw[:],
                        in1=cbT[:, g, :].unsqueeze(1).to_broadcast([128, 4, 128]), op=OP.mult),
                        reads=[dwk, "cbT"], writes=[Mk(dr, hf)])
        def emit_Y(g):
            Mg = Mm[g % 2]
            Mk = lambda dr, hf, g=g: ("Mm", g % 2, dr, hf)
            def mmY(e, g=g, Mg=Mg):
                ins = None
                for hh in range(8):
                    h = g * 8 + hh
                    e.matmul(pY[:, hh * 64:(hh + 1) * 64], lhsT=Mg[0][:, hh, :], rhs=xdt[0][:, h * 64:(h + 1) * 64],
                             start=True, stop=False)
                    ins = e.matmul(pY[:, hh * 64:(hh + 1) * 64], lhsT=Mg[1][:, hh, :], rhs=xdt[1][:, h * 64:(h + 1) * 64],
                                   start=False, stop=True)
                return ins
            pg.op("pe", mmY, reads=[Mk(dr, hf) for dr in range(2) for hf in range(2)] + ["xdt0", "xdt1"], writes=["pY"])
            pg.op("pe", lambda e, g=g: e.matmul(pOf[:], lhsT=ct[:, g, :], rhs=hpf[:, g * 512:(g + 1) * 512],
                                                start=True, stop=True), reads=[K("ct"), K("hpf")], writes=["pOf"])
            pg.op("pe", lambda e, g=g: e.matmul(pOb[:], lhsT=ct[:, g, :], rhs=hpb_[:, g * 512:(g + 1) * 512],
                                                start=True, stop=True), reads=[K("ct"), K("hpb")], writes=["pOb"])
            v3 = lambda ap: ap.rearrange("p (h d) -> p h d", d=64)
            pg.op("dve", lambda e, g=g: e.tensor_tensor(
                out=v3(t1[:]), in0=v3(pOf[:]), in1=ecum[:, g * 8:(g + 1) * 8].unsqueeze(2).to_broadcast([128, 8, 64]),
                op=OP.mult), reads=["pOf", ("ecum", 0)], writes=["t1"])
            pg.op("dve", lambda e, g=g: e.tensor_tensor(
                out=v3(t2[:]), in0=v3(pOb[:]), in1=ecum[:, 32 + g * 8:32 + (g + 1) * 8].unsqueeze(2).to_broadcast([128, 8, 64]),
                op=OP.mult), reads=["pOb", ("ecum", 1)], writes=["t2"])
            pg.op("pool", lambda e: e.tensor_tensor(out=t1[:], in0=t1[:], in1=t2[:], op=OP.add), reads=["t1", "t2"],
                  writes=["t1"])
            pg.op("dve", lambda e, g=g: e.tensor_tensor(out=yb[:, g * 512:(g + 1) * 512], in0=pY[:], in1=t1[:], op=OP.add),
                  reads=["pY", "t1"], writes=[("yb", g)])
            pg.op("pool", lambda e, g=g: e.tensor_tensor(
                out=v3(t2[:]), in0=v3(xs[:, g * 512:(g + 1) * 512]),
                in1=dsk[:, g * 8:(g + 1) * 8].unsqueeze(2).to_broadcast([128, 8, 64]), op=OP.mult),
                reads=[K("xs"), "rp", "t2"], writes=["t2"])
            pg.op("pool", lambda e, g=g: e.tensor_tensor(out=yb[:, g * 512:(g + 1) * 512], in0=yb[:, g * 512:(g + 1) * 512],
                                                        in1=t2[:], op=OP.add), reads=[("yb", g), "t2"], writes=[("yb", g)])

        emit_D(0)
        for g in range(4):
            if g + 1 < 4:
                emit_D(g + 1)
            emit_Y(g)
        ybk = [("yb", g) for g in range(4)]
        pg.op("dve", lambda e: e.tensor_tensor(out=yb[:], in0=yb[:], in1=zc[:], op=OP.mult), reads=ybk + [K("z")], writes=ybk)
        pg.op("dve", lambda e: e.memset(ss4[:], 0.0), writes=["ss4"])
        pg.op("act", lambda e: e.activation(out=yn[:], in_=yb[:], func=AF.Square, accum_out=ss4[:, 0:1]),
              reads=ybk + ["ss4"], writes=["yn", "ss4"])
        pg.op("dve", lambda e: e.tensor_scalar(out=ss4[:, 1:2], in0=ss4[:, 0:1], scalar1=1.0 / D, scalar2=EPS,
                                               op0=OP.mult, op1=OP.add), reads=["ss4"], writes=["ss4"])
        pg.op("act", lambda e: e.sqrt(out=ss4[:, 1:2], in_=ss4[:, 1:2]), reads=["ss4"], writes=["ss4"])
        pg.op("dve", lambda e: e.reciprocal(out=ss4[:, 1:2], in_=ss4[:, 1:2]), reads=["ss4"], writes=["ss4"])
        pg.op("act", lambda e: e.activation(out=yn[:], in_=yb[:], func=AF.Copy, scale=ss4[:, 1:2]),
              reads=ybk + ["ss4"], writes=["yn"])
        for q in range(4):
            pgq, pgk = pG[q % 2], pGk[q % 2]
            def mmT(e, q=q, pgq=pgq):
                ins = None
                for j in range(4):
                    kc = q * 4 + j
                    ins = e.matmul(pgq[:, j * 128:(j + 1) * 128], lhsT=yn[:, kc * 128:(kc + 1) * 128], rhs=idb[:],
                                   start=True, stop=True)
                return ins
            pg.op("pe", mmT, reads=["yn", "idb"], writes=[pgk])
            for j in range(4):
                kc = q * 4 + j
                pg.op("act", lambda e, j=j, kc=kc, pgq=pgq: e.activation(
                    out=mix[:, kc, :], in_=pgq[:, j * 128:(j + 1) * 128], func=AF.Copy,
                    scale=pv[:, PV_SNG + kc:PV_SNG + kc + 1]), reads=[pgk, "pv"], writes=[(K("mix"), kc)])
        for q in range(4):
            pgq, pgk = pG[q % 2], pGk[q % 2]
            def mmG(e, q=q, pgq=pgq):
                ins = None
                for j in range(4):
                    cc = q * 4 + j
                    ins = e.matmul(pgq[:, j * 128:(j + 1) * 128], lhsT=vc[:, cc * 128:(cc + 1) * 128], rhs=wsb[:, cc // 2, :],
                                   start=True, stop=True)
                return ins
            pg.op("pe", mmG, reads=[K("v"), "wsb"], writes=[pgk])
            pg.op("dve", lambda e, q=q, pgq=pgq: e.tensor_tensor(
                out=gt[:].rearrange("p (a b i) -> p a b i", a=2, b=2),
                in0=pgq[:].rearrange("p (a b i) -> p a b i", a=2, b=2),
                in1=rp[:, RP_BS + q * 256:RP_BS + (q + 1) * 256].rearrange("p (a i) -> p a i", a=2).unsqueeze(2)
                .to_broadcast([128, 2, 2, 128]), op=OP.add), reads=[pgk, "rp"], writes=["gt"])
            pg.op("pool", lambda e, q=q: e.tensor_tensor(
                out=mix[:, 16 + q * 4:16 + (q + 1) * 4, :], in0=gt[:].rearrange("p (c i) -> p c i", i=128),
                in1=uc[:, q * 4:(q + 1) * 4, :], op=OP.mult), reads=["gt", K("u")], writes=[(K("mix"), 16 + q)])
        dma_sp(mix_s[c], mix[:], "st_" + K("mix"),
               reads=[(K("mix"), kc) for kc in range(20)])

    nch = NCH
    wb3 = [sb4("p3w%d" % i, [128, 16, 512], BF16) for i in range(2)]

    class _MP:
        def __getitem__(self, key):
            p_, c_ = key
            return pm2[p_, 320 + c_.start:320 + c_.stop]
    mps_cur[0] = _MP()
    mps_off[0] = 64
    do_loads(0)
    for c in range(nch):
        if c + 1 < nch:
            do_loads(c + 1)
        mod_block(8 + 2 * c, wb3, part=1)
        mod_block(9 + 2 * c, wb3, part=1)
        do_chunk(c)
        mod_block(8 + 2 * c, wb3, part=2)
        mod_block(9 + 2 * c, wb3, part=2)
    mod_finish(32, 96, pm2[:, 320:512], base=32)
    ab(4, PV_N2G, 4, 3, 0)
    pg.op("dve", lambda e: e.tensor_copy(out=G12[:, 0, :], in_=mt[:, 2, :, 0]), reads=["modT"], writes=["G12a"])
    pg.op("dve", lambda e: e.tensor_copy(out=G12[:, 1, :], in_=mt[:, 5, :, 0]), reads=["modT"], writes=["G12b"])
    pg.barrier()
    pg.flush()
    st.close()
    if stage <= 4:
        return finish(nc, pg, es, out)


    st5 = ExitStack()
    x1T = st5.enter_context(nc.sbuf_tensor("x1T", [128, 16, 1024], F32))
    banks = [st5.enter_context(nc.psum_tensor("bk%d" % i, [128, 512], F32)) for i in range(8)]
    bkk = ["bk%d" % i for i in range(8)]
    st = ExitStack()
    mixblk = st.enter_context(nc.sbuf_tensor("mixblk", [128, 32, 512], BF16))
    xin5 = st.enter_context(nc.sbuf_tensor("xin5", [128, 4, 2048], F32))
    wo = [st.enter_context(nc.sbuf_tensor("wo%d" % i, [128, 32, 256], BF16)) for i in range(2)]
    tmp5 = [st.enter_context(nc.sbuf_tensor("tmp5_%d" % i, [128, 512], F32)) for i in range(2)]
    w_out_v = w_out.rearrange("(kc p) c -> p kc c", p=128)

    def do_p5(tb):
        for t in range(4):
            dma_sp(mixblk[:, :, t * 128:(t + 1) * 128], mix_s[tb * 4 + t], "ld_mixblk%d" % t, writes=[("mixblk", t)])
            r0 = 1280 + (tb * 4 + t) * 128
            dma_sp(xin5[:, t, :], x_all[r0:r0 + 128, :], "ld_xin5_%d" % t, writes=[("xin5", t)])
        for dcp in range(8):
            w = wo[dcp % 2]
            wk = "wo%d" % (dcp % 2)
            dma_cast(w[:], w_out_v[:, :, dcp * 256:(dcp + 1) * 256], "ld_" + wk, writes=[wk])
            for d2 in range(2):
                dc = dcp * 2 + d2
                i2 = dc % 2
                pa, pak = banks[i2], bkk[i2]
                px, pxk = banks[2 + i2], bkk[2 + i2]
                def mm(e, w=w, d2=d2, pa=pa):
                    ins = None
                    for kc in range(32):
                        ins = e.matmul(pa[:], lhsT=w[:, kc, d2 * 128:(d2 + 1) * 128], rhs=mixblk[:, kc, :],
                                       start=(kc == 0), stop=(kc == 31))
                    return ins
                pg.op("pe", mm, reads=[wk] + [("mixblk", t) for t in range(4)], writes=[pak])
                tm, tmk = tmp5[i2], "tmp5_%d" % i2
                pg.op("act", lambda e, tm=tm, pa=pa, dc=dc: e.activation(out=tm[:], in_=pa[:], func=AF.Copy,
                                                                         scale=G12[:, 0, dc:dc + 1]),
                      reads=[pak, "G12a"], writes=[tmk])
                def trx(e, px=px, dc=dc):
                    ins = None
                    for t in range(4):
                        ins = e.transpose(out=px[:, t * 128:(t + 1) * 128], in_=xin5[:, t, dc * 128:(dc + 1) * 128],
                                          identity=ident)
                    return ins
                pg.op("pe", trx, reads=[("xin5", t) for t in range(4)] + ["cst"], writes=[pxk])
                pg.op("dve", lambda e, px=px, tm=tm, dc=dc: e.tensor_tensor(
                    out=x1T[:, dc, tb * 512:(tb + 1) * 512], in0=px[:], in1=tm[:], op=OP.add),
                    reads=[pxk, tmk], writes=[("x1T", dc, tb)])
    for tb in range(2):
        do_p5(tb)
    if "t_x1T" in taps:
        t_x1T = nc.dram_tensor("t_x1T", [128, 16 * 1024], F32, kind="ExternalOutput").ap()
        dma_sp(t_x1T, x1T[:].rearrange("p a b -> p (a b)"), "st_tap5",
               reads=[("x1T", dc, tb) for dc in range(16) for tb in range(2)])
    pg.barrier()
    pg.flush()
    st.close()
    if stage <= 5:
        st5.close()
        return finish(nc, pg, es, out)


    st6 = ExitStack()
    h2T = st6.enter_context(nc.sbuf_tensor("h2T", [128, 16, 1024], BF16))
    cpc = st6.enter_context(nc.sbuf_tensor("cpc", [128, 1024], BF16))
    st = ExitStack()
    def sb6(name, shape, dt=F32):
        return st.enter_context(nc.sbuf_tensor(name, list(shape), dt))
    sq = [sb6("sq%d" % i, [128, 512]) for i in range(2)]
    rstd = sb6("rstd", [128, 1024])
    tmph = [sb6("tmph%d" % i, [128, 1024]) for i in range(2)]
    wrb = sb6("wrb", [128, 16, 36], BF16)
    lg = sb6("lg", [128, 8, 36])
    mg = sb6("mg", [128, 8])
    eg = sb6("eg", [128, 8, 4])
    sgm = sb6("sgm", [128, 8])
    tpg = sb6("tpg", [128, 8])
    ohg = sb6("ohg", [128, 8, 4])
    selx = sb6("selx", [128, 8, 8])
    tmp8 = sb6("tmp8", [128, 8, 8])
    m1 = sb6("m1", [128, 8])
    m2 = sb6("m2", [128, 8])
    mask1 = sb6("mask1", [128, 8, 8])
    mask2 = sb6("mask2", [128, 8, 8])
    sel2 = sb6("sel2", [128, 8, 8])
    p1 = sb6("p1", [128, 8])
    p2 = sb6("p2", [128, 8])
    wex = sb6("wex", [128, 8, 8])
    comb3 = sb6("comb3", [128, 8, 3, 32])
    ctb = sb6("ctb", [128, 1024], BF16)
    cR = sb6("cR", [128, 1024])
    cR2 = sb6("cR2", [128, 1024])
    dma_cast(wrb[:].rearrange("p a b -> p (a b)"), wr, "ld_wrb", writes=["wrb"])
    x1k = [("x1T", dc, tb) for dc in range(16) for tb in range(2)]
    for tb in range(2):
        for kc in range(16):
            s_, sk = sq[kc % 2], "sq%d" % (kc % 2)
            pg.op("act", lambda e, s_=s_, kc=kc, tb=tb: e.activation(out=s_[:], in_=x1T[:, kc, tb * 512:(tb + 1) * 512],
                                                                     func=AF.Square), reads=[("x1T", kc, tb)], writes=[sk])
            pg.op("pe", lambda e, s_=s_, kc=kc, tb=tb: e.matmul(banks[tb][:], lhsT=ones, rhs=s_[:], start=(kc == 0),
                                                               stop=(kc == 15)), reads=[sk, "cst"], writes=[bkk[tb]])
        sl = slice(tb * 512, (tb + 1) * 512)
        pg.op("dve", lambda e, tb=tb, sl=sl: e.tensor_scalar(out=rstd[:, sl], in0=banks[tb][:], scalar1=1.0 / D, scalar2=EPS,
                                                            op0=OP.mult, op1=OP.add), reads=[bkk[tb]], writes=[("rstd", tb)])
        pg.op("act", lambda e, sl=sl: e.sqrt(out=rstd[:, sl], in_=rstd[:, sl]), reads=[("rstd", tb)], writes=[("rstd", tb)])
        pg.op("dve", lambda e, sl=sl: e.reciprocal(out=rstd[:, sl], in_=rstd[:, sl]), reads=[("rstd", tb)], writes=[("rstd", tb)])
    for kc in range(16):
        th, thk = tmph[kc % 2], "tmph%d" % (kc % 2)
        pg.op("dve", lambda e, th=th, kc=kc: e.tensor_tensor(out=th[:], in0=x1T[:, kc, :], in1=rstd[:], op=OP.mult),
              reads=[("x1T", kc, 0), ("x1T", kc, 1), ("rstd", 0), ("rstd", 1)], writes=[thk])
        pg.op("act", lambda e, th=th, kc=kc: e.activation(out=h2T[:, kc, :], in_=th[:], func=AF.Identity,
                                                          bias=AB[:, 5, kc:kc + 1], scale=AB[:, 4, kc:kc + 1]),
              reads=[thk, "AB4", "AB5"], writes=[("h2T", kc)])
    h2k = [("h2T", kc) for kc in range(16)]
    for t in range(8):
        pr, prk = banks[2 + t % 2], bkk[2 + t % 2]
        def mmr(e, t=t, pr=pr):
            ins = None
            for kc in range(16):
                ins = e.matmul(pr[:, 0:36], lhsT=h2T[:, kc, t * 128:(t + 1) * 128], rhs=wrb[:, kc, :],
                               start=(kc == 0), stop=(kc == 15))
            return ins
        pg.op("pe", mmr, reads=h2k + ["wrb"], writes=[prk])
        pg.op("dve", lambda e, t=t, pr=pr: e.tensor_tensor(out=lg[:, t, :], in0=pr[:, 0:36], in1=rp[:, RP_BR:RP_BR + 36],
                                                          op=OP.add), reads=[prk, "rp"], writes=[("lg", t)])
    lgk = [("lg", t) for t in range(8)]
    lgG = lg[:, :, 0:4]
    bc = lambda ap, n: ap.unsqueeze(2).to_broadcast([128, 8, n])
    R_ = "rt"
    pg.op("dve", lambda e: e.tensor_reduce(out=mg[:], in_=lgG, axis=AX.X, op=OP.max), reads=lgk, writes=[R_])
    pg.op("dve", lambda e: e.tensor_tensor(out=eg[:], in0=lgG, in1=bc(mg[:], 4), op=OP.subtract), reads=lgk + [R_], writes=[R_])
    pg.op("act", lambda e: e.activation(out=eg[:], in_=eg[:], func=AF.Exp), reads=[R_], writes=[R_])
    pg.op("dve", lambda e: e.tensor_reduce(out=sgm[:], in_=eg[:], axis=AX.X, op=OP.add), reads=[R_], writes=[R_])
    pg.op("dve", lambda e: e.reciprocal(out=tpg[:], in_=sgm[:]), reads=[R_], writes=[R_])
    pg.op("dve", lambda e: e.tensor_tensor(out=ohg[:], in0=lgG, in1=bc(mg[:], 4), op=OP.is_equal), reads=lgk + [R_], writes=[R_])
    for g in range(4):
        lgE = lg[:, :, 4 + g * 8:4 + (g + 1) * 8]
        dst = selx if g == 0 else tmp8
        pg.op("dve", lambda e, g=g, lgE=lgE, dst=dst: e.tensor_tensor(
            out=dst[:], in0=lgE, in1=ohg[:, :, g:g + 1].to_broadcast([128, 8, 8]), op=OP.mult), reads=lgk + [R_], writes=[R_])
        if g > 0:
            pg.op("dve", lambda e: e.tensor_tensor(out=selx[:], in0=selx[:], in1=tmp8[:], op=OP.add), reads=[R_], writes=[R_])
    pg.op("dve", lambda e: e.tensor_reduce(out=m1[:], in_=selx[:], axis=AX.X, op=OP.max), reads=[R_], writes=[R_])
    pg.op("dve", lambda e: e.tensor_tensor(out=mask1[:], in0=selx[:], in1=bc(m1[:], 8), op=OP.is_equal), reads=[R_], writes=[R_])
    pg.op("dve", lambda e: e.tensor_scalar_mul(out=sel2[:], in0=mask1[:], scalar1=-1.0e30), reads=[R_], writes=[R_])
    pg.op("dve", lambda e: e.tensor_tensor(out=sel2[:], in0=sel2[:], in1=selx[:], op=OP.add), reads=[R_], writes=[R_])
    pg.op("dve", lambda e: e.tensor_reduce(out=m2[:], in_=sel2[:], axis=AX.X, op=OP.max), reads=[R_], writes=[R_])
    pg.op("dve", lambda e: e.tensor_tensor(out=mask2[:], in0=sel2[:], in1=bc(m2[:], 8), op=OP.is_equal), reads=[R_], writes=[R_])
    pg.op("dve", lambda e: e.tensor_tensor(out=p2[:], in0=m2[:], in1=m1[:], op=OP.subtract), reads=[R_], writes=[R_])
    pg.op("act", lambda e: e.activation(out=p2[:], in_=p2[:], func=AF.Exp), reads=[R_], writes=[R_])
    pg.op("dve", lambda e: e.tensor_scalar_add(out=p1[:], in0=p2[:], scalar1=1.0), reads=[R_], writes=[R_])
    pg.op("dve", lambda e: e.reciprocal(out=p1[:], in_=p1[:]), reads=[R_], writes=[R_])
    pg.op("dve", lambda e: e.tensor_tensor(out=p2[:], in0=p2[:], in1=p1[:], op=OP.mult), reads=[R_], writes=[R_])
    pg.op("dve", lambda e: e.tensor_tensor(out=p1[:], in0=p1[:], in1=tpg[:], op=OP.mult), reads=[R_], writes=[R_])
    pg.op("dve", lambda e: e.tensor_tensor(out=p2[:], in0=p2[:], in1=tpg[:], op=OP.mult), reads=[R_], writes=[R_])
    pg.op("dve", lambda e: e.tensor_tensor(out=wex[:], in0=mask1[:], in1=bc(p1[:], 8), op=OP.mult), reads=[R_], writes=[R_])
    pg.op("dve", lambda e: e.tensor_tensor(out=tmp8[:], in0=mask2[:], in1=bc(p2[:], 8), op=OP.mult), reads=[R_], writes=[R_])
    pg.op("dve", lambda e: e.tensor_tensor(out=wex[:], in0=wex[:], in1=tmp8[:], op=OP.add), reads=[R_], writes=[R_])
    for r in range(3):
        for g in range(4):
            pg.op("dve", lambda e, r=r, g=g: e.tensor_tensor(
                out=comb3[:, :, r, g * 8:(g + 1) * 8], in0=wex[:], in1=ohg[:, :, g:g + 1].to_broadcast([128, 8, 8]),
                op=OP.mult), reads=[R_], writes=[R_, ("comb3", r, g)])
    if "t_comb" in taps:
        t_comb = nc.dram_tensor("t_comb", [128, 8 * 96], F32, kind="ExternalOutput").ap()
        dma_sp(t_comb, comb3[:].rearrange("p a b c -> p (a b c)"), "st_tap6", reads=[R_])
    if "t_h2T" in taps:
        t_h2T = nc.dram_tensor("t_h2T", [128, 16 * 1024], BF16, kind="ExternalOutput").ap()
        dma_sp(t_h2T, h2T[:].rearrange("p a b -> p (a b)"), "st_tap7", reads=h2k)
    for t in range(8):
        pc_, pck = banks[4 + t // 4], bkk[4 + t // 4]
        pg.op("pe", lambda e, t=t, pc_=pc_: e.transpose(out=pc_[0:96, (t % 4) * 128:(t % 4 + 1) * 128],
                                                        in_=comb3[:, t, :, :].rearrange("p r c -> p (r c)"), identity=ident),
              reads=[R_, "cst"], writes=[(pck, t % 4)])
    for hb in range(2):
        src_ = banks[4 + hb]
        sk = [(bkk[4 + hb], j) for j in range(4)]
        sl = slice(hb * 512, (hb + 1) * 512)
        pg.op("act", lambda e, src_=src_, sl=sl: e.copy(out=cpc[0:32, sl], in_=src_[0:32, :]), reads=sk, writes=[("cpc", 0, hb)])
        for lo in (32, 64):
            pg.op("act", lambda e, src_=src_, sl=sl, lo=lo: e.copy(out=ctb[lo:lo + 32, sl], in_=src_[lo:lo + 32, :]),
                  reads=sk, writes=[("ctb", lo, hb)])
            pg.op("dve", lambda e, src_=src_, sl=sl, lo=lo: e.tensor_tensor(
                out=cR[lo:lo + 32, sl], in0=src_[lo:lo + 32, :], in1=ctb[lo:lo + 32, sl], op=OP.subtract),
                reads=sk + [("ctb", lo, hb)], writes=[("cR", lo, hb)])
        pg.op("act", lambda e, sl=sl: e.copy(out=cpc[32:64, sl], in_=cR[32:64, sl]), reads=[("cR", 32, hb)],
              writes=[("cpc", 1, hb)])
        pg.op("act", lambda e, sl=sl: e.copy(out=ctb[64:96, sl], in_=cR[64:96, sl]), reads=[("cR", 64, hb)],
              writes=[("ctb", 64, hb)])
        pg.op("dve", lambda e, sl=sl: e.tensor_tensor(out=cR2[64:96, sl], in0=cR[64:96, sl], in1=ctb[64:96, sl],
                                                      op=OP.subtract), reads=[("cR", 64, hb), ("ctb", 64, hb)],
              writes=[("cR2", hb)])
        pg.op("act", lambda e, sl=sl: e.copy(out=cpc[64:96, sl], in_=cR2[64:96, sl]), reads=[("cR2", hb)],
              writes=[("cpc", 2, hb)])
    pg.barrier()
    pg.flush()
    st.close()
    if stage <= 6:
        st6.close()
        st5.close()
        return finish(nc, pg, es, out)


    st = ExitStack()
    def sb7(name, shape, dt=F32):
        return st.enter_context(nc.sbuf_tensor(name, list(shape), dt))
    wgh = [sb7("wgh%d" % i, [128, 16, 256], BF16) for i in range(2)]
    wuh = [sb7("wuh%d" % i, [128, 16, 256], BF16) for i in range(2)]
    wdn = sb7("wdn", [128, 4, 2048], BF16)
    hid = sb7("hid", [128, 4, 1024], BF16)
    cbc = sb7("cbc", [128, 2, 512])
    sgs = [sb7("sgs%d" % i, [128, 512]) for i in range(2)]
    tus = [sb7("tus%d" % i, [128, 512]) for i in range(2)]
    tmo = [sb7("tmo%d" % i, [128, 512]) for i in range(3)]
    cpk = [("cpc", r, hb) for r in range(3) for hb in range(2)]
    cnt6 = [0]

    def do_expert(ex):
        wg_v = w_g[ex].rearrange("(kc p) f -> p kc f", p=128)
        wu_v = w_u[ex].rearrange("(kc p) f -> p kc f", p=128)
        wd_v = w_d[ex].rearrange("(fc p) d -> p fc d", p=128)
        for tb in range(2):
            pg.op("pe", lambda e, tb=tb: e.matmul(banks[6][:], lhsT=sel3b[0:96, ex * 128:(ex + 1) * 128],
                                                  rhs=cpc[0:96, tb * 512:(tb + 1) * 512], start=True, stop=True),
                  reads=cpk + ["sel3b"], writes=[bkk[6]])
            pg.op("act", lambda e, tb=tb: e.copy(out=cbc[:, tb, :], in_=banks[6][:]), reads=[bkk[6]], writes=[("cbc", tb)])
        for half in range(2):
            wg_, wu_ = wgh[half], wuh[half]
            dma_cast(wg_[:], wg_v[:, :, half * 256:(half + 1) * 256], "ld_wgh%d" % half, writes=["wgh%d" % half])
            dma_cast(wu_[:], wu_v[:, :, half * 256:(half + 1) * 256], "ld_wuh%d" % half, writes=["wuh%d" % half])
            for fcl in range(2):
                fc = half * 2 + fcl
                for tb in range(2):
                    i2 = cnt6[0] % 2
                    cnt6[0] += 1
                    pgt, pgk_ = banks[i2], bkk[i2]
                    pup, puk = banks[2 + i2], bkk[2 + i2]
                    def mmg(e, wg_=wg_, fcl=fcl, tb=tb, pgt=pgt):
                        ins = None
                        for kc in range(16):
                            ins = e.matmul(pgt[:], lhsT=wg_[:, kc, fcl * 128:(fcl + 1) * 128],
                                           rhs=h2T[:, kc, tb * 512:(tb + 1) * 512], start=(kc == 0), stop=(kc == 15))
                        return ins
                    pg.op("pe", mmg, reads=["wgh%d" % half] + h2k, writes=[pgk_])
                    def mmu(e, wu_=wu_, fcl=fcl, tb=tb, pup=pup):
                        ins = None
                        for kc in range(16):
                            ins = e.matmul(pup[:], lhsT=wu_[:, kc, fcl * 128:(fcl + 1) * 128],
                                           rhs=h2T[:, kc, tb * 512:(tb + 1) * 512], start=(kc == 0), stop=(kc == 15))
                        return ins
                    pg.op("pe", mmu, reads=["wuh%d" % half] + h2k, writes=[puk])
                    sg_, sgk = sgs[i2], "sgs%d" % i2
                    tu_, tuk = tus[i2], "tus%d" % i2
                    pg.op("act", lambda e, sg_=sg_, pgt=pgt: e.activation(out=sg_[:], in_=pgt[:], func=AF.Silu),
                          reads=[pgk_], writes=[sgk])
                    pg.op("dve", lambda e, tu_=tu_, pup=pup, sg_=sg_: e.tensor_tensor(out=tu_[:], in0=pup[:], in1=sg_[:],
                                                                                     op=OP.mult),
                          reads=[puk, sgk], writes=[tuk])
                    pg.op("dve", lambda e, tu_=tu_, fc=fc, tb=tb: e.tensor_tensor(
                        out=hid[:, fc, tb * 512:(tb + 1) * 512], in0=tu_[:], in1=cbc[:, tb, :], op=OP.mult),
                        reads=[tuk, ("cbc", tb)], writes=[("hid", fc, tb)])
        dma_cast(wdn[:], wd_v, "ld_wdn", writes=["wdn"])
        for dc in range(16):
            for tb in range(2):
                i2 = cnt6[0] % 3
                cnt6[0] += 1
                pbi = (4, 5, 7)[i2]
                po, pok = banks[pbi], bkk[pbi]
                def mmd(e, dc=dc, tb=tb, po=po):
                    ins = None
                    for fc in range(4):
                        ins = e.matmul(po[:], lhsT=wdn[:, fc, dc * 128:(dc + 1) * 128], rhs=hid[:, fc, tb * 512:(tb + 1) * 512],
                                       start=(fc == 0), stop=(fc == 3))
                    return ins
                pg.op("pe", mmd, reads=["wdn"] + [("hid", fc, tb) for fc in range(4)], writes=[pok])
                tm_, tmk = tmo[i2], "tmo%d" % i2
                pg.op("act", lambda e, tm_=tm_, po=po, dc=dc: e.activation(out=tm_[:], in_=po[:], func=AF.Copy,
                                                                           scale=G12[:, 1, dc:dc + 1]),
                      reads=[pok, "G12b"], writes=[tmk])
                pg.op("dve", lambda e, tm_=tm_, dc=dc, tb=tb: e.tensor_tensor(
                    out=x1T[:, dc, tb * 512:(tb + 1) * 512], in0=x1T[:, dc, tb * 512:(tb + 1) * 512], in1=tm_[:], op=OP.add),
                    reads=[("x1T", dc, tb), tmk], writes=[("x1T", dc, tb)])
    for ex in range(NEXP):
        do_expert(ex)
    pg.barrier()
    pg.flush()
    st.close()
    st6.close()

    st = ExitStack()
    nfr = st.enter_context(nc.sbuf_tensor("nfr", [128, 2048], F32))
    xo = [st.enter_context(nc.sbuf_tensor("xo%d" % i, [128, 2048], F32)) for i in range(2)]
    junk = st.enter_context(nc.sbuf_tensor("junk", [128, 2048], BF16))
    ss7 = st.enter_context(nc.sbuf_tensor("ss7", [128, 16], F32))
    dma_sp(nfr[:], lnrows[:, 2 * D:3 * D], "ld_nfr", writes=["nfr"])
    pg.op("dve", lambda e: e.memset(ss7[:], 0.0), writes=["ss7"])
    for t in range(8):
        xo_, xok = xo[t % 2], "xo%d" % (t % 2)
        for q in range(4):
            pf, pfk = banks[q % 2], bkk[q % 2]
            def trf(e, t=t, q=q, pf=pf):
                ins = None
                for j in range(4):
                    dc = q * 4 + j
                    ins = e.transpose(out=pf[:, j * 128:(j + 1) * 128], in_=x1T[:, dc, t * 128:(t + 1) * 128], identity=ident)
                return ins
            pg.op("pe", trf, reads=[("x1T", q * 4 + j, t // 4) for j in range(4)] + ["cst"], writes=[pfk])
            pg.op("act", lambda e, xo_=xo_, q=q, pf=pf: e.copy(out=xo_[:, q * 512:(q + 1) * 512], in_=pf[:]),
                  reads=[pfk], writes=[(xok, q)])
        xk = [(xok, q) for q in range(4)]
        pg.op("act", lambda e, xo_=xo_, t=t: e.activation(out=junk[:], in_=xo_[:], func=AF.Square, accum_out=ss7[:, t:t + 1]),
              reads=xk + ["ss7"], writes=["junk", ("ss7", t)])
        pg.op("dve", lambda e, t=t: e.tensor_scalar(out=ss7[:, 8 + t:9 + t], in0=ss7[:, t:t + 1], scalar1=1.0 / D, scalar2=EPS,
                                                    op0=OP.mult, op1=OP.add), reads=[("ss7", t)], writes=[("rs7", t)])
        pg.op("act", lambda e, t=t: e.sqrt(out=ss7[:, 8 + t:9 + t], in_=ss7[:, 8 + t:9 + t]), reads=[("rs7", t)], writes=[("rs7", t)])
        pg.op("dve", lambda e, t=t: e.reciprocal(out=ss7[:, 8 + t:9 + t], in_=ss7[:, 8 + t:9 + t]), reads=[("rs7", t)],
              writes=[("rs7", t)])
        pg.op("dve", lambda e, xo_=xo_, t=t: e.scalar_tensor_tensor(out=xo_[:], in0=xo_[:], scalar=ss7[:, 8 + t:9 + t],
                                                                    in1=nfr[:], op0=OP.mult, op1=OP.mult),
              reads=xk + [("rs7", t), "nfr"], writes=xk)
        dma_sp(out[t * 128:(t + 1) * 128, :], xo_[:], "st_out%d" % (t % 2), reads=xk)
    pg.barrier()
    pg.flush()
    st.close()
    st5.close()
    return finish(nc, pg, es, out)


def finish(nc, pg, es, out):
    es.close()
    return nc


def _consts():
    c = np.zeros((128, C_N), np.float32)
    i = np.arange(128)
    c[:, C_ID:C_ID + 128] = np.eye(128, dtype=np.float32)
    c[:, C_TU:C_TU + 128] = (i[:, None] <= i[None, :])
    c[:, C_TL:C_TL + 128] = (i[:, None] >= i[None, :])
    c[:, C_MNF:C_MNF + 128] = np.where(i[:, None] <= i[None, :], 0.0, -30000.0)
    c[:, C_MNB:C_MNB + 128] = np.where(i[:, None] >= i[None, :], 0.0, -30000.0)
    c[:, C_ONE:C_ONE + 128] = 1.0
    s3 = np.zeros((128, 32, 128), np.float32)
    for p in range(96):
        s3[p, p % 32, :] = 1.0
    return c, s3.reshape(128, 4096)


def _pp(v):
    return np.ascontiguousarray(np.asarray(v, np.float32).reshape(-1, 128).T)


def prep_inputs(inp):
    f = lambda a: np.ascontiguousarray(np.asarray(a, np.float32))
    x, c, ctx, c_ctx = f(inp["x"]), f(inp["c"]), f(inp["ctx"]), f(inp["c_ctx"])
    cst, s3 = _consts()
    conv_w = f(inp["conv_w"])[0]
    pvec = np.zeros((128, PV_N), np.float32)
    pvec[:, PV_N1G:PV_N1G + 16] = _pp(inp["norm1_g"][0])
    pvec[:, PV_N2G:PV_N2G + 16] = _pp(inp["norm2_g"][0])
    pvec[:, PV_SNG:PV_SNG + 16] = _pp(inp["ssd_norm_g"][0])
    for j in range(3):
        pvec[:, PV_CW + j * 24:PV_CW + (j + 1) * 24] = _pp(conv_w[j])
    pvec[:, PV_CB:PV_CB + 24] = _pp(inp["conv_b"][0])
    pvec[:, PV_BMOD:PV_BMOD + 96] = _pp(inp["b_mod"][0])
    row = np.zeros((RP_N,), np.float32)
    row[RP_DTB:RP_DTB + 32] = f(inp["dt_bias_f"])[0]
    row[RP_DTB + 32:RP_DTB + 64] = f(inp["dt_bias_b"])[0]
    row[RP_ALOG:RP_ALOG + 32] = f(inp["a_log_f"])[0]
    row[RP_ALOG + 32:RP_ALOG + 64] = f(inp["a_log_b"])[0]
    row[RP_DSKIP:RP_DSKIP + 32] = f(inp["d_skip"])[0]
    row[RP_BS:RP_BS + 1024] = f(inp["b_spatial"])[0].reshape(-1)
    row[RP_BR:RP_BR + 4] = f(inp["b_router_group"])[0]
    row[RP_BR + 4:RP_BR + 36] = f(inp["b_router_expert"])[0].reshape(-1)
    rowp = np.ascontiguousarray(np.broadcast_to(row[None, :], (128, RP_N)))
    lnr = np.concatenate([f(inp["cm_ln_g"])[0], f(inp["cm_ln_b"])[0], f(inp["normf_g"])])
    lnrows = np.ascontiguousarray(np.broadcast_to(lnr[None, :], (128, 3 * D)))
    w_mod = f(inp["w_mod"])[0]
    w_in = f(inp["w_in"])[0]
    w_out = f(inp["w_out"])[0]
    wsT = np.ascontiguousarray(np.transpose(f(inp["w_spatial"])[0], (2, 0, 1)).reshape(128, 1024))
    wrg = f(inp["w_router_group"])[0]
    wre = np.transpose(f(inp["w_router_expert"])[0], (1, 0, 2)).reshape(D, 32)
    wrc = np.concatenate([wrg, wre], axis=1)
    wr = np.ascontiguousarray(wrc.reshape(16, 128, 36).transpose(1, 0, 2).reshape(128, 16 * 36))
    w_g = f(inp["w_exp_gate"])[0].reshape(32, D, 512)
    w_u = f(inp["w_exp_up"])[0].reshape(32, D, 512)
    w_d = f(inp["w_exp_down"])[0].reshape(32, 512, D)
    maps = []
    for k in range(NCORES):
        b, s = k // 2, k % 2
        own = x[b, s * 1024:(s + 1) * 1024]
        oth = x[b, (1 - s) * 1024:(2 - s) * 1024]
        x_all = np.concatenate([ctx[b], oth, own], axis=0)
        fl = np.zeros((128, 2), np.float32)
        fl[:, 0] = 1.0 if s == 1 else 0.0
        fl[:, 1] = 1.0 if s == 0 else 0.0
        cvec = np.stack([_pp(c[b]), _pp(c_ctx)], axis=2).reshape(128, 32)
        maps.append(dict(x_all=x_all, flags=fl, cvec=np.ascontiguousarray(cvec), pvec=pvec, rowp=rowp,
                         lnrows=lnrows, consts=cst, sel3=s3, w_mod=w_mod, w_in=w_in, w_out=w_out,
                         wsT=wsT, wr=wr, w_g=w_g, w_u=w_u, w_d=w_d))
    return maps


def kernel(**inputs):
    maps = prep_inputs(inputs)
    nc = build_nc()
    res = run_bass_kernel_spmd(nc, maps, core_ids=list(range(NCORES)))
    outf = np.zeros((4, 2048, D), np.float32)
    for k in range(NCORES):
        b, s = k // 2, k % 2
        outf[b, s * 1024:(s + 1) * 1024] = res.results[k]["out"]
    return outf
```

```python
from contextlib import ExitStack
import numpy as np
import concourse.bass as bass
import concourse.mybir as mybir
from concourse.bass_utils import run_bass_kernel_spmd

F32 = mybir.dt.float32
BF16 = mybir.dt.bfloat16
AF = mybir.ActivationFunctionType
OP = mybir.AluOpType
AX = mybir.AxisListType

D = 2048
NCORES = 8
EPS = 1e-6
ENGS = ("pe", "act", "dve", "pool", "sp")
SUB = 99
JOBS = None
NOTR = False
NCH = 8
NEXP = 32
NOCONV = False

PV_N1G, PV_N2G, PV_SNG, PV_CW, PV_CB, PV_BMOD = 0, 16, 32, 48, 120, 144
PV_N = 240
RP_DTB, RP_ALOG, RP_DSKIP, RP_BS, RP_BR = 0, 64, 128, 160, 1184
RP_N = 1220
C_ID, C_TU, C_TL, C_MNF, C_MNB, C_ONE = 0, 128, 256, 384, 512, 640
C_N = 768


class Prog:
    def __init__(self, nc, es):
        self.nc = nc
        self.es = es
        self.sems = {}
        self.cnt = {}
        self.ops = {e: [] for e in ENGS}
        self.known = {e: {} for e in ENGS}
        self.last_w = {}
        self.readers = {}
        self.latest = {}
        for e in ENGS:
            self._sem(e)

    def _sem(self, key):
        if key not in self.sems:
            self.sems[key] = self.es.enter_context(self.nc.semaphore("s_" + str(key)))
            self.cnt[key] = 0
        return self.sems[key]

    def op(self, eng, fn, reads=(), writes=(), dma=None):
        waits = {}
        def need(tok):
            sk, v = tok
            if sk == "pe" and eng == "pe":
                return
            if self.known[eng].get(sk, 0) >= v:
                return
            waits[sk] = max(waits.get(sk, 0), v)
        for k in reads:
            if k in self.last_w:
                need(self.last_w[k])
        for k in writes:
            if k in self.last_w:
                need(self.last_w[k])
            for r in self.readers.get(k, ()):
                need(r)
        for sk, v in waits.items():
            self.known[eng][sk] = v
        if dma is not None:
            self._sem(dma)
            self.cnt[dma] += 16
            tok = (dma, self.cnt[dma])
        else:
            self.cnt[eng] += 1
            tok = (eng, self.cnt[eng])
        self.latest[tok[0]] = tok[1]
        for k in writes:
            self.last_w[k] = tok
            self.readers[k] = []
        for k in reads:
            self.readers.setdefault(k, []).append(tok)
        self.ops[eng].append((list(waits.items()), fn, tok, dma is not None))
        return tok

    def barrier(self):
        for e in ENGS:
            waits = []
            for sk, v in self.latest.items():
                if sk == e and e == "pe":
                    continue
                if self.known[e].get(sk, 0) < v:
                    waits.append((sk, v))
                    self.known[e][sk] = v
            if waits:
                self.ops[e].append((waits, None, None, False))
        self.last_w.clear()
        self.readers.clear()

    def flush(self):
        nc = self.nc
        ops = self.ops
        sems = self.sems

        def run(engh, lst):
            for waits, fn, tok, isdma in lst:
                for sk, v in waits:
                    engh.wait_ge(sems[sk], v)
                if fn is None:
                    continue
                ins = fn(engh)
                ins.then_inc(sems[tok[0]], 16 if isdma else 1)

        with nc.Block() as block:
            if ops["sp"]:
                @block.sync
                def _(e):
                    run(e, ops["sp"])
            if ops["pe"]:
                @block.tensor
                def _(e):
                    run(e, ops["pe"])
            if ops["act"]:
                @block.scalar
                def _(e):
                    run(e, ops["act"])
            if ops["dve"]:
                @block.vector
                def _(e):
                    run(e, ops["dve"])
            if ops["pool"]:
                @block.gpsimd
                def _(e):
                    run(e, ops["pool"])
        self.ops = {e: [] for e in ENGS}


def build_nc(stage=99, taps=()):
    nc = bass.Bass("TRN2", target_bir_lowering=False)
    es = ExitStack()
    pg = Prog(nc, es)

    def din(name, shape, dt=F32):
        return nc.dram_tensor(name, list(shape), dt, kind="ExternalInput").ap()

    def dscr(name, shape, dt):
        kind = "ExternalOutput" if name in taps else "Internal"
        return nc.dram_tensor(name, list(shape), dt, kind=kind).ap()

    x_all = din("x_all", [2304, D])
    flags = din("flags", [128, 2])
    cvec = din("cvec", [128, 32])
    pvec = din("pvec", [128, PV_N])
    rowp = din("rowp", [128, RP_N])
    lnrows = din("lnrows", [128, 3 * D])
    consts = din("consts", [128, C_N])
    sel3 = din("sel3", [128, 4096])
    w_mod = din("w_mod", [D, 6 * D])
    w_in = din("w_in", [D, 9280])
    w_out = din("w_out", [2 * D, D])
    wsT = din("wsT", [128, 1024])
    wr = din("wr", [128, 16 * 36])
    w_g = din("w_g", [32, D, 512])
    w_u = din("w_u", [32, D, 512])
    w_d = din("w_d", [32, 512, D])
    out = nc.dram_tensor("out", [1024, D], F32, kind="ExternalOutput").ap()

    def sb(name, shape, dt=F32):
        return es.enter_context(nc.sbuf_tensor(name, list(shape), dt))

    def ps(name, shape, dt=F32):
        return es.enter_context(nc.psum_tensor(name, list(shape), dt))

    cst = sb("cst", [128, C_N])
    idb = sb("idb", [128, 128], BF16)
    pv = sb("pv", [128, PV_N])
    rp = sb("rp", [128, RP_N])
    flg = sb("flg", [128, 2])
    cv = sb("cv", [128, 32])
    scT = sb("scT", [128, 32], BF16)
    modT = sb("modT", [128, 192])
    AB = sb("AB", [128, 6, 16])
    G12 = sb("G12", [128, 2, 16])
    dtall = sb("dtall", [128, 18, 64])
    aneg = sb("aneg", [128, 64])
    decall = sb("decall", [128, 18, 64])

    ident = cst[:, C_ID:C_ID + 128]
    ones = cst[:, C_ONE:C_ONE + 128]

    def dma_sp(out_ap, in_ap, sem, reads=(), writes=()):
        pg.op("sp", lambda e: e.dma_start(out=out_ap, in_=in_ap), reads=reads, writes=writes, dma=sem)

    def dma_cast(out_ap, in_ap, sem, reads=(), writes=()):
        pg.op("pool", lambda e: e.dma_start(out=out_ap, in_=in_ap), reads=reads, writes=writes, dma=sem)

    dma_sp(cst[:], consts, "ld_c0", writes=["cst"])
    dma_sp(pv[:], pvec, "ld_c1", writes=["pv"])
    dma_sp(rp[:], rowp, "ld_c3", writes=["rp"])
    dma_sp(flg[:], flags, "ld_c4", writes=["flg"])
    dma_sp(cv[:], cvec, "ld_c5", writes=["cv"])
    pg.op("dve", lambda e: e.tensor_copy(out=idb[:], in_=ident), reads=["cst"], writes=["idb"])
    pg.op("act", lambda e: e.activation(out=scT[:], in_=cv[:], func=AF.Silu), reads=["cv"], writes=["scT"])
    pg.op("act", lambda e: e.activation(out=aneg[:], in_=rp[:, RP_ALOG:RP_ALOG + 64], func=AF.Exp),
          reads=["rp"], writes=["aneg"])
    pg.op("dve", lambda e: e.tensor_scalar_mul(out=aneg[:], in0=aneg[:], scalar1=-1.0),
          reads=["aneg"], writes=["aneg"])

    st = ExitStack()
    wb = [st.enter_context(nc.sbuf_tensor("p0w%d" % i, [128, 16, 512], BF16)) for i in range(2)]
    modps = st.enter_context(nc.psum_tensor("modps", [128, 192], F32))
    w_mod_v = w_mod.rearrange("(kc p) c -> p kc c", p=128)
    mps_cur = [modps]
    mps_off = [0]
    def mod_block(blk, wb, part=3):
        w = wb[blk % 2]
        key = "p0w%d" % (blk % 2)
        if part & 1:
            dma_cast(w[:], w_mod_v[:, :, blk * 512:(blk + 1) * 512], "ld_" + key, writes=[key])
        if not (part & 2):
            return

        def mm(e, w=w, blk=blk):
            ins = None
            for cc in range(4):
                col = (blk * 4 + cc) * 2 - mps_off[0]
                for kc in range(16):
                    ins = e.matmul(mps_cur[0][:, col:col + 2], lhsT=w[:, kc, cc * 128:(cc + 1) * 128],
                                   rhs=scT[:, kc * 2:kc * 2 + 2], start=(kc == 0), stop=(kc == 15))
            return ins
        pg.op("pe", mm, reads=[key, "scT"], writes=["modps"])

    def mod_finish(c0, c1, modps, base=0):
        pg.op("dve", lambda e: e.tensor_tensor(
            out=modT[:, c0 * 2:c1 * 2].rearrange("p (c t) -> p c t", t=2),
            in0=modps[:, (c0 - base) * 2:(c1 - base) * 2].rearrange("p (c t) -> p c t", t=2),
            in1=pv[:, PV_BMOD + c0:PV_BMOD + c1].unsqueeze(2).to_broadcast([128, c1 - c0, 2]), op=OP.add),
            reads=["modps", "pv"], writes=["modT"])
    for blk in range(8):
        mod_block(blk, wb)
    mod_finish(0, 32, modps)
    mt = modT[:].rearrange("p (m kc t) -> p m kc t", m=6, kc=16, t=2)
    def ab(e_idx, g_off, sc_m, sh_m, which):
        pg.op("dve", lambda e: e.scalar_tensor_tensor(
            out=AB[:, e_idx, :], in0=mt[:, sc_m, :, which], scalar=1.0, in1=pv[:, g_off:g_off + 16],
            op0=OP.add, op1=OP.mult), reads=["modT", "pv"], writes=["AB%d" % e_idx])
        pg.op("dve", lambda e: e.tensor_copy(out=AB[:, e_idx + 1, :], in_=mt[:, sh_m, :, which]),
              reads=["modT"], writes=["AB%d" % (e_idx + 1)])
    ab(0, PV_N1G, 1, 0, 0)
    ab(2, PV_N1G, 1, 0, 1)
    if "t_modT" in taps:
        t_modT = nc.dram_tensor("t_modT", [128, 192], F32, kind="ExternalOutput").ap()
        dma_sp(t_modT, modT[:], "st_tap", reads=["modT"])
    pg.barrier()
    pg.flush()
    st.close()
    if stage <= 0:
        return finish(nc, pg, es, out)


    S_all = dscr("S_all", [18, 2, 128, 2048], F32)
    xs_s = dscr("xs_s", [8, 128, 2048], BF16)
    bt_s = dscr("bt_s", [8, 128, 4, 128], BF16)
    ct_s = dscr("ct_s", [8, 128, 4, 128], BF16)
    z_s = dscr("z_s", [8, 128, 2048], BF16)
    v_s = dscr("v_s", [8, 128, 2048], BF16)
    u_s = dscr("u_s", [8, 128, 16, 128], BF16)
    hp_s = dscr("hp_s", [2, 8, 128, 2048], BF16)
    mix_s = dscr("mix_s", [8, 128, 32, 128], BF16)

    st = ExitStack()
    def sb1(name, shape, dt=F32):
        return st.enter_context(nc.sbuf_tensor(name, list(shape), dt))
    def ps1(name, shape, dt=F32):
        return st.enter_context(nc.psum_tensor(name, list(shape), dt))
    xin = sb1("xin", [128, 4, 2048])
    hT = sb1("hT", [128, 16, 512], BF16)
    wb = [sb1("wb%d" % i, [128, 16, 512], BF16) for i in range(2)]
    t0s = [sb1("t0_%d" % i, [128, 512]) for i in range(2)]
    sbf = [sb1("sbf%d" % i, [128, 512], BF16) for i in range(2)]
    sraws = [sb1("sraw%d" % i, [128, 512]) for i in range(2)]
    xs_tok = sb1("xs_tok", [128, 4, 2048], BF16)
    B_tok = sb1("B_tok", [128, 4, 512], BF16)
    BTs = sb1("BTs", [128, 4, 512], BF16)
    CTs = sb1("CTs", [128, 4, 512], BF16)
    stg = sb1("stg", [128, 4, 2048], BF16)
    lnr = sb1("lnr", [128, 2, 2048])
    xw = [sb1("xw%d" % i, [128, 2048], BF16) for i in range(2)]
    vout = [sb1("vout%d" % i, [128, 2048], BF16) for i in range(2)]
    Sst = [sb1("Sst%d" % i, [128, 2048]) for i in range(2)]
    sm = sb1("sm", [128, 16, 64])
    ssq = sb1("ssq", [128, 16])
    wgt_t = sb1("wgt_t", [128, 4, 64])
    pA = [ps1("pA%d" % i, [128, 512]) for i in range(2)]
    pTf = [ps1("pTf%d" % i, [128, 512]) for i in range(2)]
    pS = [ps1("pS%d" % i, [128, 512]) for i in range(2)]
    pm = ps1("pm", [128, 512])
    stg_u = stg[:].rearrange("p t c -> p (t c)").rearrange("p (cc n) -> p cc n", cc=16)

    w_in_v = w_in.rearrange("(kc p) c -> p kc c", p=128)
    dma_sp(lnr[:].rearrange("p a c -> p (a c)"), lnrows[:, 0:2 * D], "ld_lnr", writes=["lnr"])
    wcnt = [0]
    acnt = [0]

    def load_w(c0, ncols):
        i = wcnt[0] % 2
        wcnt[0] += 1
        dma_cast(wb[i][:, :, 0:ncols], w_in_v[:, :, c0:c0 + ncols], "ld_wb%d" % i, writes=["wb%d" % i])
        return wb[i], "wb%d" % i

    def next_pA():
        i = acnt[0] % 2
        acnt[0] += 1
        return pA[i], "pA%d" % i, i

    blocks = [("ctx", 0, 2, 0), ("oth", 256, 4, 2), ("oth", 768, 4, 6), ("own", 1280, 4, 10), ("own", 1792, 4, 14)]
    if stage == 1:
        blocks = blocks[:1] + blocks[3:4]
    if SUB in (2, 3):
        blocks = blocks[:1]
    def do_block(kind, row0, NT, T0, prev_units):
        N = NT * 128
        own = kind == "own"
        Aidx = 2 if kind == "ctx" else 0
        pg.op("dve", lambda e: e.memset(ssq[:], 0.0), writes=["ssq"])
        for t in range(NT):
            dma_sp(xin[:, t, :], x_all[row0 + t * 128:row0 + (t + 1) * 128, :], "ld_xin%d" % t, writes=[("xin", t)])
            pg.op("act", lambda e, t=t: e.activation(out=stg[:, t, :], in_=xin[:, t, :], func=AF.Square,
                                                     accum_out=ssq[:, t:t + 1]),
                  reads=[("xin", t), "ssq"], writes=["stg", ("ssq", t)])
            pg.op("dve", lambda e, t=t: e.tensor_scalar(out=ssq[:, 8 + t:9 + t], in0=ssq[:, t:t + 1], scalar1=1.0 / D,
                                                        scalar2=EPS, op0=OP.mult, op1=OP.add),
                  reads=[("ssq", t)], writes=[("rs", t)])
            pg.op("act", lambda e, t=t: e.sqrt(out=ssq[:, 8 + t:9 + t], in_=ssq[:, 8 + t:9 + t]),
                  reads=[("rs", t)], writes=[("rs", t)])
            pg.op("dve", lambda e, t=t: e.reciprocal(out=ssq[:, 8 + t:9 + t], in_=ssq[:, 8 + t:9 + t]),
                  reads=[("rs", t)], writes=[("rs", t)])
            pg.op("act", lambda e, t=t: e.activation(out=xin[:, t, :], in_=xin[:, t, :], func=AF.Copy,
                                                     scale=ssq[:, 8 + t:9 + t]),
                  reads=[("xin", t), ("rs", t)], writes=[("xin", t)])
        if SUB <= 0:
            return
        for kc in range(16):
            pa, pak, _ = next_pA()
            def tr(e, pa=pa, kc=kc):
                ins = None
                for t in range(NT):
                    ins = e.transpose(out=pa[:, t * 128:(t + 1) * 128], in_=xin[:, t, kc * 128:(kc + 1) * 128],
                                      identity=ident)
                return ins
            pg.op("pe", tr, reads=[("xin", t) for t in range(NT)] + ["cst"], writes=[pak])
            pg.op("act", lambda e, pa=pa, kc=kc: e.activation(
                out=hT[:, kc, 0:N], in_=pa[:, 0:N], func=AF.Identity,
                bias=AB[:, Aidx + 1, kc:kc + 1], scale=AB[:, Aidx, kc:kc + 1]),
                reads=[pak, "AB%d" % Aidx, "AB%d" % (Aidx + 1)], writes=[("hT", kc)])
            if prev_units and kc % 2 == 1:
                prev_units.pop(0)()
        while prev_units:
            prev_units.pop(0)()
        hT_keys = [("hT", kc) for kc in range(16)]
        if SUB <= 1:
            return

        pending = []
        def fm_job(c0, jobkind, cb):
            w, wk = load_w(c0, 512)
            for cc in range(4):
                pa, pak, pi = next_pA()
                def mm(e, pa=pa, w=w, cc=cc):
                    ins = None
                    for kc in range(16):
                        ins = e.matmul(pa[:, 0:N], lhsT=w[:, kc, cc * 128:(cc + 1) * 128], rhs=hT[:, kc, 0:N],
                                       start=(kc == 0), stop=(kc == 15))
                    return ins
                pg.op("pe", mm, reads=[wk] + hT_keys, writes=[pak])
                while pending:
                    pending.pop(0)()
                if jobkind == "u":
                    pg.op("act", lambda e, pa=pa, cc=cc: e.activation(out=stg_u[:, cb * 4 + cc, 0:N], in_=pa[:, 0:N],
                                                                      func=AF.Gelu),
                          reads=[pak], writes=["stg"])
                    continue
                chn = (c0 - 2048) // 128 + cc
                L = 256 if kind == "ctx" else 64
                t0 = t0s[pi]
                t0k = "t0_%d" % pi
                sraw = sraws[pi]
                pg.op("act", lambda e, pa=pa, sraw=sraw: e.copy(out=sraw[:, 0:N], in_=pa[:, 0:N]),
                      reads=[pak], writes=["sraw%d" % pi])
                pav = sraw[:, 0:N].rearrange("p (r l) -> p r l", l=L)
                t0v = t0[:, 0:N].rearrange("p (r l) -> p r l", l=L)
                cw = lambda j, chn=chn: pv[:, PV_CW + j * 24 + chn:PV_CW + j * 24 + chn + 1]
                pg.op("act", lambda e, pa=pa, t0=t0, chn=chn, cw=cw: e.activation(
                    out=t0[:, 0:N], in_=pa[:, 0:N], func=AF.Identity,
                    bias=pv[:, PV_CB + chn:PV_CB + chn + 1], scale=cw(1)), reads=[pak, "pv"], writes=[t0k])
                if not NOCONV:
                  pg.op("dve", lambda e, pav=pav, t0v=t0v, cw=cw: e.scalar_tensor_tensor(
                    out=t0v[:, :, 1:L], in0=pav[:, :, 0:L - 1], scalar=cw(0), in1=t0v[:, :, 1:L],
                    op0=OP.mult, op1=OP.add), reads=["sraw%d" % pi, t0k, "pv"], writes=[t0k])
                if not NOCONV:
                  pg.op("dve", lambda e, pav=pav, t0v=t0v, cw=cw: e.scalar_tensor_tensor(
                    out=t0v[:, :, 0:L - 1], in0=pav[:, :, 1:L], scalar=cw(2), in1=t0v[:, :, 0:L - 1],
                    op0=OP.mult, op1=OP.add), reads=["sraw%d" % pi, t0k, "pv"], writes=[t0k])
                if jobkind == "xs":
                    dst, dk = sbf[pi][:, 0:N], "sbf%d" % pi
                elif jobkind == "B":
                    dst, dk = BTs[:, cc, 0:N], ("BTs", cc)
                else:
                    dst, dk = CTs[:, cc, 0:N], ("CTs", cc)
                pg.op("act", lambda e, t0=t0, dst=dst: e.activation(out=dst, in_=t0[:, 0:N], func=AF.Silu),
                      reads=[t0k], writes=[dk])
                if jobkind in ("xs", "B") and not NOTR:
                    def emit_tr(dst=dst, dk=dk, pi=pi, cc=cc, jobkind=jobkind, cb=cb):
                        ptf, ptk = pTf[pi], "pTf%d" % pi
                        def tr2(e):
                            ins = None
                            for t in range(NT):
                                ins = e.matmul(ptf[:, t * 128:(t + 1) * 128], lhsT=dst[:, t * 128:(t + 1) * 128],
                                               rhs=idb[:], start=True, stop=True)
                            return ins
                        pg.op("pe", tr2, reads=[dk, "idb"], writes=[ptk])
                        if jobkind == "xs":
                            o = xs_tok[:, 0:NT, cb * 512 + cc * 128:cb * 512 + (cc + 1) * 128]
                            ok = [("xs_tok", t, cb) for t in range(NT)]
                        else:
                            o = B_tok[:, 0:NT, cc * 128:(cc + 1) * 128]
                            ok = [("B_tok", t) for t in range(NT)]
                        iv = ptf[:, 0:N].rearrange("p (t c) -> p t c", c=128)
                        if cc % 2 == 0:
                            pg.op("dve", lambda e: e.tensor_copy(out=o, in_=iv), reads=[ptk], writes=ok)
                        else:
                            pg.op("act", lambda e: e.copy(out=o, in_=iv), reads=[ptk], writes=ok)
                    pending.append(emit_tr)

        def tm_job(c0, ncols, jobkind, cb):
            if jobkind == "dt":
                w, wk = load_w(4672, 512)
                wofs = 448
            else:
                w, wk = load_w(c0, ncols)
                wofs = 0
            for t in range(NT):
                pa, pak, pi = next_pA()
                def mm(e, pa=pa, w=w, t=t):
                    ins = None
                    for kc in range(16):
                        ins = e.matmul(pa[:, 0:ncols], lhsT=hT[:, kc, t * 128:(t + 1) * 128], rhs=w[:, kc, wofs:wofs + ncols],
                                       start=(kc == 0), stop=(kc == 15))
                    return ins
                pg.op("pe", mm, reads=[wk] + hT_keys, writes=[pak])
                while pending:
                    pending.pop(0)()
                if jobkind == "z":
                    pg.op("act", lambda e, pa=pa, t=t: e.activation(out=stg[:, t, cb * 512:(cb + 1) * 512], in_=pa[:],
                                                                    func=AF.Silu), reads=[pak], writes=["stg"])
                elif jobkind == "v":
                    pg.op("act", lambda e, pa=pa, t=t: e.activation(out=xin[:, t, cb * 512:(cb + 1) * 512], in_=pa[:],
                                                                    func=AF.Gelu), reads=[pak], writes=[("xin", t)])
                else:
                    T = T0 + t
                    a, b_, c_, d_ = sm[:, 0, :], sm[:, 1, :], sm[:, 2, :], sm[:, 3, :]
                    pg.op("dve", lambda e, pa=pa: e.tensor_tensor(out=a, in0=pa[:, 0:64], in1=rp[:, RP_DTB:RP_DTB + 64],
                                                                  op=OP.add), reads=[pak, "rp"], writes=["sm0"])
                    pg.op("dve", lambda e: e.tensor_scalar_mul(out=b_, in0=a, scalar1=-1.0),
                          reads=["sm0"], writes=["sm1"])
                    pg.op("dve", lambda e: e.tensor_tensor(out=b_, in0=b_, in1=a, op=OP.min),
                          reads=["sm0", "sm1"], writes=["sm1"])
                    pg.op("act", lambda e: e.activation(out=c_, in_=b_, func=AF.Exp),
                          reads=["sm1"], writes=["sm2"])
                    pg.op("dve", lambda e: e.tensor_scalar_add(out=c_, in0=c_, scalar1=1.0),
                          reads=["sm2"], writes=["sm2"])
                    pg.op("act", lambda e: e.activation(out=c_, in_=c_, func=AF.Ln),
                          reads=["sm2"], writes=["sm2"])
                    pg.op("dve", lambda e, T=T: e.scalar_tensor_tensor(out=dtall[:, T, :], in0=a, scalar=0.0, in1=c_,
                                                                       op0=OP.max, op1=OP.add),
                          reads=["sm0", "sm2"], writes=[("dtall", T)])
                    if kind == "oth":
                        for dr in range(2):
                            pg.op("dve", lambda e, T=T, dr=dr: e.tensor_scalar_mul(
                                out=dtall[:, T, dr * 32:(dr + 1) * 32], in0=dtall[:, T, dr * 32:(dr + 1) * 32],
                                scalar1=flg[:, dr:dr + 1]), reads=[("dtall", T), "flg"], writes=[("dtall", T)])

        if own:
            for cb in range(4):
                tm_job(cb * 512, 512, "z", cb)
            for t in range(NT):
                dma_sp(z_s[T0 - 10 + t], stg[:, t, :], "st_stg", reads=["stg"])
        for cb in range(4):
            if JOBS is None or "xs" in JOBS:
                fm_job(2048 + cb * 512, "xs", cb)
        if JOBS is None or "B" in JOBS:
            fm_job(4096, "B", 0)
        if own:
            fm_job(4608, "C", 0)
        if JOBS is None or "dt" in JOBS:
            tm_job(5120, 64, "dt", 0)
        while pending:
            pending.pop(0)()
        def build_units():
            units = []
            W = NT * 64
            dtab, acsb, ddb, wgtb = sm[:, 4:8, :], sm[:, 8:12, :], sm[:, 12:16, :], wgt_t[:]
            f2 = lambda ap: ap[:, 0:NT, :]
            dtk = [("dtall", T0 + t) for t in range(NT)]
            def prep_():
                pg.op("dve", lambda e: e.tensor_tensor(out=f2(dtab), in0=dtall[:, T0:T0 + NT, :],
                                                       in1=aneg[:].unsqueeze(1).to_broadcast([128, NT, 64]), op=OP.mult),
                      reads=dtk + ["aneg"], writes=["sm4"])
                def mm2(e):
                    pmv = pm[:, 0:W].rearrange("p (t c) -> p t c", c=64)
                    for dr in range(2):
                        tri = cst[:, C_TU:C_TU + 128] if dr == 0 else cst[:, C_TL:C_TL + 128]
                        e.matmul(pmv[:, :, dr * 32:(dr + 1) * 32], lhsT=tri, rhs=f2(dtab)[:, :, dr * 32:(dr + 1) * 32],
                                 start=True, stop=True)
                    return e.matmul(pm[:, 256:256 + W].rearrange("p (t c) -> p t c", c=64), lhsT=ones, rhs=f2(dtab),
                                    start=True, stop=True)
                pg.op("pe", mm2, reads=["sm4", "cst"], writes=["pm"])
                pg.op("act", lambda e: e.copy(out=f2(acsb), in_=pm[:, 0:W].rearrange("p (t c) -> p t c", c=64)),
                      reads=["pm"], writes=["sm5"])
                pg.op("act", lambda e: e.activation(out=decall[:, T0:T0 + NT, :],
                                                    in_=pm[:, 256:256 + W].rearrange("p (t c) -> p t c", c=64), func=AF.Exp),
                      reads=["pm"], writes=[("decall", T0 + t, d_) for t in range(NT) for d_ in range(2)])
                pg.op("dve", lambda e: e.tensor_tensor(out=f2(ddb), in0=pm[:, 256:256 + W].rearrange("p (t c) -> p t c", c=64),
                                                       in1=f2(acsb), op=OP.subtract), reads=["pm", "sm5"], writes=["sm6"])
                pg.op("act", lambda e: e.activation(out=f2(ddb), in_=f2(ddb), func=AF.Exp), reads=["sm6"], writes=["sm6"])
                pg.op("dve", lambda e: e.tensor_tensor(out=f2(wgtb), in0=f2(ddb), in1=dtall[:, T0:T0 + NT, :], op=OP.mult),
                      reads=["sm6"] + dtk, writes=["sm7"])

            units.append(prep_)
            for t in range(NT):
                for dr in range(2):
                    def unit_(t=t, dr=dr):
                        T = T0 + t
                        xwt = xw[dr]
                        pg.op("dve", lambda e, t=t, dr=dr, xwt=xwt: e.tensor_tensor(
                            out=xwt[:].rearrange("p (h d) -> p h d", d=64),
                            in0=xs_tok[:, t, :].rearrange("p (h d) -> p h d", d=64),
                            in1=wgtb[:, t, dr * 32:(dr + 1) * 32].unsqueeze(2).to_broadcast([128, 32, 64]), op=OP.mult),
                            reads=[("xs_tok", t, cb) for cb in range(4)] + ["sm7"], writes=["xw%d" % dr])
                        sst = Sst[dr]
                        for g in range(4):
                            psg, psk = pS[g % 2], "pS%d" % (g % 2)
                            pg.op("pe", lambda e, psg=psg, t=t, g=g, xwt=xwt: e.matmul(
                                psg[:], lhsT=B_tok[:, t, g * 128:(g + 1) * 128], rhs=xwt[:, g * 512:(g + 1) * 512],
                                start=True, stop=True), reads=[("B_tok", t), "xw%d" % dr], writes=[psk])
                            if g % 2 == 0:
                                pg.op("act", lambda e, psg=psg, g=g, sst=sst: e.copy(out=sst[:, g * 512:(g + 1) * 512], in_=psg[:]),
                                      reads=[psk], writes=[("Sst", dr, g)])
                            else:
                                pg.op("dve", lambda e, psg=psg, g=g, sst=sst: e.tensor_copy(out=sst[:, g * 512:(g + 1) * 512], in_=psg[:]),
                                      reads=[psk], writes=[("Sst", dr, g)])
                        dma_sp(S_all[T, dr], sst[:], "st_S%d" % dr, reads=[("Sst", dr, g) for g in range(4)])

                    units.append(unit_)
            return units
        units = build_units()
        while pending:
            pending.pop(0)()
        if own:
            for t in range(NT):
                dma_sp(xs_s[T0 - 10 + t], xs_tok[:, t, :], "st_xs%d" % t, reads=[("xs_tok", t, cb) for cb in range(4)])
                dma_sp(bt_s[T0 - 10 + t], BTs[:, :, t * 128:(t + 1) * 128], "st_bt",
                       reads=[("BTs", cc) for cc in range(4)])
                dma_sp(ct_s[T0 - 10 + t], CTs[:, :, t * 128:(t + 1) * 128], "st_ct",
                       reads=[("CTs", cc) for cc in range(4)])
            for cb in range(4):
                tm_job(7232 + cb * 512, 512, "v", cb)
                for _ in range(2):
                    if units:
                        units.pop(0)()
            pg.op("dve", lambda e: e.memset(ssq[:], 0.0), writes=["ssq"] + [("ssq", t) for t in range(4)])
            def ln_tile(t):
                pg.op("act", lambda e, t=t: e.activation(out=vout[t % 2][:], in_=xin[:, t, :], func=AF.Identity,
                                                         accum_out=ssq[:, t:t + 1]),
                      reads=[("xin", t), "ssq"], writes=["vout%d" % (t % 2), ("ssq", t)])
                pg.op("act", lambda e, t=t: e.activation(out=vout[t % 2][:], in_=xin[:, t, :], func=AF.Square,
                                                         accum_out=ssq[:, 4 + t:5 + t]),
                      reads=[("xin", t), "ssq"], writes=["vout%d" % (t % 2), ("ssq", t)])
                mean, var, rs_, nmr = (ssq[:, 8 + t:9 + t], ssq[:, 12 + t:13 + t], ssq[:, 12 + t:13 + t], ssq[:, 8 + t:9 + t])
                k = ("ssq", t)
                pg.op("dve", lambda e, t=t, mean=mean: e.tensor_scalar_mul(out=mean, in0=ssq[:, t:t + 1], scalar1=1.0 / D),
                      reads=[k], writes=[k])
                pg.op("dve", lambda e, t=t, mean=mean, var=var: e.tensor_tensor(out=var, in0=mean, in1=mean, op=OP.mult),
                      reads=[k], writes=[k])
                pg.op("dve", lambda e, t=t, var=var: e.scalar_tensor_tensor(
                    out=var, in0=ssq[:, 4 + t:5 + t], scalar=1.0 / D, in1=var, op0=OP.mult, op1=OP.subtract),
                    reads=[k], writes=[k])
                pg.op("dve", lambda e, var=var: e.tensor_scalar_add(out=var, in0=var, scalar1=EPS), reads=[k], writes=[k])
                pg.op("act", lambda e, var=var: e.sqrt(out=var, in_=var), reads=[k], writes=[k])
                pg.op("dve", lambda e, var=var: e.reciprocal(out=var, in_=var), reads=[k], writes=[k])
                pg.op("dve", lambda e, mean=mean, var=var: e.scalar_tensor_tensor(
                    out=mean, in0=mean, scalar=-1.0, in1=var, op0=OP.mult, op1=OP.mult), reads=[k], writes=[k])
                pg.op("act", lambda e, t=t, mean=mean, var=var: e.activation(
                    out=xin[:, t, :], in_=xin[:, t, :], func=AF.Identity, bias=mean, scale=var),
                    reads=[("xin", t), k], writes=[("xin", t)])
                pg.op("dve", lambda e, t=t: e.tensor_tensor(out=xin[:, t, :], in0=xin[:, t, :], in1=lnr[:, 0, :], op=OP.mult),
                      reads=[("xin", t), "lnr"], writes=[("xin", t)])
                pg.op("dve", lambda e, t=t: e.tensor_tensor(out=vout[t % 2][:], in0=xin[:, t, :], in1=lnr[:, 1, :], op=OP.add),
                      reads=[("xin", t), "lnr"], writes=["vout%d" % (t % 2)])
                dma_sp(v_s[T0 - 10 + t], vout[t % 2][:], "st_vout%d" % (t % 2), reads=["vout%d" % (t % 2)])

            for cb in range(4):
                fm_job(5184 + cb * 512, "u", cb)
                if units:
                    units.pop(0)()
                ln_tile(cb)
            for t in range(NT):
                dma_sp(u_s[T0 - 10 + t], stg_u[:, :, t * 128:(t + 1) * 128], "st_stg", reads=["stg"])
        return units

        if SUB <= 2:
            return
    prev_units = []
    for blk_ in blocks:
        prev_units = do_block(*blk_, prev_units)
    while prev_units:
        prev_units.pop(0)()
    if "t_dt" in taps:
        t_dt = nc.dram_tensor("t_dt", [128, 18 * 64], F32, kind="ExternalOutput").ap()
        dma_sp(t_dt, dtall[:].rearrange("p a b -> p (a b)"), "st_tap", reads=[("dtall", T) for T in range(18)])
        t_dec = nc.dram_tensor("t_dec", [128, 18 * 64], F32, kind="ExternalOutput").ap()
        dma_sp(t_dec, decall[:].rearrange("p a b -> p (a b)"), "st_tap2", reads=[("decall", T, d_) for T in range(18) for d_ in range(2)])
    pg.barrier()
    pg.flush()
    st.close()
    if stage <= 1:
        return finish(nc, pg, es, out)


    st = ExitStack()
    hsts = [st.enter_context(nc.sbuf_tensor("hst%d" % i, [128, 2048], F32)) for i in range(2)]
    Sld = [[st.enter_context(nc.sbuf_tensor("Sld%d_%d" % (d_, i), [128, 2048], F32)) for i in range(2)] for d_ in range(2)]
    hpb = [[st.enter_context(nc.sbuf_tensor("hpb%d_%d" % (d_, i), [128, 2048], BF16)) for i in range(2)] for d_ in range(2)]
    chains = [list(range(0, 18)), [1, 0] + list(range(9, 1, -1)) + list(range(17, 9, -1))]
    for dr in range(2):
        pg.op("dve" if dr == 0 else "pool", lambda e, dr=dr: e.memset(hsts[dr][:], 0.0), writes=["hst%d" % dr])
    for i in range(18):
        for dr in range(2):
            T = chains[dr][i]
            hst = hsts[dr]
            hk = "hst%d" % dr
            sl, slk = Sld[dr][i % 2], "Sld%d_%d" % (dr, i % 2)
            dma_sp(sl[:], S_all[T, dr], "ld_" + slk, writes=[slk])
            if T >= 10:
                hb, hbk = hpb[dr][i % 2], "hpb%d_%d" % (dr, i % 2)
                pg.op("act", lambda e, hb=hb, hst=hst: e.copy(out=hb[:], in_=hst[:]), reads=[hk], writes=[hbk])
                dma_sp(hp_s[dr, T - 10], hb[:], "st_" + hbk, reads=[hbk])
            pg.op("dve", lambda e, T=T, dr=dr, hst=hst: e.tensor_tensor(
                out=hst[:].rearrange("p (h d) -> p h d", d=64), in0=hst[:].rearrange("p (h d) -> p h d", d=64),
                in1=decall[:, T, dr * 32:(dr + 1) * 32].unsqueeze(2).to_broadcast([128, 32, 64]), op=OP.mult),
                reads=[hk, ("decall", T, dr)], writes=[hk])
            pg.op("dve", lambda e, sl=sl, hst=hst: e.tensor_tensor(out=hst[:], in0=hst[:], in1=sl[:], op=OP.add),
                  reads=[hk, slk], writes=[hk])
    pg.barrier()
    pg.flush()
    st.close()
    if stage <= 3:
        return finish(nc, pg, es, out)


    sel3b = sb("sel3b", [128, 4096], BF16)
    dma_cast(sel3b[:], sel3, "ld_c2", writes=["sel3b"])
    st = ExitStack()
    def sb4(name, shape, dt=F32):
        return st.enter_context(nc.sbuf_tensor(name, list(shape), dt))
    def ps4(name, shape, dt=F32):
        return st.enter_context(nc.psum_tensor(name, list(shape), dt))
    xs_c = [sb4("xs_c%d" % i, [128, 2048], BF16) for i in range(2)]
    bt_c = [sb4("bt_c%d" % i, [128, 4, 128], BF16) for i in range(2)]
    ct_c = [sb4("ct_c%d" % i, [128, 4, 128], BF16) for i in range(2)]
    hpf_c = [sb4("hpf_c%d" % i, [128, 2048], BF16) for i in range(2)]
    hpb_c = [sb4("hpb_c%d" % i, [128, 2048], BF16) for i in range(2)]
    z_c = [sb4("z_c%d" % i, [128, 2048], BF16) for i in range(2)]
    v_c = [sb4("v_c%d" % i, [128, 2048], BF16) for i in range(2)]
    u_c = [sb4("u_c%d" % i, [128, 16, 128], BF16) for i in range(2)]
    mixb = [sb4("mixb%d" % i, [128, 32, 128], BF16) for i in range(2)]
    wsb = sb4("wsb", [128, 8, 128], BF16)
    dta3 = sb4("dta3", [128, 96])
    acs = sb4("acs", [128, 64])
    nacs = sb4("nacs", [128, 64])
    ecum = sb4("ecum", [128, 64])
    xdt = [sb4("xdt%d" % i, [128, 2048], BF16) for i in range(2)]
    pcs = [sb4("pcs%d" % i, [128, 128], BF16) for i in range(2)]
    tbb = sb4("tbb", [128, 128], BF16)
    Rr = sb4("Rr", [128, 128])
    R2 = sb4("R2", [128, 128])
    cbT = sb4("cbT", [128, 4, 128])
    dws = [sb4("dw%d" % i, [128, 4, 128]) for i in range(2)]
    Mm = [[sb4("Mm%d_%d" % (i, j), [128, 8, 128], BF16) for j in range(2)] for i in range(2)]
    ucnt = [0]
    t1 = sb4("t1", [128, 512])
    t2 = sb4("t2", [128, 512])
    yb = sb4("yb", [128, 2048])
    yn = sb4("yn", [128, 2048], BF16)
    gt = sb4("gt", [128, 512])
    ss4 = sb4("ss4", [128, 4])
    pm2 = ps4("pm2", [128, 512])
    pcb = ps4("pcb", [128, 512])
    pDs = [ps4("pD%d" % i, [128, 512]) for i in range(2)]
    pY = ps4("pY", [128, 512])
    pOf = ps4("pOf", [128, 512])
    pOb = ps4("pOb", [128, 512])
    pGa = ps4("pGa", [128, 512])
    pG = [pGa, pcb]
    pGk = ["pGa", "pcb"]
    dma_cast(wsb[:].rearrange("p g i -> p (g i)"), wsT, "ld_wsb", writes=["wsb"])
    mkb = [sb4("mkb%d" % i, [128, 128], BF16) for i in range(2)]
    pg.op("dve", lambda e: e.tensor_copy(out=mkb[0][:], in_=cst[:, C_MNF:C_MNF + 128]), reads=["cst"], writes=["mkb"])
    pg.op("dve", lambda e: e.tensor_copy(out=mkb[1][:], in_=cst[:, C_MNB:C_MNB + 128]), reads=["cst"], writes=["mkb"])
    dsk = rp[:, RP_DSKIP:RP_DSKIP + 32]

    def do_loads(c):
        i2 = c % 2
        xs, bt, ct, hpf, hpb_, zc, vc, uc, mix = (xs_c[i2], bt_c[i2], ct_c[i2], hpf_c[i2], hpb_c[i2], z_c[i2],
                                                   v_c[i2], u_c[i2], mixb[i2])
        K = lambda n: "%s%d" % (n, i2)
        dma_sp(xs[:], xs_s[c], "ld_" + K("xs"), writes=[K("xs")])
        dma_sp(bt[:], bt_s[c], "ld_" + K("bt"), writes=[K("bt")])
        dma_sp(ct[:], ct_s[c], "ld_" + K("ct"), writes=[K("ct")])
        dma_sp(hpf[:], hp_s[0, c], "ld_" + K("hpf"), writes=[K("hpf")])
        dma_sp(hpb_[:], hp_s[1, c], "ld_" + K("hpb"), writes=[K("hpb")])
        dma_sp(zc[:], z_s[c], "ld_" + K("z"), writes=[K("z")])
        dma_sp(vc[:], v_s[c], "ld_" + K("v"), writes=[K("v")])
        dma_sp(uc[:], u_s[c], "ld_" + K("u"), writes=[K("u")])

    def do_chunk(c):
        T = 10 + c
        i2 = c % 2
        xs, bt, ct, hpf, hpb_, zc, vc, uc, mix = (xs_c[i2], bt_c[i2], ct_c[i2], hpf_c[i2], hpb_c[i2], z_c[i2],
                                                   v_c[i2], u_c[i2], mixb[i2])
        K = lambda n: "%s%d" % (n, i2)
        def mmcb(e):
            ins = None
            for g in range(4):
                ins = e.matmul(pcb[:, g * 128:(g + 1) * 128], lhsT=bt[:, g, :], rhs=ct[:, g, :], start=True, stop=True)
            return ins
        pg.op("pe", mmcb, reads=[K("bt"), K("ct")], writes=["pcb"])
        pg.op("act", lambda e: e.copy(out=cbT[:].rearrange("p g i -> p (g i)"), in_=pcb[:]), reads=["pcb"], writes=["cbT"])
        for dr in range(2):
            tri = cst[:, C_TU:C_TU + 128] if dr == 0 else cst[:, C_TL:C_TL + 128]
            pg.op("dve", lambda e, dr=dr: e.tensor_tensor(
                out=dta3[:].rearrange("p (r h) -> p r h", h=32),
                in0=dtall[:, T, dr * 32:(dr + 1) * 32].unsqueeze(1).to_broadcast([128, 3, 32]),
                in1=aneg[:, dr * 32:(dr + 1) * 32].unsqueeze(1).to_broadcast([128, 3, 32]), op=OP.mult),
                reads=["aneg"], writes=["dta3"])
            def mmac(e, dr=dr, tri=tri):
                e.matmul(pm2[:, dr * 32:(dr + 1) * 32], lhsT=tri, rhs=dta3[:, 0:32], start=True, stop=True)
                return e.matmul(pm2[0:96, 64 + dr * 128:64 + (dr + 1) * 128], lhsT=dta3[:, 0:96], rhs=tri,
                                start=True, stop=True)
            pg.op("pe", mmac, reads=["dta3", "cst"], writes=[("pm2", dr)])
            sl = slice(dr * 32, (dr + 1) * 32)
            pg.op("act", lambda e, sl=sl: e.copy(out=acs[:, sl], in_=pm2[:, sl]), reads=[("pm2", dr)], writes=[("acs", dr)])
            pg.op("dve", lambda e, sl=sl: e.tensor_scalar_mul(out=nacs[:, sl], in0=acs[:, sl], scalar1=-1.0),
                  reads=[("acs", dr)], writes=[("nacs", dr)])
            pg.op("act", lambda e, sl=sl: e.activation(out=ecum[:, sl], in_=acs[:, sl], func=AF.Exp),
                  reads=[("acs", dr)], writes=[("ecum", dr)])
            src_ = pm2[:, 64 + dr * 128:64 + (dr + 1) * 128]
            pc = pcs[dr]
            pk = "pcs%d" % dr
            pg.op("act", lambda e, pc=pc, src_=src_: e.copy(out=pc[0:32, :], in_=src_[0:32, :]),
                  reads=[("pm2", dr)], writes=[(pk, 0)])
            for lo in (32, 64):
                pg.op("act", lambda e, src_=src_, lo=lo: e.copy(out=tbb[lo:lo + 32, :], in_=src_[lo:lo + 32, :]),
                      reads=[("pm2", dr)], writes=[("tbb", lo)])
                pg.op("dve", lambda e, src_=src_, lo=lo: e.tensor_tensor(out=Rr[lo:lo + 32, :], in0=src_[lo:lo + 32, :],
                                                                        in1=tbb[lo:lo + 32, :], op=OP.subtract),
                      reads=[("pm2", dr), ("tbb", lo)], writes=[("Rr", lo)])
            pg.op("act", lambda e, pc=pc: e.copy(out=pc[32:64, :], in_=Rr[32:64, :]), reads=[("Rr", 32)], writes=[(pk, 1)])
            pg.op("act", lambda e: e.copy(out=tbb[64:96, :], in_=Rr[64:96, :]), reads=[("Rr", 64)], writes=[("tbb", 64)])
            pg.op("dve", lambda e: e.tensor_tensor(out=R2[64:96, :], in0=Rr[64:96, :], in1=tbb[64:96, :], op=OP.subtract),
                  reads=[("Rr", 64), ("tbb", 64)], writes=["R2"])
            pg.op("act", lambda e, pc=pc: e.copy(out=pc[64:96, :], in_=R2[64:96, :]), reads=["R2"], writes=[(pk, 2)])
            pg.op("pool", lambda e, dr=dr: e.tensor_tensor(
                out=xdt[dr][:].rearrange("p (h d) -> p h d", d=64), in0=xs[:].rearrange("p (h d) -> p h d", d=64),
                in1=dtall[:, T, dr * 32:(dr + 1) * 32].unsqueeze(2).to_broadcast([128, 32, 64]), op=OP.mult),
                reads=[K("xs")], writes=["xdt%d" % dr])
        def emit_D(g):
            Mg = Mm[g % 2]
            Mk = lambda dr, hf, g=g: ("Mm", g % 2, dr, hf)
            for dr in range(2):
                mk = cst[:, C_MNF:C_MNF + 128] if dr == 0 else cst[:, C_MNB:C_MNB + 128]
                pc = pcs[dr]
                pk = "pcs%d" % dr
                for hf in range(2):
                    bsel = ucnt[0] % 2
                    ucnt[0] += 1
                    pDh, pDk = pDs[bsel], "pD%d" % bsel
                    dw, dwk = dws[bsel], "dw%d" % bsel
                    h0 = g * 8 + hf * 4
                    def mmD(e, h0=h0, pc=pc, pDh=pDh, dr=dr):
                        ins = None
                        for j in range(4):
                            h = h0 + j
                            e.matmul(pDh[:, j * 128:(j + 1) * 128], lhsT=sel3b[0:96, h * 128:(h + 1) * 128],
                                     rhs=pc[0:96, :], start=True, stop=False)
                            ins = e.matmul(pDh[:, j * 128:(j + 1) * 128], lhsT=idb[:], rhs=mkb[dr][:],
                                           start=False, stop=True)
                        return ins
                    pg.op("pe", mmD, reads=[(pk, 0), (pk, 1), (pk, 2), "sel3b", "mkb", "idb"], writes=[pDk])
                    pg.op("dve", lambda e, dw=dw, pDh=pDh, h0=h0, dr=dr: e.tensor_tensor(
                        out=dw[:], in0=pDh[:].rearrange("p (h i) -> p h i", i=128),
                        in1=acs[:, dr * 32 + h0:dr * 32 + h0 + 4].unsqueeze(2).to_broadcast([128, 4, 128]), op=OP.subtract),
                        reads=[pDk, ("acs", dr)], writes=[dwk])
                    pg.op("act", lambda e, dw=dw: e.activation(out=dw[:], in_=dw[:], func=AF.Exp), reads=[dwk], writes=[dwk])
                    pg.op("pool", lambda e, dw=dw, g=g, dr=dr, hf=hf, Mg=Mg: e.tensor_tensor(
                        out=Mg[dr][:, hf * 4:(hf + 1) * 4, :], in0=dw[:],
                        in1=cbT[:, g, :].unsqueeze(1).to_broadcast([128, 4, 128]), op=OP.mult),
                        reads=[dwk, "cbT"], writes=[Mk(dr, hf)])
        def emit_Y(g):
            Mg = Mm[g % 2]
            Mk = lambda dr, hf, g=g: ("Mm", g % 2, dr, hf)
            def mmY(e, g=g, Mg=Mg):
                ins = None
                for hh in range(8):
                    h = g * 8 + hh
                    e.matmul(pY[:, hh * 64:(hh + 1) * 64], lhsT=Mg[0][:, hh, :], rhs=xdt[0][:, h * 64:(h + 1) * 64],
                             start=True, stop=False)
                    ins = e.matmul(pY[:, hh * 64:(hh + 1) * 64], lhsT=Mg[1][:, hh, :], rhs=xdt[1][:, h * 64:(h + 1) * 64],
                                   start=False, stop=True)
                return ins
            pg.op("pe", mmY, reads=[Mk(dr, hf) for dr in range(2) for hf in range(2)] + ["xdt0", "xdt1"], writes=["pY"])
            pg.op("pe", lambda e, g=g: e.matmul(pOf[:], lhsT=ct[:, g, :], rhs=hpf[:, g * 512:(g + 1) * 512],
                                                start=True, stop=True), reads=[K("ct"), K("hpf")], writes=["pOf"])
            pg.op("pe", lambda e, g=g: e.matmul(pOb[:], lhsT=ct[:, g, :], rhs=hpb_[:, g * 512:(g + 1) * 512],
                                                start=True, stop=True), reads=[K("ct"), K("hpb")], writes=["pOb"])
            v3 = lambda ap: ap.rearrange("p (h d) -> p h d", d=64)
            pg.op("dve", lambda e, g=g: e.tensor_tensor(
                out=v3(t1[:]), in0=v3(pOf[:]), in1=ecum[:, g * 8:(g + 1) * 8].unsqueeze(2).to_broadcast([128, 8, 64]),
                op=OP.mult), reads=["pOf", ("ecum", 0)], writes=["t1"])
            pg.op("dve", lambda e, g=g: e.tensor_tensor(
                out=v3(t2[:]), in0=v3(pOb[:]), in1=ecum[:, 32 + g * 8:32 + (g + 1) * 8].unsqueeze(2).to_broadcast([128, 8, 64]),
                op=OP.mult), reads=["pOb", ("ecum", 1)], writes=["t2"])
            pg.op("pool", lambda e: e.tensor_tensor(out=t1[:], in0=t1[:], in1=t2[:], op=OP.add), reads=["t1", "t2"],
                  writes=["t1"])
            pg.op("dve", lambda e, g=g: e.tensor_tensor(out=yb[:, g * 512:(g + 1) * 512], in0=pY[:], in1=t1[:], op=OP.add),
                  reads=["pY", "t1"], writes=[("yb", g)])
            pg.op("pool", lambda e, g=g: e.tensor_tensor(
                out=v3(t2[:]), in0=v3(xs[:, g * 512:(g + 1) * 512]),
                in1=dsk[:, g * 8:(g + 1) * 8].unsqueeze(2).to_broadcast([128, 8, 64]), op=OP.mult),
                reads=[K("xs"), "rp", "t2"], writes=["t2"])
            pg.op("pool", lambda e, g=g: e.tensor_tensor(out=yb[:, g * 512:(g + 1) * 512], in0=yb[:, g * 512:(g + 1) * 512],
                                                        in1=t2[:], op=OP.add), reads=[("yb", g), "t2"], writes=[("yb", g)])

        emit_D(0)
        for g in range(4):
            if g + 1 < 4:
                emit_D(g + 1)
            emit_Y(g)
        ybk = [("yb", g) for g in range(4)]
        pg.op("dve", lambda e: e.tensor_tensor(out=yb[:], in0=yb[:], in1=zc[:], op=OP.mult), reads=ybk + [K("z")], writes=ybk)
        pg.op("dve", lambda e: e.memset(ss4[:], 0.0), writes=["ss4"])
        pg.op("act", lambda e: e.activation(out=yn[:], in_=yb[:], func=AF.Square, accum_out=ss4[:, 0:1]),
              reads=ybk + ["ss4"], writes=["yn", "ss4"])
        pg.op("dve", lambda e: e.tensor_scalar(out=ss4[:, 1:2], in0=ss4[:, 0:1], scalar1=1.0 / D, scalar2=EPS,
                                               op0=OP.mult, op1=OP.add), reads=["ss4"], writes=["ss4"])
        pg.op("act", lambda e: e.sqrt(out=ss4[:, 1:2], in_=ss4[:, 1:2]), reads=["ss4"], writes=["ss4"])
        pg.op("dve", lambda e: e.reciprocal(out=ss4[:, 1:2], in_=ss4[:, 1:2]), reads=["ss4"], writes=["ss4"])
        pg.op("act", lambda e: e.activation(out=yn[:], in_=yb[:], func=AF.Copy, scale=ss4[:, 1:2]),
              reads=ybk + ["ss4"], writes=["yn"])
        for q in range(4):
            pgq, pgk = pG[q % 2], pGk[q % 2]
            def mmT(e, q=q, pgq=pgq):
                ins = None
                for j in range(4):
                    kc = q * 4 + j
                    ins = e.matmul(pgq[:, j * 128:(j + 1) * 128], lhsT=yn[:, kc * 128:(kc + 1) * 128], rhs=idb[:],
                                   start=True, stop=True)
                return ins
            pg.op("pe", mmT, reads=["yn", "idb"], writes=[pgk])
            for j in range(4):
                kc = q * 4 + j
                pg.op("act", lambda e, j=j, kc=kc, pgq=pgq: e.activation(
                    out=mix[:, kc, :], in_=pgq[:, j * 128:(j + 1) * 128], func=AF.Copy,
                    scale=pv[:, PV_SNG + kc:PV_SNG + kc + 1]), reads=[pgk, "pv"], writes=[(K("mix"), kc)])
        for q in range(4):
            pgq, pgk = pG[q % 2], pGk[q % 2]
            def mmG(e, q=q, pgq=pgq):
                ins = None
                for j in range(4):
                    cc = q * 4 + j
                    ins = e.matmul(pgq[:, j * 128:(j + 1) * 128], lhsT=vc[:, cc * 128:(cc + 1) * 128], rhs=wsb[:, cc // 2, :],
                                   start=True, stop=True)
                return ins
            pg.op("pe", mmG, reads=[K("v"), "wsb"], writes=[pgk])
            pg.op("dve", lambda e, q=q, pgq=pgq: e.tensor_tensor(
                out=gt[:].rearrange("p (a b i) -> p a b i", a=2, b=2),
                in0=pgq[:].rearrange("p (a b i) -> p a b i", a=2, b=2),
                in1=rp[:, RP_BS + q * 256:RP_BS + (q + 1) * 256].rearrange("p (a i) -> p a i", a=2).unsqueeze(2)
                .to_broadcast([128, 2, 2, 128]), op=OP.add), reads=[pgk, "rp"], writes=["gt"])
            pg.op("pool", lambda e, q=q: e.tensor_tensor(
                out=mix[:, 16 + q * 4:16 + (q + 1) * 4, :], in0=gt[:].rearrange("p (c i) -> p c i", i=128),
                in1=uc[:, q * 4:(q + 1) * 4, :], op=OP.mult), reads=["gt", K("u")], writes=[(K("mix"), 16 + q)])
        dma_sp(mix_s[c], mix[:], "st_" + K("mix"),
               reads=[(K("mix"), kc) for kc in range(20)])

    nch = NCH
    wb3 = [sb4("p3w%d" % i, [128, 16, 512], BF16) for i in range(2)]

    class _MP:
        def __getitem__(self, key):
            p_, c_ = key
            return pm2[p_, 320 + c_.start:320 + c_.stop]
    mps_cur[0] = _MP()
    mps_off[0] = 64
    do_loads(0)
    for c in range(nch):
        if c + 1 < nch:
            do_loads(c + 1)
        mod_block(8 + 2 * c, wb3, part=1)
        mod_block(9 + 2 * c, wb3, part=1)
        do_chunk(c)
        mod_block(8 + 2 * c, wb3, part=2)
        mod_block(9 + 2 * c, wb3, part=2)
    mod_finish(32, 96, pm2[:, 320:512], base=32)
    ab(4, PV_N2G, 4, 3, 0)
    pg.op("dve", lambda e: e.tensor_copy(out=G12[:, 0, :], in_=mt[:, 2, :, 0]), reads=["modT"], writes=["G12a"])
    pg.op("dve", lambda e: e.tensor_copy(out=G12[:, 1, :], in_=mt[:, 5, :, 0]), reads=["modT"], writes=["G12b"])
    pg.barrier()
    pg.flush()
    st.close()
    if stage <= 4:
        return finish(nc, pg, es, out)


    st5 = ExitStack()
    x1T = st5.enter_context(nc.sbuf_tensor("x1T", [128, 16, 1024], F32))
    banks = [st5.enter_context(nc.psum_tensor("bk%d" % i, [128, 512], F32)) for i in range(8)]
    bkk = ["bk%d" % i for i in range(8)]
    st = ExitStack()
    mixblk = st.enter_context(nc.sbuf_tensor("mixblk", [128, 32, 512], BF16))
    xin5 = st.enter_context(nc.sbuf_tensor("xin5", [128, 4, 2048], F32))
    wo = [st.enter_context(nc.sbuf_tensor("wo%d" % i, [128, 32, 256], BF16)) for i in range(2)]
    tmp5 = [st.enter_context(nc.sbuf_tensor("tmp5_%d" % i, [128, 512], F32)) for i in range(2)]
    w_out_v = w_out.rearrange("(kc p) c -> p kc c", p=128)

    def do_p5(tb):
        for t in range(4):
            dma_sp(mixblk[:, :, t * 128:(t + 1) * 128], mix_s[tb * 4 + t], "ld_mixblk%d" % t, writes=[("mixblk", t)])
            r0 = 1280 + (tb * 4 + t) * 128
            dma_sp(xin5[:, t, :], x_all[r0:r0 + 128, :], "ld_xin5_%d" % t, writes=[("xin5", t)])
        for dcp in range(8):
            w = wo[dcp % 2]
            wk = "wo%d" % (dcp % 2)
            dma_cast(w[:], w_out_v[:, :, dcp * 256:(dcp + 1) * 256], "ld_" + wk, writes=[wk])
            for d2 in range(2):
                dc = dcp * 2 + d2
                i2 = dc % 2
                pa, pak = banks[i2], bkk[i2]
                px, pxk = banks[2 + i2], bkk[2 + i2]
                def mm(e, w=w, d2=d2, pa=pa):
                    ins = None
                    for kc in range(32):
                        ins = e.matmul(pa[:], lhsT=w[:, kc, d2 * 128:(d2 + 1) * 128], rhs=mixblk[:, kc, :],
                                       start=(kc == 0), stop=(kc == 31))
                    return ins
                pg.op("pe", mm, reads=[wk] + [("mixblk", t) for t in range(4)], writes=[pak])
                tm, tmk = tmp5[i2], "tmp5_%d" % i2
                pg.op("act", lambda e, tm=tm, pa=pa, dc=dc: e.activation(out=tm[:], in_=pa[:], func=AF.Copy,
                                                                         scale=G12[:, 0, dc:dc + 1]),
                      reads=[pak, "G12a"], writes=[tmk])
                def trx(e, px=px, dc=dc):
                    ins = None
                    for t in range(4):
                        ins = e.transpose(out=px[:, t * 128:(t + 1) * 128], in_=xin5[:, t, dc * 128:(dc + 1) * 128],
                                          identity=ident)
                    return ins
                pg.op("pe", trx, reads=[("xin5", t) for t in range(4)] + ["cst"], writes=[pxk])
                pg.op("dve", lambda e, px=px, tm=tm, dc=dc: e.tensor_tensor(
                    out=x1T[:, dc, tb * 512:(tb + 1) * 512], in0=px[:], in1=tm[:], op=OP.add),
                    reads=[pxk, tmk], writes=[("x1T", dc, tb)])
    for tb in range(2):
        do_p5(tb)
    if "t_x1T" in taps:
        t_x1T = nc.dram_tensor("t_x1T", [128, 16 * 1024], F32, kind="ExternalOutput").ap()
        dma_sp(t_x1T, x1T[:].rearrange("p a b -> p (a b)"), "st_tap5",
               reads=[("x1T", dc, tb) for dc in range(16) for tb in range(2)])
    pg.barrier()
    pg.flush()
    st.close()
    if stage <= 5:
        st5.close()
        return finish(nc, pg, es, out)


    st6 = ExitStack()
    h2T = st6.enter_context(nc.sbuf_tensor("h2T", [128, 16, 1024], BF16))
    cpc = st6.enter_context(nc.sbuf_tensor("cpc", [128, 1024], BF16))
    st = ExitStack()
    def sb6(name, shape, dt=F32):
        return st.enter_context(nc.sbuf_tensor(name, list(shape), dt))
    sq = [sb6("sq%d" % i, [128, 512]) for i in range(2)]
    rstd = sb6("rstd", [128, 1024])
    tmph = [sb6("tmph%d" % i, [128, 1024]) for i in range(2)]
    wrb = sb6("wrb", [128, 16, 36], BF16)
    lg = sb6("lg", [128, 8, 36])
    mg = sb6("mg", [128, 8])
    eg = sb6("eg", [128, 8, 4])
    sgm = sb6("sgm", [128, 8])
    tpg = sb6("tpg", [128, 8])
    ohg = sb6("ohg", [128, 8, 4])
    selx = sb6("selx", [128, 8, 8])
    tmp8 = sb6("tmp8", [128, 8, 8])
    m1 = sb6("m1", [128, 8])
    m2 = sb6("m2", [128, 8])
    mask1 = sb6("mask1", [128, 8, 8])
    mask2 = sb6("mask2", [128, 8, 8])
    sel2 = sb6("sel2", [128, 8, 8])
    p1 = sb6("p1", [128, 8])
    p2 = sb6("p2", [128, 8])
    wex = sb6("wex", [128, 8, 8])
    comb3 = sb6("comb3", [128, 8, 3, 32])
    ctb = sb6("ctb", [128, 1024], BF16)
    cR = sb6("cR", [128, 1024])
    cR2 = sb6("cR2", [128, 1024])
    dma_cast(wrb[:].rearrange("p a b -> p (a b)"), wr, "ld_wrb", writes=["wrb"])
    x1k = [("x1T", dc, tb) for dc in range(16) for tb in range(2)]
    for tb in range(2):
        for kc in range(16):
            s_, sk = sq[kc % 2], "sq%d" % (kc % 2)
            pg.op("act", lambda e, s_=s_, kc=kc, tb=tb: e.activation(out=s_[:], in_=x1T[:, kc, tb * 512:(tb + 1) * 512],
                                                                     func=AF.Square), reads=[("x1T", kc, tb)], writes=[sk])
            pg.op("pe", lambda e, s_=s_, kc=kc, tb=tb: e.matmul(banks[tb][:], lhsT=ones, rhs=s_[:], start=(kc == 0),
                                                               stop=(kc == 15)), reads=[sk, "cst"], writes=[bkk[tb]])
        sl = slice(tb * 512, (tb + 1) * 512)
        pg.op("dve", lambda e, tb=tb, sl=sl: e.tensor_scalar(out=rstd[:, sl], in0=banks[tb][:], scalar1=1.0 / D, scalar2=EPS,
                                                            op0=OP.mult, op1=OP.add), reads=[bkk[tb]], writes=[("rstd", tb)])
        pg.op("act", lambda e, sl=sl: e.sqrt(out=rstd[:, sl], in_=rstd[:, sl]), reads=[("rstd", tb)], writes=[("rstd", tb)])
        pg.op("dve", lambda e, sl=sl: e.reciprocal(out=rstd[:, sl], in_=rstd[:, sl]), reads=[("rstd", tb)], writes=[("rstd", tb)])
    for kc in range(16):
        th, thk = tmph[kc % 2], "tmph%d" % (kc % 2)
        pg.op("dve", lambda e, th=th, kc=kc: e.tensor_tensor(out=th[:], in0=x1T[:, kc, :], in1=rstd[:], op=OP.mult),
              reads=[("x1T", kc, 0), ("x1T", kc, 1), ("rstd", 0), ("rstd", 1)], writes=[thk])
        pg.op("act", lambda e, th=th, kc=kc: e.activation(out=h2T[:, kc, :], in_=th[:], func=AF.Identity,
                                                          bias=AB[:, 5, kc:kc + 1], scale=AB[:, 4, kc:kc + 1]),
              reads=[thk, "AB4", "AB5"], writes=[("h2T", kc)])
    h2k = [("h2T", kc) for kc in range(16)]
    for t in range(8):
        pr, prk = banks[2 + t % 2], bkk[2 + t % 2]
        def mmr(e, t=t, pr=pr):
            ins = None
            for kc in range(16):
                ins = e.matmul(pr[:, 0:36], lhsT=h2T[:, kc, t * 128:(t + 1) * 128], rhs=wrb[:, kc, :],
                               start=(kc == 0), stop=(kc == 15))
            return ins
        pg.op("pe", mmr, reads=h2k + ["wrb"], writes=[prk])
        pg.op("dve", lambda e, t=t, pr=pr: e.tensor_tensor(out=lg[:, t, :], in0=pr[:, 0:36], in1=rp[:, RP_BR:RP_BR + 36],
                                                          op=OP.add), reads=[prk, "rp"], writes=[("lg", t)])
    lgk = [("lg", t) for t in range(8)]
    lgG = lg[:, :, 0:4]
    bc = lambda ap, n: ap.unsqueeze(2).to_broadcast([128, 8, n])
    R_ = "rt"
    pg.op("dve", lambda e: e.tensor_reduce(out=mg[:], in_=lgG, axis=AX.X, op=OP.max), reads=lgk, writes=[R_])
    pg.op("dve", lambda e: e.tensor_tensor(out=eg[:], in0=lgG, in1=bc(mg[:], 4), op=OP.subtract), reads=lgk + [R_], writes=[R_])
    pg.op("act", lambda e: e.activation(out=eg[:], in_=eg[:], func=AF.Exp), reads=[R_], writes=[R_])
    pg.op("dve", lambda e: e.tensor_reduce(out=sgm[:], in_=eg[:], axis=AX.X, op=OP.add), reads=[R_], writes=[R_])
    pg.op("dve", lambda e: e.reciprocal(out=tpg[:], in_=sgm[:]), reads=[R_], writes=[R_])
    pg.op("dve", lambda e: e.tensor_tensor(out=ohg[:], in0=lgG, in1=bc(mg[:], 4), op=OP.is_equal), reads=lgk + [R_], writes=[R_])
    for g in range(4):
        lgE = lg[:, :, 4 + g * 8:4 + (g + 1) * 8]
        dst = selx if g == 0 else tmp8
        pg.op("dve", lambda e, g=g, lgE=lgE, dst=dst: e.tensor_tensor(
            out=dst[:], in0=lgE, in1=ohg[:, :, g:g + 1].to_broadcast([128, 8, 8]), op=OP.mult), reads=lgk + [R_], writes=[R_])
        if g > 0:
            pg.op("dve", lambda e: e.tensor_tensor(out=selx[:], in0=selx[:], in1=tmp8[:], op=OP.add), reads=[R_], writes=[R_])
    pg.op("dve", lambda e: e.tensor_reduce(out=m1[:], in_=selx[:], axis=AX.X, op=OP.max), reads=[R_], writes=[R_])
    pg.op("dve", lambda e: e.tensor_tensor(out=mask1[:], in0=selx[:], in1=bc(m1[:], 8), op=OP.is_equal), reads=[R_], writes=[R_])
    pg.op("dve", lambda e: e.tensor_scalar_mul(out=sel2[:], in0=mask1[:], scalar1=-1.0e30), reads=[R_], writes=[R_])
    pg.op("dve", lambda e: e.tensor_tensor(out=sel2[:], in0=sel2[:], in1=selx[:], op=OP.add), reads=[R_], writes=[R_])
    pg.op("dve", lambda e: e.tensor_reduce(out=m2[:], in_=sel2[:], axis=AX.X, op=OP.max), reads=[R_], writes=[R_])
    pg.op("dve", lambda e: e.tensor_tensor(out=mask2[:], in0=sel2[:], in1=bc(m2[:], 8), op=OP.is_equal), reads=[R_], writes=[R_])
    pg.op("dve", lambda e: e.tensor_tensor(out=p2[:], in0=m2[:], in1=m1[:], op=OP.subtract), reads=[R_], writes=[R_])
    pg.op("act", lambda e: e.activation(out=p2[:], in_=p2[:], func=AF.Exp), reads=[R_], writes=[R_])
    pg.op("dve", lambda e: e.tensor_scalar_add(out=p1[:], in0=p2[:], scalar1=1.0), reads=[R_], writes=[R_])
    pg.op("dve", lambda e: e.reciprocal(out=p1[:], in_=p1[:]), reads=[R_], writes=[R_])
    pg.op("dve", lambda e: e.tensor_tensor(out=p2[:], in0=p2[:], in1=p1[:], op=OP.mult), reads=[R_], writes=[R_])
    pg.op("dve", lambda e: e.tensor_tensor(out=p1[:], in0=p1[:], in1=tpg[:], op=OP.mult), reads=[R_], writes=[R_])
    pg.op("dve", lambda e: e.tensor_tensor(out=p2[:], in0=p2[:], in1=tpg[:], op=OP.mult), reads=[R_], writes=[R_])
    pg.op("dve", lambda e: e.tensor_tensor(out=wex[:], in0=mask1[:], in1=bc(p1[:], 8), op=OP.mult), reads=[R_], writes=[R_])
    pg.op("dve", lambda e: e.tensor_tensor(out=tmp8[:], in0=mask2[:], in1=bc(p2[:], 8), op=OP.mult), reads=[R_], writes=[R_])
    pg.op("dve", lambda e: e.tensor_tensor(out=wex[:], in0=wex[:], in1=tmp8[:], op=OP.add), reads=[R_], writes=[R_])
    for r in range(3):
        for g in range(4):
            pg.op("dve", lambda e, r=r, g=g: e.tensor_tensor(
                out=comb3[:, :, r, g * 8:(g + 1) * 8], in0=wex[:], in1=ohg[:, :, g:g + 1].to_broadcast([128, 8, 8]),
                op=OP.mult), reads=[R_], writes=[R_, ("comb3", r, g)])
    if "t_comb" in taps:
        t_comb = nc.dram_tensor("t_comb", [128, 8 * 96], F32, kind="ExternalOutput").ap()
        dma_sp(t_comb, comb3[:].rearrange("p a b c -> p (a b c)"), "st_tap6", reads=[R_])
    if "t_h2T" in taps:
        t_h2T = nc.dram_tensor("t_h2T", [128, 16 * 1024], BF16, kind="ExternalOutput").ap()
        dma_sp(t_h2T, h2T[:].rearrange("p a b -> p (a b)"), "st_tap7", reads=h2k)
    for t in range(8):
        pc_, pck = banks[4 + t // 4], bkk[4 + t // 4]
        pg.op("pe", lambda e, t=t, pc_=pc_: e.transpose(out=pc_[0:96, (t % 4) * 128:(t % 4 + 1) * 128],
                                                        in_=comb3[:, t, :, :].rearrange("p r c -> p (r c)"), identity=ident),
              reads=[R_, "cst"], writes=[(pck, t % 4)])
    for hb in range(2):
        src_ = banks[4 + hb]
        sk = [(bkk[4 + hb], j) for j in range(4)]
        sl = slice(hb * 512, (hb + 1) * 512)
        pg.op("act", lambda e, src_=src_, sl=sl: e.copy(out=cpc[0:32, sl], in_=src_[0:32, :]), reads=sk, writes=[("cpc", 0, hb)])
        for lo in (32, 64):
            pg.op("act", lambda e, src_=src_, sl=sl, lo=lo: e.copy(out=ctb[lo:lo + 32, sl], in_=src_[lo:lo + 32, :]),
                  reads=sk, writes=[("ctb", lo, hb)])
            pg.op("dve", lambda e, src_=src_, sl=sl, lo=lo: e.tensor_tensor(
                out=cR[lo:lo + 32, sl], in0=src_[lo:lo + 32, :], in1=ctb[lo:lo + 32, sl], op=OP.subtract),
                reads=sk + [("ctb", lo, hb)], writes=[("cR", lo, hb)])
        pg.op("act", lambda e, sl=sl: e.copy(out=cpc[32:64, sl], in_=cR[32:64, sl]), reads=[("cR", 32, hb)],
              writes=[("cpc", 1, hb)])
        pg.op("act", lambda e, sl=sl: e.copy(out=ctb[64:96, sl], in_=cR[64:96, sl]), reads=[("cR", 64, hb)],
              writes=[("ctb", 64, hb)])
        pg.op("dve", lambda e, sl=sl: e.tensor_tensor(out=cR2[64:96, sl], in0=cR[64:96, sl], in1=ctb[64:96, sl],
                                                      op=OP.subtract), reads=[("cR", 64, hb), ("ctb", 64, hb)],
              writes=[("cR2", hb)])
        pg.op("act", lambda e, sl=sl: e.copy(out=cpc[64:96, sl], in_=cR2[64:96, sl]), reads=[("cR2", hb)],
              writes=[("cpc", 2, hb)])
    pg.barrier()
    pg.flush()
    st.close()
    if stage <= 6:
        st6.close()
        st5.close()
        return finish(nc, pg, es, out)


    st = ExitStack()
    def sb7(name, shape, dt=F32):
        return st.enter_context(nc.sbuf_tensor(name, list(shape), dt))
    wgh = [sb7("wgh%d" % i, [128, 16, 256], BF16) for i in range(2)]
    wuh = [sb7("wuh%d" % i, [128, 16, 256], BF16) for i in range(2)]
    wdn = sb7("wdn", [128, 4, 2048], BF16)
    hid = sb7("hid", [128, 4, 1024], BF16)
    cbc = sb7("cbc", [128, 2, 512])
    sgs = [sb7("sgs%d" % i, [128, 512]) for i in range(2)]
    tus = [sb7("tus%d" % i, [128, 512]) for i in range(2)]
    tmo = [sb7("tmo%d" % i, [128, 512]) for i in range(3)]
    cpk = [("cpc", r, hb) for r in range(3) for hb in range(2)]
    cnt6 = [0]

    def do_expert(ex):
        wg_v = w_g[ex].rearrange("(kc p) f -> p kc f", p=128)
        wu_v = w_u[ex].rearrange("(kc p) f -> p kc f", p=128)
        wd_v = w_d[ex].rearrange("(fc p) d -> p fc d", p=128)
        def emit_cbc(tb):
            pg.op("pe", lambda e, tb=tb: e.matmul(banks[6][:], lhsT=sel3b[0:96, ex * 128:(ex + 1) * 128],
                                                  rhs=cpc[0:96, tb * 512:(tb + 1) * 512], start=True, stop=True),
                  reads=cpk + ["sel3b"], writes=[bkk[6]])
            pg.op("act", lambda e, tb=tb: e.copy(out=cbc[:, tb, :], in_=banks[6][:]), reads=[bkk[6]], writes=[("cbc", tb)])
        emit_cbc(0)
        for half in range(2):
            wg_, wu_ = wgh[half], wuh[half]
            dma_cast(wg_[:], wg_v[:, :, half * 256:(half + 1) * 256], "ld_wgh%d" % half, writes=["wgh%d" % half])
            dma_cast(wu_[:], wu_v[:, :, half * 256:(half + 1) * 256], "ld_wuh%d" % half, writes=["wuh%d" % half])
            for fcl in range(2):
                fc = half * 2 + fcl
                for tb in range(2):
                    i2 = cnt6[0] % 2
                    cnt6[0] += 1
                    pgt, pgk_ = banks[i2], bkk[i2]
                    pup, puk = banks[2 + i2], bkk[2 + i2]
                    def mmg(e, wg_=wg_, fcl=fcl, tb=tb, pgt=pgt):
                        ins = None
                        for kc in range(16):
                            ins = e.matmul(pgt[:], lhsT=wg_[:, kc, fcl * 128:(fcl + 1) * 128],
                                           rhs=h2T[:, kc, tb * 512:(tb + 1) * 512], start=(kc == 0), stop=(kc == 15))
                        return ins
                    pg.op("pe", mmg, reads=["wgh%d" % half] + h2k, writes=[pgk_])
                    def mmu(e, wu_=wu_, fcl=fcl, tb=tb, pup=pup):
                        ins = None
                        for kc in range(16):
                            ins = e.matmul(pup[:], lhsT=wu_[:, kc, fcl * 128:(fcl + 1) * 128],
                                           rhs=h2T[:, kc, tb * 512:(tb + 1) * 512], start=(kc == 0), stop=(kc == 15))
                        return ins
                    pg.op("pe", mmu, reads=["wuh%d" % half] + h2k, writes=[puk])
                    if half == 0 and fcl == 0 and tb == 0:
                        emit_cbc(1)
                    sg_, sgk = sgs[i2], "sgs%d" % i2
                    tu_, tuk = tus[i2], "tus%d" % i2
                    pg.op("act", lambda e, sg_=sg_, pgt=pgt: e.activation(out=sg_[:], in_=pgt[:], func=AF.Silu),
                          reads=[pgk_], writes=[sgk])
                    pg.op("dve", lambda e, tu_=tu_, pup=pup, sg_=sg_: e.tensor_tensor(out=tu_[:], in0=pup[:], in1=sg_[:],
                                                                                     op=OP.mult),
                          reads=[puk, sgk], writes=[tuk])
                    pg.op("dve", lambda e, tu_=tu_, fc=fc, tb=tb: e.tensor_tensor(
                        out=hid[:, fc, tb * 512:(tb + 1) * 512], in0=tu_[:], in1=cbc[:, tb, :], op=OP.mult),
                        reads=[tuk, ("cbc", tb)], writes=[("hid", fc, tb)])
        dma_cast(wdn[:], wd_v, "ld_wdn", writes=["wdn"])
        for dc in range(16):
            for tb in range(2):
                i2 = cnt6[0] % 3
                cnt6[0] += 1
                pbi = (4, 5, 7)[i2]
                po, pok = banks[pbi], bkk[pbi]
                def mmd(e, dc=dc, tb=tb, po=po):
                    ins = None
                    for fc in range(4):
                        ins = e.matmul(po[:], lhsT=wdn[:, fc, dc * 128:(dc + 1) * 128], rhs=hid[:, fc, tb * 512:(tb + 1) * 512],
                                       start=(fc == 0), stop=(fc == 3))
                    return ins
                pg.op("pe", mmd, reads=["wdn"] + [("hid", fc, tb) for fc in range(4)], writes=[pok])
                tm_, tmk = tmo[i2], "tmo%d" % i2
                pg.op("act", lambda e, tm_=tm_, po=po, dc=dc: e.activation(out=tm_[:], in_=po[:], func=AF.Copy,
                                                                           scale=G12[:, 1, dc:dc + 1]),
                      reads=[pok, "G12b"], writes=[tmk])
                pg.op("dve", lambda e, tm_=tm_, dc=dc, tb=tb: e.tensor_tensor(
                    out=x1T[:, dc, tb * 512:(tb + 1) * 512], in0=x1T[:, dc, tb * 512:(tb + 1) * 512], in1=tm_[:], op=OP.add),
                    reads=[("x1T", dc, tb), tmk], writes=[("x1T", dc, tb)])
    for ex in range(NEXP):
        do_expert(ex)
    pg.barrier()
    pg.flush()
    st.close()
    st6.close()

    st = ExitStack()
    nfr = st.enter_context(nc.sbuf_tensor("nfr", [128, 2048], F32))
    xo = [st.enter_context(nc.sbuf_tensor("xo%d" % i, [128, 2048], F32)) for i in range(2)]
    junk = st.enter_context(nc.sbuf_tensor("junk", [128, 2048], BF16))
    ss7 = st.enter_context(nc.sbuf_tensor("ss7", [128, 16], F32))
    dma_sp(nfr[:], lnrows[:, 2 * D:3 * D], "ld_nfr", writes=["nfr"])
    pg.op("dve", lambda e: e.memset(ss7[:], 0.0), writes=["ss7"])
    for t in range(8):
        xo_, xok = xo[t % 2], "xo%d" % (t % 2)
        for q in range(4):
            pf, pfk = banks[q % 2], bkk[q % 2]
            def trf(e, t=t, q=q, pf=pf):
                ins = None
                for j in range(4):
                    dc = q * 4 + j
                    ins = e.transpose(out=pf[:, j * 128:(j + 1) * 128], in_=x1T[:, dc, t * 128:(t + 1) * 128], identity=ident)
                return ins
            pg.op("pe", trf, reads=[("x1T", q * 4 + j, t // 4) for j in range(4)] + ["cst"], writes=[pfk])
            pg.op("act", lambda e, xo_=xo_, q=q, pf=pf: e.copy(out=xo_[:, q * 512:(q + 1) * 512], in_=pf[:]),
                  reads=[pfk], writes=[(xok, q)])
        xk = [(xok, q) for q in range(4)]
        pg.op("act", lambda e, xo_=xo_, t=t: e.activation(out=junk[:], in_=xo_[:], func=AF.Square, accum_out=ss7[:, t:t + 1]),
              reads=xk + ["ss7"], writes=["junk", ("ss7", t)])
        pg.op("dve", lambda e, t=t: e.tensor_scalar(out=ss7[:, 8 + t:9 + t], in0=ss7[:, t:t + 1], scalar1=1.0 / D, scalar2=EPS,
                                                    op0=OP.mult, op1=OP.add), reads=[("ss7", t)], writes=[("rs7", t)])
        pg.op("act", lambda e, t=t: e.sqrt(out=ss7[:, 8 + t:9 + t], in_=ss7[:, 8 + t:9 + t]), reads=[("rs7", t)], writes=[("rs7", t)])
        pg.op("dve", lambda e, t=t: e.reciprocal(out=ss7[:, 8 + t:9 + t], in_=ss7[:, 8 + t:9 + t]), reads=[("rs7", t)],
              writes=[("rs7", t)])
        pg.op("dve", lambda e, xo_=xo_, t=t: e.scalar_tensor_tensor(out=xo_[:], in0=xo_[:], scalar=ss7[:, 8 + t:9 + t],
                                                                    in1=nfr[:], op0=OP.mult, op1=OP.mult),
              reads=xk + [("rs7", t), "nfr"], writes=xk)
        dma_sp(out[t * 128:(t + 1) * 128, :], xo_[:], "st_out%d" % (t % 2), reads=xk)
    pg.barrier()
    pg.flush()
    st.close()
    st5.close()
    return finish(nc, pg, es, out)


def finish(nc, pg, es, out):
    es.close()
    return nc


def _consts():
    c = np.zeros((128, C_N), np.float32)
    i = np.arange(128)
    c[:, C_ID:C_ID + 128] = np.eye(128, dtype=np.float32)
    c[:, C_TU:C_TU + 128] = (i[:, None] <= i[None, :])
    c[:, C_TL:C_TL + 128] = (i[:, None] >= i[None, :])
    c[:, C_MNF:C_MNF + 128] = np.where(i[:, None] <= i[None, :], 0.0, -30000.0)
    c[:, C_MNB:C_MNB + 128] = np.where(i[:, None] >= i[None, :], 0.0, -30000.0)
    c[:, C_ONE:C_ONE + 128] = 1.0
    s3 = np.zeros((128, 32, 128), np.float32)
    for p in range(96):
        s3[p, p % 32, :] = 1.0
    return c, s3.reshape(128, 4096)


def _pp(v):
    return np.ascontiguousarray(np.asarray(v, np.float32).reshape(-1, 128).T)


def prep_inputs(inp):
    f = lambda a: np.ascontiguousarray(np.asarray(a, np.float32))
    x, c, ctx, c_ctx = f(inp["x"]), f(inp["c"]), f(inp["ctx"]), f(inp["c_ctx"])
    cst, s3 = _consts()
    conv_w = f(inp["conv_w"])[0]
    pvec = np.zeros((128, PV_N), np.float32)
    pvec[:, PV_N1G:PV_N1G + 16] = _pp(inp["norm1_g"][0])
    pvec[:, PV_N2G:PV_N2G + 16] = _pp(inp["norm2_g"][0])
    pvec[:, PV_SNG:PV_SNG + 16] = _pp(inp["ssd_norm_g"][0])
    for j in range(3):
        pvec[:, PV_CW + j * 24:PV_CW + (j + 1) * 24] = _pp(conv_w[j])
    pvec[:, PV_CB:PV_CB + 24] = _pp(inp["conv_b"][0])
    pvec[:, PV_BMOD:PV_BMOD + 96] = _pp(inp["b_mod"][0])
    row = np.zeros((RP_N,), np.float32)
    row[RP_DTB:RP_DTB + 32] = f(inp["dt_bias_f"])[0]
    row[RP_DTB + 32:RP_DTB + 64] = f(inp["dt_bias_b"])[0]
    row[RP_ALOG:RP_ALOG + 32] = f(inp["a_log_f"])[0]
    row[RP_ALOG + 32:RP_ALOG + 64] = f(inp["a_log_b"])[0]
    row[RP_DSKIP:RP_DSKIP + 32] = f(inp["d_skip"])[0]
    row[RP_BS:RP_BS + 1024] = f(inp["b_spatial"])[0].reshape(-1)
    row[RP_BR:RP_BR + 4] = f(inp["b_router_group"])[0]
    row[RP_BR + 4:RP_BR + 36] = f(inp["b_router_expert"])[0].reshape(-1)
    rowp = np.ascontiguousarray(np.broadcast_to(row[None, :], (128, RP_N)))
    lnr = np.concatenate([f(inp["cm_ln_g"])[0], f(inp["cm_ln_b"])[0], f(inp["normf_g"])])
    lnrows = np.ascontiguousarray(np.broadcast_to(lnr[None, :], (128, 3 * D)))
    w_mod = f(inp["w_mod"])[0]
    w_in = f(inp["w_in"])[0]
    w_out = f(inp["w_out"])[0]
    wsT = np.ascontiguousarray(np.transpose(f(inp["w_spatial"])[0], (2, 0, 1)).reshape(128, 1024))
    wrg = f(inp["w_router_group"])[0]
    wre = np.transpose(f(inp["w_router_expert"])[0], (1, 0, 2)).reshape(D, 32)
    wrc = np.concatenate([wrg, wre], axis=1)
    wr = np.ascontiguousarray(wrc.reshape(16, 128, 36).transpose(1, 0, 2).reshape(128, 16 * 36))
    w_g = f(inp["w_exp_gate"])[0].reshape(32, D, 512)
    w_u = f(inp["w_exp_up"])[0].reshape(32, D, 512)
    w_d = f(inp["w_exp_down"])[0].reshape(32, 512, D)
    maps = []
    for k in range(NCORES):
        b, s = k // 2, k % 2
        own = x[b, s * 1024:(s + 1) * 1024]
        oth = x[b, (1 - s) * 1024:(2 - s) * 1024]
        x_all = np.concatenate([ctx[b], oth, own], axis=0)
        fl = np.zeros((128, 2), np.float32)
        fl[:, 0] = 1.0 if s == 1 else 0.0
        fl[:, 1] = 1.0 if s == 0 else 0.0
        cvec = np.stack([_pp(c[b]), _pp(c_ctx)], axis=2).reshape(128, 32)
        maps.append(dict(x_all=x_all, flags=fl, cvec=np.ascontiguousarray(cvec), pvec=pvec, rowp=rowp,
                         lnrows=lnrows, consts=cst, sel3=s3, w_mod=w_mod, w_in=w_in, w_out=w_out,
                         wsT=wsT, wr=wr, w_g=w_g, w_u=w_u, w_d=w_d))
    return maps


def kernel(**inputs):
    maps = prep_inputs(inputs)
    nc = build_nc()
    res = run_bass_kernel_spmd(nc, maps, core_ids=list(range(NCORES)))
    outf = np.zeros((4, 2048, D), np.float32)
    for k in range(NCORES):
        b, s = k // 2, k % 2
        outf[b, s * 1024:(s + 1) * 1024] = res.results[k]["out"]
    return outf
```
